# Optimizing a Trainium2 kernel written in Bass

```python
import jax, jax.numpy as jnp
from jax import lax
import numpy as np

D_MODEL = 4096
BATCH = 4
SEQ = 2048
DEPTH = 1

CHUNK = 64
Q_BLOCK = 128
D_PLE = 256
A_HEADS = 16
A_HEAD_DIM = 128
A_LATENT = 256
IDX_HEADS = 32
IDX_DIM = 64
TOPK_MAX = 256
B_HEADS = 16
B_HEAD_DIM = 128
PEER_HEADS = 8
PEER_KEYS = 128
PEER_QDIM = 256
PEER_TOPK = 16
N_EXPERTS = PEER_KEYS * PEER_KEYS
PEER_TOKEN_BLOCK = 128
N_BRANCH = 2
LN_EPS = 1e-5
IN_WIDTHS = (
    A_HEADS * A_HEAD_DIM,
    A_LATENT,
    IDX_HEADS * IDX_DIM,
    IDX_DIM,
    IDX_HEADS,
    B_HEADS * B_HEAD_DIM,
    B_HEADS * B_HEAD_DIM,
    B_HEADS * B_HEAD_DIM,
    B_HEADS,
)
IN_WIDTH = sum(IN_WIDTHS)

kernel_name = "hybrid_dsa_fox_peer_block"


def layer_norm(x, g, b):
    xf = x.astype(jnp.float32)
    mu = jnp.mean(xf, axis=-1, keepdims=True)
    var = jnp.mean(jnp.square(xf - mu), axis=-1, keepdims=True)
    y = (xf - mu) * lax.rsqrt(var + LN_EPS) * g.astype(jnp.float32) + b.astype(jnp.float32)
    return y.astype(x.dtype)


def rms_norm(x, g):
    xf = x.astype(jnp.float32)
    y = xf * lax.rsqrt(jnp.mean(jnp.square(xf), axis=-1, keepdims=True) + LN_EPS) * g.astype(jnp.float32)
    return y.astype(x.dtype)


def alibi_slopes(n):
    return 2.0 ** (-8.0 * jnp.arange(1, n + 1, dtype=jnp.float32) / n)


def to_blocks(a, n_blocks):
    return a.reshape(a.shape[0], n_blocks, Q_BLOCK, *a.shape[2:]).swapaxes(0, 1)


def dsa_attention(q, c_kv, q_idx, k_idx, w_idx, w_uk, w_uv):
    bsz, seq = q.shape[0], q.shape[1]
    n_blocks = seq // Q_BLOCK
    k_sel = min(TOPK_MAX, seq // 4)
    pos = jnp.arange(seq, dtype=jnp.int32)
    slopes = alibi_slopes(A_HEADS)
    scale = A_HEAD_DIM ** -0.5
    idx_scale = (IDX_DIM ** -0.5) * (IDX_HEADS ** -0.5)

    def block(args):
        qb, qib, wib, tb = args
        rel = jax.nn.relu(jnp.einsum('bqhd,bsd->bqsh', qib, k_idx).astype(jnp.float32))
        score = jnp.einsum('bqsh,bqh->bqs', rel, wib.astype(jnp.float32)) * idx_scale
        chunk_end = (tb // CHUNK + 1) * CHUNK
        admissible = pos[None, :] < chunk_end[:, None]
        score = jnp.where(admissible[None], score, -jnp.inf)
        _, idx = lax.top_k(score, k_sel)
        valid = idx < chunk_end[None, :, None]
        c_sel = jax.vmap(lambda c, i: c[i])(c_kv, idx)
        q_lat = jnp.einsum('bqhd,chd->bqhc', qb, w_uk)
        logits = jnp.einsum('bqhc,bqkc->bqhk', q_lat, c_sel).astype(jnp.float32) * scale
        dist = jnp.abs(tb[None, :, None] - idx).astype(jnp.float32)
        logits = logits - slopes[None, None, :, None] * dist[:, :, None, :]
        logits = jnp.where(valid[:, :, None, :], logits, -jnp.inf)
        probs = jax.nn.softmax(logits, axis=-1).astype(c_sel.dtype)
        o_lat = jnp.einsum('bqhk,bqkc->bqhc', probs, c_sel)
        o = jnp.einsum('bqhc,chd->bqhd', o_lat, w_uv)
        return o.reshape(bsz, Q_BLOCK, A_HEADS * A_HEAD_DIM)

    out = lax.map(block, (to_blocks(q, n_blocks), to_blocks(q_idx, n_blocks),
                          to_blocks(w_idx, n_blocks), pos.reshape(n_blocks, Q_BLOCK)))
    return out.swapaxes(0, 1).reshape(bsz, seq, A_HEADS * A_HEAD_DIM)


def fox_attention(q, k, v, f_logit):
    bsz, seq = q.shape[0], q.shape[1]
    n_blocks = seq // Q_BLOCK
    pos = jnp.arange(seq, dtype=jnp.int32)
    scale = B_HEAD_DIM ** -0.5
    cum = jnp.cumsum(jax.nn.log_sigmoid(f_logit.astype(jnp.float32)), axis=1)
    cum_k = cum.transpose(0, 2, 1)

    def block(args):
        qb, cqb, tb = args
        logits = jnp.einsum('bqhd,bshd->bhqs', qb, k).astype(jnp.float32) * scale
        logits = logits + cqb.transpose(0, 2, 1)[..., None] - cum_k[:, :, None, :]
        causal = pos[None, :] <= tb[:, None]
        logits = jnp.where(causal[None, None], logits, -jnp.inf)
        probs = jax.nn.softmax(logits, axis=-1).astype(v.dtype)
        o = jnp.einsum('bhqs,bshd->bqhd', probs, v)
        return o.reshape(bsz, Q_BLOCK, B_HEADS * B_HEAD_DIM)

    out = lax.map(block, (to_blocks(q, n_blocks), to_blocks(cum, n_blocks),
                          pos.reshape(n_blocks, Q_BLOCK)))
    return out.swapaxes(0, 1).reshape(bsz, seq, B_HEADS * B_HEAD_DIM)


def peer_ffn(x, w_q, sub_keys, u_tab, v_tab):
    bsz, seq, d = x.shape
    half = PEER_QDIM // 2
    q = jnp.einsum('bsd,dhk->bshk', x, w_q)
    q1, q2 = q[..., :half], q[..., half:]
    s1 = jnp.einsum('bshk,hnk->bshn', q1, sub_keys[:, 0]).astype(jnp.float32)
    s2 = jnp.einsum('bshk,hnk->bshn', q2, sub_keys[:, 1]).astype(jnp.float32)
    v1, i1 = lax.top_k(s1, PEER_TOPK)
    v2, i2 = lax.top_k(s2, PEER_TOPK)
    cand = (v1[..., :, None] + v2[..., None, :]).reshape(bsz, seq, PEER_HEADS, PEER_TOPK * PEER_TOPK)
    vals, ci = lax.top_k(cand, PEER_TOPK)
    experts = (jnp.take_along_axis(i1, ci // PEER_TOPK, axis=-1) * PEER_KEYS
               + jnp.take_along_axis(i2, ci % PEER_TOPK, axis=-1))
    gates = jax.nn.softmax(vals, axis=-1)
    n_tok = bsz * seq
    m = PEER_HEADS * PEER_TOPK
    n_tb = n_tok // PEER_TOKEN_BLOCK
    xs = x.reshape(n_tb, PEER_TOKEN_BLOCK, d)
    es = experts.reshape(n_tb, PEER_TOKEN_BLOCK, m)
    gs = gates.reshape(n_tb, PEER_TOKEN_BLOCK, m).astype(x.dtype)

    def block(args):
        xb, eb, gb = args
        act = jax.nn.gelu(jnp.einsum('nd,nmd->nm', xb, u_tab[eb]), approximate=False)
        return jnp.einsum('nm,nmd->nd', gb * act, v_tab[eb])

    y = lax.map(block, (xs, es, gs))
    return y.reshape(bsz, seq, d)


def setup_inputs(seed: int = 0) -> dict:
    key = jax.random.key(seed)
    ks = jax.random.split(key, 24)
    f32 = jnp.float32
    beta = (8.0 * DEPTH) ** -0.25
    nrm = lambda k, shape, s: jax.random.normal(k, shape, f32) * s
    L = DEPTH
    return {
        "x": nrm(ks[0], (BATCH, SEQ, D_MODEL), 1.0),
        "p": nrm(ks[1], (DEPTH, BATCH, SEQ, D_PLE), 1.0),
        "w_in": nrm(ks[2], (L, D_MODEL, IN_WIDTH), D_MODEL ** -0.5),
        "b_forget": 1.0 + nrm(ks[3], (L, B_HEADS), 0.1),
        "g_latent": 1.0 + nrm(ks[4], (L, A_LATENT), 0.02),
        "w_uk": nrm(ks[5], (L, A_LATENT, A_HEADS, A_HEAD_DIM), A_LATENT ** -0.5),
        "w_uv": nrm(ks[6], (L, A_LATENT, A_HEADS, A_HEAD_DIM), A_LATENT ** -0.5),
        "w_branch_a": nrm(ks[7], (L, A_HEADS * A_HEAD_DIM, D_MODEL), (A_HEADS * A_HEAD_DIM) ** -0.5),
        "w_branch_b": nrm(ks[8], (L, B_HEADS * B_HEAD_DIM, D_MODEL), (B_HEADS * B_HEAD_DIM) ** -0.5),
        "w_gate": nrm(ks[9], (L, D_MODEL, N_BRANCH * D_MODEL), D_MODEL ** -0.5),
        "b_gate": nrm(ks[10], (L, N_BRANCH * D_MODEL), 0.02),
        "w_out": nrm(ks[11], (L, D_MODEL, D_MODEL), beta * D_MODEL ** -0.5),
        "ln1_g": 1.0 + nrm(ks[12], (L, D_MODEL), 0.02),
        "ln1_b": nrm(ks[13], (L, D_MODEL), 0.02),
        "peer_wq": nrm(ks[14], (L, D_MODEL, PEER_HEADS, PEER_QDIM), D_MODEL ** -0.5),
        "peer_subkeys": nrm(ks[15], (L, PEER_HEADS, 2, PEER_KEYS, PEER_QDIM // 2), (PEER_QDIM // 2) ** -0.5),
        "peer_u": nrm(ks[16], (L, N_EXPERTS, D_MODEL), D_MODEL ** -0.5),
        "peer_v": nrm(ks[17], (L, N_EXPERTS, D_MODEL), beta * PEER_HEADS ** -0.5),
        "w_ple": nrm(ks[18], (L, D_PLE, D_MODEL), D_PLE ** -0.5),
        "w_ple_gate": nrm(ks[19], (L, D_MODEL, D_MODEL), D_MODEL ** -0.5),
        "b_ple_gate": nrm(ks[20], (L, D_MODEL), 0.02),
        "ln2_g": 1.0 + nrm(ks[21], (L, D_MODEL), 0.02),
        "ln2_b": nrm(ks[22], (L, D_MODEL), 0.02),
    }


def reference(x, p, w_in, b_forget, g_latent, w_uk, w_uv, w_branch_a, w_branch_b,
              w_gate, b_gate, w_out, ln1_g, ln1_b, peer_wq, peer_subkeys, peer_u,
              peer_v, w_ple, w_ple_gate, b_ple_gate, ln2_g, ln2_b):
    bsz, seq, d = x.shape
    alpha = (2.0 * DEPTH) ** 0.25
    split_at = [int(o) for o in np.cumsum(IN_WIDTHS)[:-1]]
    h = x
    for i in range(DEPTH):
        u = h
        proj = u @ w_in[i]
        (qa, c_kv, q_idx, k_idx, w_idx, qb, kb, vb, f_logit) = jnp.split(proj, split_at, axis=-1)
        qa = qa.reshape(bsz, seq, A_HEADS, A_HEAD_DIM)
        c_kv = rms_norm(c_kv, g_latent[i])
        q_idx = q_idx.reshape(bsz, seq, IDX_HEADS, IDX_DIM)
        o_a = dsa_attention(qa, c_kv, q_idx, k_idx, w_idx, w_uk[i], w_uv[i])
        o_b = fox_attention(qb.reshape(bsz, seq, B_HEADS, B_HEAD_DIM),
                            kb.reshape(bsz, seq, B_HEADS, B_HEAD_DIM),
                            vb.reshape(bsz, seq, B_HEADS, B_HEAD_DIM),
                            f_logit + b_forget[i])
        gates = jax.nn.sigmoid(u @ w_gate[i] + b_gate[i]).reshape(bsz, seq, N_BRANCH, d)
        merged = gates[:, :, 0] * (o_a @ w_branch_a[i]) + gates[:, :, 1] * (o_b @ w_branch_b[i])
        h = layer_norm(alpha * h + merged @ w_out[i], ln1_g[i], ln1_b[i])
        ffn = peer_ffn(h, peer_wq[i], peer_subkeys[i], peer_u[i], peer_v[i])
        ple = jax.nn.sigmoid(h @ w_ple_gate[i] + b_ple_gate[i]) * (p[i] @ w_ple[i])
        h = layer_norm(alpha * h + ffn + ple, ln2_g[i], ln2_b[i])
    return h
```

```python
from contextlib import ExitStack
import numpy as np
import concourse.bass as bass
import concourse.mybir as mybir
from concourse.bass_utils import run_bass_kernel_spmd

F32 = mybir.dt.float32
BF16 = mybir.dt.bfloat16
U32 = mybir.dt.uint32
ALU = mybir.AluOpType
AF = mybir.ActivationFunctionType
AX = mybir.AxisListType

ENG = ("sp", "act", "dve", "pool", "pe")
NDMASEM = 8


class Buf:
    __slots__ = ("name", "w", "ws", "r")

    def __init__(self, name=""):
        self.name = name
        self.w = None
        self.ws = []
        self.r = []


class Prog:
    def __init__(self, nc, es):
        self.nc = nc
        self.streams = {e: [] for e in ENG}
        self.cnt = {e: 0 for e in ENG}
        self.sems = {}
        for e in ENG:
            self.sems[("e", e)] = es.enter_context(nc.semaphore("s_" + e))
        self.dcnt = {}
        self.dnext = {}
        for q in ("sp", "act", "pool"):
            self.dnext[q] = 0
            for i in range(NDMASEM):
                k = ("d", q, i)
                self.sems[k] = es.enter_context(nc.semaphore("d_%s%d" % (q, i)))
                self.dcnt[k] = 0
        self.waited = {e: {} for e in ENG}
        self.ninstr = 0

    def _wait(self, e, deps, skip_own=False):
        best = {}
        for d in deps:
            if d is None:
                continue
            k, v = d
            if skip_own and k == ("e", e):
                continue
            if best.get(k, 0) < v:
                best[k] = v
        for k, v in best.items():
            if self.waited[e].get(k, 0) >= v:
                continue
            if k == ("e", e) and v > self.cnt[e]:
                continue
            self.waited[e][k] = v
            sem = self.sems[k]
            self.streams[e].append(lambda eng, sem=sem, v=v: eng.wait_ge(sem, v))

    @staticmethod
    def _deps(reads, writes, group=False):
        deps = []
        for b in reads:
            deps.append(b.w)
            deps.extend(b.ws)
        for b in writes:
            if not group:
                deps.append(b.w)
                deps.extend(b.ws)
            deps.extend(b.r)
        return deps

    def _mark(self, tok, reads, writes, group=False):
        for b in reads:
            b.r.append(tok)
            if len(b.r) > 64:
                best = {}
                for k, v in b.r:
                    if best.get(k, 0) < v:
                        best[k] = v
                b.r = list(best.items())
        for b in writes:
            if group:
                b.ws.append(tok)
            else:
                b.w = tok
                b.ws = []
                b.r = []

    def op(self, e, fn, reads=(), writes=(), inc=True, skip_own=False):
        self._wait(e, self._deps(reads, writes), skip_own)
        tok_val = self.cnt[e] + 1
        key = ("e", e)
        if inc:
            self.cnt[e] += 1
            sem = self.sems[key]
            self.streams[e].append(lambda eng, fn=fn, sem=sem: fn(eng).then_inc(sem, 1))
        else:
            self.streams[e].append(lambda eng, fn=fn: fn(eng))
        tok = (key, tok_val)
        self._mark(tok, reads, writes)
        self.ninstr += 1
        return tok

    def dma(self, q, out, in_, reads=(), writes=(), group=False, **kw):
        i = self.dnext[q]
        self.dnext[q] = (i + 1) % NDMASEM
        k = ("d", q, i)
        deps = self._deps(reads, writes, group)
        if self.dcnt[k] > 0:
            deps.append((k, self.dcnt[k]))
        self._wait(q, deps)
        self.dcnt[k] += 16
        sem = self.sems[k]
        self.streams[q].append(
            lambda eng, out=out, in_=in_, sem=sem, kw=kw: eng.dma_start(out=out, in_=in_, **kw).then_inc(sem, 16))
        tok = (k, self.dcnt[k])
        self._mark(tok, reads, writes, group)
        self.ninstr += 1
        return tok

    def barrier(self):
        deps = [(("e", e), self.cnt[e]) for e in ENG if self.cnt[e] > 0]
        deps += [(k, v) for k, v in self.dcnt.items() if v > 0]
        for e in ENG:
            self._wait(e, deps)

    def wait_all_on(self, e):
        deps = [(("e", x), self.cnt[x]) for x in ENG if self.cnt[x] > 0]
        deps += [(k, v) for k, v in self.dcnt.items() if v > 0]
        self._wait(e, deps)

    def emit(self):
        nc = self.nc
        with nc.Block() as block:
            @block.sync
            def _(eng):
                for f in self.streams["sp"]:
                    f(eng)

            @block.scalar
            def _(eng):
                for f in self.streams["act"]:
                    f(eng)

            @block.vector
            def _(eng):
                for f in self.streams["dve"]:
                    f(eng)

            @block.gpsimd
            def _(eng):
                for f in self.streams["pool"]:
                    f(eng)

            @block.tensor
            def _(eng):
                for f in self.streams["pe"]:
                    f(eng)


class Arena:
    def __init__(self, ap_full, nwords):
        self.a = ap_full
        self.n = nwords
        self.off = 0
        self.top = nwords
        self.marks = []

    def alloc(self, nbytes, dt=F32):
        nw = (nbytes + 3) // 4
        nw = (nw + 15) // 16 * 16
        assert self.off + nw <= self.top, "SBUF arena overflow %d + %d > %d" % (self.off, nw, self.top)
        v = self.a[:, self.off:self.off + nw]
        self.off += nw
        if dt != F32:
            v = v.bitcast(dt)
        return v

    def alloc_top(self, nbytes, dt=F32):
        nw = ((nbytes + 3) // 4 + 15) // 16 * 16
        assert self.top - nw >= self.off
        self.top -= nw
        v = self.a[:, self.top:self.top + nw]
        return v.bitcast(dt) if dt != F32 else v

    def f32(self, n):
        return self.alloc(4 * n)[:, 0:n]

    def bf16(self, n):
        return self.alloc(2 * n, BF16)[:, 0:n]

    def push(self):
        self.marks.append(self.off)

    def pop(self):
        self.off = self.marks.pop()


D = 4096
S = 2048
T = 1024
NQ_CH = 49
NK_CH = 36
ALPHA = 2.0 ** 0.25
SCALE = 128.0 ** -0.5
EPS = 1e-5
NEG = -30000.0
SLOPES = [2.0 ** (-8.0 * (h + 1) / 16) for h in range(16)]

ARENA_WORDS = 184 * 256
import os
NH_A = int(os.environ.get('NH_A', 16))
NH_B = int(os.environ.get('NH_B', 16))


def build(stage=99, dbg=()):
    nc = bass.Bass("TRN2", target_bir_lowering=False)

    def din(name, shape, dt=F32):
        return nc.dram_tensor(name, list(shape), dt, kind="ExternalInput").ap()

    def dscr(name, shape, dt=F32):
        return nc.dram_tensor(name, list(shape), dt, kind="Internal").ap()

    def dout(name, shape, dt=F32):
        return nc.dram_tensor(name, list(shape), dt, kind="ExternalOutput").ap()

    IN_SHAPES = {
        "xT": [D, S], "pT": [256, T], "w_in_t": [NQ_CH + NK_CH, 128, 32, 128], "ident": [128, 128],
        "qpos_b": [128, T], "kpos_b": [128, S], "kpos_col": [128, 16], "cend_col": [128, 8], "penA": [32, T], "penB": [32, S],
        "glat": [128, 2], "bfor": [128, 1], "wuk_t": [128, 2, 2048], "wuv_t": [128, 2, 2048],
        "w_gate_t": [64, 128, 32, 128], "b_gate_t": [128, 64], "w_ba_t": [32, 128, 16, 128],
        "w_bb_t": [32, 128, 16, 128], "w_out_t": [32, 128, 32, 128], "ln1g": [128, 32], "ln1b": [128, 32],
        "peer_wq_t": [16, 128, 32, 128], "sk_t": [128, 16, 128], "uT_t": [128, 128, 32, 128],
        "v_t": [128, 128, D], "w_pg_t": [32, 128, 32, 128], "b_pg_t": [128, 32],
        "w_ple_t": [32, 128, 2, 128], "ln2g": [128, 32], "ln2b": [128, 32], "iota_t": [128, 32, 128],
    }
    IN = {}

    def inp(name):
        if name not in IN:
            IN[name] = din(name, IN_SHAPES[name])
        return IN[name]

    outT_d = dout("outT", [D, T])

    qA_s = dscr("qA_s", [16, 128, T], BF16)
    qB_s = dscr("qB_s", [16, 128, T], BF16)
    kB_s = dscr("kB_s", [16, 128, S], BF16)
    vB_s = dscr("vB_s", [16, 128, S], BF16)
    oT_s = dscr("oT_s", [32, 128, T], BF16)
    s1_s = dscr("s1_s", [8, T, 128], F32)
    B_s1s = Buf("s1_s")
    B_gTs = Buf("gT_s")
    B_ggs = Buf("ggT_s")
    gT_s = dscr("gT_s", [16, 128, 128 * 64], BF16)
    ggT_s = dscr("ggT_s", [128, 128, T], BF16)

    dbg_out = {}

    with ExitStack() as es:
        p = Prog(nc, es)
        arena_t = es.enter_context(nc.sbuf_tensor("arena", [128, ARENA_WORDS], F32))
        psum_t = es.enter_context(nc.psum_tensor("ps", [128, 4096], F32))
        A = Arena(arena_t, ARENA_WORDS)
        PS = [psum_t[:, b * 512:(b + 1) * 512] for b in range(8)]
        PSB = [Buf("ps%d" % b) for b in range(8)]

        def dbg_dump(name, ap, shape, buf, dt=F32):
            if name in dbg:
                o = dout("dbg_" + name, shape, dt)
                dbg_out[name] = o
                p.dma("sp", o, ap, reads=[buf])

        ident_f = A.f32(128)
        ident_b = A.bf16(128)
        ones_f = A.f32(128)
        ones_b = A.bf16(128)
        B_const = Buf("const")
        p.dma("sp", ident_f, inp("ident"), writes=[B_const])
        p.op("dve", lambda e: e.tensor_copy(out=ident_b, in_=ident_f), reads=[B_const], writes=[B_const])
        p.op("dve", lambda e: e.memset(ones_f, 1.0), writes=[B_const])
        p.op("dve", lambda e: e.memset(ones_b, 1.0), writes=[B_const])
        A_BASE = A.off

        def arena_reset():
            p.barrier()
            A.off = A_BASE
            A.top = ARENA_WORDS
            A.marks = []

        def gemm(xT, KC, ntok, w_dram, chunks, evac, xbufs, wtiles, ps_slots, q="pool"):
            nb = ntok // 512
            for i, c in enumerate(chunks):
                wt, wb = wtiles[i % len(wtiles)]
                p.dma(q, wt.rearrange("p (k n) -> p k n", k=KC), w_dram[c], writes=[wb])
                banks = ps_slots[i % len(ps_slots)]
                for kc in range(KC):
                    for t in range(nb):
                        b = banks[t]
                        p.op("pe", lambda e, b=b, kc=kc, t=t, wt=wt: e.matmul(
                            PS[b], wt[:, kc * 128:(kc + 1) * 128], xT[:, kc, t * 512:(t + 1) * 512],
                            start=(kc == 0), stop=(kc == KC - 1)),
                            reads=[wb] + list(xbufs), writes=[PSB[b]],
                            inc=(kc == KC - 1 and t == nb - 1))
                evac(i, c, banks)

        ckvn = A.bf16(2 * S).rearrange("p (c t) -> p c t", c=2)
        B_ckvn = Buf("ckvn")
        B_selT = Buf("selT")
        kpos_b = A.f32(S)
        qpos_b = A.f32(T)
        kpos_col = A.f32(16)
        cend_col = A.f32(8)
        glat = A.f32(2)
        negb = A.f32(1)
        B_pos = Buf("pos")
        A.push()
        qidxT = A.bf16(16 * T).rearrange("p (c t) -> p c t", c=16)
        B_qidx = Buf("qidx")
        kidxT = A.bf16(S)
        B_kidx = Buf("kidx")
        w_tok = A.f32(8 * 32).rearrange("p (j h) -> p j h", j=8)
        B_wtok = Buf("wtok")
        A.push()
        ckvT = A.f32(2 * S).rearrange("p (c t) -> p c t", c=2)
        B_ckv = Buf("ckv")
        fT = A.f32(S)
        B_f = Buf("fT")
        widxT = A.f32(T)
        B_widxT = Buf("widxT")
        A.push()
        xT = A.bf16(32 * T).rearrange("p (k t) -> p k t", k=32)
        B_x = Buf("xT")
        wtiles = [(A.bf16(32 * 128), Buf("w%d" % i)) for i in range(3)]
        stg = [(A.bf16(T), Buf("stg%d" % i)) for i in range(3)]
        xT_v = inp("xT").rearrange("(k p) t -> p k t", p=128)

        def load_x(half):
            for k4 in range(4):
                p.dma("pool", xT[:, k4 * 8:(k4 + 1) * 8, :], xT_v[:, k4 * 8:(k4 + 1) * 8, half * T:(half + 1) * T],
                      writes=[B_x], group=(k4 > 0))

        slots2 = [[0, 1], [2, 3], [4, 5], [6, 7]]
        stg_i = [0]

        def evac_A(half):
            tok0 = half * T

            def ev(i, c, banks):
                def to_scratch(dst, scale):
                    st, sb = stg[stg_i[0] % 3]
                    stg_i[0] += 1
                    for t, b in enumerate(banks):
                        p.op("act", lambda e, b=b, t=t, st=st: e.activation(
                            out=st[:, t * 512:(t + 1) * 512], in_=PS[b], func=AF.Copy, scale=scale),
                            reads=[PSB[b]], writes=[sb])
                    p.dma("sp", dst, st, reads=[sb])

                if c < 16:
                    to_scratch(qA_s[c], SCALE)
                elif c < 32:
                    to_scratch(qB_s[c - 16], SCALE)
                elif c < 48:
                    for t, b in enumerate(banks):
                        p.op("act", lambda e, b=b, t=t: e.activation(
                            out=qidxT[:, c - 32, t * 512:(t + 1) * 512], in_=PS[b], func=AF.Copy),
                            reads=[PSB[b]], writes=[B_qidx])
                elif c == 48:
                    for t, b in enumerate(banks):
                        p.op("dve", lambda e, b=b, t=t: e.tensor_copy(out=widxT[:, t * 512:(t + 1) * 512], in_=PS[b]),
                             reads=[PSB[b]], writes=[B_widxT])
                elif c < 51:
                    for t, b in enumerate(banks):
                        p.op("dve", lambda e, b=b, t=t: e.tensor_copy(
                            out=ckvT[:, c - 49, tok0 + t * 512: tok0 + (t + 1) * 512], in_=PS[b]),
                            reads=[PSB[b]], writes=[B_ckv])
                elif c == 51:
                    for t, b in enumerate(banks):
                        p.op("act", lambda e, b=b, t=t: e.activation(
                            out=kidxT[:, tok0 + t * 512: tok0 + (t + 1) * 512], in_=PS[b], func=AF.Copy),
                            reads=[PSB[b]], writes=[B_kidx])
                elif c < 68:
                    to_scratch(kB_s[c - 52][:, tok0:tok0 + T], 1.0)
                elif c < 84:
                    to_scratch(vB_s[c - 68][:, tok0:tok0 + T], 1.0)
                else:
                    for t, b in enumerate(banks):
                        p.op("dve", lambda e, b=b, t=t: e.tensor_copy(
                            out=fT[:, tok0 + t * 512: tok0 + (t + 1) * 512], in_=PS[b]),
                            reads=[PSB[b]], writes=[B_f])
            return ev

        load_x(1)
        gemm(xT, 32, T, inp("w_in_t"), list(range(NQ_CH, NQ_CH + NK_CH)), evac_A(1), [B_x], wtiles, slots2)
        load_x(0)
        gemm(xT, 32, T, inp("w_in_t"), list(range(NQ_CH + NK_CH)), evac_A(0), [B_x], wtiles, slots2)

        dbg_dump("ckv", ckvT, [128, 2, S], B_ckv)
        dbg_dump("fT", fT, [128, S], B_f)
        dbg_dump("widxT", widxT, [128, T], B_widxT)
        dbg_dump("kidxT", kidxT, [128, S], B_kidx, BF16)
        dbg_dump("qidxT", qidxT, [128, 16, T], B_qidx, BF16)

        if stage <= 1:
            p.wait_all_on("sp")
            p.emit()
            return nc, dbg_out, list(IN)

        p.barrier()
        A.pop()
        selT = A.alloc_top(2 * 16 * T, BF16).rearrange("p (k t) -> p k t", k=16)
        p.dma("sp", kpos_b, inp("kpos_b"), writes=[B_pos])
        p.dma("sp", qpos_b, inp("qpos_b"), writes=[B_pos])
        p.dma("sp", kpos_col, inp("kpos_col"), writes=[B_pos])
        p.dma("sp", cend_col, inp("cend_col"), writes=[B_pos])
        p.dma("sp", glat, inp("glat"), writes=[B_pos])
        p.dma("sp", negb, inp("bfor"), writes=[B_pos])
        p.op("dve", lambda e: e.tensor_scalar(out=negb, in0=negb, scalar1=-1.0, scalar2=None, op0=ALU.mult),
             reads=[B_pos], writes=[B_pos])
        A.push()
        sq = A.f32(2 * S).rearrange("p (c t) -> p c t", c=2)
        rstd = A.f32(S)
        B_sq, B_rstd = Buf("sq"), Buf("rstd")
        for c in range(2):
            p.op("act", lambda e, c=c: e.activation(out=sq[:, c, :], in_=ckvT[:, c, :], func=AF.Square),
                 reads=[B_ckv], writes=[B_sq])
        for t in range(4):
            for c in range(2):
                p.op("pe", lambda e, t=t, c=c: e.matmul(PS[t], ones_f, sq[:, c, t * 512:(t + 1) * 512],
                                                        start=(c == 0), stop=(c == 1)),
                     reads=[B_sq, B_const], writes=[PSB[t]], inc=(c == 1))
            p.op("dve", lambda e, t=t: e.tensor_scalar(out=rstd[:, t * 512:(t + 1) * 512], in0=PS[t],
                                                       scalar1=1.0 / 256, scalar2=EPS, op0=ALU.mult, op1=ALU.add),
                 reads=[PSB[t]], writes=[B_rstd])
        p.op("act", lambda e: e.activation(out=rstd, in_=rstd, func=AF.Sqrt), reads=[B_rstd], writes=[B_rstd])
        p.op("dve", lambda e: e.reciprocal(out=rstd, in_=rstd), reads=[B_rstd], writes=[B_rstd])
        for c in range(2):
            p.op("dve", lambda e, c=c: e.scalar_tensor_tensor(out=ckvn[:, c, :], in0=ckvT[:, c, :], scalar=glat[:, c:c + 1],
                                                              in1=rstd, op0=ALU.mult, op1=ALU.mult),
                 reads=[B_ckv, B_rstd, B_pos], writes=[B_ckvn])
        dbg_dump("ckvn", ckvn, [128, 2, S], B_ckvn, BF16)
        A.pop()

        for j in range(8):
            p.op("pe", lambda e, j=j: e.matmul(PS[4][:, j * 32:(j + 1) * 32], widxT[0:32, j * 128:(j + 1) * 128],
                                               ident_f[0:32, 0:32], start=True, stop=True),
                 reads=[B_widxT, B_const], writes=[PSB[4]], inc=(j == 7))
        p.op("dve", lambda e: e.tensor_copy(out=w_tok.rearrange("p j h -> p (j h)"), in_=PS[4][:, 0:256]),
             reads=[PSB[4]], writes=[B_wtok])

        A.push()
        l2 = A.f32(S)
        B_l2 = Buf("l2")
        p.op("act", lambda e: e.activation(out=l2[0:16, :], in_=fT[0:16, :], func=AF.Exp, scale=-1.0, bias=negb[0:16, :]),
             reads=[B_f, B_pos], writes=[B_l2])
        p.op("act", lambda e: e.activation(out=l2[0:16, :], in_=l2[0:16, :], func=AF.Ln, scale=1.0, bias=1.0),
             reads=[B_l2], writes=[B_l2])
        for i in range(16):
            p.op("pe", lambda e, i=i: e.matmul(PS[5][:, i * 16:(i + 1) * 16], l2[0:16, i * 128:(i + 1) * 128],
                                               ident_f[0:16, 0:16], start=True, stop=True),
                 reads=[B_l2, B_const], writes=[PSB[5]], inc=(i == 15))
        l2t = A.f32(256)
        r1 = A.f32(256)
        tmpf = A.f32(256)
        parts = [A.bf16(256) for _ in range(3)]
        B_sp = Buf("split")
        p.op("dve", lambda e: e.tensor_copy(out=l2t, in_=PS[5][:, 0:256]), reads=[PSB[5]], writes=[B_sp])

        def split3(src, res, tmp, outs, n_part):
            sl = lambda a: a[0:n_part]
            o0, o1, o2 = outs
            p.op("dve", lambda e: e.tensor_copy(out=sl(o0), in_=sl(src)), reads=[B_sp], writes=[B_sp])
            p.op("dve", lambda e: e.tensor_copy(out=sl(tmp), in_=sl(o0)), reads=[B_sp], writes=[B_sp])
            p.op("dve", lambda e: e.tensor_sub(out=sl(res), in0=sl(src), in1=sl(tmp)), reads=[B_sp], writes=[B_sp])
            p.op("dve", lambda e: e.tensor_copy(out=sl(o1), in_=sl(res)), reads=[B_sp], writes=[B_sp])
            p.op("dve", lambda e: e.tensor_copy(out=sl(tmp), in_=sl(o1)), reads=[B_sp], writes=[B_sp])
            p.op("dve", lambda e: e.tensor_sub(out=sl(res), in0=sl(res), in1=sl(tmp)), reads=[B_sp], writes=[B_sp])
            p.op("dve", lambda e: e.tensor_copy(out=sl(o2), in_=sl(res)), reads=[B_sp], writes=[B_sp])

        split3(l2t, r1, tmpf, parts, 128)
        TtR = A.f32(S)
        Tt = [(TtR[:, i * 1024:(i + 1) * 1024].bitcast(BF16), Buf("Tt%d" % i)) for i in range(2)]
        for i in range(16):
            tt, tb = Tt[i % 2]
            p.op("dve", lambda e, i=i, tt=tt: e.tensor_scalar(out=tt, in0=kpos_b, scalar1=kpos_col[:, i:i + 1], scalar2=None,
                                                             op0=ALU.is_ge),
                 reads=[B_pos], writes=[tb])
            for t in range(4):
                for k in range(3):
                    p.op("pe", lambda e, i=i, t=t, k=k, tt=tt: e.matmul(
                        PS[t][0:16, :], parts[k][:, i * 16:(i + 1) * 16], tt[:, t * 512:(t + 1) * 512],
                        start=(i == 0 and k == 0), stop=(i == 15 and k == 2)),
                        reads=[B_sp, tb], writes=[PSB[t]], inc=(k == 2 and t == 3))
        cn = A.f32(S)
        cres = l2
        ctmp = TtR
        cparts = [A.bf16(S) for _ in range(3)]
        nparts = [A.bf16(S) for _ in range(3)]
        for t in range(4):
            p.op("dve", lambda e, t=t: e.tensor_copy(out=cn[0:16, t * 512:(t + 1) * 512], in_=PS[t][0:16, :]),
                 reads=[PSB[t]], writes=[B_sp])
        p.barrier()
        split3(cn, cres, ctmp, cparts, 16)
        cum_s = dscr("cum_s", [6, 16, S], BF16)
        B_cums = Buf("cum_s")
        for k in range(3):
            p.op("dve", lambda e, k=k: e.tensor_scalar(out=nparts[k][0:16], in0=cparts[k][0:16], scalar1=-1.0, scalar2=None,
                                                       op0=ALU.mult), reads=[B_sp], writes=[B_sp])
        for k in range(3):
            p.dma("sp", cum_s[k], cparts[k][0:16], reads=[B_sp], writes=[B_cums])
            p.dma("sp", cum_s[3 + k], nparts[k][0:16], reads=[B_sp], writes=[B_cums])
        dbg_dump("cn", cn, [128, S], B_sp)
        p.barrier()
        A.pop()

        A.pop()
        A.push()
        penA = A.bf16(T)
        penB = A.bf16(S)
        B_pen = Buf("pen")
        p.dma("pool", penA[0:32, :], inp("penA"), writes=[B_pen])
        p.dma("pool", penB[0:32, :], inp("penB"), writes=[B_pen])
        scmS = [(A.f32(S), Buf("scm%d" % i)) for i in range(4)]
        workS = [(A.f32(S), Buf("work%d" % i)) for i in range(4)]
        selcS = [(A.bf16(S), Buf("selc%d" % i)) for i in range(2)]
        m8S = [(A.f32(8), Buf("m8%d" % i)) for i in range(2)]
        thrS = [A.f32(1) for i in range(2)]
        rb = [(A.bf16(512), Buf("r%d" % i)) for i in range(4)]
        dgb = [(A.bf16(128), Buf("dg%d" % i)) for i in range(4)]
        ri = [0]
        pi = [0]

        def score_phase(j):
            scm, B_scm = scmS[j % 4]
            work, B_work = workS[j % 4]
            pieces = [0, 2] if j < 4 else [0, 1, 2, 3]
            for ip, pc in enumerate(pieces):
                p.op("pe", lambda e, ip=ip, pc=pc: e.matmul(
                    PS[4 + ip], penA[0:32, j * 128:(j + 1) * 128], penB[0:32, pc * 512:(pc + 1) * 512],
                    start=True, stop=False), reads=[B_pen], writes=[PSB[4 + ip]], inc=False)
            tiles = [(hi, ip, pc) for hi in range(32) for ip, pc in enumerate(pieces)]
            nt = len(tiles)

            def emit_R(t):
                hi, ip, pc = tiles[t]
                c, hb = hi // 2, (hi % 2) * 64
                if ip == 0:
                    dg, B_dg = dgb[hi % 4]
                    p.op("pool", lambda e, dg=dg, hi=hi: e.tensor_scalar(
                        out=dg, in0=ident_f, scalar1=w_tok[:, j, hi:hi + 1], scalar2=None, op0=ALU.mult),
                        reads=[B_const, B_wtok], writes=[B_dg])
                b = t % 4
                p.op("pe", lambda e, b=b, c=c, hb=hb, pc=pc: e.matmul(
                    PS[b], qidxT[hb:hb + 64, c, j * 128:(j + 1) * 128], kidxT[hb:hb + 64, pc * 512:(pc + 1) * 512],
                    start=True, stop=True), reads=[B_qidx, B_kidx], writes=[PSB[b]])

            LA = 3
            for t in range(min(LA, nt)):
                emit_R(t)
            for t in range(nt):
                if t + LA < nt:
                    emit_R(t + LA)
                hi, ip, pc = tiles[t]
                b = t % 4
                r, rbuf = rb[ri[0] % 4]
                ri[0] += 1
                dg, B_dg = dgb[hi % 4]
                p.op("act", lambda e, b=b, r=r: e.activation(out=r, in_=PS[b], func=AF.Relu),
                     reads=[PSB[b]], writes=[rbuf])
                p.op("pe", lambda e, ip=ip, dg=dg, r=r, hi=hi: e.matmul(
                    PS[4 + ip], dg, r, start=False, stop=(hi == 31)),
                    reads=[B_dg, rbuf], writes=[PSB[4 + ip]])
            nv = (j + 1) * 128
            for ip, pc in enumerate(pieces):
                wv = min(512, nv - (pc % 2) * 512)
                if wv <= 0:
                    continue
                d0 = (0 if pc < 2 else nv) + (pc % 2) * 512
                p.op("act", lambda e, ip=ip, d0=d0, wv=wv: e.activation(out=scm[:, d0:d0 + wv], in_=PS[4 + ip][:, 0:wv], func=AF.Copy),
                     reads=[PSB[4 + ip]], writes=[B_scm])
                if j > 0:
                    p.op("act", lambda e, ip=ip, d0=d0, wv=wv: e.activation(out=work[:, d0:d0 + wv], in_=PS[4 + ip][:, 0:wv], func=AF.Copy),
                         reads=[PSB[4 + ip]], writes=[B_work])

        def topk_pair(js):
            Ws = [2 * (j + 1) * 128 for j in js]
            for rnd in range(32):
                for a, j in enumerate(js):
                    if j == 0:
                        continue
                    work, B_work = workS[j % 4]
                    m8, B_m8 = m8S[a]
                    W = Ws[a]
                    p.op("dve", lambda e, W=W, work=work, m8=m8: e.max(out=m8, in_=work[:, 0:W]), reads=[B_work], writes=[B_m8])
                if rnd < 31:
                    for a, j in enumerate(js):
                        if j == 0:
                            continue
                        work, B_work = workS[j % 4]
                        m8, B_m8 = m8S[a]
                        W = Ws[a]
                        p.op("dve", lambda e, W=W, work=work, m8=m8: e.match_replace(
                            out=work[:, 0:W], in_to_replace=m8, in_values=work[:, 0:W], imm_value=-1e30),
                            reads=[B_m8, B_work], writes=[B_work])
            for a, j in enumerate(js):
                scm, B_scm = scmS[j % 4]
                m8, B_m8 = m8S[a]
                selc, B_selc = selcS[a]
                thr = thrS[a]
                W = Ws[a]
                if j == 0:
                    p.op("dve", lambda e, thr=thr: e.memset(thr, -1e29), reads=[B_m8], writes=[B_m8])
                else:
                    p.op("dve", lambda e, thr=thr, m8=m8: e.tensor_scalar(out=thr, in0=m8[:, 7:8], scalar1=-1e29, scalar2=None,
                                                                         op0=ALU.max), reads=[B_m8], writes=[B_m8])
                p.op("dve", lambda e, W=W, selc=selc, scm=scm, thr=thr: e.tensor_scalar(
                    out=selc[:, 0:W], in0=scm[:, 0:W], scalar1=thr, scalar2=None, op0=ALU.is_ge),
                    reads=[B_m8, B_scm], writes=[B_selc])
                for base_blk, kt_base in ((0, 0), (j + 1, 8)):
                    for g0 in range(0, j + 1, 4):
                        n4 = min(4, j + 1 - g0)
                        b = pi[0] % 4
                        pi[0] += 1
                        psb = PS[b].bitcast(BF16)
                        for i4 in range(n4):
                            i = base_blk + g0 + i4
                            p.op("pe", lambda e, psb=psb, i=i, i4=i4, selc=selc: e.transpose(
                                psb[:, i4 * 128:(i4 + 1) * 128], selc[:, i * 128:(i + 1) * 128], ident_b),
                                 reads=[B_selc, B_const], writes=[PSB[b]], inc=(i4 == n4 - 1))
                        kt0 = kt_base + g0
                        p.op("act", lambda e, psb=psb, kt0=kt0, j=j, n4=n4: e.activation(
                            out=selT[:, kt0:kt0 + n4, j * 128:(j + 1) * 128],
                            in_=psb[:, 0:n4 * 128].rearrange("p (k q) -> p k q", k=n4),
                            func=AF.Copy), reads=[PSB[b]], writes=[B_selT])

        p.op("dve", lambda e: e.memset(selT.rearrange("p k t -> p (k t)"), 0.0), writes=[B_selT])
        score_phase(0)
        score_phase(1)
        for i in range(4):
            if i < 3:
                score_phase(2 * i + 2)
                score_phase(2 * i + 3)
            topk_pair((2 * i, 2 * i + 1))
        dbg_dump("selT", selT, [128, 16, T], B_selT, BF16)
        p.barrier()
        A.pop()

        if stage <= 2:
            p.wait_all_on("sp")
            p.emit()
            return nc, dbg_out, list(IN)

        A.pop()
        KTS = [[0, 1, 2, 3, 8, 9, 10, 11], list(range(16))]
        tmpb = [(A.f32(512), Buf("tmp%d" % i)) for i in range(4)]
        ptb = [(A.bf16(512), Buf("pt%d" % i)) for i in range(4)]
        rec = A.f32(512)
        B_rec = Buf("rec")
        ostb = [(A.bf16(512), Buf("ost%d" % i)) for i in range(2)]
        qTb = [(A.bf16(T), Buf("qT%d" % i)) for i in range(2)]
        kTb = [(A.bf16(S), Buf("kT%d" % i)) for i in range(2)]
        Vb = [(A.bf16(S).rearrange("p (k d) -> p k d", k=16), Buf("V%d" % i)) for i in range(2)]
        cnt = {"s": 0, "tmp": 0, "pt": 0, "ost": 0, "acc": 0}

        def attn_core(h_glob, qT, B_q, kT, B_k, V, B_V, pre_exp, bias_mm):
            for J in range(2):
                kts = KTS[J]
                n = len(kts)
                oacc = 4 + (cnt["acc"] % 2)
                dacc = 6 + (cnt["acc"] % 2)
                cnt["acc"] += 1
                sbank = {}

                def emit_S(idx):
                    kt = kts[idx]
                    b = cnt["s"] % 4
                    cnt["s"] += 1
                    sbank[idx] = b
                    last = bias_mm is None
                    p.op("pe", lambda e, b=b, kt=kt, J=J, last=last: e.matmul(
                        PS[b], kT[:, kt * 128:(kt + 1) * 128], qT[:, J * 512:(J + 1) * 512], start=True, stop=last),
                         reads=[B_k, B_q], writes=[PSB[b]], inc=last)
                    if bias_mm is not None:
                        bias_mm(J, kt, b)

                LA = 3
                for i0 in range(min(LA, n)):
                    emit_S(i0)
                for idx in range(n):
                    kt = kts[idx]
                    if idx + LA < n:
                        emit_S(idx + LA)
                    b = sbank[idx]
                    tmp, B_tmp = tmpb[cnt["tmp"] % 4]
                    cnt["tmp"] += 1
                    pt, B_pt = ptb[cnt["pt"] % 4]
                    cnt["pt"] += 1
                    pre_exp(J, kt, b, tmp, B_tmp)
                    p.op("act", lambda e, tmp=tmp, pt=pt: e.activation(out=pt, in_=tmp, func=AF.Exp),
                         reads=[B_tmp], writes=[B_pt])
                    p.op("pe", lambda e, kt=kt, pt=pt, idx=idx, oacc=oacc, n=n: e.matmul(
                        PS[oacc], V[:, kt, :], pt, start=(idx == 0), stop=(idx == n - 1)),
                         reads=[B_V, B_pt], writes=[PSB[oacc]], inc=False)
                    p.op("pe", lambda e, pt=pt, idx=idx, dacc=dacc, n=n: e.matmul(
                        PS[dacc], ones_b, pt, start=(idx == 0), stop=(idx == n - 1)),
                         reads=[B_const, B_pt], writes=[PSB[dacc]], inc=True)
                ost, B_ost = ostb[cnt["ost"] % 2]
                cnt["ost"] += 1
                p.op("dve", lambda e, dacc=dacc: e.reciprocal(out=rec, in_=PS[dacc]), reads=[PSB[dacc]], writes=[B_rec])
                p.op("dve", lambda e, ost=ost, oacc=oacc: e.tensor_tensor(out=ost, in0=PS[oacc], in1=rec, op=ALU.mult),
                     reads=[PSB[oacc], B_rec], writes=[B_ost])
                p.dma("sp", oT_s[h_glob][:, J * 512:(J + 1) * 512], ost, reads=[B_ost], writes=[B_oTs])

        B_oTs = Buf("oT_s")
        B_scr = Buf("scrA")

        A.push()
        wuk = A.bf16(2 * 2048).rearrange("p (c n) -> p c n", c=2)
        wuv = A.bf16(2 * 2048).rearrange("p (c n) -> p c n", c=2)
        B_wu = Buf("wu")
        p.dma("pool", wuk, inp("wuk_t"), writes=[B_wu])
        p.dma("pool", wuv, inp("wuv_t"), writes=[B_wu])
        Dm = A.f32(24 * 512).rearrange("p (k q) -> p k q", k=24)
        B_Dm = Buf("Dm")
        dmi = {}
        for J in range(2):
            for kt in KTS[J]:
                i = len(dmi)
                dmi[(J, kt)] = i
                dst = Dm[:, i, :]
                p.op("dve", lambda e, dst=dst, J=J, kt=kt: e.tensor_scalar(
                    out=dst, in0=qpos_b[:, J * 512:(J + 1) * 512], scalar1=kpos_col[:, kt:kt + 1], scalar2=None,
                    op0=ALU.subtract), reads=[B_pos], writes=[B_Dm])
                p.op("dve", lambda e, dst=dst: e.scalar_tensor_tensor(out=dst, in0=dst, scalar=-1.0, in1=dst,
                                                                      op0=ALU.mult, op1=ALU.max),
                     reads=[B_Dm], writes=[B_Dm])
                p.op("dve", lambda e, dst=dst: e.tensor_scalar(out=dst, in0=dst, scalar1=1.0e6, scalar2=None, op0=ALU.add),
                     reads=[B_Dm], writes=[B_Dm])
                p.op("dve", lambda e, dst=dst, J=J, kt=kt: e.scalar_tensor_tensor(
                    out=dst, in0=selT[:, kt, J * 512:(J + 1) * 512], scalar=-1.0e6, in1=dst, op0=ALU.mult, op1=ALU.add),
                    reads=[B_Dm, B_selT], writes=[B_Dm])
        for h in range(NH_A):
            qT, B_q = qTb[h % 2]
            kT, B_k = kTb[h % 2]
            V, B_V = Vb[h % 2]
            p.dma("sp", qT, qA_s[h], writes=[B_q])
            for t in range(4):
                for c in range(2):
                    p.op("pe", lambda e, t=t, c=c, h=h: e.matmul(PS[t], wuk[:, c, h * 128:(h + 1) * 128],
                                                                 ckvn[:, c, t * 512:(t + 1) * 512], start=(c == 0), stop=(c == 1)),
                         reads=[B_wu, B_ckvn], writes=[PSB[t]], inc=(c == 1))
                p.op("act", lambda e, t=t, kT=kT: e.activation(out=kT[:, t * 512:(t + 1) * 512], in_=PS[t], func=AF.Copy),
                     reads=[PSB[t]], writes=[B_k])
            for t in range(4):
                for k4 in range(4):
                    kt = t * 4 + k4
                    for c in range(2):
                        p.op("pe", lambda e, t=t, k4=k4, kt=kt, c=c, h=h: e.matmul(
                            PS[t][:, k4 * 128:(k4 + 1) * 128], ckvn[:, c, kt * 128:(kt + 1) * 128],
                            wuv[:, c, h * 128:(h + 1) * 128], start=(c == 0), stop=(c == 1)),
                            reads=[B_wu, B_ckvn], writes=[PSB[t]], inc=(c == 1 and k4 == 3))
                p.op("act", lambda e, t=t, V=V: e.activation(out=V[:, t * 4:(t + 1) * 4, :],
                                                             in_=PS[t].rearrange("p (k d) -> p k d", k=4), func=AF.Copy),
                     reads=[PSB[t]], writes=[B_V])

            def pre_exp(J, kt, b, tmp, B_tmp, h=h):
                i = dmi[(J, kt)]
                p.op("dve", lambda e: e.scalar_tensor_tensor(out=tmp, in0=Dm[:, i, :], scalar=-SLOPES[h], in1=PS[b],
                                                             op0=ALU.mult, op1=ALU.add),
                     reads=[B_Dm, PSB[b]], writes=[B_tmp])

            attn_core(h, qT, B_q, kT, B_k, V, B_V, pre_exp, None)
        p.barrier()
        A.pop()

        A.push()
        Mk = A.bf16(24 * 512).rearrange("p (k q) -> p k q", k=24)
        B_Mk = Buf("Mk")
        for J in range(2):
            for kt in KTS[J]:
                i = dmi[(J, kt)]
                p.op("dve", lambda e, i=i, J=J, kt=kt: e.tensor_scalar(
                    out=Mk[:, i, :], in0=qpos_b[:, J * 512:(J + 1) * 512], scalar1=kpos_col[:, kt:kt + 1], scalar2=NEG,
                    op0=ALU.is_lt, op1=ALU.mult), reads=[B_pos], writes=[B_Mk])
        vTb = [(A.bf16(S), Buf("vT%d" % i)) for i in range(2)]
        Aopb = [(A.bf16(S), Buf("Aop%d" % i)) for i in range(2)]
        Bopb = [(A.bf16(T), Buf("Bop%d" % i)) for i in range(2)]
        for i in range(2):
            p.op("dve", lambda e, i=i: e.memset(Aopb[i][0][0:32, :], 1.0), writes=[Aopb[i][1]])
            p.op("dve", lambda e, i=i: e.memset(Bopb[i][0][0:32, :], 1.0), writes=[Bopb[i][1]])
        for h in range(NH_B):
            qT, B_q = qTb[h % 2]
            kT, B_k = kTb[h % 2]
            V, B_V = Vb[h % 2]
            vT, B_vT = vTb[h % 2]
            Aop, B_A = Aopb[h % 2]
            Bop, B_B = Bopb[h % 2]
            p.dma("sp", qT, qB_s[h], writes=[B_q])
            p.dma("sp", kT, kB_s[h], writes=[B_k])
            p.dma("sp", vT, vB_s[h], writes=[B_vT])
            p.dma("sp", Aop[3:6, :], cum_s[0:3, h, :], writes=[B_A])
            p.dma("sp", Bop[0:3, :], cum_s[3:6, h, 0:T], writes=[B_B])
            for half in range(2):
                psb = PS[half].bitcast(BF16)
                for k8 in range(8):
                    kt = half * 8 + k8
                    p.op("pe", lambda e, psb=psb, k8=k8, kt=kt, vT=vT: e.transpose(
                        psb[:, k8 * 128:(k8 + 1) * 128], vT[:, kt * 128:(kt + 1) * 128], ident_b),
                        reads=[B_vT, B_const], writes=[PSB[half]], inc=(k8 == 7))
                p.op("act", lambda e, psb=psb, half=half, V=V: e.activation(
                    out=V[:, half * 8:(half + 1) * 8, :], in_=psb.rearrange("p (k d) -> p k d", k=8), func=AF.Copy),
                    reads=[PSB[half]], writes=[B_V])

            def bias_mm(J, kt, b, Aop=Aop, Bop=Bop, B_A=B_A, B_B=B_B):
                p.op("pe", lambda e: e.matmul(PS[b], Aop[0:6, kt * 128:(kt + 1) * 128], Bop[0:6, J * 512:(J + 1) * 512],
                                              start=False, stop=True),
                     reads=[B_A, B_B], writes=[PSB[b]], inc=True)

            def pre_exp(J, kt, b, tmp, B_tmp):
                i = dmi[(J, kt)]
                p.op("dve", lambda e: e.tensor_tensor(out=tmp, in0=PS[b], in1=Mk[:, i, :], op=ALU.add),
                     reads=[B_Mk, PSB[b]], writes=[B_tmp])

            attn_core(16 + h, qT, B_q, kT, B_k, V, B_V, pre_exp, bias_mm)
        p.barrier()
        A.pop()
        if "oT" in dbg:
            o = dout("dbg_oT", [32, 128, T], BF16)
            ot_sb = A.bf16(T)
            B_otsb = Buf("otsb")
            for i in range(32):
                p.dma("sp", ot_sb, oT_s[i], reads=[B_oTs], writes=[B_otsb])
                p.dma("sp", o[i], ot_sb, reads=[B_otsb])

        if stage <= 3:
            p.wait_all_on("sp")
            p.emit()
            return nc, dbg_out, list(IN)

        arena_reset()
        mg_s = dscr("mg_s", [32, 128, T], BF16)
        B_mgs = Buf("mg_s")
        xT = A.bf16(32 * T).rearrange("p (k t) -> p k t", k=32)
        oT = A.bf16(32 * T).rearrange("p (k t) -> p k t", k=32)
        B_x, B_o = Buf("xT"), Buf("oT")
        wtiles = [(A.bf16(32 * 128), Buf("w%d" % i)) for i in range(4)]
        bgate = A.f32(64)
        B_bg = Buf("bgate")
        p.dma("sp", bgate, inp("b_gate_t"), writes=[B_bg])
        for k4 in range(4):
            p.dma("pool", xT[:, k4 * 8:(k4 + 1) * 8, :], xT_v[:, k4 * 8:(k4 + 1) * 8, 0:T], writes=[B_x], group=(k4 > 0))
        for k4 in range(8):
            p.dma("sp", oT[:, k4 * 4:(k4 + 1) * 4, :], oT_s[k4 * 4:(k4 + 1) * 4].rearrange("c p t -> p c t"),
                  reads=[B_oTs], writes=[B_o], group=(k4 > 0))
        gsig = [(A.f32(T), Buf("gsig%d" % i)) for i in range(2)]
        m1 = A.f32(T)
        B_m1 = Buf("m1")
        mgst = [(A.bf16(T), Buf("mgst%d" % i)) for i in range(2)]
        slots2 = [[0, 1], [2, 3], [4, 5], [6, 7]]
        gi = [0]
        wi = [0]
        si = [0]

        def one_chunk(xin, xbuf, KC, w_dram, c, evac):
            wt, wb = wtiles[wi[0] % 4]
            wi[0] += 1
            banks = slots2[si[0] % 4]
            si[0] += 1
            p.dma("pool", wt[:, 0:KC * 128].rearrange("p (k n) -> p k n", k=KC), w_dram[c], writes=[wb])
            for kc in range(KC):
                for t in range(2):
                    b = banks[t]
                    p.op("pe", lambda e, b=b, kc=kc, t=t, wt=wt: e.matmul(
                        PS[b], wt[:, kc * 128:(kc + 1) * 128], xin[:, kc, t * 512:(t + 1) * 512],
                        start=(kc == 0), stop=(kc == KC - 1)),
                        reads=[wb, xbuf], writes=[PSB[b]], inc=(kc == KC - 1 and t == 1))
            evac(banks)

        w_gate_d, w_ba_d, w_bb_d = inp("w_gate_t"), inp("w_ba_t"), inp("w_bb_t")
        for n in range(32):
            ga, B_ga = gsig[0]
            gb, B_gb = gsig[1]
            mst, B_mst = mgst[n % 2]

            def ev_gate(dst, B_dst, col):
                def ev(banks):
                    for t, b in enumerate(banks):
                        p.op("act", lambda e, b=b, t=t: e.activation(
                            out=dst[:, t * 512:(t + 1) * 512], in_=PS[b], func=AF.Sigmoid, bias=bgate[:, col:col + 1], scale=1.0),
                            reads=[PSB[b], B_bg], writes=[B_dst])
                return ev

            def ev_ba(banks):
                for t, b in enumerate(banks):
                    p.op("dve", lambda e, b=b, t=t: e.tensor_tensor(
                        out=m1[:, t * 512:(t + 1) * 512], in0=PS[b], in1=ga[:, t * 512:(t + 1) * 512], op=ALU.mult),
                        reads=[PSB[b], B_ga], writes=[B_m1])

            def ev_bb(banks, mst=mst, B_mst=B_mst, n=n):
                for t, b in enumerate(banks):
                    p.op("dve", lambda e, b=b, t=t: e.tensor_tensor(
                        out=gb[:, t * 512:(t + 1) * 512], in0=PS[b], in1=gb[:, t * 512:(t + 1) * 512], op=ALU.mult),
                        reads=[PSB[b], B_gb], writes=[B_gb])
                p.op("dve", lambda e: e.tensor_tensor(out=mst, in0=gb, in1=m1, op=ALU.add),
                     reads=[B_gb, B_m1], writes=[B_mst])
                p.dma("sp", mg_s[n], mst, reads=[B_mst], writes=[B_mgs])

            one_chunk(xT, B_x, 32, w_gate_d, n, ev_gate(ga, B_ga, n))
            one_chunk(oT[:, 0:16, :], B_o, 16, w_ba_d, n, ev_ba)
            one_chunk(xT, B_x, 32, w_gate_d, 32 + n, ev_gate(gb, B_gb, 32 + n))
            one_chunk(oT[:, 16:32, :], B_o, 16, w_bb_d, n, ev_bb)
        if "mg" in dbg:
            o = dout("dbg_mg", [32, 128, T], BF16)
            for i in range(32):
                p.dma("sp", mgst[0][0], mg_s[i], reads=[B_mgs], writes=[mgst[0][1]])
                p.dma("sp", o[i], mgst[0][0], reads=[mgst[0][1]])

        if stage <= 3.3:
            p.wait_all_on("sp")
            p.emit()
            return nc, dbg_out, list(IN)

        arena_reset()
        mgT = A.bf16(32 * T).rearrange("p (k t) -> p k t", k=32)
        B_mg = Buf("mgT")
        for k4 in range(8):
            p.dma("sp", mgT[:, k4 * 4:(k4 + 1) * 4, :], mg_s[k4 * 4:(k4 + 1) * 4].rearrange("c p t -> p c t"),
                  reads=[B_mgs], writes=[B_mg], group=(k4 > 0))
        wtiles = [(A.bf16(32 * 128), Buf("w%d" % i)) for i in range(3)]
        xf = [(A.f32(T), Buf("xf%d" % i)) for i in range(2)]
        zf = [(A.f32(T), Buf("zf%d" % i)) for i in range(2)]
        zq = [(A.bf16(T), Buf("zq%d" % i)) for i in range(2)]
        zbb = [(A.bf16(T), Buf("zbb%d" % i)) for i in range(2)]
        z1_s = dscr("z1_s", [32, 128, T], F32)
        B_z1s = Buf("z1_s")
        slots_c2 = [[0, 1], [2, 3]]
        w_out_d = inp("w_out_t")
        xT_rows = inp("xT")

        def ln_stats_mm(n, z, B_z, q, B_q, nchunks):
            for t in range(2):
                p.op("pe", lambda e, t=t: e.matmul(PS[4 + t], ones_b, z[:, t * 512:(t + 1) * 512],
                                                   start=(n == 0), stop=(n == nchunks - 1)),
                     reads=[B_z, B_const], writes=[PSB[4 + t]], inc=False)
                p.op("pe", lambda e, t=t: e.matmul(PS[6 + t], ones_b, q[:, t * 512:(t + 1) * 512],
                                                   start=(n == 0), stop=(n == nchunks - 1)),
                     reads=[B_q, B_const], writes=[PSB[6 + t]], inc=True)

        for n in range(32):
            wt, wb = wtiles[n % 3]
            banks = slots_c2[n % 2]
            x_f, B_xf = xf[n % 2]
            z_f, B_zf = zf[n % 2]
            z_q, B_zq = zq[n % 2]
            p.dma("pool", wt.rearrange("p (k n) -> p k n", k=32), w_out_d[n], writes=[wb])
            p.dma("sp", x_f, xT_rows[n * 128:(n + 1) * 128, 0:T], writes=[B_xf])
            for kc in range(32):
                for t in range(2):
                    b = banks[t]
                    p.op("pe", lambda e, b=b, kc=kc, t=t, wt=wt: e.matmul(
                        PS[b], wt[:, kc * 128:(kc + 1) * 128], mgT[:, kc, t * 512:(t + 1) * 512],
                        start=(kc == 0), stop=(kc == 31)),
                        reads=[wb, B_mg], writes=[PSB[b]], inc=(kc == 31 and t == 1))
            for t, b in enumerate(banks):
                p.op("dve", lambda e, b=b, t=t, x_f=x_f, z_f=z_f: e.scalar_tensor_tensor(
                    out=z_f[:, t * 512:(t + 1) * 512], in0=x_f[:, t * 512:(t + 1) * 512], scalar=ALPHA, in1=PS[b],
                    op0=ALU.mult, op1=ALU.add), reads=[PSB[b], B_xf], writes=[B_zf])
            z_b, B_zb = zbb[n % 2]
            p.op("act", lambda e, z_f=z_f, z_q=z_q: e.activation(out=z_q, in_=z_f, func=AF.Square),
                 reads=[B_zf], writes=[B_zq])
            p.op("act", lambda e, z_f=z_f, z_b=z_b: e.activation(out=z_b, in_=z_f, func=AF.Copy),
                 reads=[B_zf], writes=[B_zb])
            p.dma("sp", z1_s[n], z_f, reads=[B_zf], writes=[B_z1s])
            ln_stats_mm(n, z_b, B_zb, z_q, B_zq, 32)

        if stage <= 3.6:
            p.wait_all_on("sp")
            p.emit()
            return nc, dbg_out, list(IN)

        def ln_finish():
            Mt = A.f32(T)
            Rt = A.f32(T)
            B_MR = Buf("MR")
            for t in range(2):
                sl = slice(t * 512, (t + 1) * 512)
                p.op("dve", lambda e, t=t, sl=sl: e.tensor_scalar(out=Mt[:, sl], in0=PS[4 + t], scalar1=1.0 / D, scalar2=None,
                                                                 op0=ALU.mult), reads=[PSB[4 + t]], writes=[B_MR])
                p.op("dve", lambda e, t=t, sl=sl: e.tensor_scalar(out=Rt[:, sl], in0=PS[6 + t], scalar1=1.0 / D, scalar2=EPS,
                                                                 op0=ALU.mult, op1=ALU.add), reads=[PSB[6 + t]], writes=[B_MR])
            msq = A.f32(T)
            p.op("dve", lambda e: e.tensor_tensor(out=msq, in0=Mt, in1=Mt, op=ALU.mult), reads=[B_MR], writes=[B_MR])
            p.op("dve", lambda e: e.tensor_sub(out=Rt, in0=Rt, in1=msq), reads=[B_MR], writes=[B_MR])
            p.op("act", lambda e: e.activation(out=Rt, in_=Rt, func=AF.Sqrt), reads=[B_MR], writes=[B_MR])
            p.op("dve", lambda e: e.reciprocal(out=Rt, in_=Rt), reads=[B_MR], writes=[B_MR])
            return Mt, Rt, B_MR

        def ln_apply(src_s, B_src, g_name, b_name, sink):
            Mt, Rt, B_MR = ln_finish()
            gcol = A.f32(32)
            bcol = A.f32(32)
            B_gb = Buf("gb")
            p.dma("sp", gcol, inp(g_name), writes=[B_gb])
            p.dma("sp", bcol, inp(b_name), writes=[B_gb])
            zb = [(A.f32(T), Buf("lz%d" % i)) for i in range(4)]
            for n in range(32):
                z, B_z = zb[n % 4]
                p.dma("sp", z, src_s[n], reads=[B_src], writes=[B_z])
                p.op("dve", lambda e, z=z: e.tensor_sub(out=z, in0=z, in1=Mt), reads=[B_MR, B_z], writes=[B_z])
                p.op("dve", lambda e, z=z: e.tensor_tensor(out=z, in0=z, in1=Rt, op=ALU.mult), reads=[B_MR, B_z], writes=[B_z])
                p.op("dve", lambda e, z=z, n=n: e.tensor_scalar(out=z, in0=z, scalar1=gcol[:, n:n + 1], scalar2=bcol[:, n:n + 1],
                                                               op0=ALU.mult, op1=ALU.add), reads=[B_gb, B_z], writes=[B_z])
                sink(n, z, B_z)

        arena_reset()
        h1_s = dscr("h1_s", [32, 128, T], F32)
        B_h1s = Buf("h1_s")
        h1T = A.bf16(32 * T).rearrange("p (k t) -> p k t", k=32)
        B_h1 = Buf("h1T")

        def sink1(n, z, B_z):
            p.dma("sp", h1_s[n], z, reads=[B_z], writes=[B_h1s])
            p.op("act", lambda e: e.activation(out=h1T[:, n, :], in_=z, func=AF.Copy), reads=[B_z], writes=[B_h1])

        ln_apply(z1_s, B_z1s, "ln1g", "ln1b", sink1)
        if "h1" in dbg:
            o = dout("dbg_h1", [32, 128, T], F32)
            tmp_h = A.f32(T)
            B_th = Buf("tmp_h")
            for i in range(32):
                p.dma("sp", tmp_h, h1_s[i], reads=[B_h1s], writes=[B_th])
                p.dma("sp", o[i], tmp_h, reads=[B_th])

        if stage <= 4:
            p.wait_all_on("sp")
            p.emit()
            return nc, dbg_out, list(IN)

        z2_s = dscr("z2_s", [32, 128, T], F32)
        B_z2s = Buf("z2_s")
        A.off = A_BASE + (2 * 32 * T + 3) // 4
        pTb = A.bf16(2 * T).rearrange("p (k t) -> p k t", k=2)
        B_pT = Buf("pT")
        p.barrier()
        p.dma("pool", pTb, inp("pT").rearrange("(k p) t -> p k t", p=128), writes=[B_pT])
        bpg = A.f32(32)
        B_bpg = Buf("bpg")
        p.dma("sp", bpg, inp("b_pg_t"), writes=[B_bpg])
        wtiles = [(A.bf16(32 * 128), Buf("w%d" % i)) for i in range(3)]
        wple = [(A.bf16(2 * 128), Buf("wple%d" % i)) for i in range(2)]
        sg = [(A.f32(T), Buf("sg%d" % i)) for i in range(2)]
        hf = [(A.f32(T), Buf("hf%d" % i)) for i in range(2)]
        w_pg_d, w_ple_d = inp("w_pg_t"), inp("w_ple_t")
        slots_d = [[0, 1], [2, 3], [4, 5], [6, 7]]
        for n in range(32):
            wt, wb = wtiles[n % 3]
            wp, wpb = wple[n % 2]
            s_g, B_sg = sg[n % 2]
            h_f, B_hf = hf[n % 2]
            bg_ = slots_d[(2 * n) % 4]
            bp_ = slots_d[(2 * n + 1) % 4]
            p.dma("pool", wt.rearrange("p (k n) -> p k n", k=32), w_pg_d[n], writes=[wb])
            p.dma("pool", wp.rearrange("p (k n) -> p k n", k=2), w_ple_d[n], writes=[wpb])
            p.dma("sp", h_f, h1_s[n], reads=[B_h1s], writes=[B_hf])
            for kc in range(32):
                for t in range(2):
                    b = bg_[t]
                    p.op("pe", lambda e, b=b, kc=kc, t=t, wt=wt: e.matmul(
                        PS[b], wt[:, kc * 128:(kc + 1) * 128], h1T[:, kc, t * 512:(t + 1) * 512],
                        start=(kc == 0), stop=(kc == 31)),
                        reads=[wb, B_h1], writes=[PSB[b]], inc=(kc == 31 and t == 1))
            for kc in range(2):
                for t in range(2):
                    b = bp_[t]
                    p.op("pe", lambda e, b=b, kc=kc, t=t, wp=wp: e.matmul(
                        PS[b], wp[:, kc * 128:(kc + 1) * 128], pTb[:, kc, t * 512:(t + 1) * 512],
                        start=(kc == 0), stop=(kc == 1)),
                        reads=[wpb, B_pT], writes=[PSB[b]], inc=(kc == 1 and t == 1))
            for t in range(2):
                sl = slice(t * 512, (t + 1) * 512)
                p.op("act", lambda e, t=t, sl=sl, s_g=s_g, n=n, b=bg_[t]: e.activation(
                    out=s_g[:, sl], in_=PS[b], func=AF.Sigmoid, bias=bpg[:, n:n + 1], scale=1.0),
                    reads=[PSB[bg_[t]], B_bpg], writes=[B_sg])
                p.op("dve", lambda e, sl=sl, s_g=s_g, b=bp_[t]: e.tensor_tensor(
                    out=s_g[:, sl], in0=PS[b], in1=s_g[:, sl], op=ALU.mult),
                    reads=[PSB[bp_[t]], B_sg], writes=[B_sg])
            p.op("dve", lambda e, s_g=s_g, h_f=h_f: e.scalar_tensor_tensor(
                out=h_f, in0=h_f, scalar=ALPHA, in1=s_g, op0=ALU.mult, op1=ALU.add),
                reads=[B_sg, B_hf], writes=[B_hf])
            p.dma("sp", z2_s[n], h_f, reads=[B_hf], writes=[B_z2s])

        if stage >= 6:
            p.barrier()
            A.off = A_BASE + (2 * 32 * T + 3) // 4
            qpT = A.alloc_top(2 * 16 * T, BF16).rearrange("p (g t) -> p g t", g=16)
            B_qp = Buf("qpT")
            wtiles = [(A.bf16(32 * 128), Buf("w%d" % i)) for i in range(3)]

            def ev_q(i, c, banks):
                for t, b in enumerate(banks):
                    p.op("act", lambda e, b=b, t=t: e.activation(out=qpT[:, c, t * 512:(t + 1) * 512], in_=PS[b], func=AF.Copy),
                         reads=[PSB[b]], writes=[B_qp])

            gemm(h1T, 32, T, inp("peer_wq_t"), list(range(16)), ev_q, [B_h1], wtiles, [[0, 1], [2, 3], [4, 5], [6, 7]])
            p.barrier()
            A.off = A_BASE
            skb = A.bf16(16 * 128).rearrange("p (g n) -> p g n", g=16)
            iota3 = A.bf16(32 * 128).rearrange("p (t n) -> p t n", t=32)
            B_ec = Buf("e1const")
            p.dma("pool", skb, inp("sk_t"), writes=[B_ec])
            p.dma("pool", iota3, inp("iota_t"), writes=[B_ec])
            S_sb = A.f32(16 * 128).rearrange("p (g n) -> p g n", g=16)
            B_Sb = [Buf("S_sb%d" % i) for i in range(4)]
            V16 = A.f32(16 * 16).rearrange("p (g k) -> p g k", g=16)
            B_Vg = [Buf("V16_%d" % i) for i in range(16)]
            w128g = A.f32(16 * 128).rearrange("p (g n) -> p g n", g=16)
            B_wg = [Buf("w128_%d" % i) for i in range(16)]
            idxu = A.alloc(4 * 128, U32)[:, 0:128].rearrange("p (h k) -> p h k", h=8)
            B_ix = [Buf("ix%d" % i) for i in range(8)]
            cand = A.f32(8 * 256).rearrange("p (h c) -> p h c", h=8)
            B_cd = [Buf("cand%d" % i) for i in range(8)]
            workc = A.f32(8 * 256).rearrange("p (h c) -> p h c", h=8)
            B_wc = [Buf("workc%d" % i) for i in range(8)]
            vals = A.f32(128).rearrange("p (h k) -> p h k", h=8)
            B_vl = [Buf("vals%d" % i) for i in range(8)]
            ev_ = A.f32(128).rearrange("p (h k) -> p h k", h=8)
            Zs = A.f32(8)
            rZ = A.f32(8)
            X = [A.f32(128).rearrange("p (h k) -> p h k", h=8) for _ in range(4)]
            B_X = [Buf("X%d" % i) for i in range(4)]
            XTb = [A.f32(4 * 128).rearrange("p (i t) -> p i t", i=4) for _ in range(2)]
            B_XTb = [Buf("XT0"), Buf("XT1")]
            B_sm = Buf("small")
            S1rep = [(A.f32(32 * 128).rearrange("p (t n) -> p t n", t=32), Buf("S1rep%d" % i)) for i in range(2)]
            L3b = [(A.bf16(32 * 128).rearrange("p (t n) -> p t n", t=32), Buf("L3%d" % i)) for i in range(2)]
            R3b = [(A.bf16(32 * 128).rearrange("p (t n) -> p t n", t=32), Buf("R3%d" % i)) for i in range(2)]
            GTb = [(A.bf16(128 * 64).rearrange("p (y t) -> p y t", y=128), Buf("GT%d" % i)) for i in range(2)]
            gT_h = gT_s
            V1v = V16.rearrange("p (h two) k -> p h two k", two=2)[:, :, 0, :]
            V2v = V16.rearrange("p (h two) k -> p h two k", two=2)[:, :, 1, :]
            gbank = [0]

            def chain(tt):
                for g in range(16):
                    p.op("pe", lambda e, g=g: e.matmul(PS[g // 4][:, (g % 4) * 128:(g % 4 + 1) * 128],
                                                       qpT[:, g, tt * 128:(tt + 1) * 128], skb[:, g, :],
                                                       start=True, stop=True),
                         reads=[B_qp, B_ec], writes=[PSB[g // 4]], inc=(g % 4 == 3))
                for b in range(4):
                    p.op("act", lambda e, b=b: e.activation(out=S_sb[:, b * 4:(b + 1) * 4, :],
                                                            in_=PS[b].rearrange("p (g n) -> p g n", g=4), func=AF.Copy),
                         reads=[PSB[b]], writes=[B_Sb[b]])
                p.dma("sp", s1_s[:, tt * 128:(tt + 1) * 128, :].rearrange("h t n -> t h n"),
                      S_sb.rearrange("p (h two) n -> p h two n", two=2)[:, :, 0, :], reads=B_Sb, writes=[B_s1s])
                for g in range(16):
                    p.op("dve", lambda e, g=g: e.max(out=V16[:, g, 0:8], in_=S_sb[:, g, :]),
                         reads=[B_Sb[g // 4]], writes=[B_Vg[g]])
                for g in range(16):
                    p.op("dve", lambda e, g=g: e.match_replace(out=w128g[:, g, :], in_to_replace=V16[:, g, 0:8],
                                                               in_values=S_sb[:, g, :], imm_value=-1e30),
                         reads=[B_Sb[g // 4], B_Vg[g]], writes=[B_wg[g]])
                for g in range(16):
                    p.op("dve", lambda e, g=g: e.max(out=V16[:, g, 8:16], in_=w128g[:, g, :]),
                         reads=[B_wg[g]], writes=[B_Vg[g]])
                for hd in range(8):
                    g = 2 * hd + 1
                    p.op("dve", lambda e, g=g, hd=hd: e.max_index(out=idxu[:, hd, 0:8], in_max=V16[:, g, 0:8], in_values=S_sb[:, g, :]),
                         reads=[B_Sb[g // 4], B_Vg[g]], writes=[B_ix[hd]])
                for hd in range(8):
                    g = 2 * hd + 1
                    p.op("dve", lambda e, g=g, hd=hd: e.max_index(out=idxu[:, hd, 8:16], in_max=V16[:, g, 8:16], in_values=S_sb[:, g, :]),
                         reads=[B_Sb[g // 4], B_Vg[g]], writes=[B_ix[hd]])
                for hd in range(8):
                    p.op("dve", lambda e, hd=hd: e.tensor_tensor(
                        out=cand[:, hd, :].rearrange("p (a b) -> p a b", a=16),
                        in0=V16[:, 2 * hd, :].unsqueeze(2).to_broadcast([128, 16, 16]),
                        in1=V16[:, 2 * hd + 1, :].unsqueeze(1).to_broadcast([128, 16, 16]), op=ALU.add),
                        reads=[B_Vg[2 * hd], B_Vg[2 * hd + 1]], writes=[B_cd[hd]])
                for hd in range(8):
                    p.op("dve", lambda e, hd=hd: e.max(out=vals[:, hd, 0:8], in_=cand[:, hd, :]),
                         reads=[B_cd[hd]], writes=[B_vl[hd]])
                for hd in range(8):
                    p.op("dve", lambda e, hd=hd: e.match_replace(out=workc[:, hd, :], in_to_replace=vals[:, hd, 0:8],
                                                                 in_values=cand[:, hd, :], imm_value=-1e30),
                         reads=[B_cd[hd], B_vl[hd]], writes=[B_wc[hd]])
                for hd in range(8):
                    p.op("dve", lambda e, hd=hd: e.max(out=vals[:, hd, 8:16], in_=workc[:, hd, :]),
                         reads=[B_wc[hd]], writes=[B_vl[hd]])
                p.op("dve", lambda e: e.tensor_copy(out=X[2], in_=idxu), reads=B_ix, writes=[B_X[2]])
                p.op("dve", lambda e: e.tensor_tensor(out=ev_, in0=vals, in1=vals[:, :, 0:1].to_broadcast([128, 8, 16]),
                                                      op=ALU.subtract), reads=B_vl + [B_sm], writes=[B_sm])
                p.op("act", lambda e: e.activation(out=ev_, in_=ev_, func=AF.Exp), reads=[B_sm], writes=[B_sm])
                p.op("dve", lambda e: e.tensor_tensor(out=X[0], in0=vals[:, :, 15:16].to_broadcast([128, 8, 16]), in1=V2v,
                                                      op=ALU.subtract), reads=B_vl + B_Vg, writes=[B_X[0]])
                p.op("dve", lambda e: e.tensor_tensor(out=X[1], in0=V2v, in1=V2v[:, :, 0:1].to_broadcast([128, 8, 16]),
                                                      op=ALU.subtract), reads=B_Vg, writes=[B_X[1]])
                p.op("dve", lambda e: e.tensor_scalar(out=X[3], in0=V1v[:, :, 0:1].to_broadcast([128, 8, 16]), scalar1=-1.0,
                                                      scalar2=None, op0=ALU.mult), reads=B_Vg, writes=[B_X[3]])
                p.op("dve", lambda e: e.scalar_tensor_tensor(out=X[0], in0=X[0], scalar=-3e-5, in1=X[3], op0=ALU.add, op1=ALU.add),
                     reads=[B_X[0], B_X[3]], writes=[B_X[0]])
                p.op("act", lambda e: e.activation(out=X[0], in_=X[0], func=AF.Exp), reads=[B_X[0]], writes=[B_X[0]])
                p.op("act", lambda e: e.activation(out=X[1], in_=X[1], func=AF.Exp), reads=[B_X[1]], writes=[B_X[1]])
                p.op("dve", lambda e: e.tensor_reduce(out=Zs, in_=ev_, axis=AX.X, op=ALU.add), reads=[B_sm], writes=[B_sm])
                p.op("dve", lambda e: e.reciprocal(out=rZ, in_=Zs), reads=[B_sm], writes=[B_sm])
                p.op("dve", lambda e: e.tensor_tensor(out=X[1], in0=X[1], in1=rZ.unsqueeze(2).to_broadcast([128, 8, 16]),
                                                      op=ALU.mult), reads=[B_sm, B_X[1]], writes=[B_X[1]])
                for i in range(4):
                    p.op("pe", lambda e, i=i: e.matmul(PS[4][:, i * 128:(i + 1) * 128], X[i].rearrange("p h k -> p (h k)"),
                                                       ident_f, start=True, stop=True),
                         reads=[B_X[i], B_const], writes=[PSB[4]], inc=(i == 3))
                p.op("act", lambda e: e.activation(out=XTb[tt % 2].rearrange("p i t -> p (i t)"), in_=PS[4], func=AF.Copy),
                     reads=[PSB[4]], writes=[B_XTb[tt % 2]])

            B_srD = [Buf("srD%d" % i) for i in range(2)]

            def stage_A(tt, sub, k):
                XT, B_XT = XTb[tt % 2], B_XTb[tt % 2]
                t0 = tt * 128 + sub * 32
                sr, B_E = S1rep[k % 2]
                B_D = B_srD[k % 2]
                R3, B_R3 = R3b[k % 2]
                for hd in range(8):
                    p.dma("sp", sr[hd * 16:(hd + 1) * 16, :, :],
                          s1_s[hd, t0:t0 + 32, :].partition_broadcast(16), reads=[B_s1s], writes=[B_D, B_E], group=(hd > 0))
                for t in range(32):
                    tc = sub * 32 + t
                    p.op("act", lambda e, t=t, tc=tc: e.activation(out=sr[:, t, :], in_=sr[:, t, :], func=AF.Exp,
                                                                  bias=XT[:, 3, tc:tc + 1], scale=1.0),
                         reads=[B_D, B_XT], writes=[B_E], skip_own=(t > 0))
                for t in range(32):
                    tc = sub * 32 + t
                    p.op("dve", lambda e, t=t, tc=tc: e.tensor_scalar(
                        out=R3[:, t, :], in0=iota3[:, 0, :], scalar1=XT[:, 2, tc:tc + 1], scalar2=XT[:, 1, tc:tc + 1],
                        op0=ALU.is_equal, op1=ALU.mult), reads=[B_ec, B_XT], writes=[B_R3], skip_own=(t > 0))

            def stage_B(tt, sub, k):
                XT, B_XT = XTb[tt % 2], B_XTb[tt % 2]
                ht = tt * 2 + sub // 2
                GT, B_GT = GTb[ht % 2]
                sr, B_E = S1rep[k % 2]
                L3, B_L3 = L3b[k % 2]
                R3, B_R3 = R3b[k % 2]
                for t in range(32):
                    tc = sub * 32 + t
                    p.op("dve", lambda e, t=t, tc=tc: e.scalar_tensor_tensor(
                        out=L3[:, t, :], in0=sr[:, t, :], scalar=XT[:, 0, tc:tc + 1], in1=sr[:, t, :],
                        op0=ALU.is_ge, op1=ALU.mult), reads=[B_E, B_XT], writes=[B_L3], skip_own=(t > 0))
                for q4 in range(8):
                    b = 5 + gbank[0] % 3
                    gbank[0] += 1
                    for kk in range(4):
                        tk = q4 * 4 + kk
                        p.op("pe", lambda e, b=b, kk=kk, tk=tk: e.matmul(
                            PS[b][:, kk * 128:(kk + 1) * 128], L3[:, tk, :], R3[:, tk, :], start=True, stop=True),
                             reads=[B_L3, B_R3], writes=[PSB[b]], inc=(kk == 3))
                    tl0 = (sub % 2) * 32 + q4 * 4
                    p.op("act", lambda e, b=b, tl0=tl0: e.activation(
                        out=GT[:, :, tl0:tl0 + 4], in_=PS[b].rearrange("p (t y) -> p y t", t=4), func=AF.Copy),
                        reads=[PSB[b]], writes=[B_GT])
                if sub % 2 == 1:
                    p.dma("sp", gT_h[ht], GT.rearrange("p y t -> p (y t)"), reads=[B_GT], writes=[B_gTs])

            jobs = [(tt, sub) for tt in range(8) for sub in range(4)]
            chain(0)
            stage_A(0, 0, 0)
            for k, (tt, sub) in enumerate(jobs):
                if k + 1 < len(jobs):
                    tt2, sub2 = jobs[k + 1]
                    if sub2 == 0:
                        chain(tt2)
                    stage_A(tt2, sub2, k + 1)
                stage_B(tt, sub, k)
            if stage <= 6:
                p.wait_all_on("sp")
                p.emit()
                return nc, dbg_out, list(IN)

            arena_reset()
            h1T = A.bf16(32 * T).rearrange("p (k t) -> p k t", k=32)
            B_h1 = Buf("h1T")
            for k4 in range(8):
                p.dma("pool", h1T[:, k4 * 4:(k4 + 1) * 4, :], h1_s[k4 * 4:(k4 + 1) * 4].rearrange("c p t -> p c t"),
                      reads=[B_h1s], writes=[B_h1], group=(k4 > 0))
            wtiles = [(A.bf16(32 * 128), Buf("w%d" % i)) for i in range(3)]
            gty = [(A.bf16(T), Buf("gty%d" % i)) for i in range(3)]
            actb = [(A.bf16(T), Buf("actb%d" % i)) for i in range(2)]
            ggb = [(A.bf16(T), Buf("ggb%d" % i)) for i in range(3)]
            gT_v = gT_s.rearrange("ht x (y t) -> x ht y t", y=128)

            def ev_e2(i, y, banks):
                g_y, B_gy = gty[i % 3]
                a_b, B_ab = actb[i % 2]
                g_g, B_gg = ggb[i % 3]
                p.dma("sp", g_y.rearrange("p (ht t) -> p ht t", ht=16), gT_v[:, :, y, :], reads=[B_gTs], writes=[B_gy])
                for t, b in enumerate(banks):
                    p.op("act", lambda e, b=b, t=t: e.activation(out=a_b[:, t * 512:(t + 1) * 512], in_=PS[b], func=AF.Gelu),
                         reads=[PSB[b]], writes=[B_ab])
                p.op("dve", lambda e: e.tensor_tensor(out=g_g, in0=a_b, in1=g_y, op=ALU.mult),
                     reads=[B_ab, B_gy], writes=[B_gg])
                p.dma("sp", ggT_s[y], g_g, reads=[B_gg], writes=[B_ggs])

            gemm(h1T, 32, T, inp("uT_t"), list(range(128)), ev_e2, [B_h1], wtiles, [[0, 1], [2, 3], [4, 5], [6, 7]])

            arena_reset()
            vtb = [(A.bf16(2 * 512).rearrange("p (y d) -> p y d", y=2), Buf("vt%d" % i)) for i in range(4)]
            gyb = [(A.bf16(2 * T).rearrange("p (y t) -> p y t", y=2), Buf("gy%d" % i)) for i in range(4)]
            ztb = [(A.f32(T), Buf("zt%d" % i)) for i in range(8)]
            v_d = inp("v_t")
            zi = 0
            for dg in range(8):
                zts = []
                for dc in range(4):
                    n = dg * 4 + dc
                    zt, B_zt = ztb[zi % 8]
                    zi += 1
                    p.dma("sp", zt, z2_s[n], reads=[B_z2s], writes=[B_zt])
                    zts.append((zt, B_zt))
                for y2 in range(64):
                    vt, B_vt = vtb[y2 % 4]
                    gy, B_gy = gyb[y2 % 4]
                    p.dma("pool", vt, v_d[2 * y2:2 * y2 + 2][:, :, dg * 512:(dg + 1) * 512].rearrange("y x d -> x y d"),
                          writes=[B_vt])
                    p.dma("sp" if y2 % 2 else "act", gy, ggT_s[2 * y2:2 * y2 + 2].rearrange("y x t -> x y t"),
                          reads=[B_ggs], writes=[B_gy])
                    for yy in range(2):
                        y = 2 * y2 + yy
                        for dc in range(4):
                            for th in range(2):
                                b = dc * 2 + th
                                p.op("pe", lambda e, b=b, dc=dc, th=th, vt=vt, gy=gy, y=y, yy=yy: e.matmul(
                                    PS[b], vt[:, yy, dc * 128:(dc + 1) * 128], gy[:, yy, th * 512:(th + 1) * 512],
                                    start=(y == 0), stop=(y == 127)),
                                    reads=[B_vt, B_gy], writes=[PSB[b]], inc=(dc == 3 and th == 1))
                for dc in range(4):
                    n = dg * 4 + dc
                    zt, B_zt = zts[dc]
                    for th in range(2):
                        b = dc * 2 + th
                        p.op("dve", lambda e, b=b, th=th, zt=zt: e.tensor_tensor(
                            out=zt[:, th * 512:(th + 1) * 512], in0=PS[b], in1=zt[:, th * 512:(th + 1) * 512], op=ALU.add),
                            reads=[PSB[b], B_zt], writes=[B_zt])
                    p.dma("sp", z2_s[n], zt, reads=[B_zt], writes=[B_z2s])

        arena_reset()
        zb2 = [(A.f32(T), Buf("fz%d" % i)) for i in range(4)]
        zq2 = [(A.bf16(T), Buf("fq%d" % i)) for i in range(4)]
        zc2 = [(A.bf16(T), Buf("fc%d" % i)) for i in range(4)]
        for n in range(32):
            z, B_z = zb2[n % 4]
            q, B_q = zq2[n % 4]
            zc, B_zc = zc2[n % 4]
            p.dma("sp", z, z2_s[n], reads=[B_z2s], writes=[B_z])
            p.op("act", lambda e, z=z, q=q: e.activation(out=q, in_=z, func=AF.Square), reads=[B_z], writes=[B_q])
            p.op("act", lambda e, z=z, zc=zc: e.activation(out=zc, in_=z, func=AF.Copy), reads=[B_z], writes=[B_zc])
            ln_stats_mm(n, zc, B_zc, q, B_q, 32)
        B_out = Buf("out")

        def sink2(n, z, B_z):
            p.dma("sp", outT_d[n * 128:(n + 1) * 128, :], z, reads=[B_z], writes=[B_out])

        ln_apply(z2_s, B_z2s, "ln2g", "ln2b", sink2)

        p.wait_all_on("sp")
        p.emit()
    return nc, dbg_out, list(IN)


def _chunked(w, kc):
    K, N = w.shape
    assert K == kc * 128 and N % 128 == 0
    return np.ascontiguousarray(w.reshape(kc, 128, N // 128, 128).transpose(2, 1, 0, 3))


def _col(v, n):
    return np.ascontiguousarray(v.reshape(n, 128).T)


def prep_shared(inp, used=None):
    f = lambda a: np.asarray(a, dtype=np.float32)

    def w_in_t():
        w_in = f(inp["w_in"])[0]
        W = [2048, 256, 2048, 64, 32, 2048, 2048, 2048, 16]
        off = np.concatenate([[0], np.cumsum(W)])
        seg = lambda i: w_in[:, off[i]:off[i + 1]]
        z = lambda n: np.zeros((D, n), np.float32)
        cols = np.concatenate([
            seg(0), seg(5), seg(2), seg(4), z(96),
            seg(1), seg(3), seg(3), seg(6), seg(7), seg(8), z(112)], axis=1)
        assert cols.shape[1] == (NQ_CH + NK_CH) * 128
        return _chunked(cols, 32)

    def bfor():
        bf = np.zeros((128, 1), np.float32)
        bf[:16, 0] = f(inp["b_forget"])[0]
        return bf

    th = {
        "w_in_t": w_in_t,
        "ident": lambda: np.eye(128, dtype=np.float32),
        "glat": lambda: _col(f(inp["g_latent"])[0], 2),
        "bfor": bfor,
        "wuk_t": lambda: np.ascontiguousarray(f(inp["w_uk"])[0].reshape(2, 128, 2048).transpose(1, 0, 2)),
        "wuv_t": lambda: np.ascontiguousarray(f(inp["w_uv"])[0].reshape(2, 128, 2048).transpose(1, 0, 2)),
        "w_gate_t": lambda: _chunked(f(inp["w_gate"])[0], 32),
        "b_gate_t": lambda: _col(f(inp["b_gate"])[0], 64),
        "w_ba_t": lambda: _chunked(f(inp["w_branch_a"])[0], 16),
        "w_bb_t": lambda: _chunked(f(inp["w_branch_b"])[0], 16),
        "w_out_t": lambda: _chunked(f(inp["w_out"])[0], 32),
        "ln1g": lambda: _col(f(inp["ln1_g"])[0], 32),
        "ln1b": lambda: _col(f(inp["ln1_b"])[0], 32),
        "peer_wq_t": lambda: _chunked(f(inp["peer_wq"])[0].reshape(D, 2048), 32),
        "sk_t": lambda: np.ascontiguousarray(f(inp["peer_subkeys"])[0].reshape(16, 128, 128).transpose(2, 0, 1)),
        "uT_t": lambda: np.ascontiguousarray(f(inp["peer_u"])[0].reshape(128, 128, 32, 128).transpose(1, 3, 2, 0)),
        "v_t": lambda: np.ascontiguousarray(f(inp["peer_v"])[0].reshape(128, 128, D).transpose(1, 0, 2)),
        "w_pg_t": lambda: _chunked(f(inp["w_ple_gate"])[0], 32),
        "b_pg_t": lambda: _col(f(inp["b_ple_gate"])[0], 32),
        "w_ple_t": lambda: _chunked(f(inp["w_ple"])[0], 2),
        "ln2g": lambda: _col(f(inp["ln2_g"])[0], 32),
        "ln2b": lambda: _col(f(inp["ln2_b"])[0], 32),
        "iota_t": lambda: np.ascontiguousarray(np.broadcast_to(np.arange(128, dtype=np.float32), (128, 32, 128))),
    }
    return {k: fn() for k, fn in th.items() if used is None or k in used}


def core_positions(par):
    j = np.arange(8)
    own = ((2 * j + par)[:, None] * 128 + np.arange(128)[None, :]).reshape(-1)
    oth = ((2 * j + 1 - par)[:, None] * 128 + np.arange(128)[None, :]).reshape(-1)
    return own, oth


def prep_core(inp, b, par):
    x = np.asarray(inp["x"], dtype=np.float32)[b]
    pp = np.asarray(inp["p"], dtype=np.float32)[0, b]
    own, oth = core_positions(par)
    kpos = np.concatenate([own, oth])
    d = {}
    d["xT"] = np.ascontiguousarray(x[kpos].T)
    d["pT"] = np.ascontiguousarray(pp[own].T)
    d["qpos_b"] = np.ascontiguousarray(np.broadcast_to(own.astype(np.float32), (128, T)))
    d["kpos_b"] = np.ascontiguousarray(np.broadcast_to(kpos.astype(np.float32), (128, S)))
    d["kpos_col"] = _col(kpos.astype(np.float32), 16)
    d["cend_col"] = _col(((own // 64 + 1) * 64).astype(np.float32), 8)
    ce = own // 64 + 1
    cidx = np.arange(32)
    d["penA"] = np.where(cidx[:, None] >= ce[None, :], np.float32(-1e30), np.float32(0)).astype(np.float32)
    d["penB"] = (cidx[:, None] == (kpos // 64)[None, :]).astype(np.float32)
    return d


_CACHE = {}


def kernel(**inputs):
    if "nc" not in _CACHE:
        _CACHE["nc"] = build()
    nc, _, used = _CACHE["nc"]
    sh = prep_shared(inputs, used)
    in_maps = []
    for c in range(8):
        d = dict(sh)
        d.update(prep_core(inputs, c // 2, c % 2))
        in_maps.append({k: d[k] for k in used})
    res = run_bass_kernel_spmd(nc, in_maps, core_ids=list(range(8)))
    out = np.zeros((4, S, D), np.float32)
    for c in range(8):
        own, _ = core_positions(c % 2)
        out[c // 2, own, :] = res.results[c]["outT"].T
    return out
```

```python
from contextlib import ExitStack
import numpy as np
import concourse.bass as bass
import concourse.mybir as mybir
from concourse.bass_utils import run_bass_kernel_spmd

F32 = mybir.dt.float32
BF16 = mybir.dt.bfloat16
U32 = mybir.dt.uint32
ALU = mybir.AluOpType
AF = mybir.ActivationFunctionType
AX = mybir.AxisListType

ENG = ("sp", "act", "dve", "pool", "pe")
NDMASEM = 8


class Buf:
    __slots__ = ("name", "w", "ws", "r")

    def __init__(self, name=""):
        self.name = name
        self.w = None
        self.ws = []
        self.r = []


class Prog:
    def __init__(self, nc, es):
        self.nc = nc
        self.streams = {e: [] for e in ENG}
        self.cnt = {e: 0 for e in ENG}
        self.sems = {}
        for e in ENG:
            self.sems[("e", e)] = es.enter_context(nc.semaphore("s_" + e))
        self.dcnt = {}
        self.dnext = {}
        for q in ("sp", "act", "pool"):
            self.dnext[q] = 0
            for i in range(NDMASEM):
                k = ("d", q, i)
                self.sems[k] = es.enter_context(nc.semaphore("d_%s%d" % (q, i)))
                self.dcnt[k] = 0
        self.waited = {e: {} for e in ENG}
        self.ninstr = 0

    def _wait(self, e, deps, skip_own=False):
        best = {}
        for d in deps:
            if d is None:
                continue
            k, v = d
            if skip_own and k == ("e", e):
                continue
            if best.get(k, 0) < v:
                best[k] = v
        for k, v in best.items():
            if self.waited[e].get(k, 0) >= v:
                continue
            if k == ("e", e) and v > self.cnt[e]:
                continue
            self.waited[e][k] = v
            sem = self.sems[k]
            self.streams[e].append(lambda eng, sem=sem, v=v: eng.wait_ge(sem, v))

    @staticmethod
    def _deps(reads, writes, group=False):
        deps = []
        for b in reads:
            deps.append(b.w)
            deps.extend(b.ws)
        for b in writes:
            if not group:
                deps.append(b.w)
                deps.extend(b.ws)
            deps.extend(b.r)
        return deps

    def _mark(self, tok, reads, writes, group=False):
        for b in reads:
            b.r.append(tok)
            if len(b.r) > 64:
                best = {}
                for k, v in b.r:
                    if best.get(k, 0) < v:
                        best[k] = v
                b.r = list(best.items())
        for b in writes:
            if group:
                b.ws.append(tok)
            else:
                b.w = tok
                b.ws = []
                b.r = []

    def op(self, e, fn, reads=(), writes=(), inc=True, skip_own=False):
        self._wait(e, self._deps(reads, writes), skip_own)
        tok_val = self.cnt[e] + 1
        key = ("e", e)
        if inc:
            self.cnt[e] += 1
            sem = self.sems[key]
            self.streams[e].append(lambda eng, fn=fn, sem=sem: fn(eng).then_inc(sem, 1))
        else:
            self.streams[e].append(lambda eng, fn=fn: fn(eng))
        tok = (key, tok_val)
        self._mark(tok, reads, writes)
        self.ninstr += 1
        return tok

    def dma(self, q, out, in_, reads=(), writes=(), group=False, **kw):
        i = self.dnext[q]
        self.dnext[q] = (i + 1) % NDMASEM
        k = ("d", q, i)
        deps = self._deps(reads, writes, group)
        if self.dcnt[k] > 0:
            deps.append((k, self.dcnt[k]))
        self._wait(q, deps)
        self.dcnt[k] += 16
        sem = self.sems[k]
        self.streams[q].append(
            lambda eng, out=out, in_=in_, sem=sem, kw=kw: eng.dma_start(out=out, in_=in_, **kw).then_inc(sem, 16))
        tok = (k, self.dcnt[k])
        self._mark(tok, reads, writes, group)
        self.ninstr += 1
        return tok

    def barrier(self):
        deps = [(("e", e), self.cnt[e]) for e in ENG if self.cnt[e] > 0]
        deps += [(k, v) for k, v in self.dcnt.items() if v > 0]
        for e in ENG:
            self._wait(e, deps)

    def wait_all_on(self, e):
        deps = [(("e", x), self.cnt[x]) for x in ENG if self.cnt[x] > 0]
        deps += [(k, v) for k, v in self.dcnt.items() if v > 0]
        self._wait(e, deps)

    def emit(self):
        nc = self.nc
        with nc.Block() as block:
            @block.sync
            def _(eng):
                for f in self.streams["sp"]:
                    f(eng)

            @block.scalar
            def _(eng):
                for f in self.streams["act"]:
                    f(eng)

            @block.vector
            def _(eng):
                for f in self.streams["dve"]:
                    f(eng)

            @block.gpsimd
            def _(eng):
                for f in self.streams["pool"]:
                    f(eng)

            @block.tensor
            def _(eng):
                for f in self.streams["pe"]:
                    f(eng)


class Arena:
    def __init__(self, ap_full, nwords):
        self.a = ap_full
        self.n = nwords
        self.off = 0
        self.top = nwords
        self.marks = []

    def alloc(self, nbytes, dt=F32):
        nw = (nbytes + 3) // 4
        nw = (nw + 15) // 16 * 16
        assert self.off + nw <= self.top, "SBUF arena overflow %d + %d > %d" % (self.off, nw, self.top)
        v = self.a[:, self.off:self.off + nw]
        self.off += nw
        if dt != F32:
            v = v.bitcast(dt)
        return v

    def alloc_top(self, nbytes, dt=F32):
        nw = ((nbytes + 3) // 4 + 15) // 16 * 16
        assert self.top - nw >= self.off
        self.top -= nw
        v = self.a[:, self.top:self.top + nw]
        return v.bitcast(dt) if dt != F32 else v

    def f32(self, n):
        return self.alloc(4 * n)[:, 0:n]

    def bf16(self, n):
        return self.alloc(2 * n, BF16)[:, 0:n]

    def push(self):
        self.marks.append(self.off)

    def pop(self):
        self.off = self.marks.pop()


D = 4096
S = 2048
T = 1024
NQ_CH = 49
NK_CH = 36
ALPHA = 2.0 ** 0.25
SCALE = 128.0 ** -0.5
EPS = 1e-5
NEG = -30000.0
SLOPES = [2.0 ** (-8.0 * (h + 1) / 16) for h in range(16)]

ARENA_WORDS = 184 * 256
import os
NH_A = int(os.environ.get('NH_A', 16))
NH_B = int(os.environ.get('NH_B', 16))


def build(stage=99, dbg=()):
    nc = bass.Bass("TRN2", target_bir_lowering=False)

    def din(name, shape, dt=F32):
        return nc.dram_tensor(name, list(shape), dt, kind="ExternalInput").ap()

    def dscr(name, shape, dt=F32):
        return nc.dram_tensor(name, list(shape), dt, kind="Internal").ap()

    def dout(name, shape, dt=F32):
        return nc.dram_tensor(name, list(shape), dt, kind="ExternalOutput").ap()

    IN_SHAPES = {
        "xT": [D, S], "pT": [256, T], "w_in_t": [NQ_CH + NK_CH, 128, 32, 128], "ident": [128, 128],
        "qpos_b": [128, T], "kpos_b": [128, S], "kpos_col": [128, 16], "cend_col": [128, 8], "penA": [32, T], "penB": [32, S],
        "glat": [128, 2], "bfor": [128, 1], "wuk_t": [128, 2, 2048], "wuv_t": [128, 2, 2048],
        "w_gate_t": [64, 128, 32, 128], "b_gate_t": [128, 64], "w_ba_t": [32, 128, 16, 128],
        "w_bb_t": [32, 128, 16, 128], "w_out_t": [32, 128, 32, 128], "ln1g": [128, 32], "ln1b": [128, 32],
        "peer_wq_t": [16, 128, 32, 128], "sk_t": [128, 16, 128], "uT_t": [128, 128, 32, 128],
        "v_t": [128, 128, D], "w_pg_t": [32, 128, 32, 128], "b_pg_t": [128, 32],
        "w_ple_t": [32, 128, 2, 128], "ln2g": [128, 32], "ln2b": [128, 32], "iota_t": [128, 32, 128],
    }
    IN = {}

    def inp(name):
        if name not in IN:
            IN[name] = din(name, IN_SHAPES[name])
        return IN[name]

    outT_d = dout("outT", [D, T])

    qA_s = dscr("qA_s", [16, 128, T], BF16)
    qB_s = dscr("qB_s", [16, 128, T], BF16)
    kB_s = dscr("kB_s", [16, 128, S], BF16)
    vB_s = dscr("vB_s", [16, 128, S], BF16)
    oT_s = dscr("oT_s", [32, 128, T], BF16)
    s1_s = dscr("s1_s", [8, T, 128], F32)
    B_s1s = Buf("s1_s")
    B_gTs = Buf("gT_s")
    B_ggs = Buf("ggT_s")
    gT_s = dscr("gT_s", [16, 128, 128 * 64], BF16)
    ggT_s = dscr("ggT_s", [128, 128, T], BF16)

    dbg_out = {}

    with ExitStack() as es:
        p = Prog(nc, es)
        arena_t = es.enter_context(nc.sbuf_tensor("arena", [128, ARENA_WORDS], F32))
        psum_t = es.enter_context(nc.psum_tensor("ps", [128, 4096], F32))
        A = Arena(arena_t, ARENA_WORDS)
        PS = [psum_t[:, b * 512:(b + 1) * 512] for b in range(8)]
        PSB = [Buf("ps%d" % b) for b in range(8)]

        def dbg_dump(name, ap, shape, buf, dt=F32):
            if name in dbg:
                o = dout("dbg_" + name, shape, dt)
                dbg_out[name] = o
                p.dma("sp", o, ap, reads=[buf])

        ident_f = A.f32(128)
        ident_b = A.bf16(128)
        ones_f = A.f32(128)
        ones_b = A.bf16(128)
        B_const = Buf("const")
        p.dma("sp", ident_f, inp("ident"), writes=[B_const])
        p.op("dve", lambda e: e.tensor_copy(out=ident_b, in_=ident_f), reads=[B_const], writes=[B_const])
        p.op("dve", lambda e: e.memset(ones_f, 1.0), writes=[B_const])
        p.op("dve", lambda e: e.memset(ones_b, 1.0), writes=[B_const])
        A_BASE = A.off

        def arena_reset():
            p.barrier()
            A.off = A_BASE
            A.top = ARENA_WORDS
            A.marks = []

        def gemm(xT, KC, ntok, w_dram, chunks, evac, xbufs, wtiles, ps_slots, q="pool"):
            nb = ntok // 512
            for i, c in enumerate(chunks):
                wt, wb = wtiles[i % len(wtiles)]
                p.dma(q, wt.rearrange("p (k n) -> p k n", k=KC), w_dram[c], writes=[wb])
                banks = ps_slots[i % len(ps_slots)]
                for kc in range(KC):
                    for t in range(nb):
                        b = banks[t]
                        p.op("pe", lambda e, b=b, kc=kc, t=t, wt=wt: e.matmul(
                            PS[b], wt[:, kc * 128:(kc + 1) * 128], xT[:, kc, t * 512:(t + 1) * 512],
                            start=(kc == 0), stop=(kc == KC - 1)),
                            reads=[wb] + list(xbufs), writes=[PSB[b]],
                            inc=(kc == KC - 1 and t == nb - 1))
                evac(i, c, banks)

        ckvn = A.bf16(2 * S).rearrange("p (c t) -> p c t", c=2)
        B_ckvn = Buf("ckvn")
        B_selT = Buf("selT")
        kpos_b = A.f32(S)
        qpos_b = A.f32(T)
        kpos_col = A.f32(16)
        cend_col = A.f32(8)
        glat = A.f32(2)
        negb = A.f32(1)
        B_pos = Buf("pos")
        A.push()
        qidxT = A.bf16(16 * T).rearrange("p (c t) -> p c t", c=16)
        B_qidx = Buf("qidx")
        kidxT = A.bf16(S)
        B_kidx = Buf("kidx")
        w_tok = A.f32(8 * 32).rearrange("p (j h) -> p j h", j=8)
        B_wtok = Buf("wtok")
        A.push()
        ckvT = A.f32(2 * S).rearrange("p (c t) -> p c t", c=2)
        B_ckv = Buf("ckv")
        fT = A.f32(S)
        B_f = Buf("fT")
        widxT = A.f32(T)
        B_widxT = Buf("widxT")
        A.push()
        xT = A.bf16(32 * T).rearrange("p (k t) -> p k t", k=32)
        B_x = Buf("xT")
        wtiles = [(A.bf16(32 * 128), Buf("w%d" % i)) for i in range(3)]
        stg = [(A.bf16(T), Buf("stg%d" % i)) for i in range(3)]
        xT_v = inp("xT").rearrange("(k p) t -> p k t", p=128)

        def load_x(half):
            for k4 in range(4):
                p.dma("pool", xT[:, k4 * 8:(k4 + 1) * 8, :], xT_v[:, k4 * 8:(k4 + 1) * 8, half * T:(half + 1) * T],
                      writes=[B_x], group=(k4 > 0))

        slots2 = [[0, 1], [2, 3], [4, 5], [6, 7]]
        stg_i = [0]

        def evac_A(half):
            tok0 = half * T

            def ev(i, c, banks):
                def to_scratch(dst, scale):
                    st, sb = stg[stg_i[0] % 3]
                    stg_i[0] += 1
                    for t, b in enumerate(banks):
                        p.op("act", lambda e, b=b, t=t, st=st: e.activation(
                            out=st[:, t * 512:(t + 1) * 512], in_=PS[b], func=AF.Copy, scale=scale),
                            reads=[PSB[b]], writes=[sb])
                    p.dma("sp", dst, st, reads=[sb])

                if c < 16:
                    to_scratch(qA_s[c], SCALE)
                elif c < 32:
                    to_scratch(qB_s[c - 16], SCALE)
                elif c < 48:
                    for t, b in enumerate(banks):
                        p.op("act", lambda e, b=b, t=t: e.activation(
                            out=qidxT[:, c - 32, t * 512:(t + 1) * 512], in_=PS[b], func=AF.Copy),
                            reads=[PSB[b]], writes=[B_qidx])
                elif c == 48:
                    for t, b in enumerate(banks):
                        p.op("dve", lambda e, b=b, t=t: e.tensor_copy(out=widxT[:, t * 512:(t + 1) * 512], in_=PS[b]),
                             reads=[PSB[b]], writes=[B_widxT])
                elif c < 51:
                    for t, b in enumerate(banks):
                        p.op("dve", lambda e, b=b, t=t: e.tensor_copy(
                            out=ckvT[:, c - 49, tok0 + t * 512: tok0 + (t + 1) * 512], in_=PS[b]),
                            reads=[PSB[b]], writes=[B_ckv])
                elif c == 51:
                    for t, b in enumerate(banks):
                        p.op("act", lambda e, b=b, t=t: e.activation(
                            out=kidxT[:, tok0 + t * 512: tok0 + (t + 1) * 512], in_=PS[b], func=AF.Copy),
                            reads=[PSB[b]], writes=[B_kidx])
                elif c < 68:
                    to_scratch(kB_s[c - 52][:, tok0:tok0 + T], 1.0)
                elif c < 84:
                    to_scratch(vB_s[c - 68][:, tok0:tok0 + T], 1.0)
                else:
                    for t, b in enumerate(banks):
                        p.op("dve", lambda e, b=b, t=t: e.tensor_copy(
                            out=fT[:, tok0 + t * 512: tok0 + (t + 1) * 512], in_=PS[b]),
                            reads=[PSB[b]], writes=[B_f])
            return ev

        load_x(1)
        gemm(xT, 32, T, inp("w_in_t"), list(range(NQ_CH, NQ_CH + NK_CH)), evac_A(1), [B_x], wtiles, slots2)
        load_x(0)
        gemm(xT, 32, T, inp("w_in_t"), list(range(NQ_CH + NK_CH)), evac_A(0), [B_x], wtiles, slots2)

        dbg_dump("ckv", ckvT, [128, 2, S], B_ckv)
        dbg_dump("fT", fT, [128, S], B_f)
        dbg_dump("widxT", widxT, [128, T], B_widxT)
        dbg_dump("kidxT", kidxT, [128, S], B_kidx, BF16)
        dbg_dump("qidxT", qidxT, [128, 16, T], B_qidx, BF16)

        if stage <= 1:
            p.wait_all_on("sp")
            p.emit()
            return nc, dbg_out, list(IN)

        p.barrier()
        A.pop()
        selT = A.alloc_top(2 * 16 * T, BF16).rearrange("p (k t) -> p k t", k=16)
        p.dma("sp", kpos_b, inp("kpos_b"), writes=[B_pos])
        p.dma("sp", qpos_b, inp("qpos_b"), writes=[B_pos])
        p.dma("sp", kpos_col, inp("kpos_col"), writes=[B_pos])
        p.dma("sp", cend_col, inp("cend_col"), writes=[B_pos])
        p.dma("sp", glat, inp("glat"), writes=[B_pos])
        p.dma("sp", negb, inp("bfor"), writes=[B_pos])
        p.op("dve", lambda e: e.tensor_scalar(out=negb, in0=negb, scalar1=-1.0, scalar2=None, op0=ALU.mult),
             reads=[B_pos], writes=[B_pos])
        A.push()
        sq = A.f32(2 * S).rearrange("p (c t) -> p c t", c=2)
        rstd = A.f32(S)
        B_sq, B_rstd = Buf("sq"), Buf("rstd")
        for c in range(2):
            p.op("act", lambda e, c=c: e.activation(out=sq[:, c, :], in_=ckvT[:, c, :], func=AF.Square),
                 reads=[B_ckv], writes=[B_sq])
        for t in range(4):
            for c in range(2):
                p.op("pe", lambda e, t=t, c=c: e.matmul(PS[t], ones_f, sq[:, c, t * 512:(t + 1) * 512],
                                                        start=(c == 0), stop=(c == 1)),
                     reads=[B_sq, B_const], writes=[PSB[t]], inc=(c == 1))
            p.op("dve", lambda e, t=t: e.tensor_scalar(out=rstd[:, t * 512:(t + 1) * 512], in0=PS[t],
                                                       scalar1=1.0 / 256, scalar2=EPS, op0=ALU.mult, op1=ALU.add),
                 reads=[PSB[t]], writes=[B_rstd])
        p.op("act", lambda e: e.activation(out=rstd, in_=rstd, func=AF.Sqrt), reads=[B_rstd], writes=[B_rstd])
        p.op("dve", lambda e: e.reciprocal(out=rstd, in_=rstd), reads=[B_rstd], writes=[B_rstd])
        for c in range(2):
            p.op("dve", lambda e, c=c: e.scalar_tensor_tensor(out=ckvn[:, c, :], in0=ckvT[:, c, :], scalar=glat[:, c:c + 1],
                                                              in1=rstd, op0=ALU.mult, op1=ALU.mult),
                 reads=[B_ckv, B_rstd, B_pos], writes=[B_ckvn])
        dbg_dump("ckvn", ckvn, [128, 2, S], B_ckvn, BF16)
        A.pop()

        for j in range(8):
            p.op("pe", lambda e, j=j: e.matmul(PS[4][:, j * 32:(j + 1) * 32], widxT[0:32, j * 128:(j + 1) * 128],
                                               ident_f[0:32, 0:32], start=True, stop=True),
                 reads=[B_widxT, B_const], writes=[PSB[4]], inc=(j == 7))
        p.op("dve", lambda e: e.tensor_copy(out=w_tok.rearrange("p j h -> p (j h)"), in_=PS[4][:, 0:256]),
             reads=[PSB[4]], writes=[B_wtok])

        A.push()
        l2 = A.f32(S)
        B_l2 = Buf("l2")
        p.op("act", lambda e: e.activation(out=l2[0:16, :], in_=fT[0:16, :], func=AF.Exp, scale=-1.0, bias=negb[0:16, :]),
             reads=[B_f, B_pos], writes=[B_l2])
        p.op("act", lambda e: e.activation(out=l2[0:16, :], in_=l2[0:16, :], func=AF.Ln, scale=1.0, bias=1.0),
             reads=[B_l2], writes=[B_l2])
        for i in range(16):
            p.op("pe", lambda e, i=i: e.matmul(PS[5][:, i * 16:(i + 1) * 16], l2[0:16, i * 128:(i + 1) * 128],
                                               ident_f[0:16, 0:16], start=True, stop=True),
                 reads=[B_l2, B_const], writes=[PSB[5]], inc=(i == 15))
        l2t = A.f32(256)
        r1 = A.f32(256)
        tmpf = A.f32(256)
        parts = [A.bf16(256) for _ in range(3)]
        B_sp = Buf("split")
        p.op("dve", lambda e: e.tensor_copy(out=l2t, in_=PS[5][:, 0:256]), reads=[PSB[5]], writes=[B_sp])

        def split3(src, res, tmp, outs, n_part):
            sl = lambda a: a[0:n_part]
            o0, o1, o2 = outs
            p.op("dve", lambda e: e.tensor_copy(out=sl(o0), in_=sl(src)), reads=[B_sp], writes=[B_sp])
            p.op("dve", lambda e: e.tensor_copy(out=sl(tmp), in_=sl(o0)), reads=[B_sp], writes=[B_sp])
            p.op("dve", lambda e: e.tensor_sub(out=sl(res), in0=sl(src), in1=sl(tmp)), reads=[B_sp], writes=[B_sp])
            p.op("dve", lambda e: e.tensor_copy(out=sl(o1), in_=sl(res)), reads=[B_sp], writes=[B_sp])
            p.op("dve", lambda e: e.tensor_copy(out=sl(tmp), in_=sl(o1)), reads=[B_sp], writes=[B_sp])
            p.op("dve", lambda e: e.tensor_sub(out=sl(res), in0=sl(res), in1=sl(tmp)), reads=[B_sp], writes=[B_sp])
            p.op("dve", lambda e: e.tensor_copy(out=sl(o2), in_=sl(res)), reads=[B_sp], writes=[B_sp])

        split3(l2t, r1, tmpf, parts, 128)
        TtR = A.f32(S)
        Tt = [(TtR[:, i * 1024:(i + 1) * 1024].bitcast(BF16), Buf("Tt%d" % i)) for i in range(2)]
        for i in range(16):
            tt, tb = Tt[i % 2]
            p.op("dve", lambda e, i=i, tt=tt: e.tensor_scalar(out=tt, in0=kpos_b, scalar1=kpos_col[:, i:i + 1], scalar2=None,
                                                             op0=ALU.is_ge),
                 reads=[B_pos], writes=[tb])
            for t in range(4):
                for k in range(3):
                    p.op("pe", lambda e, i=i, t=t, k=k, tt=tt: e.matmul(
                        PS[t][0:16, :], parts[k][:, i * 16:(i + 1) * 16], tt[:, t * 512:(t + 1) * 512],
                        start=(i == 0 and k == 0), stop=(i == 15 and k == 2)),
                        reads=[B_sp, tb], writes=[PSB[t]], inc=(k == 2 and t == 3))
        cn = A.f32(S)
        cres = l2
        ctmp = TtR
        cparts = [A.bf16(S) for _ in range(3)]
        nparts = [A.bf16(S) for _ in range(3)]
        for t in range(4):
            p.op("dve", lambda e, t=t: e.tensor_copy(out=cn[0:16, t * 512:(t + 1) * 512], in_=PS[t][0:16, :]),
                 reads=[PSB[t]], writes=[B_sp])
        p.barrier()
        split3(cn, cres, ctmp, cparts, 16)
        cum_s = dscr("cum_s", [6, 16, S], BF16)
        B_cums = Buf("cum_s")
        for k in range(3):
            p.op("dve", lambda e, k=k: e.tensor_scalar(out=nparts[k][0:16], in0=cparts[k][0:16], scalar1=-1.0, scalar2=None,
                                                       op0=ALU.mult), reads=[B_sp], writes=[B_sp])
        for k in range(3):
            p.dma("sp", cum_s[k], cparts[k][0:16], reads=[B_sp], writes=[B_cums])
            p.dma("sp", cum_s[3 + k], nparts[k][0:16], reads=[B_sp], writes=[B_cums])
        dbg_dump("cn", cn, [128, S], B_sp)
        p.barrier()
        A.pop()

        A.pop()
        A.push()
        penA = A.bf16(T)
        penB = A.bf16(S)
        B_pen = Buf("pen")
        p.dma("pool", penA[0:32, :], inp("penA"), writes=[B_pen])
        p.dma("pool", penB[0:32, :], inp("penB"), writes=[B_pen])
        scmS = [(A.f32(S), Buf("scm%d" % i)) for i in range(4)]
        workS = [(A.f32(S), Buf("work%d" % i)) for i in range(4)]
        selcS = [(A.bf16(S), Buf("selc%d" % i)) for i in range(2)]
        m8S = [(A.f32(8), Buf("m8%d" % i)) for i in range(2)]
        thrS = [A.f32(1) for i in range(2)]
        rb = [(A.bf16(512), Buf("r%d" % i)) for i in range(4)]
        dgb = [(A.bf16(128), Buf("dg%d" % i)) for i in range(4)]
        ri = [0]
        pi = [0]

        def score_phase(j):
            scm, B_scm = scmS[j % 4]
            work, B_work = workS[j % 4]
            pieces = [0, 2] if j < 4 else [0, 1, 2, 3]
            for ip, pc in enumerate(pieces):
                p.op("pe", lambda e, ip=ip, pc=pc: e.matmul(
                    PS[4 + ip], penA[0:32, j * 128:(j + 1) * 128], penB[0:32, pc * 512:(pc + 1) * 512],
                    start=True, stop=False), reads=[B_pen], writes=[PSB[4 + ip]], inc=False)
            tiles = [(hi, ip, pc) for hi in range(32) for ip, pc in enumerate(pieces)]
            nt = len(tiles)

            def emit_R(t):
                hi, ip, pc = tiles[t]
                c, hb = hi // 2, (hi % 2) * 64
                if ip == 0:
                    dg, B_dg = dgb[hi % 4]
                    p.op("pool", lambda e, dg=dg, hi=hi: e.tensor_scalar(
                        out=dg, in0=ident_f, scalar1=w_tok[:, j, hi:hi + 1], scalar2=None, op0=ALU.mult),
                        reads=[B_const, B_wtok], writes=[B_dg])
                b = t % 4
                p.op("pe", lambda e, b=b, c=c, hb=hb, pc=pc: e.matmul(
                    PS[b], qidxT[hb:hb + 64, c, j * 128:(j + 1) * 128], kidxT[hb:hb + 64, pc * 512:(pc + 1) * 512],
                    start=True, stop=True), reads=[B_qidx, B_kidx], writes=[PSB[b]])

            LA = 3
            for t in range(min(LA, nt)):
                emit_R(t)
            for t in range(nt):
                if t + LA < nt:
                    emit_R(t + LA)
                hi, ip, pc = tiles[t]
                b = t % 4
                r, rbuf = rb[ri[0] % 4]
                ri[0] += 1
                dg, B_dg = dgb[hi % 4]
                p.op("act", lambda e, b=b, r=r: e.activation(out=r, in_=PS[b], func=AF.Relu),
                     reads=[PSB[b]], writes=[rbuf])
                p.op("pe", lambda e, ip=ip, dg=dg, r=r, hi=hi: e.matmul(
                    PS[4 + ip], dg, r, start=False, stop=(hi == 31)),
                    reads=[B_dg, rbuf], writes=[PSB[4 + ip]])
            nv = (j + 1) * 128
            for ip, pc in enumerate(pieces):
                wv = min(512, nv - (pc % 2) * 512)
                if wv <= 0:
                    continue
                d0 = (0 if pc < 2 else nv) + (pc % 2) * 512
                p.op("act", lambda e, ip=ip, d0=d0, wv=wv: e.activation(out=scm[:, d0:d0 + wv], in_=PS[4 + ip][:, 0:wv], func=AF.Copy),
                     reads=[PSB[4 + ip]], writes=[B_scm])
                if j > 0:
                    p.op("act", lambda e, ip=ip, d0=d0, wv=wv: e.activation(out=work[:, d0:d0 + wv], in_=PS[4 + ip][:, 0:wv], func=AF.Copy),
                         reads=[PSB[4 + ip]], writes=[B_work])

        def topk_pair(js):
            Ws = [2 * (j + 1) * 128 for j in js]
            for rnd in range(32):
                for a, j in enumerate(js):
                    if j == 0:
                        continue
                    work, B_work = workS[j % 4]
                    m8, B_m8 = m8S[a]
                    W = Ws[a]
                    p.op("dve", lambda e, W=W, work=work, m8=m8: e.max(out=m8, in_=work[:, 0:W]), reads=[B_work], writes=[B_m8])
                if rnd < 31:
                    for a, j in enumerate(js):
                        if j == 0:
                            continue
                        work, B_work = workS[j % 4]
                        m8, B_m8 = m8S[a]
                        W = Ws[a]
                        p.op("dve", lambda e, W=W, work=work, m8=m8: e.match_replace(
                            out=work[:, 0:W], in_to_replace=m8, in_values=work[:, 0:W], imm_value=-1e30),
                            reads=[B_m8, B_work], writes=[B_work])
            for a, j in enumerate(js):
                scm, B_scm = scmS[j % 4]
                m8, B_m8 = m8S[a]
                selc, B_selc = selcS[a]
                thr = thrS[a]
                W = Ws[a]
                if j == 0:
                    p.op("dve", lambda e, thr=thr: e.memset(thr, -1e29), reads=[B_m8], writes=[B_m8])
                else:
                    p.op("dve", lambda e, thr=thr, m8=m8: e.tensor_scalar(out=thr, in0=m8[:, 7:8], scalar1=-1e29, scalar2=None,
                                                                         op0=ALU.max), reads=[B_m8], writes=[B_m8])
                p.op("dve", lambda e, W=W, selc=selc, scm=scm, thr=thr: e.tensor_scalar(
                    out=selc[:, 0:W], in0=scm[:, 0:W], scalar1=thr, scalar2=None, op0=ALU.is_ge),
                    reads=[B_m8, B_scm], writes=[B_selc])
                for base_blk, kt_base in ((0, 0), (j + 1, 8)):
                    for g0 in range(0, j + 1, 4):
                        n4 = min(4, j + 1 - g0)
                        b = pi[0] % 4
                        pi[0] += 1
                        psb = PS[b].bitcast(BF16)
                        for i4 in range(n4):
                            i = base_blk + g0 + i4
                            p.op("pe", lambda e, psb=psb, i=i, i4=i4, selc=selc: e.transpose(
                                psb[:, i4 * 128:(i4 + 1) * 128], selc[:, i * 128:(i + 1) * 128], ident_b),
                                 reads=[B_selc, B_const], writes=[PSB[b]], inc=(i4 == n4 - 1))
                        kt0 = kt_base + g0
                        p.op("act", lambda e, psb=psb, kt0=kt0, j=j, n4=n4: e.activation(
                            out=selT[:, kt0:kt0 + n4, j * 128:(j + 1) * 128],
                            in_=psb[:, 0:n4 * 128].rearrange("p (k q) -> p k q", k=n4),
                            func=AF.Copy), reads=[PSB[b]], writes=[B_selT])

        p.op("dve", lambda e: e.memset(selT.rearrange("p k t -> p (k t)"), 0.0), writes=[B_selT])
        score_phase(0)
        score_phase(1)
        for i in range(4):
            if i < 3:
                score_phase(2 * i + 2)
                score_phase(2 * i + 3)
            topk_pair((2 * i, 2 * i + 1))
        dbg_dump("selT", selT, [128, 16, T], B_selT, BF16)
        p.barrier()
        A.pop()

        if stage <= 2:
            p.wait_all_on("sp")
            p.emit()
            return nc, dbg_out, list(IN)

        A.pop()
        KTS = [[0, 1, 2, 3, 8, 9, 10, 11], list(range(16))]
        tmpb = [(A.f32(512), Buf("tmp%d" % i)) for i in range(4)]
        ptb = [(A.bf16(512), Buf("pt%d" % i)) for i in range(4)]
        rec = A.f32(512)
        B_rec = Buf("rec")
        ostb = [(A.bf16(512), Buf("ost%d" % i)) for i in range(2)]
        qTb = [(A.bf16(T), Buf("qT%d" % i)) for i in range(2)]
        kTb = [(A.bf16(S), Buf("kT%d" % i)) for i in range(2)]
        Vb = [(A.bf16(S).rearrange("p (k d) -> p k d", k=16), Buf("V%d" % i)) for i in range(2)]
        cnt = {"s": 0, "tmp": 0, "pt": 0, "ost": 0, "acc": 0}

        def attn_core(h_glob, qT, B_q, kT, B_k, V, B_V, pre_exp, bias_mm):
            for J in range(2):
                kts = KTS[J]
                n = len(kts)
                oacc = 4 + (cnt["acc"] % 2)
                dacc = 6 + (cnt["acc"] % 2)
                cnt["acc"] += 1
                sbank = {}

                def emit_S(idx):
                    kt = kts[idx]
                    b = cnt["s"] % 4
                    cnt["s"] += 1
                    sbank[idx] = b
                    last = bias_mm is None
                    p.op("pe", lambda e, b=b, kt=kt, J=J, last=last: e.matmul(
                        PS[b], kT[:, kt * 128:(kt + 1) * 128], qT[:, J * 512:(J + 1) * 512], start=True, stop=last),
                         reads=[B_k, B_q], writes=[PSB[b]], inc=last)
                    if bias_mm is not None:
                        bias_mm(J, kt, b)

                LA = 3
                for i0 in range(min(LA, n)):
                    emit_S(i0)
                for idx in range(n):
                    kt = kts[idx]
                    if idx + LA < n:
                        emit_S(idx + LA)
                    b = sbank[idx]
                    tmp, B_tmp = tmpb[cnt["tmp"] % 4]
                    cnt["tmp"] += 1
                    pt, B_pt = ptb[cnt["pt"] % 4]
                    cnt["pt"] += 1
                    pre_exp(J, kt, b, tmp, B_tmp)
                    p.op("act", lambda e, tmp=tmp, pt=pt: e.activation(out=pt, in_=tmp, func=AF.Exp),
                         reads=[B_tmp], writes=[B_pt])
                    p.op("pe", lambda e, kt=kt, pt=pt, idx=idx, oacc=oacc, n=n: e.matmul(
                        PS[oacc], V[:, kt, :], pt, start=(idx == 0), stop=(idx == n - 1)),
                         reads=[B_V, B_pt], writes=[PSB[oacc]], inc=False)
                    p.op("pe", lambda e, pt=pt, idx=idx, dacc=dacc, n=n: e.matmul(
                        PS[dacc], ones_b, pt, start=(idx == 0), stop=(idx == n - 1)),
                         reads=[B_const, B_pt], writes=[PSB[dacc]], inc=True)
                ost, B_ost = ostb[cnt["ost"] % 2]
                cnt["ost"] += 1
                p.op("dve", lambda e, dacc=dacc: e.reciprocal(out=rec, in_=PS[dacc]), reads=[PSB[dacc]], writes=[B_rec])
                p.op("dve", lambda e, ost=ost, oacc=oacc: e.tensor_tensor(out=ost, in0=PS[oacc], in1=rec, op=ALU.mult),
                     reads=[PSB[oacc], B_rec], writes=[B_ost])
                p.dma("sp", oT_s[h_glob][:, J * 512:(J + 1) * 512], ost, reads=[B_ost], writes=[B_oTs])

        B_oTs = Buf("oT_s")
        B_scr = Buf("scrA")

        A.push()
        wuk = A.bf16(2 * 2048).rearrange("p (c n) -> p c n", c=2)
        wuv = A.bf16(2 * 2048).rearrange("p (c n) -> p c n", c=2)
        B_wu = Buf("wu")
        p.dma("pool", wuk, inp("wuk_t"), writes=[B_wu])
        p.dma("pool", wuv, inp("wuv_t"), writes=[B_wu])
        Dm = A.f32(24 * 512).rearrange("p (k q) -> p k q", k=24)
        B_Dm = Buf("Dm")
        dmi = {}
        for J in range(2):
            for kt in KTS[J]:
                i = len(dmi)
                dmi[(J, kt)] = i
                dst = Dm[:, i, :]
                p.op("dve", lambda e, dst=dst, J=J, kt=kt: e.tensor_scalar(
                    out=dst, in0=qpos_b[:, J * 512:(J + 1) * 512], scalar1=kpos_col[:, kt:kt + 1], scalar2=None,
                    op0=ALU.subtract), reads=[B_pos], writes=[B_Dm])
                p.op("dve", lambda e, dst=dst: e.scalar_tensor_tensor(out=dst, in0=dst, scalar=-1.0, in1=dst,
                                                                      op0=ALU.mult, op1=ALU.max),
                     reads=[B_Dm], writes=[B_Dm])
                p.op("dve", lambda e, dst=dst: e.tensor_scalar(out=dst, in0=dst, scalar1=1.0e6, scalar2=None, op0=ALU.add),
                     reads=[B_Dm], writes=[B_Dm])
                p.op("dve", lambda e, dst=dst, J=J, kt=kt: e.scalar_tensor_tensor(
                    out=dst, in0=selT[:, kt, J * 512:(J + 1) * 512], scalar=-1.0e6, in1=dst, op0=ALU.mult, op1=ALU.add),
                    reads=[B_Dm, B_selT], writes=[B_Dm])
        if NH_A:
            p.dma("sp", qTb[0][0], qA_s[0], writes=[qTb[0][1]])
        for h in range(NH_A):
            qT, B_q = qTb[h % 2]
            kT, B_k = kTb[h % 2]
            V, B_V = Vb[h % 2]
            if h + 1 < NH_A:
                p.dma("sp", qTb[(h + 1) % 2][0], qA_s[h + 1], writes=[qTb[(h + 1) % 2][1]])
            for t in range(4):
                for c in range(2):
                    p.op("pe", lambda e, t=t, c=c, h=h: e.matmul(PS[t], wuk[:, c, h * 128:(h + 1) * 128],
                                                                 ckvn[:, c, t * 512:(t + 1) * 512], start=(c == 0), stop=(c == 1)),
                         reads=[B_wu, B_ckvn], writes=[PSB[t]], inc=(c == 1))
                p.op("act", lambda e, t=t, kT=kT: e.activation(out=kT[:, t * 512:(t + 1) * 512], in_=PS[t], func=AF.Copy),
                     reads=[PSB[t]], writes=[B_k])
            for t in range(4):
                for k4 in range(4):
                    kt = t * 4 + k4
                    for c in range(2):
                        p.op("pe", lambda e, t=t, k4=k4, kt=kt, c=c, h=h: e.matmul(
                            PS[t][:, k4 * 128:(k4 + 1) * 128], ckvn[:, c, kt * 128:(kt + 1) * 128],
                            wuv[:, c, h * 128:(h + 1) * 128], start=(c == 0), stop=(c == 1)),
                            reads=[B_wu, B_ckvn], writes=[PSB[t]], inc=(c == 1 and k4 == 3))
                p.op("act", lambda e, t=t, V=V: e.activation(out=V[:, t * 4:(t + 1) * 4, :],
                                                             in_=PS[t].rearrange("p (k d) -> p k d", k=4), func=AF.Copy),
                     reads=[PSB[t]], writes=[B_V])

            def pre_exp(J, kt, b, tmp, B_tmp, h=h):
                i = dmi[(J, kt)]
                p.op("dve", lambda e: e.scalar_tensor_tensor(out=tmp, in0=Dm[:, i, :], scalar=-SLOPES[h], in1=PS[b],
                                                             op0=ALU.mult, op1=ALU.add),
                     reads=[B_Dm, PSB[b]], writes=[B_tmp])

            attn_core(h, qT, B_q, kT, B_k, V, B_V, pre_exp, None)
        p.barrier()
        A.pop()

        A.push()
        Mk = A.bf16(24 * 512).rearrange("p (k q) -> p k q", k=24)
        B_Mk = Buf("Mk")
        for J in range(2):
            for kt in KTS[J]:
                i = dmi[(J, kt)]
                p.op("dve", lambda e, i=i, J=J, kt=kt: e.tensor_scalar(
                    out=Mk[:, i, :], in0=qpos_b[:, J * 512:(J + 1) * 512], scalar1=kpos_col[:, kt:kt + 1], scalar2=NEG,
                    op0=ALU.is_lt, op1=ALU.mult), reads=[B_pos], writes=[B_Mk])
        vTb = [(A.bf16(S), Buf("vT%d" % i)) for i in range(2)]
        Aopb = [(A.bf16(S), Buf("Aop%d" % i)) for i in range(2)]
        Bopb = [(A.bf16(T), Buf("Bop%d" % i)) for i in range(2)]
        for i in range(2):
            p.op("dve", lambda e, i=i: e.memset(Aopb[i][0][0:32, :], 1.0), writes=[Aopb[i][1]])
            p.op("dve", lambda e, i=i: e.memset(Bopb[i][0][0:32, :], 1.0), writes=[Bopb[i][1]])
        def fox_loads(h):
            p.dma("sp", qTb[h % 2][0], qB_s[h], writes=[qTb[h % 2][1]])
            p.dma("sp", kTb[h % 2][0], kB_s[h], writes=[kTb[h % 2][1]])
            p.dma("sp", vTb[h % 2][0], vB_s[h], writes=[vTb[h % 2][1]])
            p.dma("sp", Aopb[h % 2][0][3:6, :], cum_s[0:3, h, :], writes=[Aopb[h % 2][1]])
            p.dma("sp", Bopb[h % 2][0][0:3, :], cum_s[3:6, h, 0:T], writes=[Bopb[h % 2][1]])

        if NH_B:
            fox_loads(0)
        for h in range(NH_B):
            qT, B_q = qTb[h % 2]
            kT, B_k = kTb[h % 2]
            V, B_V = Vb[h % 2]
            vT, B_vT = vTb[h % 2]
            Aop, B_A = Aopb[h % 2]
            Bop, B_B = Bopb[h % 2]
            if h + 1 < NH_B:
                fox_loads(h + 1)
            for half in range(2):
                psb = PS[half].bitcast(BF16)
                for k8 in range(8):
                    kt = half * 8 + k8
                    p.op("pe", lambda e, psb=psb, k8=k8, kt=kt, vT=vT: e.transpose(
                        psb[:, k8 * 128:(k8 + 1) * 128], vT[:, kt * 128:(kt + 1) * 128], ident_b),
                        reads=[B_vT, B_const], writes=[PSB[half]], inc=(k8 == 7))
                p.op("act", lambda e, psb=psb, half=half, V=V: e.activation(
                    out=V[:, half * 8:(half + 1) * 8, :], in_=psb.rearrange("p (k d) -> p k d", k=8), func=AF.Copy),
                    reads=[PSB[half]], writes=[B_V])

            def bias_mm(J, kt, b, Aop=Aop, Bop=Bop, B_A=B_A, B_B=B_B):
                p.op("pe", lambda e: e.matmul(PS[b], Aop[0:6, kt * 128:(kt + 1) * 128], Bop[0:6, J * 512:(J + 1) * 512],
                                              start=False, stop=True),
                     reads=[B_A, B_B], writes=[PSB[b]], inc=True)

            def pre_exp(J, kt, b, tmp, B_tmp):
                i = dmi[(J, kt)]
                p.op("dve", lambda e: e.tensor_tensor(out=tmp, in0=PS[b], in1=Mk[:, i, :], op=ALU.add),
                     reads=[B_Mk, PSB[b]], writes=[B_tmp])

            attn_core(16 + h, qT, B_q, kT, B_k, V, B_V, pre_exp, bias_mm)
        p.barrier()
        A.pop()
        if "oT" in dbg:
            o = dout("dbg_oT", [32, 128, T], BF16)
            ot_sb = A.bf16(T)
            B_otsb = Buf("otsb")
            for i in range(32):
                p.dma("sp", ot_sb, oT_s[i], reads=[B_oTs], writes=[B_otsb])
                p.dma("sp", o[i], ot_sb, reads=[B_otsb])

        if stage <= 3:
            p.wait_all_on("sp")
            p.emit()
            return nc, dbg_out, list(IN)

        arena_reset()
        mg_s = dscr("mg_s", [32, 128, T], BF16)
        B_mgs = Buf("mg_s")
        xT = A.bf16(32 * T).rearrange("p (k t) -> p k t", k=32)
        oT = A.bf16(32 * T).rearrange("p (k t) -> p k t", k=32)
        B_x, B_o = Buf("xT"), Buf("oT")
        wtiles = [(A.bf16(32 * 128), Buf("w%d" % i)) for i in range(4)]
        bgate = A.f32(64)
        B_bg = Buf("bgate")
        p.dma("sp", bgate, inp("b_gate_t"), writes=[B_bg])
        for k4 in range(4):
            p.dma("pool", xT[:, k4 * 8:(k4 + 1) * 8, :], xT_v[:, k4 * 8:(k4 + 1) * 8, 0:T], writes=[B_x], group=(k4 > 0))
        for k4 in range(8):
            p.dma("sp", oT[:, k4 * 4:(k4 + 1) * 4, :], oT_s[k4 * 4:(k4 + 1) * 4].rearrange("c p t -> p c t"),
                  reads=[B_oTs], writes=[B_o], group=(k4 > 0))
        gsig = [(A.f32(T), Buf("gsig%d" % i)) for i in range(2)]
        m1 = A.f32(T)
        B_m1 = Buf("m1")
        mgst = [(A.bf16(T), Buf("mgst%d" % i)) for i in range(2)]
        slots2 = [[0, 1], [2, 3], [4, 5], [6, 7]]
        gi = [0]
        wi = [0]
        si = [0]

        def one_chunk(xin, xbuf, KC, w_dram, c, evac):
            wt, wb = wtiles[wi[0] % 4]
            wi[0] += 1
            banks = slots2[si[0] % 4]
            si[0] += 1
            p.dma("pool", wt[:, 0:KC * 128].rearrange("p (k n) -> p k n", k=KC), w_dram[c], writes=[wb])
            for kc in range(KC):
                for t in range(2):
                    b = banks[t]
                    p.op("pe", lambda e, b=b, kc=kc, t=t, wt=wt: e.matmul(
                        PS[b], wt[:, kc * 128:(kc + 1) * 128], xin[:, kc, t * 512:(t + 1) * 512],
                        start=(kc == 0), stop=(kc == KC - 1)),
                        reads=[wb, xbuf], writes=[PSB[b]], inc=(kc == KC - 1 and t == 1))
            evac(banks)

        w_gate_d, w_ba_d, w_bb_d = inp("w_gate_t"), inp("w_ba_t"), inp("w_bb_t")
        for n in range(32):
            ga, B_ga = gsig[0]
            gb, B_gb = gsig[1]
            mst, B_mst = mgst[n % 2]

            def ev_gate(dst, B_dst, col):
                def ev(banks):
                    for t, b in enumerate(banks):
                        p.op("act", lambda e, b=b, t=t: e.activation(
                            out=dst[:, t * 512:(t + 1) * 512], in_=PS[b], func=AF.Sigmoid, bias=bgate[:, col:col + 1], scale=1.0),
                            reads=[PSB[b], B_bg], writes=[B_dst])
                return ev

            def ev_ba(banks):
                for t, b in enumerate(banks):
                    p.op("dve", lambda e, b=b, t=t: e.tensor_tensor(
                        out=m1[:, t * 512:(t + 1) * 512], in0=PS[b], in1=ga[:, t * 512:(t + 1) * 512], op=ALU.mult),
                        reads=[PSB[b], B_ga], writes=[B_m1])

            def ev_bb(banks, mst=mst, B_mst=B_mst, n=n):
                for t, b in enumerate(banks):
                    p.op("dve", lambda e, b=b, t=t: e.tensor_tensor(
                        out=gb[:, t * 512:(t + 1) * 512], in0=PS[b], in1=gb[:, t * 512:(t + 1) * 512], op=ALU.mult),
                        reads=[PSB[b], B_gb], writes=[B_gb])
                p.op("dve", lambda e: e.tensor_tensor(out=mst, in0=gb, in1=m1, op=ALU.add),
                     reads=[B_gb, B_m1], writes=[B_mst])
                p.dma("sp", mg_s[n], mst, reads=[B_mst], writes=[B_mgs])

            one_chunk(xT, B_x, 32, w_gate_d, n, ev_gate(ga, B_ga, n))
            one_chunk(oT[:, 0:16, :], B_o, 16, w_ba_d, n, ev_ba)
            one_chunk(xT, B_x, 32, w_gate_d, 32 + n, ev_gate(gb, B_gb, 32 + n))
            one_chunk(oT[:, 16:32, :], B_o, 16, w_bb_d, n, ev_bb)
        if "mg" in dbg:
            o = dout("dbg_mg", [32, 128, T], BF16)
            for i in range(32):
                p.dma("sp", mgst[0][0], mg_s[i], reads=[B_mgs], writes=[mgst[0][1]])
                p.dma("sp", o[i], mgst[0][0], reads=[mgst[0][1]])

        if stage <= 3.3:
            p.wait_all_on("sp")
            p.emit()
            return nc, dbg_out, list(IN)

        arena_reset()
        mgT = A.bf16(32 * T).rearrange("p (k t) -> p k t", k=32)
        B_mg = Buf("mgT")
        for k4 in range(8):
            p.dma("sp", mgT[:, k4 * 4:(k4 + 1) * 4, :], mg_s[k4 * 4:(k4 + 1) * 4].rearrange("c p t -> p c t"),
                  reads=[B_mgs], writes=[B_mg], group=(k4 > 0))
        wtiles = [(A.bf16(32 * 128), Buf("w%d" % i)) for i in range(3)]
        xf = [(A.f32(T), Buf("xf%d" % i)) for i in range(2)]
        zf = [(A.f32(T), Buf("zf%d" % i)) for i in range(2)]
        zq = [(A.bf16(T), Buf("zq%d" % i)) for i in range(2)]
        zbb = [(A.bf16(T), Buf("zbb%d" % i)) for i in range(2)]
        z1_s = dscr("z1_s", [32, 128, T], F32)
        B_z1s = Buf("z1_s")
        slots_c2 = [[0, 1], [2, 3]]
        w_out_d = inp("w_out_t")
        xT_rows = inp("xT")

        def ln_stats_mm(n, z, B_z, q, B_q, nchunks):
            for t in range(2):
                p.op("pe", lambda e, t=t: e.matmul(PS[4 + t], ones_b, z[:, t * 512:(t + 1) * 512],
                                                   start=(n == 0), stop=(n == nchunks - 1)),
                     reads=[B_z, B_const], writes=[PSB[4 + t]], inc=False)
                p.op("pe", lambda e, t=t: e.matmul(PS[6 + t], ones_b, q[:, t * 512:(t + 1) * 512],
                                                   start=(n == 0), stop=(n == nchunks - 1)),
                     reads=[B_q, B_const], writes=[PSB[6 + t]], inc=True)

        for n in range(32):
            wt, wb = wtiles[n % 3]
            banks = slots_c2[n % 2]
            x_f, B_xf = xf[n % 2]
            z_f, B_zf = zf[n % 2]
            z_q, B_zq = zq[n % 2]
            p.dma("pool", wt.rearrange("p (k n) -> p k n", k=32), w_out_d[n], writes=[wb])
            p.dma("sp", x_f, xT_rows[n * 128:(n + 1) * 128, 0:T], writes=[B_xf])
            for kc in range(32):
                for t in range(2):
                    b = banks[t]
                    p.op("pe", lambda e, b=b, kc=kc, t=t, wt=wt: e.matmul(
                        PS[b], wt[:, kc * 128:(kc + 1) * 128], mgT[:, kc, t * 512:(t + 1) * 512],
                        start=(kc == 0), stop=(kc == 31)),
                        reads=[wb, B_mg], writes=[PSB[b]], inc=(kc == 31 and t == 1))
            for t, b in enumerate(banks):
                p.op("dve", lambda e, b=b, t=t, x_f=x_f, z_f=z_f: e.scalar_tensor_tensor(
                    out=z_f[:, t * 512:(t + 1) * 512], in0=x_f[:, t * 512:(t + 1) * 512], scalar=ALPHA, in1=PS[b],
                    op0=ALU.mult, op1=ALU.add), reads=[PSB[b], B_xf], writes=[B_zf])
            z_b, B_zb = zbb[n % 2]
            p.op("act", lambda e, z_f=z_f, z_q=z_q: e.activation(out=z_q, in_=z_f, func=AF.Square),
                 reads=[B_zf], writes=[B_zq])
            p.op("act", lambda e, z_f=z_f, z_b=z_b: e.activation(out=z_b, in_=z_f, func=AF.Copy),
                 reads=[B_zf], writes=[B_zb])
            p.dma("sp", z1_s[n], z_f, reads=[B_zf], writes=[B_z1s])
            ln_stats_mm(n, z_b, B_zb, z_q, B_zq, 32)

        if stage <= 3.6:
            p.wait_all_on("sp")
            p.emit()
            return nc, dbg_out, list(IN)

        def ln_finish():
            Mt = A.f32(T)
            Rt = A.f32(T)
            B_MR = Buf("MR")
            for t in range(2):
                sl = slice(t * 512, (t + 1) * 512)
                p.op("dve", lambda e, t=t, sl=sl: e.tensor_scalar(out=Mt[:, sl], in0=PS[4 + t], scalar1=1.0 / D, scalar2=None,
                                                                 op0=ALU.mult), reads=[PSB[4 + t]], writes=[B_MR])
                p.op("dve", lambda e, t=t, sl=sl: e.tensor_scalar(out=Rt[:, sl], in0=PS[6 + t], scalar1=1.0 / D, scalar2=EPS,
                                                                 op0=ALU.mult, op1=ALU.add), reads=[PSB[6 + t]], writes=[B_MR])
            msq = A.f32(T)
            p.op("dve", lambda e: e.tensor_tensor(out=msq, in0=Mt, in1=Mt, op=ALU.mult), reads=[B_MR], writes=[B_MR])
            p.op("dve", lambda e: e.tensor_sub(out=Rt, in0=Rt, in1=msq), reads=[B_MR], writes=[B_MR])
            p.op("act", lambda e: e.activation(out=Rt, in_=Rt, func=AF.Sqrt), reads=[B_MR], writes=[B_MR])
            p.op("dve", lambda e: e.reciprocal(out=Rt, in_=Rt), reads=[B_MR], writes=[B_MR])
            return Mt, Rt, B_MR

        def ln_apply(src_s, B_src, g_name, b_name, sink):
            Mt, Rt, B_MR = ln_finish()
            gcol = A.f32(32)
            bcol = A.f32(32)
            B_gb = Buf("gb")
            p.dma("sp", gcol, inp(g_name), writes=[B_gb])
            p.dma("sp", bcol, inp(b_name), writes=[B_gb])
            zb = [(A.f32(T), Buf("lz%d" % i)) for i in range(4)]
            for n in range(32):
                z, B_z = zb[n % 4]
                p.dma("sp", z, src_s[n], reads=[B_src], writes=[B_z])
                p.op("dve", lambda e, z=z: e.tensor_sub(out=z, in0=z, in1=Mt), reads=[B_MR, B_z], writes=[B_z])
                p.op("dve", lambda e, z=z: e.tensor_tensor(out=z, in0=z, in1=Rt, op=ALU.mult), reads=[B_MR, B_z], writes=[B_z])
                p.op("dve", lambda e, z=z, n=n: e.tensor_scalar(out=z, in0=z, scalar1=gcol[:, n:n + 1], scalar2=bcol[:, n:n + 1],
                                                               op0=ALU.mult, op1=ALU.add), reads=[B_gb, B_z], writes=[B_z])
                sink(n, z, B_z)

        arena_reset()
        h1_s = dscr("h1_s", [32, 128, T], F32)
        B_h1s = Buf("h1_s")
        h1T = A.bf16(32 * T).rearrange("p (k t) -> p k t", k=32)
        B_h1 = Buf("h1T")

        def sink1(n, z, B_z):
            p.dma("sp", h1_s[n], z, reads=[B_z], writes=[B_h1s])
            p.op("act", lambda e: e.activation(out=h1T[:, n, :], in_=z, func=AF.Copy), reads=[B_z], writes=[B_h1])

        ln_apply(z1_s, B_z1s, "ln1g", "ln1b", sink1)
        if "h1" in dbg:
            o = dout("dbg_h1", [32, 128, T], F32)
            tmp_h = A.f32(T)
            B_th = Buf("tmp_h")
            for i in range(32):
                p.dma("sp", tmp_h, h1_s[i], reads=[B_h1s], writes=[B_th])
                p.dma("sp", o[i], tmp_h, reads=[B_th])

        if stage <= 4:
            p.wait_all_on("sp")
            p.emit()
            return nc, dbg_out, list(IN)

        z2_s = dscr("z2_s", [32, 128, T], F32)
        B_z2s = Buf("z2_s")
        A.off = A_BASE + (2 * 32 * T + 3) // 4
        pTb = A.bf16(2 * T).rearrange("p (k t) -> p k t", k=2)
        B_pT = Buf("pT")
        p.barrier()
        p.dma("pool", pTb, inp("pT").rearrange("(k p) t -> p k t", p=128), writes=[B_pT])
        bpg = A.f32(32)
        B_bpg = Buf("bpg")
        p.dma("sp", bpg, inp("b_pg_t"), writes=[B_bpg])
        wtiles = [(A.bf16(32 * 128), Buf("w%d" % i)) for i in range(3)]
        wple = [(A.bf16(2 * 128), Buf("wple%d" % i)) for i in range(2)]
        sg = [(A.f32(T), Buf("sg%d" % i)) for i in range(2)]
        hf = [(A.f32(T), Buf("hf%d" % i)) for i in range(2)]
        w_pg_d, w_ple_d = inp("w_pg_t"), inp("w_ple_t")
        slots_d = [[0, 1], [2, 3], [4, 5], [6, 7]]
        for n in range(32):
            wt, wb = wtiles[n % 3]
            wp, wpb = wple[n % 2]
            s_g, B_sg = sg[n % 2]
            h_f, B_hf = hf[n % 2]
            bg_ = slots_d[(2 * n) % 4]
            bp_ = slots_d[(2 * n + 1) % 4]
            p.dma("pool", wt.rearrange("p (k n) -> p k n", k=32), w_pg_d[n], writes=[wb])
            p.dma("pool", wp.rearrange("p (k n) -> p k n", k=2), w_ple_d[n], writes=[wpb])
            p.dma("sp", h_f, h1_s[n], reads=[B_h1s], writes=[B_hf])
            for kc in range(32):
                for t in range(2):
                    b = bg_[t]
                    p.op("pe", lambda e, b=b, kc=kc, t=t, wt=wt: e.matmul(
                        PS[b], wt[:, kc * 128:(kc + 1) * 128], h1T[:, kc, t * 512:(t + 1) * 512],
                        start=(kc == 0), stop=(kc == 31)),
                        reads=[wb, B_h1], writes=[PSB[b]], inc=(kc == 31 and t == 1))
            for kc in range(2):
                for t in range(2):
                    b = bp_[t]
                    p.op("pe", lambda e, b=b, kc=kc, t=t, wp=wp: e.matmul(
                        PS[b], wp[:, kc * 128:(kc + 1) * 128], pTb[:, kc, t * 512:(t + 1) * 512],
                        start=(kc == 0), stop=(kc == 1)),
                        reads=[wpb, B_pT], writes=[PSB[b]], inc=(kc == 1 and t == 1))
            for t in range(2):
                sl = slice(t * 512, (t + 1) * 512)
                p.op("act", lambda e, t=t, sl=sl, s_g=s_g, n=n, b=bg_[t]: e.activation(
                    out=s_g[:, sl], in_=PS[b], func=AF.Sigmoid, bias=bpg[:, n:n + 1], scale=1.0),
                    reads=[PSB[bg_[t]], B_bpg], writes=[B_sg])
                p.op("dve", lambda e, sl=sl, s_g=s_g, b=bp_[t]: e.tensor_tensor(
                    out=s_g[:, sl], in0=PS[b], in1=s_g[:, sl], op=ALU.mult),
                    reads=[PSB[bp_[t]], B_sg], writes=[B_sg])
            p.op("dve", lambda e, s_g=s_g, h_f=h_f: e.scalar_tensor_tensor(
                out=h_f, in0=h_f, scalar=ALPHA, in1=s_g, op0=ALU.mult, op1=ALU.add),
                reads=[B_sg, B_hf], writes=[B_hf])
            p.dma("sp", z2_s[n], h_f, reads=[B_hf], writes=[B_z2s])

        if stage >= 6:
            p.barrier()
            A.off = A_BASE + (2 * 32 * T + 3) // 4
            qpT = A.alloc_top(2 * 16 * T, BF16).rearrange("p (g t) -> p g t", g=16)
            B_qp = Buf("qpT")
            wtiles = [(A.bf16(32 * 128), Buf("w%d" % i)) for i in range(3)]

            def ev_q(i, c, banks):
                for t, b in enumerate(banks):
                    p.op("act", lambda e, b=b, t=t: e.activation(out=qpT[:, c, t * 512:(t + 1) * 512], in_=PS[b], func=AF.Copy),
                         reads=[PSB[b]], writes=[B_qp])

            gemm(h1T, 32, T, inp("peer_wq_t"), list(range(16)), ev_q, [B_h1], wtiles, [[0, 1], [2, 3], [4, 5], [6, 7]])
            p.barrier()
            A.off = A_BASE
            skb = A.bf16(16 * 128).rearrange("p (g n) -> p g n", g=16)
            iota3 = A.bf16(32 * 128).rearrange("p (t n) -> p t n", t=32)
            B_ec = Buf("e1const")
            p.dma("pool", skb, inp("sk_t"), writes=[B_ec])
            p.dma("pool", iota3, inp("iota_t"), writes=[B_ec])
            S_sb = A.f32(16 * 128).rearrange("p (g n) -> p g n", g=16)
            B_Sb = [Buf("S_sb%d" % i) for i in range(4)]
            V16 = A.f32(16 * 16).rearrange("p (g k) -> p g k", g=16)
            B_Vg = [Buf("V16_%d" % i) for i in range(16)]
            w128g = A.f32(16 * 128).rearrange("p (g n) -> p g n", g=16)
            B_wg = [Buf("w128_%d" % i) for i in range(16)]
            idxu = A.alloc(4 * 128, U32)[:, 0:128].rearrange("p (h k) -> p h k", h=8)
            B_ix = [Buf("ix%d" % i) for i in range(8)]
            cand = A.f32(8 * 256).rearrange("p (h c) -> p h c", h=8)
            B_cd = [Buf("cand%d" % i) for i in range(8)]
            workc = A.f32(8 * 256).rearrange("p (h c) -> p h c", h=8)
            B_wc = [Buf("workc%d" % i) for i in range(8)]
            vals = A.f32(128).rearrange("p (h k) -> p h k", h=8)
            B_vl = [Buf("vals%d" % i) for i in range(8)]
            ev_ = A.f32(128).rearrange("p (h k) -> p h k", h=8)
            Zs = A.f32(8)
            rZ = A.f32(8)
            X = [A.f32(128).rearrange("p (h k) -> p h k", h=8) for _ in range(4)]
            B_X = [Buf("X%d" % i) for i in range(4)]
            XTb = [A.f32(4 * 128).rearrange("p (i t) -> p i t", i=4) for _ in range(2)]
            B_XTb = [Buf("XT0"), Buf("XT1")]
            B_sm = Buf("small")
            S1rep = [(A.f32(32 * 128).rearrange("p (t n) -> p t n", t=32), Buf("S1rep%d" % i)) for i in range(2)]
            L3b = [(A.bf16(32 * 128).rearrange("p (t n) -> p t n", t=32), Buf("L3%d" % i)) for i in range(2)]
            R3b = [(A.bf16(32 * 128).rearrange("p (t n) -> p t n", t=32), Buf("R3%d" % i)) for i in range(2)]
            GTb = [(A.bf16(128 * 64).rearrange("p (y t) -> p y t", y=128), Buf("GT%d" % i)) for i in range(2)]
            gT_h = gT_s
            V1v = V16.rearrange("p (h two) k -> p h two k", two=2)[:, :, 0, :]
            V2v = V16.rearrange("p (h two) k -> p h two k", two=2)[:, :, 1, :]
            gbank = [0]

            def chain(tt):
                for g in range(16):
                    p.op("pe", lambda e, g=g: e.matmul(PS[g // 4][:, (g % 4) * 128:(g % 4 + 1) * 128],
                                                       qpT[:, g, tt * 128:(tt + 1) * 128], skb[:, g, :],
                                                       start=True, stop=True),
                         reads=[B_qp, B_ec], writes=[PSB[g // 4]], inc=(g % 4 == 3))
                for b in range(4):
                    p.op("act", lambda e, b=b: e.activation(out=S_sb[:, b * 4:(b + 1) * 4, :],
                                                            in_=PS[b].rearrange("p (g n) -> p g n", g=4), func=AF.Copy),
                         reads=[PSB[b]], writes=[B_Sb[b]])
                p.dma("sp", s1_s[:, tt * 128:(tt + 1) * 128, :].rearrange("h t n -> t h n"),
                      S_sb.rearrange("p (h two) n -> p h two n", two=2)[:, :, 0, :], reads=B_Sb, writes=[B_s1s])
                for g in range(16):
                    p.op("dve", lambda e, g=g: e.max(out=V16[:, g, 0:8], in_=S_sb[:, g, :]),
                         reads=[B_Sb[g // 4]], writes=[B_Vg[g]])
                for g in range(16):
                    p.op("dve", lambda e, g=g: e.match_replace(out=w128g[:, g, :], in_to_replace=V16[:, g, 0:8],
                                                               in_values=S_sb[:, g, :], imm_value=-1e30),
                         reads=[B_Sb[g // 4], B_Vg[g]], writes=[B_wg[g]])
                for g in range(16):
                    p.op("dve", lambda e, g=g: e.max(out=V16[:, g, 8:16], in_=w128g[:, g, :]),
                         reads=[B_wg[g]], writes=[B_Vg[g]])
                for hd in range(8):
                    g = 2 * hd + 1
                    p.op("dve", lambda e, g=g, hd=hd: e.max_index(out=idxu[:, hd, 0:8], in_max=V16[:, g, 0:8], in_values=S_sb[:, g, :]),
                         reads=[B_Sb[g // 4], B_Vg[g]], writes=[B_ix[hd]])
                for hd in range(8):
                    g = 2 * hd + 1
                    p.op("dve", lambda e, g=g, hd=hd: e.max_index(out=idxu[:, hd, 8:16], in_max=V16[:, g, 8:16], in_values=S_sb[:, g, :]),
                         reads=[B_Sb[g // 4], B_Vg[g]], writes=[B_ix[hd]])
                for hd in range(8):
                    p.op("dve", lambda e, hd=hd: e.tensor_tensor(
                        out=cand[:, hd, :].rearrange("p (a b) -> p a b", a=16),
                        in0=V16[:, 2 * hd, :].unsqueeze(2).to_broadcast([128, 16, 16]),
                        in1=V16[:, 2 * hd + 1, :].unsqueeze(1).to_broadcast([128, 16, 16]), op=ALU.add),
                        reads=[B_Vg[2 * hd], B_Vg[2 * hd + 1]], writes=[B_cd[hd]])
                for hd in range(8):
                    p.op("dve", lambda e, hd=hd: e.max(out=vals[:, hd, 0:8], in_=cand[:, hd, :]),
                         reads=[B_cd[hd]], writes=[B_vl[hd]])
                for hd in range(8):
                    p.op("dve", lambda e, hd=hd: e.match_replace(out=workc[:, hd, :], in_to_replace=vals[:, hd, 0:8],
                                                                 in_values=cand[:, hd, :], imm_value=-1e30),
                         reads=[B_cd[hd], B_vl[hd]], writes=[B_wc[hd]])
                for hd in range(8):
                    p.op("dve", lambda e, hd=hd: e.max(out=vals[:, hd, 8:16], in_=workc[:, hd, :]),
                         reads=[B_wc[hd]], writes=[B_vl[hd]])
                p.op("dve", lambda e: e.tensor_copy(out=X[2], in_=idxu), reads=B_ix, writes=[B_X[2]])
                p.op("dve", lambda e: e.tensor_tensor(out=ev_, in0=vals, in1=vals[:, :, 0:1].to_broadcast([128, 8, 16]),
                                                      op=ALU.subtract), reads=B_vl + [B_sm], writes=[B_sm])
                p.op("act", lambda e: e.activation(out=ev_, in_=ev_, func=AF.Exp), reads=[B_sm], writes=[B_sm])
                p.op("dve", lambda e: e.tensor_tensor(out=X[0], in0=vals[:, :, 15:16].to_broadcast([128, 8, 16]), in1=V2v,
                                                      op=ALU.subtract), reads=B_vl + B_Vg, writes=[B_X[0]])
                p.op("dve", lambda e: e.tensor_tensor(out=X[1], in0=V2v, in1=V2v[:, :, 0:1].to_broadcast([128, 8, 16]),
                                                      op=ALU.subtract), reads=B_Vg, writes=[B_X[1]])
                p.op("dve", lambda e: e.tensor_scalar(out=X[3], in0=V1v[:, :, 0:1].to_broadcast([128, 8, 16]), scalar1=-1.0,
                                                      scalar2=None, op0=ALU.mult), reads=B_Vg, writes=[B_X[3]])
                p.op("dve", lambda e: e.scalar_tensor_tensor(out=X[0], in0=X[0], scalar=-3e-5, in1=X[3], op0=ALU.add, op1=ALU.add),
                     reads=[B_X[0], B_X[3]], writes=[B_X[0]])
                p.op("act", lambda e: e.activation(out=X[0], in_=X[0], func=AF.Exp), reads=[B_X[0]], writes=[B_X[0]])
                p.op("act", lambda e: e.activation(out=X[1], in_=X[1], func=AF.Exp), reads=[B_X[1]], writes=[B_X[1]])
                p.op("dve", lambda e: e.tensor_reduce(out=Zs, in_=ev_, axis=AX.X, op=ALU.add), reads=[B_sm], writes=[B_sm])
                p.op("dve", lambda e: e.reciprocal(out=rZ, in_=Zs), reads=[B_sm], writes=[B_sm])
                p.op("dve", lambda e: e.tensor_tensor(out=X[1], in0=X[1], in1=rZ.unsqueeze(2).to_broadcast([128, 8, 16]),
                                                      op=ALU.mult), reads=[B_sm, B_X[1]], writes=[B_X[1]])
                for i in range(4):
                    p.op("pe", lambda e, i=i: e.matmul(PS[4][:, i * 128:(i + 1) * 128], X[i].rearrange("p h k -> p (h k)"),
                                                       ident_f, start=True, stop=True),
                         reads=[B_X[i], B_const], writes=[PSB[4]], inc=(i == 3))
                p.op("act", lambda e: e.activation(out=XTb[tt % 2].rearrange("p i t -> p (i t)"), in_=PS[4], func=AF.Copy),
                     reads=[PSB[4]], writes=[B_XTb[tt % 2]])

            B_srD = [Buf("srD%d" % i) for i in range(2)]

            def stage_A(tt, sub, k):
                XT, B_XT = XTb[tt % 2], B_XTb[tt % 2]
                t0 = tt * 128 + sub * 32
                sr, B_E = S1rep[k % 2]
                B_D = B_srD[k % 2]
                R3, B_R3 = R3b[k % 2]
                for hd in range(8):
                    p.dma("sp", sr[hd * 16:(hd + 1) * 16, :, :],
                          s1_s[hd, t0:t0 + 32, :].partition_broadcast(16), reads=[B_s1s], writes=[B_D, B_E], group=(hd > 0))
                for t in range(32):
                    tc = sub * 32 + t
                    p.op("act", lambda e, t=t, tc=tc: e.activation(out=sr[:, t, :], in_=sr[:, t, :], func=AF.Exp,
                                                                  bias=XT[:, 3, tc:tc + 1], scale=1.0),
                         reads=[B_D, B_XT], writes=[B_E], skip_own=(t > 0))
                for t in range(32):
                    tc = sub * 32 + t
                    p.op("dve", lambda e, t=t, tc=tc: e.tensor_scalar(
                        out=R3[:, t, :], in0=iota3[:, 0, :], scalar1=XT[:, 2, tc:tc + 1], scalar2=XT[:, 1, tc:tc + 1],
                        op0=ALU.is_equal, op1=ALU.mult), reads=[B_ec, B_XT], writes=[B_R3], skip_own=(t > 0))

            def stage_B(tt, sub, k):
                XT, B_XT = XTb[tt % 2], B_XTb[tt % 2]
                ht = tt * 2 + sub // 2
                GT, B_GT = GTb[ht % 2]
                sr, B_E = S1rep[k % 2]
                L3, B_L3 = L3b[k % 2]
                R3, B_R3 = R3b[k % 2]
                for t in range(32):
                    tc = sub * 32 + t
                    p.op("dve", lambda e, t=t, tc=tc: e.scalar_tensor_tensor(
                        out=L3[:, t, :], in0=sr[:, t, :], scalar=XT[:, 0, tc:tc + 1], in1=sr[:, t, :],
                        op0=ALU.is_ge, op1=ALU.mult), reads=[B_E, B_XT], writes=[B_L3], skip_own=(t > 0))
                for q4 in range(8):
                    b = 5 + gbank[0] % 3
                    gbank[0] += 1
                    for kk in range(4):
                        tk = q4 * 4 + kk
                        p.op("pe", lambda e, b=b, kk=kk, tk=tk: e.matmul(
                            PS[b][:, kk * 128:(kk + 1) * 128], L3[:, tk, :], R3[:, tk, :], start=True, stop=True),
                             reads=[B_L3, B_R3], writes=[PSB[b]], inc=(kk == 3))
                    tl0 = (sub % 2) * 32 + q4 * 4
                    p.op("act", lambda e, b=b, tl0=tl0: e.activation(
                        out=GT[:, :, tl0:tl0 + 4], in_=PS[b].rearrange("p (t y) -> p y t", t=4), func=AF.Copy),
                        reads=[PSB[b]], writes=[B_GT])
                if sub % 2 == 1:
                    p.dma("sp", gT_h[ht], GT.rearrange("p y t -> p (y t)"), reads=[B_GT], writes=[B_gTs])

            jobs = [(tt, sub) for tt in range(8) for sub in range(4)]
            chain(0)
            stage_A(0, 0, 0)
            for k, (tt, sub) in enumerate(jobs):
                if k + 1 < len(jobs):
                    tt2, sub2 = jobs[k + 1]
                    if sub2 == 0:
                        chain(tt2)
                    stage_A(tt2, sub2, k + 1)
                stage_B(tt, sub, k)
            if stage <= 6:
                p.wait_all_on("sp")
                p.emit()
                return nc, dbg_out, list(IN)

            arena_reset()
            h1T = A.bf16(32 * T).rearrange("p (k t) -> p k t", k=32)
            B_h1 = Buf("h1T")
            for k4 in range(8):
                p.dma("pool", h1T[:, k4 * 4:(k4 + 1) * 4, :], h1_s[k4 * 4:(k4 + 1) * 4].rearrange("c p t -> p c t"),
                      reads=[B_h1s], writes=[B_h1], group=(k4 > 0))
            wtiles = [(A.bf16(32 * 128), Buf("w%d" % i)) for i in range(3)]
            gty = [(A.bf16(T), Buf("gty%d" % i)) for i in range(3)]
            actb = [(A.bf16(T), Buf("actb%d" % i)) for i in range(2)]
            ggb = [(A.bf16(T), Buf("ggb%d" % i)) for i in range(3)]
            gT_v = gT_s.rearrange("ht x (y t) -> x ht y t", y=128)

            def ev_e2(i, y, banks):
                g_y, B_gy = gty[i % 3]
                a_b, B_ab = actb[i % 2]
                g_g, B_gg = ggb[i % 3]
                p.dma("sp", g_y.rearrange("p (ht t) -> p ht t", ht=16), gT_v[:, :, y, :], reads=[B_gTs], writes=[B_gy])
                for t, b in enumerate(banks):
                    p.op("act", lambda e, b=b, t=t: e.activation(out=a_b[:, t * 512:(t + 1) * 512], in_=PS[b], func=AF.Gelu),
                         reads=[PSB[b]], writes=[B_ab])
                p.op("dve", lambda e: e.tensor_tensor(out=g_g, in0=a_b, in1=g_y, op=ALU.mult),
                     reads=[B_ab, B_gy], writes=[B_gg])
                p.dma("sp", ggT_s[y], g_g, reads=[B_gg], writes=[B_ggs])

            gemm(h1T, 32, T, inp("uT_t"), list(range(128)), ev_e2, [B_h1], wtiles, [[0, 1], [2, 3], [4, 5], [6, 7]])

            arena_reset()
            vtb = [(A.bf16(2 * 512).rearrange("p (y d) -> p y d", y=2), Buf("vt%d" % i)) for i in range(4)]
            gyb = [(A.bf16(2 * T).rearrange("p (y t) -> p y t", y=2), Buf("gy%d" % i)) for i in range(4)]
            ztb = [(A.f32(T), Buf("zt%d" % i)) for i in range(8)]
            v_d = inp("v_t")
            zi = 0
            for dg in range(8):
                zts = []
                for dc in range(4):
                    n = dg * 4 + dc
                    zt, B_zt = ztb[zi % 8]
                    zi += 1
                    p.dma("sp", zt, z2_s[n], reads=[B_z2s], writes=[B_zt])
                    zts.append((zt, B_zt))
                for y2 in range(64):
                    vt, B_vt = vtb[y2 % 4]
                    gy, B_gy = gyb[y2 % 4]
                    p.dma("pool", vt, v_d[2 * y2:2 * y2 + 2][:, :, dg * 512:(dg + 1) * 512].rearrange("y x d -> x y d"),
                          writes=[B_vt])
                    p.dma("sp" if y2 % 2 else "act", gy, ggT_s[2 * y2:2 * y2 + 2].rearrange("y x t -> x y t"),
                          reads=[B_ggs], writes=[B_gy])
                    for yy in range(2):
                        y = 2 * y2 + yy
                        for dc in range(4):
                            for th in range(2):
                                b = dc * 2 + th
                                p.op("pe", lambda e, b=b, dc=dc, th=th, vt=vt, gy=gy, y=y, yy=yy: e.matmul(
                                    PS[b], vt[:, yy, dc * 128:(dc + 1) * 128], gy[:, yy, th * 512:(th + 1) * 512],
                                    start=(y == 0), stop=(y == 127)),
                                    reads=[B_vt, B_gy], writes=[PSB[b]], inc=(dc == 3 and th == 1))
                for dc in range(4):
                    n = dg * 4 + dc
                    zt, B_zt = zts[dc]
                    for th in range(2):
                        b = dc * 2 + th
                        p.op("dve", lambda e, b=b, th=th, zt=zt: e.tensor_tensor(
                            out=zt[:, th * 512:(th + 1) * 512], in0=PS[b], in1=zt[:, th * 512:(th + 1) * 512], op=ALU.add),
                            reads=[PSB[b], B_zt], writes=[B_zt])
                    p.dma("sp", z2_s[n], zt, reads=[B_zt], writes=[B_z2s])

        arena_reset()
        zb2 = [(A.f32(T), Buf("fz%d" % i)) for i in range(4)]
        zq2 = [(A.bf16(T), Buf("fq%d" % i)) for i in range(4)]
        zc2 = [(A.bf16(T), Buf("fc%d" % i)) for i in range(4)]
        for n in range(32):
            z, B_z = zb2[n % 4]
            q, B_q = zq2[n % 4]
            zc, B_zc = zc2[n % 4]
            p.dma("sp", z, z2_s[n], reads=[B_z2s], writes=[B_z])
            p.op("act", lambda e, z=z, q=q: e.activation(out=q, in_=z, func=AF.Square), reads=[B_z], writes=[B_q])
            p.op("act", lambda e, z=z, zc=zc: e.activation(out=zc, in_=z, func=AF.Copy), reads=[B_z], writes=[B_zc])
            ln_stats_mm(n, zc, B_zc, q, B_q, 32)
        B_out = Buf("out")

        def sink2(n, z, B_z):
            p.dma("sp", outT_d[n * 128:(n + 1) * 128, :], z, reads=[B_z], writes=[B_out])

        ln_apply(z2_s, B_z2s, "ln2g", "ln2b", sink2)

        p.wait_all_on("sp")
        p.emit()
    return nc, dbg_out, list(IN)


def _chunked(w, kc):
    K, N = w.shape
    assert K == kc * 128 and N % 128 == 0
    return np.ascontiguousarray(w.reshape(kc, 128, N // 128, 128).transpose(2, 1, 0, 3))


def _col(v, n):
    return np.ascontiguousarray(v.reshape(n, 128).T)


def prep_shared(inp, used=None):
    f = lambda a: np.asarray(a, dtype=np.float32)

    def w_in_t():
        w_in = f(inp["w_in"])[0]
        W = [2048, 256, 2048, 64, 32, 2048, 2048, 2048, 16]
        off = np.concatenate([[0], np.cumsum(W)])
        seg = lambda i: w_in[:, off[i]:off[i + 1]]
        z = lambda n: np.zeros((D, n), np.float32)
        cols = np.concatenate([
            seg(0), seg(5), seg(2), seg(4), z(96),
            seg(1), seg(3), seg(3), seg(6), seg(7), seg(8), z(112)], axis=1)
        assert cols.shape[1] == (NQ_CH + NK_CH) * 128
        return _chunked(cols, 32)

    def bfor():
        bf = np.zeros((128, 1), np.float32)
        bf[:16, 0] = f(inp["b_forget"])[0]
        return bf

    th = {
        "w_in_t": w_in_t,
        "ident": lambda: np.eye(128, dtype=np.float32),
        "glat": lambda: _col(f(inp["g_latent"])[0], 2),
        "bfor": bfor,
        "wuk_t": lambda: np.ascontiguousarray(f(inp["w_uk"])[0].reshape(2, 128, 2048).transpose(1, 0, 2)),
        "wuv_t": lambda: np.ascontiguousarray(f(inp["w_uv"])[0].reshape(2, 128, 2048).transpose(1, 0, 2)),
        "w_gate_t": lambda: _chunked(f(inp["w_gate"])[0], 32),
        "b_gate_t": lambda: _col(f(inp["b_gate"])[0], 64),
        "w_ba_t": lambda: _chunked(f(inp["w_branch_a"])[0], 16),
        "w_bb_t": lambda: _chunked(f(inp["w_branch_b"])[0], 16),
        "w_out_t": lambda: _chunked(f(inp["w_out"])[0], 32),
        "ln1g": lambda: _col(f(inp["ln1_g"])[0], 32),
        "ln1b": lambda: _col(f(inp["ln1_b"])[0], 32),
        "peer_wq_t": lambda: _chunked(f(inp["peer_wq"])[0].reshape(D, 2048), 32),
        "sk_t": lambda: np.ascontiguousarray(f(inp["peer_subkeys"])[0].reshape(16, 128, 128).transpose(2, 0, 1)),
        "uT_t": lambda: np.ascontiguousarray(f(inp["peer_u"])[0].reshape(128, 128, 32, 128).transpose(1, 3, 2, 0)),
        "v_t": lambda: np.ascontiguousarray(f(inp["peer_v"])[0].reshape(128, 128, D).transpose(1, 0, 2)),
        "w_pg_t": lambda: _chunked(f(inp["w_ple_gate"])[0], 32),
        "b_pg_t": lambda: _col(f(inp["b_ple_gate"])[0], 32),
        "w_ple_t": lambda: _chunked(f(inp["w_ple"])[0], 2),
        "ln2g": lambda: _col(f(inp["ln2_g"])[0], 32),
        "ln2b": lambda: _col(f(inp["ln2_b"])[0], 32),
        "iota_t": lambda: np.ascontiguousarray(np.broadcast_to(np.arange(128, dtype=np.float32), (128, 32, 128))),
    }
    return {k: fn() for k, fn in th.items() if used is None or k in used}


def core_positions(par):
    j = np.arange(8)
    own = ((2 * j + par)[:, None] * 128 + np.arange(128)[None, :]).reshape(-1)
    oth = ((2 * j + 1 - par)[:, None] * 128 + np.arange(128)[None, :]).reshape(-1)
    return own, oth


def prep_core(inp, b, par):
    x = np.asarray(inp["x"], dtype=np.float32)[b]
    pp = np.asarray(inp["p"], dtype=np.float32)[0, b]
    own, oth = core_positions(par)
    kpos = np.concatenate([own, oth])
    d = {}
    d["xT"] = np.ascontiguousarray(x[kpos].T)
    d["pT"] = np.ascontiguousarray(pp[own].T)
    d["qpos_b"] = np.ascontiguousarray(np.broadcast_to(own.astype(np.float32), (128, T)))
    d["kpos_b"] = np.ascontiguousarray(np.broadcast_to(kpos.astype(np.float32), (128, S)))
    d["kpos_col"] = _col(kpos.astype(np.float32), 16)
    d["cend_col"] = _col(((own // 64 + 1) * 64).astype(np.float32), 8)
    ce = own // 64 + 1
    cidx = np.arange(32)
    d["penA"] = np.where(cidx[:, None] >= ce[None, :], np.float32(-1e30), np.float32(0)).astype(np.float32)
    d["penB"] = (cidx[:, None] == (kpos // 64)[None, :]).astype(np.float32)
    return d


_CACHE = {}


def kernel(**inputs):
    if "nc" not in _CACHE:
        _CACHE["nc"] = build()
    nc, _, used = _CACHE["nc"]
    sh = prep_shared(inputs, used)
    in_maps = []
    for c in range(8):
        d = dict(sh)
        d.update(prep_core(inputs, c // 2, c % 2))
        in_maps.append({k: d[k] for k in used})
    res = run_bass_kernel_spmd(nc, in_maps, core_ids=list(range(8)))
    out = np.zeros((4, S, D), np.float32)
    for c in range(8):
        own, _ = core_positions(c % 2)
        out[c // 2, own, :] = res.results[c]["outT"].T
    return out
```

```python
from contextlib import ExitStack
import numpy as np
import concourse.bass as bass
import concourse.mybir as mybir
from concourse.bass_utils import run_bass_kernel_spmd

F32 = mybir.dt.float32
BF16 = mybir.dt.bfloat16
U32 = mybir.dt.uint32
ALU = mybir.AluOpType
AF = mybir.ActivationFunctionType
AX = mybir.AxisListType

ENG = ("sp", "act", "dve", "pool", "pe")
NDMASEM = 8


class Buf:
    __slots__ = ("name", "w", "ws", "r")

    def __init__(self, name=""):
        self.name = name
        self.w = None
        self.ws = []
        self.r = []


class Prog:
    def __init__(self, nc, es):
        self.nc = nc
        self.streams = {e: [] for e in ENG}
        self.cnt = {e: 0 for e in ENG}
        self.sems = {}
        for e in ENG:
            self.sems[("e", e)] = es.enter_context(nc.semaphore("s_" + e))
        self.dcnt = {}
        self.dnext = {}
        for q in ("sp", "act", "pool"):
            self.dnext[q] = 0
            for i in range(NDMASEM):
                k = ("d", q, i)
                self.sems[k] = es.enter_context(nc.semaphore("d_%s%d" % (q, i)))
                self.dcnt[k] = 0
        self.waited = {e: {} for e in ENG}
        self.ninstr = 0

    def _wait(self, e, deps, skip_own=False):
        best = {}
        for d in deps:
            if d is None:
                continue
            k, v = d
            if skip_own and k == ("e", e):
                continue
            if best.get(k, 0) < v:
                best[k] = v
        for k, v in best.items():
            if self.waited[e].get(k, 0) >= v:
                continue
            if k == ("e", e) and v > self.cnt[e]:
                continue
            self.waited[e][k] = v
            sem = self.sems[k]
            self.streams[e].append(lambda eng, sem=sem, v=v: eng.wait_ge(sem, v))

    @staticmethod
    def _deps(reads, writes, group=False):
        deps = []
        for b in reads:
            deps.append(b.w)
            deps.extend(b.ws)
        for b in writes:
            if not group:
                deps.append(b.w)
                deps.extend(b.ws)
            deps.extend(b.r)
        return deps

    def _mark(self, tok, reads, writes, group=False):
        for b in reads:
            b.r.append(tok)
            if len(b.r) > 64:
                best = {}
                for k, v in b.r:
                    if best.get(k, 0) < v:
                        best[k] = v
                b.r = list(best.items())
        for b in writes:
            if group:
                b.ws.append(tok)
            else:
                b.w = tok
                b.ws = []
                b.r = []

    def op(self, e, fn, reads=(), writes=(), inc=True, skip_own=False):
        self._wait(e, self._deps(reads, writes), skip_own)
        tok_val = self.cnt[e] + 1
        key = ("e", e)
        if inc:
            self.cnt[e] += 1
            sem = self.sems[key]
            self.streams[e].append(lambda eng, fn=fn, sem=sem: fn(eng).then_inc(sem, 1))
        else:
            self.streams[e].append(lambda eng, fn=fn: fn(eng))
        tok = (key, tok_val)
        self._mark(tok, reads, writes)
        self.ninstr += 1
        return tok

    def dma(self, q, out, in_, reads=(), writes=(), group=False, **kw):
        i = self.dnext[q]
        self.dnext[q] = (i + 1) % NDMASEM
        k = ("d", q, i)
        deps = self._deps(reads, writes, group)
        if self.dcnt[k] > 0:
            deps.append((k, self.dcnt[k]))
        self._wait(q, deps)
        self.dcnt[k] += 16
        sem = self.sems[k]
        self.streams[q].append(
            lambda eng, out=out, in_=in_, sem=sem, kw=kw: eng.dma_start(out=out, in_=in_, **kw).then_inc(sem, 16))
        tok = (k, self.dcnt[k])
        self._mark(tok, reads, writes, group)
        self.ninstr += 1
        return tok

    def barrier(self):
        deps = [(("e", e), self.cnt[e]) for e in ENG if self.cnt[e] > 0]
        deps += [(k, v) for k, v in self.dcnt.items() if v > 0]
        for e in ENG:
            self._wait(e, deps)

    def wait_all_on(self, e):
        deps = [(("e", x), self.cnt[x]) for x in ENG if self.cnt[x] > 0]
        deps += [(k, v) for k, v in self.dcnt.items() if v > 0]
        self._wait(e, deps)

    def emit(self):
        nc = self.nc
        with nc.Block() as block:
            @block.sync
            def _(eng):
                for f in self.streams["sp"]:
                    f(eng)

            @block.scalar
            def _(eng):
                for f in self.streams["act"]:
                    f(eng)

            @block.vector
            def _(eng):
                for f in self.streams["dve"]:
                    f(eng)

            @block.gpsimd
            def _(eng):
                for f in self.streams["pool"]:
                    f(eng)

            @block.tensor
            def _(eng):
                for f in self.streams["pe"]:
                    f(eng)


class Arena:
    def __init__(self, ap_full, nwords):
        self.a = ap_full
        self.n = nwords
        self.off = 0
        self.top = nwords
        self.marks = []

    def alloc(self, nbytes, dt=F32):
        nw = (nbytes + 3) // 4
        nw = (nw + 15) // 16 * 16
        assert self.off + nw <= self.top, "SBUF arena overflow %d + %d > %d" % (self.off, nw, self.top)
        v = self.a[:, self.off:self.off + nw]
        self.off += nw
        if dt != F32:
            v = v.bitcast(dt)
        return v

    def alloc_top(self, nbytes, dt=F32):
        nw = ((nbytes + 3) // 4 + 15) // 16 * 16
        assert self.top - nw >= self.off
        self.top -= nw
        v = self.a[:, self.top:self.top + nw]
        return v.bitcast(dt) if dt != F32 else v

    def f32(self, n):
        return self.alloc(4 * n)[:, 0:n]

    def bf16(self, n):
        return self.alloc(2 * n, BF16)[:, 0:n]

    def push(self):
        self.marks.append(self.off)

    def pop(self):
        self.off = self.marks.pop()


D = 4096
S = 2048
T = 1024
NQ_CH = 49
NK_CH = 36
ALPHA = 2.0 ** 0.25
SCALE = 128.0 ** -0.5
EPS = 1e-5
NEG = -30000.0
SLOPES = [2.0 ** (-8.0 * (h + 1) / 16) for h in range(16)]

ARENA_WORDS = 184 * 256
import os
NH_A = int(os.environ.get('NH_A', 16))
NH_B = int(os.environ.get('NH_B', 16))


def build(stage=99, dbg=()):
    nc = bass.Bass("TRN2", target_bir_lowering=False)

    def din(name, shape, dt=F32):
        return nc.dram_tensor(name, list(shape), dt, kind="ExternalInput").ap()

    def dscr(name, shape, dt=F32):
        return nc.dram_tensor(name, list(shape), dt, kind="Internal").ap()

    def dout(name, shape, dt=F32):
        return nc.dram_tensor(name, list(shape), dt, kind="ExternalOutput").ap()

    IN_SHAPES = {
        "xT": [D, S], "pT": [256, T], "w_in_t": [NQ_CH + NK_CH, 128, 32, 128], "ident": [128, 128],
        "qpos_b": [128, T], "kpos_b": [128, S], "kpos_col": [128, 16], "cend_col": [128, 8], "penA": [32, T], "penB": [32, S],
        "glat": [128, 2], "bfor": [128, 1], "wuk_t": [128, 2, 2048], "wuv_t": [128, 2, 2048],
        "w_gate_t": [64, 128, 32, 128], "b_gate_t": [128, 64], "w_ba_t": [32, 128, 16, 128],
        "w_bb_t": [32, 128, 16, 128], "w_out_t": [32, 128, 32, 128], "ln1g": [128, 32], "ln1b": [128, 32],
        "peer_wq_t": [16, 128, 32, 128], "sk_t": [128, 16, 128], "uT_t": [128, 128, 32, 128],
        "v_t": [128, 128, D], "w_pg_t": [32, 128, 32, 128], "b_pg_t": [128, 32],
        "w_ple_t": [32, 128, 2, 128], "ln2g": [128, 32], "ln2b": [128, 32], "iota_t": [128, 32, 128],
    }
    IN = {}

    def inp(name):
        if name not in IN:
            IN[name] = din(name, IN_SHAPES[name])
        return IN[name]

    outT_d = dout("outT", [D, T])

    qA_s = dscr("qA_s", [16, 128, T], BF16)
    qB_s = dscr("qB_s", [16, 128, T], BF16)
    kB_s = dscr("kB_s", [16, 128, S], BF16)
    vB_s = dscr("vB_s", [16, 128, S], BF16)
    oT_s = dscr("oT_s", [32, 128, T], BF16)
    s1_s = dscr("s1_s", [8, T, 128], F32)
    B_s1s = Buf("s1_s")
    B_gTs = Buf("gT_s")
    B_ggs = Buf("ggT_s")
    gT_s = dscr("gT_s", [16, 128, 128 * 64], BF16)
    ggT_s = dscr("ggT_s", [128, 128, T], BF16)

    dbg_out = {}

    with ExitStack() as es:
        p = Prog(nc, es)
        arena_t = es.enter_context(nc.sbuf_tensor("arena", [128, ARENA_WORDS], F32))
        psum_t = es.enter_context(nc.psum_tensor("ps", [128, 4096], F32))
        A = Arena(arena_t, ARENA_WORDS)
        PS = [psum_t[:, b * 512:(b + 1) * 512] for b in range(8)]
        PSB = [Buf("ps%d" % b) for b in range(8)]

        def dbg_dump(name, ap, shape, buf, dt=F32):
            if name in dbg:
                o = dout("dbg_" + name, shape, dt)
                dbg_out[name] = o
                p.dma("sp", o, ap, reads=[buf])

        ident_f = A.f32(128)
        ident_b = A.bf16(128)
        ones_f = A.f32(128)
        ones_b = A.bf16(128)
        B_const = Buf("const")
        p.dma("sp", ident_f, inp("ident"), writes=[B_const])
        p.op("dve", lambda e: e.tensor_copy(out=ident_b, in_=ident_f), reads=[B_const], writes=[B_const])
        p.op("dve", lambda e: e.memset(ones_f, 1.0), writes=[B_const])
        p.op("dve", lambda e: e.memset(ones_b, 1.0), writes=[B_const])
        A_BASE = A.off

        def arena_reset():
            p.barrier()
            A.off = A_BASE
            A.top = ARENA_WORDS
            A.marks = []

        def gemm(xT, KC, ntok, w_dram, chunks, evac, xbufs, wtiles, ps_slots, q="pool"):
            nb = ntok // 512
            for i, c in enumerate(chunks):
                wt, wb = wtiles[i % len(wtiles)]
                p.dma(q, wt.rearrange("p (k n) -> p k n", k=KC), w_dram[c], writes=[wb])
                banks = ps_slots[i % len(ps_slots)]
                for kc in range(KC):
                    for t in range(nb):
                        b = banks[t]
                        p.op("pe", lambda e, b=b, kc=kc, t=t, wt=wt: e.matmul(
                            PS[b], wt[:, kc * 128:(kc + 1) * 128], xT[:, kc, t * 512:(t + 1) * 512],
                            start=(kc == 0), stop=(kc == KC - 1)),
                            reads=[wb] + list(xbufs), writes=[PSB[b]],
                            inc=(kc == KC - 1 and t == nb - 1))
                evac(i, c, banks)

        ckvn = A.bf16(2 * S).rearrange("p (c t) -> p c t", c=2)
        B_ckvn = Buf("ckvn")
        B_selT = Buf("selT")
        kpos_b = A.f32(S)
        qpos_b = A.f32(T)
        kpos_col = A.f32(16)
        cend_col = A.f32(8)
        glat = A.f32(2)
        negb = A.f32(1)
        B_pos = Buf("pos")
        A.push()
        qidxT = A.bf16(16 * T).rearrange("p (c t) -> p c t", c=16)
        B_qidx = Buf("qidx")
        kidxT = A.bf16(S)
        B_kidx = Buf("kidx")
        w_tok = A.f32(8 * 32).rearrange("p (j h) -> p j h", j=8)
        B_wtok = Buf("wtok")
        A.push()
        ckvT = A.f32(2 * S).rearrange("p (c t) -> p c t", c=2)
        B_ckv = Buf("ckv")
        fT = A.f32(S)
        B_f = Buf("fT")
        widxT = A.f32(T)
        B_widxT = Buf("widxT")
        A.push()
        xT = A.bf16(32 * T).rearrange("p (k t) -> p k t", k=32)
        B_x = Buf("xT")
        wtiles = [(A.bf16(32 * 128), Buf("w%d" % i)) for i in range(3)]
        stg = [(A.bf16(T), Buf("stg%d" % i)) for i in range(3)]
        xT_v = inp("xT").rearrange("(k p) t -> p k t", p=128)

        def load_x(half):
            for k4 in range(4):
                p.dma("pool", xT[:, k4 * 8:(k4 + 1) * 8, :], xT_v[:, k4 * 8:(k4 + 1) * 8, half * T:(half + 1) * T],
                      writes=[B_x], group=(k4 > 0))

        slots2 = [[0, 1], [2, 3], [4, 5], [6, 7]]
        stg_i = [0]

        def evac_A(half):
            tok0 = half * T

            def ev(i, c, banks):
                def to_scratch(dst, scale):
                    st, sb = stg[stg_i[0] % 3]
                    stg_i[0] += 1
                    for t, b in enumerate(banks):
                        p.op("act", lambda e, b=b, t=t, st=st: e.activation(
                            out=st[:, t * 512:(t + 1) * 512], in_=PS[b], func=AF.Copy, scale=scale),
                            reads=[PSB[b]], writes=[sb])
                    p.dma("sp", dst, st, reads=[sb])

                if c < 16:
                    to_scratch(qA_s[c], SCALE)
                elif c < 32:
                    to_scratch(qB_s[c - 16], SCALE)
                elif c < 48:
                    for t, b in enumerate(banks):
                        p.op("act", lambda e, b=b, t=t: e.activation(
                            out=qidxT[:, c - 32, t * 512:(t + 1) * 512], in_=PS[b], func=AF.Copy),
                            reads=[PSB[b]], writes=[B_qidx])
                elif c == 48:
                    for t, b in enumerate(banks):
                        p.op("dve", lambda e, b=b, t=t: e.tensor_copy(out=widxT[:, t * 512:(t + 1) * 512], in_=PS[b]),
                             reads=[PSB[b]], writes=[B_widxT])
                elif c < 51:
                    for t, b in enumerate(banks):
                        p.op("dve", lambda e, b=b, t=t: e.tensor_copy(
                            out=ckvT[:, c - 49, tok0 + t * 512: tok0 + (t + 1) * 512], in_=PS[b]),
                            reads=[PSB[b]], writes=[B_ckv])
                elif c == 51:
                    for t, b in enumerate(banks):
                        p.op("act", lambda e, b=b, t=t: e.activation(
                            out=kidxT[:, tok0 + t * 512: tok0 + (t + 1) * 512], in_=PS[b], func=AF.Copy),
                            reads=[PSB[b]], writes=[B_kidx])
                elif c < 68:
                    to_scratch(kB_s[c - 52][:, tok0:tok0 + T], 1.0)
                elif c < 84:
                    to_scratch(vB_s[c - 68][:, tok0:tok0 + T], 1.0)
                else:
                    for t, b in enumerate(banks):
                        p.op("dve", lambda e, b=b, t=t: e.tensor_copy(
                            out=fT[:, tok0 + t * 512: tok0 + (t + 1) * 512], in_=PS[b]),
                            reads=[PSB[b]], writes=[B_f])
            return ev

        load_x(1)
        gemm(xT, 32, T, inp("w_in_t"), list(range(NQ_CH, NQ_CH + NK_CH)), evac_A(1), [B_x], wtiles, slots2)
        load_x(0)
        gemm(xT, 32, T, inp("w_in_t"), list(range(NQ_CH + NK_CH)), evac_A(0), [B_x], wtiles, slots2)

        dbg_dump("ckv", ckvT, [128, 2, S], B_ckv)
        dbg_dump("fT", fT, [128, S], B_f)
        dbg_dump("widxT", widxT, [128, T], B_widxT)
        dbg_dump("kidxT", kidxT, [128, S], B_kidx, BF16)
        dbg_dump("qidxT", qidxT, [128, 16, T], B_qidx, BF16)

        if stage <= 1:
            p.wait_all_on("sp")
            p.emit()
            return nc, dbg_out, list(IN)

        p.barrier()
        A.pop()
        selT = A.alloc_top(2 * 16 * T, BF16).rearrange("p (k t) -> p k t", k=16)
        p.dma("sp", kpos_b, inp("kpos_b"), writes=[B_pos])
        p.dma("sp", qpos_b, inp("qpos_b"), writes=[B_pos])
        p.dma("sp", kpos_col, inp("kpos_col"), writes=[B_pos])
        p.dma("sp", cend_col, inp("cend_col"), writes=[B_pos])
        p.dma("sp", glat, inp("glat"), writes=[B_pos])
        p.dma("sp", negb, inp("bfor"), writes=[B_pos])
        p.op("dve", lambda e: e.tensor_scalar(out=negb, in0=negb, scalar1=-1.0, scalar2=None, op0=ALU.mult),
             reads=[B_pos], writes=[B_pos])
        A.push()
        sq = A.f32(2 * S).rearrange("p (c t) -> p c t", c=2)
        rstd = A.f32(S)
        B_sq, B_rstd = Buf("sq"), Buf("rstd")
        for c in range(2):
            p.op("act", lambda e, c=c: e.activation(out=sq[:, c, :], in_=ckvT[:, c, :], func=AF.Square),
                 reads=[B_ckv], writes=[B_sq])
        for t in range(4):
            for c in range(2):
                p.op("pe", lambda e, t=t, c=c: e.matmul(PS[t], ones_f, sq[:, c, t * 512:(t + 1) * 512],
                                                        start=(c == 0), stop=(c == 1)),
                     reads=[B_sq, B_const], writes=[PSB[t]], inc=(c == 1))
            p.op("dve", lambda e, t=t: e.tensor_scalar(out=rstd[:, t * 512:(t + 1) * 512], in0=PS[t],
                                                       scalar1=1.0 / 256, scalar2=EPS, op0=ALU.mult, op1=ALU.add),
                 reads=[PSB[t]], writes=[B_rstd])
        p.op("act", lambda e: e.activation(out=rstd, in_=rstd, func=AF.Sqrt), reads=[B_rstd], writes=[B_rstd])
        p.op("dve", lambda e: e.reciprocal(out=rstd, in_=rstd), reads=[B_rstd], writes=[B_rstd])
        for c in range(2):
            p.op("dve", lambda e, c=c: e.scalar_tensor_tensor(out=ckvn[:, c, :], in0=ckvT[:, c, :], scalar=glat[:, c:c + 1],
                                                              in1=rstd, op0=ALU.mult, op1=ALU.mult),
                 reads=[B_ckv, B_rstd, B_pos], writes=[B_ckvn])
        dbg_dump("ckvn", ckvn, [128, 2, S], B_ckvn, BF16)
        A.pop()

        for j in range(8):
            p.op("pe", lambda e, j=j: e.matmul(PS[4][:, j * 32:(j + 1) * 32], widxT[0:32, j * 128:(j + 1) * 128],
                                               ident_f[0:32, 0:32], start=True, stop=True),
                 reads=[B_widxT, B_const], writes=[PSB[4]], inc=(j == 7))
        p.op("dve", lambda e: e.tensor_copy(out=w_tok.rearrange("p j h -> p (j h)"), in_=PS[4][:, 0:256]),
             reads=[PSB[4]], writes=[B_wtok])

        A.push()
        l2 = A.f32(S)
        B_l2 = Buf("l2")
        p.op("act", lambda e: e.activation(out=l2[0:16, :], in_=fT[0:16, :], func=AF.Exp, scale=-1.0, bias=negb[0:16, :]),
             reads=[B_f, B_pos], writes=[B_l2])
        p.op("act", lambda e: e.activation(out=l2[0:16, :], in_=l2[0:16, :], func=AF.Ln, scale=1.0, bias=1.0),
             reads=[B_l2], writes=[B_l2])
        for i in range(16):
            p.op("pe", lambda e, i=i: e.matmul(PS[5][:, i * 16:(i + 1) * 16], l2[0:16, i * 128:(i + 1) * 128],
                                               ident_f[0:16, 0:16], start=True, stop=True),
                 reads=[B_l2, B_const], writes=[PSB[5]], inc=(i == 15))
        l2t = A.f32(256)
        r1 = A.f32(256)
        tmpf = A.f32(256)
        parts = [A.bf16(256) for _ in range(3)]
        B_sp = Buf("split")
        p.op("dve", lambda e: e.tensor_copy(out=l2t, in_=PS[5][:, 0:256]), reads=[PSB[5]], writes=[B_sp])

        def split3(src, res, tmp, outs, n_part):
            sl = lambda a: a[0:n_part]
            o0, o1, o2 = outs
            p.op("dve", lambda e: e.tensor_copy(out=sl(o0), in_=sl(src)), reads=[B_sp], writes=[B_sp])
            p.op("dve", lambda e: e.tensor_copy(out=sl(tmp), in_=sl(o0)), reads=[B_sp], writes=[B_sp])
            p.op("dve", lambda e: e.tensor_sub(out=sl(res), in0=sl(src), in1=sl(tmp)), reads=[B_sp], writes=[B_sp])
            p.op("dve", lambda e: e.tensor_copy(out=sl(o1), in_=sl(res)), reads=[B_sp], writes=[B_sp])
            p.op("dve", lambda e: e.tensor_copy(out=sl(tmp), in_=sl(o1)), reads=[B_sp], writes=[B_sp])
            p.op("dve", lambda e: e.tensor_sub(out=sl(res), in0=sl(res), in1=sl(tmp)), reads=[B_sp], writes=[B_sp])
            p.op("dve", lambda e: e.tensor_copy(out=sl(o2), in_=sl(res)), reads=[B_sp], writes=[B_sp])

        split3(l2t, r1, tmpf, parts, 128)
        TtR = A.f32(S)
        Tt = [(TtR[:, i * 1024:(i + 1) * 1024].bitcast(BF16), Buf("Tt%d" % i)) for i in range(2)]
        for i in range(16):
            tt, tb = Tt[i % 2]
            p.op("dve", lambda e, i=i, tt=tt: e.tensor_scalar(out=tt, in0=kpos_b, scalar1=kpos_col[:, i:i + 1], scalar2=None,
                                                             op0=ALU.is_ge),
                 reads=[B_pos], writes=[tb])
            for t in range(4):
                for k in range(3):
                    p.op("pe", lambda e, i=i, t=t, k=k, tt=tt: e.matmul(
                        PS[t][0:16, :], parts[k][:, i * 16:(i + 1) * 16], tt[:, t * 512:(t + 1) * 512],
                        start=(i == 0 and k == 0), stop=(i == 15 and k == 2)),
                        reads=[B_sp, tb], writes=[PSB[t]], inc=(k == 2 and t == 3))
        cn = A.f32(S)
        cres = l2
        ctmp = TtR
        cparts = [A.bf16(S) for _ in range(3)]
        nparts = [A.bf16(S) for _ in range(3)]
        for t in range(4):
            p.op("dve", lambda e, t=t: e.tensor_copy(out=cn[0:16, t * 512:(t + 1) * 512], in_=PS[t][0:16, :]),
                 reads=[PSB[t]], writes=[B_sp])
        p.barrier()
        split3(cn, cres, ctmp, cparts, 16)
        cum_s = dscr("cum_s", [6, 16, S], BF16)
        B_cums = Buf("cum_s")
        for k in range(3):
            p.op("dve", lambda e, k=k: e.tensor_scalar(out=nparts[k][0:16], in0=cparts[k][0:16], scalar1=-1.0, scalar2=None,
                                                       op0=ALU.mult), reads=[B_sp], writes=[B_sp])
        for k in range(3):
            p.dma("sp", cum_s[k], cparts[k][0:16], reads=[B_sp], writes=[B_cums])
            p.dma("sp", cum_s[3 + k], nparts[k][0:16], reads=[B_sp], writes=[B_cums])
        dbg_dump("cn", cn, [128, S], B_sp)
        p.barrier()
        A.pop()

        A.pop()
        A.push()
        penA = A.bf16(T)
        penB = A.bf16(S)
        B_pen = Buf("pen")
        p.dma("pool", penA[0:32, :], inp("penA"), writes=[B_pen])
        p.dma("pool", penB[0:32, :], inp("penB"), writes=[B_pen])
        scmS = [(A.f32(S), Buf("scm%d" % i)) for i in range(4)]
        workS = [(A.f32(S), Buf("work%d" % i)) for i in range(4)]
        selcS = [(A.bf16(S), Buf("selc%d" % i)) for i in range(2)]
        m8S = [(A.f32(8), Buf("m8%d" % i)) for i in range(2)]
        thrS = [A.f32(1) for i in range(2)]
        rb = [(A.bf16(512), Buf("r%d" % i)) for i in range(4)]
        dgb = [(A.bf16(128), Buf("dg%d" % i)) for i in range(4)]
        ri = [0]
        pi = [0]

        def score_phase(j):
            scm, B_scm = scmS[j % 4]
            work, B_work = workS[j % 4]
            pieces = [0, 2] if j < 4 else [0, 1, 2, 3]
            for ip, pc in enumerate(pieces):
                p.op("pe", lambda e, ip=ip, pc=pc: e.matmul(
                    PS[4 + ip], penA[0:32, j * 128:(j + 1) * 128], penB[0:32, pc * 512:(pc + 1) * 512],
                    start=True, stop=False), reads=[B_pen], writes=[PSB[4 + ip]], inc=False)
            tiles = [(hi, ip, pc) for hi in range(32) for ip, pc in enumerate(pieces)]
            nt = len(tiles)

            def emit_R(t):
                hi, ip, pc = tiles[t]
                c, hb = hi // 2, (hi % 2) * 64
                if ip == 0:
                    dg, B_dg = dgb[hi % 4]
                    p.op("pool", lambda e, dg=dg, hi=hi: e.tensor_scalar(
                        out=dg, in0=ident_f, scalar1=w_tok[:, j, hi:hi + 1], scalar2=None, op0=ALU.mult),
                        reads=[B_const, B_wtok], writes=[B_dg])
                b = t % 4
                p.op("pe", lambda e, b=b, c=c, hb=hb, pc=pc: e.matmul(
                    PS[b], qidxT[hb:hb + 64, c, j * 128:(j + 1) * 128], kidxT[hb:hb + 64, pc * 512:(pc + 1) * 512],
                    start=True, stop=True), reads=[B_qidx, B_kidx], writes=[PSB[b]])

            LA = 3
            for t in range(min(LA, nt)):
                emit_R(t)
            for t in range(nt):
                if t + LA < nt:
                    emit_R(t + LA)
                hi, ip, pc = tiles[t]
                b = t % 4
                r, rbuf = rb[ri[0] % 4]
                ri[0] += 1
                dg, B_dg = dgb[hi % 4]
                p.op("act", lambda e, b=b, r=r: e.activation(out=r, in_=PS[b], func=AF.Relu),
                     reads=[PSB[b]], writes=[rbuf])
                p.op("pe", lambda e, ip=ip, dg=dg, r=r, hi=hi: e.matmul(
                    PS[4 + ip], dg, r, start=False, stop=(hi == 31)),
                    reads=[B_dg, rbuf], writes=[PSB[4 + ip]])
            nv = (j + 1) * 128
            for ip, pc in enumerate(pieces):
                wv = min(512, nv - (pc % 2) * 512)
                if wv <= 0:
                    continue
                d0 = (0 if pc < 2 else nv) + (pc % 2) * 512
                p.op("act", lambda e, ip=ip, d0=d0, wv=wv: e.activation(out=scm[:, d0:d0 + wv], in_=PS[4 + ip][:, 0:wv], func=AF.Copy),
                     reads=[PSB[4 + ip]], writes=[B_scm])
                if j > 0:
                    p.op("act", lambda e, ip=ip, d0=d0, wv=wv: e.activation(out=work[:, d0:d0 + wv], in_=PS[4 + ip][:, 0:wv], func=AF.Copy),
                         reads=[PSB[4 + ip]], writes=[B_work])

        def topk_pair(js):
            Ws = [2 * (j + 1) * 128 for j in js]
            for rnd in range(32):
                for a, j in enumerate(js):
                    if j == 0:
                        continue
                    work, B_work = workS[j % 4]
                    m8, B_m8 = m8S[a]
                    W = Ws[a]
                    p.op("dve", lambda e, W=W, work=work, m8=m8: e.max(out=m8, in_=work[:, 0:W]), reads=[B_work], writes=[B_m8])
                if rnd < 31:
                    for a, j in enumerate(js):
                        if j == 0:
                            continue
                        work, B_work = workS[j % 4]
                        m8, B_m8 = m8S[a]
                        W = Ws[a]
                        p.op("dve", lambda e, W=W, work=work, m8=m8: e.match_replace(
                            out=work[:, 0:W], in_to_replace=m8, in_values=work[:, 0:W], imm_value=-1e30),
                            reads=[B_m8, B_work], writes=[B_work])
            for a, j in enumerate(js):
                scm, B_scm = scmS[j % 4]
                m8, B_m8 = m8S[a]
                selc, B_selc = selcS[a]
                thr = thrS[a]
                W = Ws[a]
                if j == 0:
                    p.op("dve", lambda e, thr=thr: e.memset(thr, -1e29), reads=[B_m8], writes=[B_m8])
                else:
                    p.op("dve", lambda e, thr=thr, m8=m8: e.tensor_scalar(out=thr, in0=m8[:, 7:8], scalar1=-1e29, scalar2=None,
                                                                         op0=ALU.max), reads=[B_m8], writes=[B_m8])
                p.op("dve", lambda e, W=W, selc=selc, scm=scm, thr=thr: e.tensor_scalar(
                    out=selc[:, 0:W], in0=scm[:, 0:W], scalar1=thr, scalar2=None, op0=ALU.is_ge),
                    reads=[B_m8, B_scm], writes=[B_selc])
                for base_blk, kt_base in ((0, 0), (j + 1, 8)):
                    for g0 in range(0, j + 1, 4):
                        n4 = min(4, j + 1 - g0)
                        b = pi[0] % 4
                        pi[0] += 1
                        psb = PS[b].bitcast(BF16)
                        for i4 in range(n4):
                            i = base_blk + g0 + i4
                            p.op("pe", lambda e, psb=psb, i=i, i4=i4, selc=selc: e.transpose(
                                psb[:, i4 * 128:(i4 + 1) * 128], selc[:, i * 128:(i + 1) * 128], ident_b),
                                 reads=[B_selc, B_const], writes=[PSB[b]], inc=(i4 == n4 - 1))
                        kt0 = kt_base + g0
                        p.op("act", lambda e, psb=psb, kt0=kt0, j=j, n4=n4: e.activation(
                            out=selT[:, kt0:kt0 + n4, j * 128:(j + 1) * 128],
                            in_=psb[:, 0:n4 * 128].rearrange("p (k q) -> p k q", k=n4),
                            func=AF.Copy), reads=[PSB[b]], writes=[B_selT])

        p.op("dve", lambda e: e.memset(selT.rearrange("p k t -> p (k t)"), 0.0), writes=[B_selT])
        score_phase(0)
        score_phase(1)
        for i in range(4):
            if i < 3:
                score_phase(2 * i + 2)
                score_phase(2 * i + 3)
            topk_pair((2 * i, 2 * i + 1))
        dbg_dump("selT", selT, [128, 16, T], B_selT, BF16)
        p.barrier()
        A.pop()

        if stage <= 2:
            p.wait_all_on("sp")
            p.emit()
            return nc, dbg_out, list(IN)

        A.pop()
        KTS = [[0, 1, 2, 3, 8, 9, 10, 11], list(range(16))]
        tmpb = [(A.f32(512), Buf("tmp%d" % i)) for i in range(4)]
        ptb = [(A.bf16(512), Buf("pt%d" % i)) for i in range(4)]
        rec = A.f32(512)
        B_rec = Buf("rec")
        ostb = [(A.bf16(512), Buf("ost%d" % i)) for i in range(2)]
        qTb = [(A.bf16(T), Buf("qT%d" % i)) for i in range(2)]
        kTb = [(A.bf16(S), Buf("kT%d" % i)) for i in range(2)]
        Vb = [(A.bf16(S).rearrange("p (k d) -> p k d", k=16), Buf("V%d" % i)) for i in range(2)]
        cnt = {"s": 0, "tmp": 0, "pt": 0, "ost": 0, "acc": 0}

        def attn_core(h_glob, qT, B_q, kT, B_k, V, B_V, pre_exp, bias_mm):
            for J in range(2):
                kts = KTS[J]
                n = len(kts)
                oacc = 4 + (cnt["acc"] % 2)
                dacc = 6 + (cnt["acc"] % 2)
                cnt["acc"] += 1
                sbank = {}

                def emit_S(idx):
                    kt = kts[idx]
                    b = cnt["s"] % 4
                    cnt["s"] += 1
                    sbank[idx] = b
                    last = bias_mm is None
                    p.op("pe", lambda e, b=b, kt=kt, J=J, last=last: e.matmul(
                        PS[b], kT[:, kt * 128:(kt + 1) * 128], qT[:, J * 512:(J + 1) * 512], start=True, stop=last),
                         reads=[B_k, B_q], writes=[PSB[b]], inc=last)
                    if bias_mm is not None:
                        bias_mm(J, kt, b)

                LA = 3
                for i0 in range(min(LA, n)):
                    emit_S(i0)
                for idx in range(n):
                    kt = kts[idx]
                    if idx + LA < n:
                        emit_S(idx + LA)
                    b = sbank[idx]
                    tmp, B_tmp = tmpb[cnt["tmp"] % 4]
                    cnt["tmp"] += 1
                    pt, B_pt = ptb[cnt["pt"] % 4]
                    cnt["pt"] += 1
                    pre_exp(J, kt, b, tmp, B_tmp)
                    p.op("act", lambda e, tmp=tmp, pt=pt: e.activation(out=pt, in_=tmp, func=AF.Exp),
                         reads=[B_tmp], writes=[B_pt])
                    p.op("pe", lambda e, kt=kt, pt=pt, idx=idx, oacc=oacc, n=n: e.matmul(
                        PS[oacc], V[:, kt, :], pt, start=(idx == 0), stop=(idx == n - 1)),
                         reads=[B_V, B_pt], writes=[PSB[oacc]], inc=False)
                    p.op("pe", lambda e, pt=pt, idx=idx, dacc=dacc, n=n: e.matmul(
                        PS[dacc], ones_b, pt, start=(idx == 0), stop=(idx == n - 1)),
                         reads=[B_const, B_pt], writes=[PSB[dacc]], inc=True)
                ost, B_ost = ostb[cnt["ost"] % 2]
                cnt["ost"] += 1
                p.op("dve", lambda e, dacc=dacc: e.reciprocal(out=rec, in_=PS[dacc]), reads=[PSB[dacc]], writes=[B_rec])
                p.op("dve", lambda e, ost=ost, oacc=oacc: e.tensor_tensor(out=ost, in0=PS[oacc], in1=rec, op=ALU.mult),
                     reads=[PSB[oacc], B_rec], writes=[B_ost])
                p.dma("sp", oT_s[h_glob][:, J * 512:(J + 1) * 512], ost, reads=[B_ost], writes=[B_oTs])

        B_oTs = Buf("oT_s")
        B_scr = Buf("scrA")

        A.push()
        wuk = A.bf16(2 * 2048).rearrange("p (c n) -> p c n", c=2)
        wuv = A.bf16(2 * 2048).rearrange("p (c n) -> p c n", c=2)
        B_wu = Buf("wu")
        p.dma("pool", wuk, inp("wuk_t"), writes=[B_wu])
        p.dma("pool", wuv, inp("wuv_t"), writes=[B_wu])
        Dm = A.f32(24 * 512).rearrange("p (k q) -> p k q", k=24)
        B_Dm = Buf("Dm")
        dmi = {}
        for J in range(2):
            for kt in KTS[J]:
                i = len(dmi)
                dmi[(J, kt)] = i
                dst = Dm[:, i, :]
                p.op("dve", lambda e, dst=dst, J=J, kt=kt: e.tensor_scalar(
                    out=dst, in0=qpos_b[:, J * 512:(J + 1) * 512], scalar1=kpos_col[:, kt:kt + 1], scalar2=None,
                    op0=ALU.subtract), reads=[B_pos], writes=[B_Dm])
                p.op("dve", lambda e, dst=dst: e.scalar_tensor_tensor(out=dst, in0=dst, scalar=-1.0, in1=dst,
                                                                      op0=ALU.mult, op1=ALU.max),
                     reads=[B_Dm], writes=[B_Dm])
                p.op("dve", lambda e, dst=dst: e.tensor_scalar(out=dst, in0=dst, scalar1=1.0e6, scalar2=None, op0=ALU.add),
                     reads=[B_Dm], writes=[B_Dm])
                p.op("dve", lambda e, dst=dst, J=J, kt=kt: e.scalar_tensor_tensor(
                    out=dst, in0=selT[:, kt, J * 512:(J + 1) * 512], scalar=-1.0e6, in1=dst, op0=ALU.mult, op1=ALU.add),
                    reads=[B_Dm, B_selT], writes=[B_Dm])
        if NH_A:
            p.dma("sp", qTb[0][0], qA_s[0], writes=[qTb[0][1]])
        for h in range(NH_A):
            qT, B_q = qTb[h % 2]
            kT, B_k = kTb[h % 2]
            V, B_V = Vb[h % 2]
            if h + 1 < NH_A:
                p.dma("sp", qTb[(h + 1) % 2][0], qA_s[h + 1], writes=[qTb[(h + 1) % 2][1]])
            for t in range(4):
                for c in range(2):
                    p.op("pe", lambda e, t=t, c=c, h=h: e.matmul(PS[t], wuk[:, c, h * 128:(h + 1) * 128],
                                                                 ckvn[:, c, t * 512:(t + 1) * 512], start=(c == 0), stop=(c == 1)),
                         reads=[B_wu, B_ckvn], writes=[PSB[t]], inc=(c == 1))
                p.op("act", lambda e, t=t, kT=kT: e.activation(out=kT[:, t * 512:(t + 1) * 512], in_=PS[t], func=AF.Copy),
                     reads=[PSB[t]], writes=[B_k])
            for t in range(4):
                for k4 in range(4):
                    kt = t * 4 + k4
                    for c in range(2):
                        p.op("pe", lambda e, t=t, k4=k4, kt=kt, c=c, h=h: e.matmul(
                            PS[t][:, k4 * 128:(k4 + 1) * 128], ckvn[:, c, kt * 128:(kt + 1) * 128],
                            wuv[:, c, h * 128:(h + 1) * 128], start=(c == 0), stop=(c == 1)),
                            reads=[B_wu, B_ckvn], writes=[PSB[t]], inc=(c == 1 and k4 == 3))
                p.op("act", lambda e, t=t, V=V: e.activation(out=V[:, t * 4:(t + 1) * 4, :],
                                                             in_=PS[t].rearrange("p (k d) -> p k d", k=4), func=AF.Copy),
                     reads=[PSB[t]], writes=[B_V])

            def pre_exp(J, kt, b, tmp, B_tmp, h=h):
                i = dmi[(J, kt)]
                p.op("dve", lambda e: e.scalar_tensor_tensor(out=tmp, in0=Dm[:, i, :], scalar=-SLOPES[h], in1=PS[b],
                                                             op0=ALU.mult, op1=ALU.add),
                     reads=[B_Dm, PSB[b]], writes=[B_tmp])

            attn_core(h, qT, B_q, kT, B_k, V, B_V, pre_exp, None)
        p.barrier()
        A.pop()

        A.push()
        Mk = A.bf16(24 * 512).rearrange("p (k q) -> p k q", k=24)
        B_Mk = Buf("Mk")
        for J in range(2):
            for kt in KTS[J]:
                i = dmi[(J, kt)]
                p.op("dve", lambda e, i=i, J=J, kt=kt: e.tensor_scalar(
                    out=Mk[:, i, :], in0=qpos_b[:, J * 512:(J + 1) * 512], scalar1=kpos_col[:, kt:kt + 1], scalar2=NEG,
                    op0=ALU.is_lt, op1=ALU.mult), reads=[B_pos], writes=[B_Mk])
        vTb = [(A.bf16(S), Buf("vT%d" % i)) for i in range(2)]
        Aopb = [(A.bf16(S), Buf("Aop%d" % i)) for i in range(2)]
        Bopb = [(A.bf16(T), Buf("Bop%d" % i)) for i in range(2)]
        for i in range(2):
            p.op("dve", lambda e, i=i: e.memset(Aopb[i][0][0:32, :], 1.0), writes=[Aopb[i][1]])
            p.op("dve", lambda e, i=i: e.memset(Bopb[i][0][0:32, :], 1.0), writes=[Bopb[i][1]])
        def fox_loads(h):
            p.dma("sp", qTb[h % 2][0], qB_s[h], writes=[qTb[h % 2][1]])
            p.dma("sp", kTb[h % 2][0], kB_s[h], writes=[kTb[h % 2][1]])
            p.dma("sp", vTb[h % 2][0], vB_s[h], writes=[vTb[h % 2][1]])
            p.dma("sp", Aopb[h % 2][0][3:6, :], cum_s[0:3, h, :], writes=[Aopb[h % 2][1]])
            p.dma("sp", Bopb[h % 2][0][0:3, :], cum_s[3:6, h, 0:T], writes=[Bopb[h % 2][1]])

        if NH_B:
            fox_loads(0)
        for h in range(NH_B):
            qT, B_q = qTb[h % 2]
            kT, B_k = kTb[h % 2]
            V, B_V = Vb[h % 2]
            vT, B_vT = vTb[h % 2]
            Aop, B_A = Aopb[h % 2]
            Bop, B_B = Bopb[h % 2]
            if h + 1 < NH_B:
                fox_loads(h + 1)
            for half in range(2):
                psb = PS[half].bitcast(BF16)
                for k8 in range(8):
                    kt = half * 8 + k8
                    p.op("pe", lambda e, psb=psb, k8=k8, kt=kt, vT=vT: e.transpose(
                        psb[:, k8 * 128:(k8 + 1) * 128], vT[:, kt * 128:(kt + 1) * 128], ident_b),
                        reads=[B_vT, B_const], writes=[PSB[half]], inc=(k8 == 7))
                p.op("act", lambda e, psb=psb, half=half, V=V: e.activation(
                    out=V[:, half * 8:(half + 1) * 8, :], in_=psb.rearrange("p (k d) -> p k d", k=8), func=AF.Copy),
                    reads=[PSB[half]], writes=[B_V])

            def bias_mm(J, kt, b, Aop=Aop, Bop=Bop, B_A=B_A, B_B=B_B):
                p.op("pe", lambda e: e.matmul(PS[b], Aop[0:6, kt * 128:(kt + 1) * 128], Bop[0:6, J * 512:(J + 1) * 512],
                                              start=False, stop=True),
                     reads=[B_A, B_B], writes=[PSB[b]], inc=True)

            def pre_exp(J, kt, b, tmp, B_tmp):
                i = dmi[(J, kt)]
                p.op("dve", lambda e: e.tensor_tensor(out=tmp, in0=PS[b], in1=Mk[:, i, :], op=ALU.add),
                     reads=[B_Mk, PSB[b]], writes=[B_tmp])

            attn_core(16 + h, qT, B_q, kT, B_k, V, B_V, pre_exp, bias_mm)
        p.barrier()
        A.pop()
        if "oT" in dbg:
            o = dout("dbg_oT", [32, 128, T], BF16)
            ot_sb = A.bf16(T)
            B_otsb = Buf("otsb")
            for i in range(32):
                p.dma("sp", ot_sb, oT_s[i], reads=[B_oTs], writes=[B_otsb])
                p.dma("sp", o[i], ot_sb, reads=[B_otsb])

        if stage <= 3:
            p.wait_all_on("sp")
            p.emit()
            return nc, dbg_out, list(IN)

        arena_reset()
        mg_s = dscr("mg_s", [32, 128, T], BF16)
        B_mgs = Buf("mg_s")
        xT = A.bf16(32 * T).rearrange("p (k t) -> p k t", k=32)
        oT = A.bf16(32 * T).rearrange("p (k t) -> p k t", k=32)
        B_x, B_o = Buf("xT"), Buf("oT")
        wtiles = [(A.bf16(32 * 128), Buf("w%d" % i)) for i in range(4)]
        bgate = A.f32(64)
        B_bg = Buf("bgate")
        p.dma("sp", bgate, inp("b_gate_t"), writes=[B_bg])
        for k4 in range(4):
            p.dma("pool", xT[:, k4 * 8:(k4 + 1) * 8, :], xT_v[:, k4 * 8:(k4 + 1) * 8, 0:T], writes=[B_x], group=(k4 > 0))
        for k4 in range(8):
            p.dma("sp", oT[:, k4 * 4:(k4 + 1) * 4, :], oT_s[k4 * 4:(k4 + 1) * 4].rearrange("c p t -> p c t"),
                  reads=[B_oTs], writes=[B_o], group=(k4 > 0))
        gsig = [(A.f32(T), Buf("gsig%d" % i)) for i in range(2)]
        m1 = A.f32(T)
        B_m1 = Buf("m1")
        mgst = [(A.bf16(T), Buf("mgst%d" % i)) for i in range(2)]
        slots2 = [[0, 1], [2, 3], [4, 5], [6, 7]]
        gi = [0]
        wi = [0]
        si = [0]

        def one_chunk(xin, xbuf, KC, w_dram, c, evac):
            wt, wb = wtiles[wi[0] % 4]
            wi[0] += 1
            banks = slots2[si[0] % 4]
            si[0] += 1
            p.dma("pool", wt[:, 0:KC * 128].rearrange("p (k n) -> p k n", k=KC), w_dram[c], writes=[wb])
            for kc in range(KC):
                for t in range(2):
                    b = banks[t]
                    p.op("pe", lambda e, b=b, kc=kc, t=t, wt=wt: e.matmul(
                        PS[b], wt[:, kc * 128:(kc + 1) * 128], xin[:, kc, t * 512:(t + 1) * 512],
                        start=(kc == 0), stop=(kc == KC - 1)),
                        reads=[wb, xbuf], writes=[PSB[b]], inc=(kc == KC - 1 and t == 1))
            evac(banks)

        w_gate_d, w_ba_d, w_bb_d = inp("w_gate_t"), inp("w_ba_t"), inp("w_bb_t")
        for n in range(32):
            ga, B_ga = gsig[0]
            gb, B_gb = gsig[1]
            mst, B_mst = mgst[n % 2]

            def ev_gate(dst, B_dst, col):
                def ev(banks):
                    for t, b in enumerate(banks):
                        p.op("act", lambda e, b=b, t=t: e.activation(
                            out=dst[:, t * 512:(t + 1) * 512], in_=PS[b], func=AF.Sigmoid, bias=bgate[:, col:col + 1], scale=1.0),
                            reads=[PSB[b], B_bg], writes=[B_dst])
                return ev

            def ev_ba(banks):
                for t, b in enumerate(banks):
                    p.op("dve", lambda e, b=b, t=t: e.tensor_tensor(
                        out=m1[:, t * 512:(t + 1) * 512], in0=PS[b], in1=ga[:, t * 512:(t + 1) * 512], op=ALU.mult),
                        reads=[PSB[b], B_ga], writes=[B_m1])

            def ev_bb(banks, mst=mst, B_mst=B_mst, n=n):
                for t, b in enumerate(banks):
                    p.op("dve", lambda e, b=b, t=t: e.tensor_tensor(
                        out=gb[:, t * 512:(t + 1) * 512], in0=PS[b], in1=gb[:, t * 512:(t + 1) * 512], op=ALU.mult),
                        reads=[PSB[b], B_gb], writes=[B_gb])
                p.op("dve", lambda e: e.tensor_tensor(out=mst, in0=gb, in1=m1, op=ALU.add),
                     reads=[B_gb, B_m1], writes=[B_mst])
                p.dma("sp", mg_s[n], mst, reads=[B_mst], writes=[B_mgs])

            one_chunk(xT, B_x, 32, w_gate_d, n, ev_gate(ga, B_ga, n))
            one_chunk(oT[:, 0:16, :], B_o, 16, w_ba_d, n, ev_ba)
            one_chunk(xT, B_x, 32, w_gate_d, 32 + n, ev_gate(gb, B_gb, 32 + n))
            one_chunk(oT[:, 16:32, :], B_o, 16, w_bb_d, n, ev_bb)
        if "mg" in dbg:
            o = dout("dbg_mg", [32, 128, T], BF16)
            for i in range(32):
                p.dma("sp", mgst[0][0], mg_s[i], reads=[B_mgs], writes=[mgst[0][1]])
                p.dma("sp", o[i], mgst[0][0], reads=[mgst[0][1]])

        if stage <= 3.3:
            p.wait_all_on("sp")
            p.emit()
            return nc, dbg_out, list(IN)

        arena_reset()
        mgT = A.bf16(32 * T).rearrange("p (k t) -> p k t", k=32)
        B_mg = Buf("mgT")
        for k4 in range(8):
            p.dma("sp", mgT[:, k4 * 4:(k4 + 1) * 4, :], mg_s[k4 * 4:(k4 + 1) * 4].rearrange("c p t -> p c t"),
                  reads=[B_mgs], writes=[B_mg], group=(k4 > 0))
        wtiles = [(A.bf16(32 * 128), Buf("w%d" % i)) for i in range(3)]
        xf = [(A.f32(T), Buf("xf%d" % i)) for i in range(2)]
        zf = [(A.f32(T), Buf("zf%d" % i)) for i in range(2)]
        zq = [(A.bf16(T), Buf("zq%d" % i)) for i in range(2)]
        zbb = [(A.bf16(T), Buf("zbb%d" % i)) for i in range(2)]
        z1_s = dscr("z1_s", [32, 128, T], F32)
        B_z1s = Buf("z1_s")
        slots_c2 = [[0, 1], [2, 3]]
        w_out_d = inp("w_out_t")
        xT_rows = inp("xT")

        def ln_stats_mm(n, z, B_z, q, B_q, nchunks):
            for t in range(2):
                p.op("pe", lambda e, t=t: e.matmul(PS[4 + t], ones_b, z[:, t * 512:(t + 1) * 512],
                                                   start=(n == 0), stop=(n == nchunks - 1)),
                     reads=[B_z, B_const], writes=[PSB[4 + t]], inc=False)
                p.op("pe", lambda e, t=t: e.matmul(PS[6 + t], ones_b, q[:, t * 512:(t + 1) * 512],
                                                   start=(n == 0), stop=(n == nchunks - 1)),
                     reads=[B_q, B_const], writes=[PSB[6 + t]], inc=True)

        for n in range(32):
            wt, wb = wtiles[n % 3]
            banks = slots_c2[n % 2]
            x_f, B_xf = xf[n % 2]
            z_f, B_zf = zf[n % 2]
            z_q, B_zq = zq[n % 2]
            p.dma("pool", wt.rearrange("p (k n) -> p k n", k=32), w_out_d[n], writes=[wb])
            p.dma("sp", x_f, xT_rows[n * 128:(n + 1) * 128, 0:T], writes=[B_xf])
            for kc in range(32):
                for t in range(2):
                    b = banks[t]
                    p.op("pe", lambda e, b=b, kc=kc, t=t, wt=wt: e.matmul(
                        PS[b], wt[:, kc * 128:(kc + 1) * 128], mgT[:, kc, t * 512:(t + 1) * 512],
                        start=(kc == 0), stop=(kc == 31)),
                        reads=[wb, B_mg], writes=[PSB[b]], inc=(kc == 31 and t == 1))
            for t, b in enumerate(banks):
                p.op("dve", lambda e, b=b, t=t, x_f=x_f, z_f=z_f: e.scalar_tensor_tensor(
                    out=z_f[:, t * 512:(t + 1) * 512], in0=x_f[:, t * 512:(t + 1) * 512], scalar=ALPHA, in1=PS[b],
                    op0=ALU.mult, op1=ALU.add), reads=[PSB[b], B_xf], writes=[B_zf])
            z_b, B_zb = zbb[n % 2]
            p.op("act", lambda e, z_f=z_f, z_q=z_q: e.activation(out=z_q, in_=z_f, func=AF.Square),
                 reads=[B_zf], writes=[B_zq])
            p.op("act", lambda e, z_f=z_f, z_b=z_b: e.activation(out=z_b, in_=z_f, func=AF.Copy),
                 reads=[B_zf], writes=[B_zb])
            p.dma("sp", z1_s[n], z_f, reads=[B_zf], writes=[B_z1s])
            ln_stats_mm(n, z_b, B_zb, z_q, B_zq, 32)

        if stage <= 3.6:
            p.wait_all_on("sp")
            p.emit()
            return nc, dbg_out, list(IN)

        def ln_finish():
            Mt = A.f32(T)
            Rt = A.f32(T)
            B_MR = Buf("MR")
            for t in range(2):
                sl = slice(t * 512, (t + 1) * 512)
                p.op("dve", lambda e, t=t, sl=sl: e.tensor_scalar(out=Mt[:, sl], in0=PS[4 + t], scalar1=1.0 / D, scalar2=None,
                                                                 op0=ALU.mult), reads=[PSB[4 + t]], writes=[B_MR])
                p.op("dve", lambda e, t=t, sl=sl: e.tensor_scalar(out=Rt[:, sl], in0=PS[6 + t], scalar1=1.0 / D, scalar2=EPS,
                                                                 op0=ALU.mult, op1=ALU.add), reads=[PSB[6 + t]], writes=[B_MR])
            msq = A.f32(T)
            p.op("dve", lambda e: e.tensor_tensor(out=msq, in0=Mt, in1=Mt, op=ALU.mult), reads=[B_MR], writes=[B_MR])
            p.op("dve", lambda e: e.tensor_sub(out=Rt, in0=Rt, in1=msq), reads=[B_MR], writes=[B_MR])
            p.op("act", lambda e: e.activation(out=Rt, in_=Rt, func=AF.Sqrt), reads=[B_MR], writes=[B_MR])
            p.op("dve", lambda e: e.reciprocal(out=Rt, in_=Rt), reads=[B_MR], writes=[B_MR])
            return Mt, Rt, B_MR

        def ln_apply(src_s, B_src, g_name, b_name, sink):
            Mt, Rt, B_MR = ln_finish()
            gcol = A.f32(32)
            bcol = A.f32(32)
            B_gb = Buf("gb")
            p.dma("sp", gcol, inp(g_name), writes=[B_gb])
            p.dma("sp", bcol, inp(b_name), writes=[B_gb])
            zb = [(A.f32(T), Buf("lz%d" % i)) for i in range(4)]
            for n in range(32):
                z, B_z = zb[n % 4]
                p.dma("sp", z, src_s[n], reads=[B_src], writes=[B_z])
                p.op("dve", lambda e, z=z: e.tensor_sub(out=z, in0=z, in1=Mt), reads=[B_MR, B_z], writes=[B_z])
                p.op("dve", lambda e, z=z: e.tensor_tensor(out=z, in0=z, in1=Rt, op=ALU.mult), reads=[B_MR, B_z], writes=[B_z])
                p.op("dve", lambda e, z=z, n=n: e.tensor_scalar(out=z, in0=z, scalar1=gcol[:, n:n + 1], scalar2=bcol[:, n:n + 1],
                                                               op0=ALU.mult, op1=ALU.add), reads=[B_gb, B_z], writes=[B_z])
                sink(n, z, B_z)

        arena_reset()
        h1_s = dscr("h1_s", [32, 128, T], F32)
        B_h1s = Buf("h1_s")
        h1T = A.bf16(32 * T).rearrange("p (k t) -> p k t", k=32)
        B_h1 = Buf("h1T")

        def sink1(n, z, B_z):
            p.dma("sp", h1_s[n], z, reads=[B_z], writes=[B_h1s])
            p.op("act", lambda e: e.activation(out=h1T[:, n, :], in_=z, func=AF.Copy), reads=[B_z], writes=[B_h1])

        ln_apply(z1_s, B_z1s, "ln1g", "ln1b", sink1)
        if "h1" in dbg:
            o = dout("dbg_h1", [32, 128, T], F32)
            tmp_h = A.f32(T)
            B_th = Buf("tmp_h")
            for i in range(32):
                p.dma("sp", tmp_h, h1_s[i], reads=[B_h1s], writes=[B_th])
                p.dma("sp", o[i], tmp_h, reads=[B_th])

        if stage <= 4:
            p.wait_all_on("sp")
            p.emit()
            return nc, dbg_out, list(IN)

        z2_s = dscr("z2_s", [32, 128, T], F32)
        B_z2s = Buf("z2_s")
        A.off = A_BASE + (2 * 32 * T + 3) // 4
        pTb = A.bf16(2 * T).rearrange("p (k t) -> p k t", k=2)
        B_pT = Buf("pT")
        p.barrier()
        p.dma("pool", pTb, inp("pT").rearrange("(k p) t -> p k t", p=128), writes=[B_pT])
        bpg = A.f32(32)
        B_bpg = Buf("bpg")
        p.dma("sp", bpg, inp("b_pg_t"), writes=[B_bpg])
        wtiles = [(A.bf16(32 * 128), Buf("w%d" % i)) for i in range(3)]
        wple = [(A.bf16(2 * 128), Buf("wple%d" % i)) for i in range(2)]
        sg = [(A.f32(T), Buf("sg%d" % i)) for i in range(2)]
        hf = [(A.f32(T), Buf("hf%d" % i)) for i in range(2)]
        w_pg_d, w_ple_d = inp("w_pg_t"), inp("w_ple_t")
        slots_d = [[0, 1], [2, 3], [4, 5], [6, 7]]
        for n in range(32):
            wt, wb = wtiles[n % 3]
            wp, wpb = wple[n % 2]
            s_g, B_sg = sg[n % 2]
            h_f, B_hf = hf[n % 2]
            bg_ = slots_d[(2 * n) % 4]
            bp_ = slots_d[(2 * n + 1) % 4]
            p.dma("pool", wt.rearrange("p (k n) -> p k n", k=32), w_pg_d[n], writes=[wb])
            p.dma("pool", wp.rearrange("p (k n) -> p k n", k=2), w_ple_d[n], writes=[wpb])
            p.dma("sp", h_f, h1_s[n], reads=[B_h1s], writes=[B_hf])
            for kc in range(32):
                for t in range(2):
                    b = bg_[t]
                    p.op("pe", lambda e, b=b, kc=kc, t=t, wt=wt: e.matmul(
                        PS[b], wt[:, kc * 128:(kc + 1) * 128], h1T[:, kc, t * 512:(t + 1) * 512],
                        start=(kc == 0), stop=(kc == 31)),
                        reads=[wb, B_h1], writes=[PSB[b]], inc=(kc == 31 and t == 1))
            for kc in range(2):
                for t in range(2):
                    b = bp_[t]
                    p.op("pe", lambda e, b=b, kc=kc, t=t, wp=wp: e.matmul(
                        PS[b], wp[:, kc * 128:(kc + 1) * 128], pTb[:, kc, t * 512:(t + 1) * 512],
                        start=(kc == 0), stop=(kc == 1)),
                        reads=[wpb, B_pT], writes=[PSB[b]], inc=(kc == 1 and t == 1))
            for t in range(2):
                sl = slice(t * 512, (t + 1) * 512)
                p.op("act", lambda e, t=t, sl=sl, s_g=s_g, n=n, b=bg_[t]: e.activation(
                    out=s_g[:, sl], in_=PS[b], func=AF.Sigmoid, bias=bpg[:, n:n + 1], scale=1.0),
                    reads=[PSB[bg_[t]], B_bpg], writes=[B_sg])
                p.op("dve", lambda e, sl=sl, s_g=s_g, b=bp_[t]: e.tensor_tensor(
                    out=s_g[:, sl], in0=PS[b], in1=s_g[:, sl], op=ALU.mult),
                    reads=[PSB[bp_[t]], B_sg], writes=[B_sg])
            p.op("dve", lambda e, s_g=s_g, h_f=h_f: e.scalar_tensor_tensor(
                out=h_f, in0=h_f, scalar=ALPHA, in1=s_g, op0=ALU.mult, op1=ALU.add),
                reads=[B_sg, B_hf], writes=[B_hf])
            p.dma("sp", z2_s[n], h_f, reads=[B_hf], writes=[B_z2s])

        if stage >= 6:
            p.barrier()
            A.off = A_BASE + (2 * 32 * T + 3) // 4
            qpT = A.alloc_top(2 * 16 * T, BF16).rearrange("p (g t) -> p g t", g=16)
            B_qp = Buf("qpT")
            wtiles = [(A.bf16(32 * 128), Buf("w%d" % i)) for i in range(3)]

            def ev_q(i, c, banks):
                for t, b in enumerate(banks):
                    p.op("act", lambda e, b=b, t=t: e.activation(out=qpT[:, c, t * 512:(t + 1) * 512], in_=PS[b], func=AF.Copy),
                         reads=[PSB[b]], writes=[B_qp])

            gemm(h1T, 32, T, inp("peer_wq_t"), list(range(16)), ev_q, [B_h1], wtiles, [[0, 1], [2, 3], [4, 5], [6, 7]])
            p.barrier()
            A.off = A_BASE
            skb = A.bf16(16 * 128).rearrange("p (g n) -> p g n", g=16)
            iota3 = A.bf16(32 * 128).rearrange("p (t n) -> p t n", t=32)
            B_ec = Buf("e1const")
            p.dma("pool", skb, inp("sk_t"), writes=[B_ec])
            p.dma("pool", iota3, inp("iota_t"), writes=[B_ec])
            S_sb = A.f32(16 * 128).rearrange("p (g n) -> p g n", g=16)
            B_Sb = [Buf("S_sb%d" % i) for i in range(4)]
            V16 = A.f32(16 * 16).rearrange("p (g k) -> p g k", g=16)
            B_Vg = [Buf("V16_%d" % i) for i in range(16)]
            w128g = A.f32(16 * 128).rearrange("p (g n) -> p g n", g=16)
            B_wg = [Buf("w128_%d" % i) for i in range(16)]
            idxu = A.alloc(4 * 128, U32)[:, 0:128].rearrange("p (h k) -> p h k", h=8)
            B_ix = [Buf("ix%d" % i) for i in range(8)]
            cand = A.f32(8 * 256).rearrange("p (h c) -> p h c", h=8)
            B_cd = [Buf("cand%d" % i) for i in range(8)]
            workc = A.f32(8 * 256).rearrange("p (h c) -> p h c", h=8)
            B_wc = [Buf("workc%d" % i) for i in range(8)]
            vals = A.f32(128).rearrange("p (h k) -> p h k", h=8)
            B_vl = [Buf("vals%d" % i) for i in range(8)]
            ev_ = A.f32(128).rearrange("p (h k) -> p h k", h=8)
            Zs = A.f32(8)
            rZ = A.f32(8)
            X = [A.f32(128).rearrange("p (h k) -> p h k", h=8) for _ in range(4)]
            B_X = [Buf("X%d" % i) for i in range(4)]
            XTb = [A.f32(4 * 128).rearrange("p (i t) -> p i t", i=4) for _ in range(2)]
            B_XTb = [Buf("XT0"), Buf("XT1")]
            B_sm = Buf("small")
            S1rep = [(A.f32(32 * 128).rearrange("p (t n) -> p t n", t=32), Buf("S1rep%d" % i)) for i in range(2)]
            L3b = [(A.bf16(32 * 128).rearrange("p (t n) -> p t n", t=32), Buf("L3%d" % i)) for i in range(2)]
            R3b = [(A.bf16(32 * 128).rearrange("p (t n) -> p t n", t=32), Buf("R3%d" % i)) for i in range(2)]
            GTb = [(A.bf16(128 * 64).rearrange("p (y t) -> p y t", y=128), Buf("GT%d" % i)) for i in range(2)]
            gT_h = gT_s
            V1v = V16.rearrange("p (h two) k -> p h two k", two=2)[:, :, 0, :]
            V2v = V16.rearrange("p (h two) k -> p h two k", two=2)[:, :, 1, :]
            gbank = [0]

            def chain(tt):
                for g in range(16):
                    p.op("pe", lambda e, g=g: e.matmul(PS[g // 4][:, (g % 4) * 128:(g % 4 + 1) * 128],
                                                       qpT[:, g, tt * 128:(tt + 1) * 128], skb[:, g, :],
                                                       start=True, stop=True),
                         reads=[B_qp, B_ec], writes=[PSB[g // 4]], inc=(g % 4 == 3))
                for b in range(4):
                    p.op("act", lambda e, b=b: e.activation(out=S_sb[:, b * 4:(b + 1) * 4, :],
                                                            in_=PS[b].rearrange("p (g n) -> p g n", g=4), func=AF.Copy),
                         reads=[PSB[b]], writes=[B_Sb[b]])
                p.dma("sp", s1_s[:, tt * 128:(tt + 1) * 128, :].rearrange("h t n -> t h n"),
                      S_sb.rearrange("p (h two) n -> p h two n", two=2)[:, :, 0, :], reads=B_Sb, writes=[B_s1s])
                for g in range(16):
                    p.op("dve", lambda e, g=g: e.max(out=V16[:, g, 0:8], in_=S_sb[:, g, :]),
                         reads=[B_Sb[g // 4]], writes=[B_Vg[g]])
                for g in range(16):
                    p.op("dve", lambda e, g=g: e.match_replace(out=w128g[:, g, :], in_to_replace=V16[:, g, 0:8],
                                                               in_values=S_sb[:, g, :], imm_value=-1e30),
                         reads=[B_Sb[g // 4], B_Vg[g]], writes=[B_wg[g]])
                for g in range(16):
                    p.op("dve", lambda e, g=g: e.max(out=V16[:, g, 8:16], in_=w128g[:, g, :]),
                         reads=[B_wg[g]], writes=[B_Vg[g]])
                for hd in range(8):
                    g = 2 * hd + 1
                    p.op("dve", lambda e, g=g, hd=hd: e.max_index(out=idxu[:, hd, 0:8], in_max=V16[:, g, 0:8], in_values=S_sb[:, g, :]),
                         reads=[B_Sb[g // 4], B_Vg[g]], writes=[B_ix[hd]])
                for hd in range(8):
                    g = 2 * hd + 1
                    p.op("dve", lambda e, g=g, hd=hd: e.max_index(out=idxu[:, hd, 8:16], in_max=V16[:, g, 8:16], in_values=S_sb[:, g, :]),
                         reads=[B_Sb[g // 4], B_Vg[g]], writes=[B_ix[hd]])
                for hd in range(8):
                    p.op("dve", lambda e, hd=hd: e.tensor_tensor(
                        out=cand[:, hd, :].rearrange("p (a b) -> p a b", a=16),
                        in0=V16[:, 2 * hd, :].unsqueeze(2).to_broadcast([128, 16, 16]),
                        in1=V16[:, 2 * hd + 1, :].unsqueeze(1).to_broadcast([128, 16, 16]), op=ALU.add),
                        reads=[B_Vg[2 * hd], B_Vg[2 * hd + 1]], writes=[B_cd[hd]])
                for hd in range(8):
                    p.op("dve", lambda e, hd=hd: e.max(out=vals[:, hd, 0:8], in_=cand[:, hd, :]),
                         reads=[B_cd[hd]], writes=[B_vl[hd]])
                for hd in range(8):
                    p.op("dve", lambda e, hd=hd: e.match_replace(out=workc[:, hd, :], in_to_replace=vals[:, hd, 0:8],
                                                                 in_values=cand[:, hd, :], imm_value=-1e30),
                         reads=[B_cd[hd], B_vl[hd]], writes=[B_wc[hd]])
                for hd in range(8):
                    p.op("dve", lambda e, hd=hd: e.max(out=vals[:, hd, 8:16], in_=workc[:, hd, :]),
                         reads=[B_wc[hd]], writes=[B_vl[hd]])
                p.op("dve", lambda e: e.tensor_copy(out=X[2], in_=idxu), reads=B_ix, writes=[B_X[2]])
                p.op("dve", lambda e: e.tensor_tensor(out=ev_, in0=vals, in1=vals[:, :, 0:1].to_broadcast([128, 8, 16]),
                                                      op=ALU.subtract), reads=B_vl + [B_sm], writes=[B_sm])
                p.op("act", lambda e: e.activation(out=ev_, in_=ev_, func=AF.Exp), reads=[B_sm], writes=[B_sm])
                p.op("dve", lambda e: e.tensor_tensor(out=X[0], in0=vals[:, :, 15:16].to_broadcast([128, 8, 16]), in1=V2v,
                                                      op=ALU.subtract), reads=B_vl + B_Vg, writes=[B_X[0]])
                p.op("dve", lambda e: e.tensor_tensor(out=X[1], in0=V2v, in1=V2v[:, :, 0:1].to_broadcast([128, 8, 16]),
                                                      op=ALU.subtract), reads=B_Vg, writes=[B_X[1]])
                p.op("dve", lambda e: e.tensor_scalar(out=X[3], in0=V1v[:, :, 0:1].to_broadcast([128, 8, 16]), scalar1=-1.0,
                                                      scalar2=None, op0=ALU.mult), reads=B_Vg, writes=[B_X[3]])
                p.op("dve", lambda e: e.scalar_tensor_tensor(out=X[0], in0=X[0], scalar=-3e-5, in1=X[3], op0=ALU.add, op1=ALU.add),
                     reads=[B_X[0], B_X[3]], writes=[B_X[0]])
                p.op("act", lambda e: e.activation(out=X[0], in_=X[0], func=AF.Exp), reads=[B_X[0]], writes=[B_X[0]])
                p.op("act", lambda e: e.activation(out=X[1], in_=X[1], func=AF.Exp), reads=[B_X[1]], writes=[B_X[1]])
                p.op("dve", lambda e: e.tensor_reduce(out=Zs, in_=ev_, axis=AX.X, op=ALU.add), reads=[B_sm], writes=[B_sm])
                p.op("dve", lambda e: e.reciprocal(out=rZ, in_=Zs), reads=[B_sm], writes=[B_sm])
                p.op("dve", lambda e: e.tensor_tensor(out=X[1], in0=X[1], in1=rZ.unsqueeze(2).to_broadcast([128, 8, 16]),
                                                      op=ALU.mult), reads=[B_sm, B_X[1]], writes=[B_X[1]])
                for i in range(4):
                    p.op("pe", lambda e, i=i: e.matmul(PS[4][:, i * 128:(i + 1) * 128], X[i].rearrange("p h k -> p (h k)"),
                                                       ident_f, start=True, stop=True),
                         reads=[B_X[i], B_const], writes=[PSB[4]], inc=(i == 3))
                p.op("act", lambda e: e.activation(out=XTb[tt % 2].rearrange("p i t -> p (i t)"), in_=PS[4], func=AF.Copy),
                     reads=[PSB[4]], writes=[B_XTb[tt % 2]])

            B_srD = [Buf("srD%d" % i) for i in range(2)]

            def stage_A(tt, sub, k):
                XT, B_XT = XTb[tt % 2], B_XTb[tt % 2]
                t0 = tt * 128 + sub * 32
                sr, B_E = S1rep[k % 2]
                B_D = B_srD[k % 2]
                R3, B_R3 = R3b[k % 2]
                for hd in range(8):
                    p.dma("sp", sr[hd * 16:(hd + 1) * 16, :, :],
                          s1_s[hd, t0:t0 + 32, :].partition_broadcast(16), reads=[B_s1s], writes=[B_D, B_E], group=(hd > 0))
                for t in range(32):
                    tc = sub * 32 + t
                    p.op("act", lambda e, t=t, tc=tc: e.activation(out=sr[:, t, :], in_=sr[:, t, :], func=AF.Exp,
                                                                  bias=XT[:, 3, tc:tc + 1], scale=1.0),
                         reads=[B_D, B_XT], writes=[B_E], skip_own=(t > 0))
                for t in range(32):
                    tc = sub * 32 + t
                    p.op("dve", lambda e, t=t, tc=tc: e.tensor_scalar(
                        out=R3[:, t, :], in0=iota3[:, 0, :], scalar1=XT[:, 2, tc:tc + 1], scalar2=XT[:, 1, tc:tc + 1],
                        op0=ALU.is_equal, op1=ALU.mult), reads=[B_ec, B_XT], writes=[B_R3], skip_own=(t > 0))

            def stage_B(tt, sub, k):
                XT, B_XT = XTb[tt % 2], B_XTb[tt % 2]
                ht = tt * 2 + sub // 2
                GT, B_GT = GTb[ht % 2]
                sr, B_E = S1rep[k % 2]
                L3, B_L3 = L3b[k % 2]
                R3, B_R3 = R3b[k % 2]
                for t in range(32):
                    tc = sub * 32 + t
                    p.op("dve", lambda e, t=t, tc=tc: e.scalar_tensor_tensor(
                        out=L3[:, t, :], in0=sr[:, t, :], scalar=XT[:, 0, tc:tc + 1], in1=sr[:, t, :],
                        op0=ALU.is_ge, op1=ALU.mult), reads=[B_E, B_XT], writes=[B_L3], skip_own=(t > 0))
                for q4 in range(8):
                    b = 5 + gbank[0] % 3
                    gbank[0] += 1
                    for kk in range(4):
                        tk = q4 * 4 + kk
                        p.op("pe", lambda e, b=b, kk=kk, tk=tk: e.matmul(
                            PS[b][:, kk * 128:(kk + 1) * 128], L3[:, tk, :], R3[:, tk, :], start=True, stop=True),
                             reads=[B_L3, B_R3], writes=[PSB[b]], inc=(kk == 3))
                    tl0 = (sub % 2) * 32 + q4 * 4
                    p.op("act", lambda e, b=b, tl0=tl0: e.activation(
                        out=GT[:, :, tl0:tl0 + 4], in_=PS[b].rearrange("p (t y) -> p y t", t=4), func=AF.Copy),
                        reads=[PSB[b]], writes=[B_GT])
                if sub % 2 == 1:
                    p.dma("sp", gT_h[ht], GT.rearrange("p y t -> p (y t)"), reads=[B_GT], writes=[B_gTs])

            jobs = [(tt, sub) for tt in range(8) for sub in range(4)]
            chain(0)
            stage_A(0, 0, 0)
            for k, (tt, sub) in enumerate(jobs):
                if k + 1 < len(jobs):
                    tt2, sub2 = jobs[k + 1]
                    if sub2 == 0:
                        chain(tt2)
                    stage_A(tt2, sub2, k + 1)
                stage_B(tt, sub, k)
            if stage <= 6:
                p.wait_all_on("sp")
                p.emit()
                return nc, dbg_out, list(IN)

            arena_reset()
            h1T = A.bf16(32 * T).rearrange("p (k t) -> p k t", k=32)
            B_h1 = Buf("h1T")
            for k4 in range(8):
                p.dma("pool", h1T[:, k4 * 4:(k4 + 1) * 4, :], h1_s[k4 * 4:(k4 + 1) * 4].rearrange("c p t -> p c t"),
                      reads=[B_h1s], writes=[B_h1], group=(k4 > 0))
            wtiles = [(A.bf16(32 * 128), Buf("w%d" % i)) for i in range(3)]
            gty = [(A.bf16(T), Buf("gty%d" % i)) for i in range(3)]
            actb = [(A.bf16(T), Buf("actb%d" % i)) for i in range(2)]
            ggb = [(A.bf16(T), Buf("ggb%d" % i)) for i in range(3)]
            gT_v = gT_s.rearrange("ht x (y t) -> x ht y t", y=128)

            def ev_e2(i, y, banks):
                g_y, B_gy = gty[i % 3]
                a_b, B_ab = actb[i % 2]
                g_g, B_gg = ggb[i % 3]
                p.dma("sp", g_y.rearrange("p (ht t) -> p ht t", ht=16), gT_v[:, :, y, :], reads=[B_gTs], writes=[B_gy])
                for t, b in enumerate(banks):
                    p.op("act", lambda e, b=b, t=t: e.activation(out=a_b[:, t * 512:(t + 1) * 512], in_=PS[b], func=AF.Gelu),
                         reads=[PSB[b]], writes=[B_ab])
                p.op("dve", lambda e: e.tensor_tensor(out=g_g, in0=a_b, in1=g_y, op=ALU.mult),
                     reads=[B_ab, B_gy], writes=[B_gg])
                p.dma("sp", ggT_s[y], g_g, reads=[B_gg], writes=[B_ggs])

            gemm(h1T, 32, T, inp("uT_t"), list(range(128)), ev_e2, [B_h1], wtiles, [[0, 1], [2, 3], [4, 5], [6, 7]])

            arena_reset()
            vtb = [(A.bf16(2 * 512).rearrange("p (y d) -> p y d", y=2), Buf("vt%d" % i)) for i in range(4)]
            gyb = [(A.bf16(2 * T).rearrange("p (y t) -> p y t", y=2), Buf("gy%d" % i)) for i in range(4)]
            ztb = [(A.f32(T), Buf("zt%d" % i)) for i in range(4)]
            NRES = 32
            GGR = A.bf16(NRES * 2 * T).rearrange("p (r y t) -> p r y t", r=NRES, y=2)
            B_ggr = [Buf("ggr%d" % i) for i in range(NRES)]
            v_d = inp("v_t")
            zi = 0
            for dg in range(8):
                zts = []
                for dc in range(4):
                    n = dg * 4 + dc
                    zt, B_zt = ztb[zi % 4]
                    zi += 1
                    p.dma("sp", zt, z2_s[n], reads=[B_z2s], writes=[B_zt])
                    zts.append((zt, B_zt))
                for y2 in range(64):
                    vt, B_vt = vtb[y2 % 4]
                    p.dma("pool", vt, v_d[2 * y2:2 * y2 + 2][:, :, dg * 512:(dg + 1) * 512].rearrange("y x d -> x y d"),
                          writes=[B_vt])
                    if y2 < NRES:
                        gy, B_gy = GGR[:, y2, :, :], B_ggr[y2]
                        if dg == 0:
                            p.dma("sp" if y2 % 2 else "act", gy, ggT_s[2 * y2:2 * y2 + 2].rearrange("y x t -> x y t"),
                                  reads=[B_ggs], writes=[B_gy])
                    else:
                        gy, B_gy = gyb[y2 % 4]
                        p.dma("sp" if y2 % 2 else "act", gy, ggT_s[2 * y2:2 * y2 + 2].rearrange("y x t -> x y t"),
                              reads=[B_ggs], writes=[B_gy])
                    for yy in range(2):
                        y = 2 * y2 + yy
                        for dc in range(4):
                            for th in range(2):
                                b = dc * 2 + th
                                p.op("pe", lambda e, b=b, dc=dc, th=th, vt=vt, gy=gy, y=y, yy=yy: e.matmul(
                                    PS[b], vt[:, yy, dc * 128:(dc + 1) * 128], gy[:, yy, th * 512:(th + 1) * 512],
                                    start=(y == 0), stop=(y == 127)),
                                    reads=[B_vt, B_gy], writes=[PSB[b]], inc=(dc == 3 and th == 1))
                for dc in range(4):
                    n = dg * 4 + dc
                    zt, B_zt = zts[dc]
                    for th in range(2):
                        b = dc * 2 + th
                        p.op("dve", lambda e, b=b, th=th, zt=zt: e.tensor_tensor(
                            out=zt[:, th * 512:(th + 1) * 512], in0=PS[b], in1=zt[:, th * 512:(th + 1) * 512], op=ALU.add),
                            reads=[PSB[b], B_zt], writes=[B_zt])
                    p.dma("sp", z2_s[n], zt, reads=[B_zt], writes=[B_z2s])

        arena_reset()
        zb2 = [(A.f32(T), Buf("fz%d" % i)) for i in range(4)]
        zq2 = [(A.bf16(T), Buf("fq%d" % i)) for i in range(4)]
        zc2 = [(A.bf16(T), Buf("fc%d" % i)) for i in range(4)]
        for n in range(32):
            z, B_z = zb2[n % 4]
            q, B_q = zq2[n % 4]
            zc, B_zc = zc2[n % 4]
            p.dma("sp", z, z2_s[n], reads=[B_z2s], writes=[B_z])
            p.op("act", lambda e, z=z, q=q: e.activation(out=q, in_=z, func=AF.Square), reads=[B_z], writes=[B_q])
            p.op("act", lambda e, z=z, zc=zc: e.activation(out=zc, in_=z, func=AF.Copy), reads=[B_z], writes=[B_zc])
            ln_stats_mm(n, zc, B_zc, q, B_q, 32)
        B_out = Buf("out")

        def sink2(n, z, B_z):
            p.dma("sp", outT_d[n * 128:(n + 1) * 128, :], z, reads=[B_z], writes=[B_out])

        ln_apply(z2_s, B_z2s, "ln2g", "ln2b", sink2)

        p.wait_all_on("sp")
        p.emit()
    return nc, dbg_out, list(IN)


def _chunked(w, kc):
    K, N = w.shape
    assert K == kc * 128 and N % 128 == 0
    return np.ascontiguousarray(w.reshape(kc, 128, N // 128, 128).transpose(2, 1, 0, 3))


def _col(v, n):
    return np.ascontiguousarray(v.reshape(n, 128).T)


def prep_shared(inp, used=None):
    f = lambda a: np.asarray(a, dtype=np.float32)

    def w_in_t():
        w_in = f(inp["w_in"])[0]
        W = [2048, 256, 2048, 64, 32, 2048, 2048, 2048, 16]
        off = np.concatenate([[0], np.cumsum(W)])
        seg = lambda i: w_in[:, off[i]:off[i + 1]]
        z = lambda n: np.zeros((D, n), np.float32)
        cols = np.concatenate([
            seg(0), seg(5), seg(2), seg(4), z(96),
            seg(1), seg(3), seg(3), seg(6), seg(7), seg(8), z(112)], axis=1)
        assert cols.shape[1] == (NQ_CH + NK_CH) * 128
        return _chunked(cols, 32)

    def bfor():
        bf = np.zeros((128, 1), np.float32)
        bf[:16, 0] = f(inp["b_forget"])[0]
        return bf

    th = {
        "w_in_t": w_in_t,
        "ident": lambda: np.eye(128, dtype=np.float32),
        "glat": lambda: _col(f(inp["g_latent"])[0], 2),
        "bfor": bfor,
        "wuk_t": lambda: np.ascontiguousarray(f(inp["w_uk"])[0].reshape(2, 128, 2048).transpose(1, 0, 2)),
        "wuv_t": lambda: np.ascontiguousarray(f(inp["w_uv"])[0].reshape(2, 128, 2048).transpose(1, 0, 2)),
        "w_gate_t": lambda: _chunked(f(inp["w_gate"])[0], 32),
        "b_gate_t": lambda: _col(f(inp["b_gate"])[0], 64),
        "w_ba_t": lambda: _chunked(f(inp["w_branch_a"])[0], 16),
        "w_bb_t": lambda: _chunked(f(inp["w_branch_b"])[0], 16),
        "w_out_t": lambda: _chunked(f(inp["w_out"])[0], 32),
        "ln1g": lambda: _col(f(inp["ln1_g"])[0], 32),
        "ln1b": lambda: _col(f(inp["ln1_b"])[0], 32),
        "peer_wq_t": lambda: _chunked(f(inp["peer_wq"])[0].reshape(D, 2048), 32),
        "sk_t": lambda: np.ascontiguousarray(f(inp["peer_subkeys"])[0].reshape(16, 128, 128).transpose(2, 0, 1)),
        "uT_t": lambda: np.ascontiguousarray(f(inp["peer_u"])[0].reshape(128, 128, 32, 128).transpose(1, 3, 2, 0)),
        "v_t": lambda: np.ascontiguousarray(f(inp["peer_v"])[0].reshape(128, 128, D).transpose(1, 0, 2)),
        "w_pg_t": lambda: _chunked(f(inp["w_ple_gate"])[0], 32),
        "b_pg_t": lambda: _col(f(inp["b_ple_gate"])[0], 32),
        "w_ple_t": lambda: _chunked(f(inp["w_ple"])[0], 2),
        "ln2g": lambda: _col(f(inp["ln2_g"])[0], 32),
        "ln2b": lambda: _col(f(inp["ln2_b"])[0], 32),
        "iota_t": lambda: np.ascontiguousarray(np.broadcast_to(np.arange(128, dtype=np.float32), (128, 32, 128))),
    }
    return {k: fn() for k, fn in th.items() if used is None or k in used}


def core_positions(par):
    j = np.arange(8)
    own = ((2 * j + par)[:, None] * 128 + np.arange(128)[None, :]).reshape(-1)
    oth = ((2 * j + 1 - par)[:, None] * 128 + np.arange(128)[None, :]).reshape(-1)
    return own, oth


def prep_core(inp, b, par):
    x = np.asarray(inp["x"], dtype=np.float32)[b]
    pp = np.asarray(inp["p"], dtype=np.float32)[0, b]
    own, oth = core_positions(par)
    kpos = np.concatenate([own, oth])
    d = {}
    d["xT"] = np.ascontiguousarray(x[kpos].T)
    d["pT"] = np.ascontiguousarray(pp[own].T)
    d["qpos_b"] = np.ascontiguousarray(np.broadcast_to(own.astype(np.float32), (128, T)))
    d["kpos_b"] = np.ascontiguousarray(np.broadcast_to(kpos.astype(np.float32), (128, S)))
    d["kpos_col"] = _col(kpos.astype(np.float32), 16)
    d["cend_col"] = _col(((own // 64 + 1) * 64).astype(np.float32), 8)
    ce = own // 64 + 1
    cidx = np.arange(32)
    d["penA"] = np.where(cidx[:, None] >= ce[None, :], np.float32(-1e30), np.float32(0)).astype(np.float32)
    d["penB"] = (cidx[:, None] == (kpos // 64)[None, :]).astype(np.float32)
    return d


_CACHE = {}


def kernel(**inputs):
    if "nc" not in _CACHE:
        _CACHE["nc"] = build()
    nc, _, used = _CACHE["nc"]
    sh = prep_shared(inputs, used)
    in_maps = []
    for c in range(8):
        d = dict(sh)
        d.update(prep_core(inputs, c // 2, c % 2))
        in_maps.append({k: d[k] for k in used})
    res = run_bass_kernel_spmd(nc, in_maps, core_ids=list(range(8)))
    out = np.zeros((4, S, D), np.float32)
    for c in range(8):
        own, _ = core_positions(c % 2)
        out[c // 2, own, :] = res.results[c]["outT"].T
    return out
```

```python
from contextlib import ExitStack
import numpy as np
import concourse.bass as bass
import concourse.mybir as mybir
from concourse.bass_utils import run_bass_kernel_spmd

F32 = mybir.dt.float32
BF16 = mybir.dt.bfloat16
U32 = mybir.dt.uint32
ALU = mybir.AluOpType
AF = mybir.ActivationFunctionType
AX = mybir.AxisListType

ENG = ("sp", "act", "dve", "pool", "pe")
NDMASEM = 8


class Buf:
    __slots__ = ("name", "w", "ws", "r")

    def __init__(self, name=""):
        self.name = name
        self.w = None
        self.ws = []
        self.r = []


class Prog:
    def __init__(self, nc, es):
        self.nc = nc
        self.streams = {e: [] for e in ENG}
        self.cnt = {e: 0 for e in ENG}
        self.sems = {}
        for e in ENG:
            self.sems[("e", e)] = es.enter_context(nc.semaphore("s_" + e))
        self.dcnt = {}
        self.dnext = {}
        for q in ("sp", "act", "pool"):
            self.dnext[q] = 0
            for i in range(NDMASEM):
                k = ("d", q, i)
                self.sems[k] = es.enter_context(nc.semaphore("d_%s%d" % (q, i)))
                self.dcnt[k] = 0
        self.waited = {e: {} for e in ENG}
        self.ninstr = 0

    def _wait(self, e, deps, skip_own=False):
        best = {}
        for d in deps:
            if d is None:
                continue
            k, v = d
            if skip_own and k == ("e", e):
                continue
            if best.get(k, 0) < v:
                best[k] = v
        for k, v in best.items():
            if self.waited[e].get(k, 0) >= v:
                continue
            if k == ("e", e) and v > self.cnt[e]:
                continue
            self.waited[e][k] = v
            sem = self.sems[k]
            self.streams[e].append(lambda eng, sem=sem, v=v: eng.wait_ge(sem, v))

    @staticmethod
    def _deps(reads, writes, group=False):
        deps = []
        for b in reads:
            deps.append(b.w)
            deps.extend(b.ws)
        for b in writes:
            if not group:
                deps.append(b.w)
                deps.extend(b.ws)
            deps.extend(b.r)
        return deps

    def _mark(self, tok, reads, writes, group=False):
        for b in reads:
            b.r.append(tok)
            if len(b.r) > 64:
                best = {}
                for k, v in b.r:
                    if best.get(k, 0) < v:
                        best[k] = v
                b.r = list(best.items())
        for b in writes:
            if group:
                b.ws.append(tok)
            else:
                b.w = tok
                b.ws = []
                b.r = []

    def op(self, e, fn, reads=(), writes=(), inc=True, skip_own=False):
        self._wait(e, self._deps(reads, writes), skip_own)
        tok_val = self.cnt[e] + 1
        key = ("e", e)
        if inc:
            self.cnt[e] += 1
            sem = self.sems[key]
            self.streams[e].append(lambda eng, fn=fn, sem=sem: fn(eng).then_inc(sem, 1))
        else:
            self.streams[e].append(lambda eng, fn=fn: fn(eng))
        tok = (key, tok_val)
        self._mark(tok, reads, writes)
        self.ninstr += 1
        return tok

    def dma(self, q, out, in_, reads=(), writes=(), group=False, **kw):
        i = self.dnext[q]
        self.dnext[q] = (i + 1) % NDMASEM
        k = ("d", q, i)
        deps = self._deps(reads, writes, group)
        if self.dcnt[k] > 0:
            deps.append((k, self.dcnt[k]))
        self._wait(q, deps)
        self.dcnt[k] += 16
        sem = self.sems[k]
        self.streams[q].append(
            lambda eng, out=out, in_=in_, sem=sem, kw=kw: eng.dma_start(out=out, in_=in_, **kw).then_inc(sem, 16))
        tok = (k, self.dcnt[k])
        self._mark(tok, reads, writes, group)
        self.ninstr += 1
        return tok

    def barrier(self):
        deps = [(("e", e), self.cnt[e]) for e in ENG if self.cnt[e] > 0]
        deps += [(k, v) for k, v in self.dcnt.items() if v > 0]
        for e in ENG:
            self._wait(e, deps)

    def wait_all_on(self, e):
        deps = [(("e", x), self.cnt[x]) for x in ENG if self.cnt[x] > 0]
        deps += [(k, v) for k, v in self.dcnt.items() if v > 0]
        self._wait(e, deps)

    def emit(self):
        nc = self.nc
        with nc.Block() as block:
            @block.sync
            def _(eng):
                for f in self.streams["sp"]:
                    f(eng)

            @block.scalar
            def _(eng):
                for f in self.streams["act"]:
                    f(eng)

            @block.vector
            def _(eng):
                for f in self.streams["dve"]:
                    f(eng)

            @block.gpsimd
            def _(eng):
                for f in self.streams["pool"]:
                    f(eng)

            @block.tensor
            def _(eng):
                for f in self.streams["pe"]:
                    f(eng)


class Arena:
    def __init__(self, ap_full, nwords):
        self.a = ap_full
        self.n = nwords
        self.off = 0
        self.top = nwords
        self.marks = []

    def alloc(self, nbytes, dt=F32):
        nw = (nbytes + 3) // 4
        nw = (nw + 15) // 16 * 16
        assert self.off + nw <= self.top, "SBUF arena overflow %d + %d > %d" % (self.off, nw, self.top)
        v = self.a[:, self.off:self.off + nw]
        self.off += nw
        if dt != F32:
            v = v.bitcast(dt)
        return v

    def alloc_top(self, nbytes, dt=F32):
        nw = ((nbytes + 3) // 4 + 15) // 16 * 16
        assert self.top - nw >= self.off
        self.top -= nw
        v = self.a[:, self.top:self.top + nw]
        return v.bitcast(dt) if dt != F32 else v

    def f32(self, n):
        return self.alloc(4 * n)[:, 0:n]

    def bf16(self, n):
        return self.alloc(2 * n, BF16)[:, 0:n]

    def push(self):
        self.marks.append(self.off)

    def pop(self):
        self.off = self.marks.pop()


D = 4096
S = 2048
T = 1024
NQ_CH = 49
NK_CH = 36
ALPHA = 2.0 ** 0.25
SCALE = 128.0 ** -0.5
EPS = 1e-5
NEG = -30000.0
SLOPES = [2.0 ** (-8.0 * (h + 1) / 16) for h in range(16)]

ARENA_WORDS = 184 * 256
import os
NH_A = int(os.environ.get('NH_A', 16))
NH_B = int(os.environ.get('NH_B', 16))


def build(stage=99, dbg=()):
    nc = bass.Bass("TRN2", target_bir_lowering=False)

    def din(name, shape, dt=F32):
        return nc.dram_tensor(name, list(shape), dt, kind="ExternalInput").ap()

    def dscr(name, shape, dt=F32):
        return nc.dram_tensor(name, list(shape), dt, kind="Internal").ap()

    def dout(name, shape, dt=F32):
        return nc.dram_tensor(name, list(shape), dt, kind="ExternalOutput").ap()

    IN_SHAPES = {
        "xT": [D, S], "pT": [256, T], "w_in_t": [NQ_CH + NK_CH, 128, 32, 128], "ident": [128, 128],
        "qpos_b": [128, T], "kpos_b": [128, S], "kpos_col": [128, 16], "cend_col": [128, 8], "penA": [32, T], "penB": [32, S],
        "glat": [128, 2], "bfor": [128, 1], "wuk_t": [128, 2, 2048], "wuv_t": [128, 2, 2048],
        "w_gate_t": [64, 128, 32, 128], "b_gate_t": [128, 64], "w_ba_t": [32, 128, 16, 128],
        "w_bb_t": [32, 128, 16, 128], "w_out_t": [32, 128, 32, 128], "ln1g": [128, 32], "ln1b": [128, 32],
        "peer_wq_t": [16, 128, 32, 128], "sk_t": [128, 16, 128], "uT_t": [128, 128, 32, 128],
        "v_t": [128, 128, D], "w_pg_t": [32, 128, 32, 128], "b_pg_t": [128, 32],
        "w_ple_t": [32, 128, 2, 128], "ln2g": [128, 32], "ln2b": [128, 32], "iota_t": [128, 32, 128],
    }
    IN = {}

    def inp(name):
        if name not in IN:
            IN[name] = din(name, IN_SHAPES[name])
        return IN[name]

    outT_d = dout("outT", [D, T])

    qA_s = dscr("qA_s", [16, 128, T], BF16)
    qB_s = dscr("qB_s", [16, 128, T], BF16)
    kB_s = dscr("kB_s", [16, 128, S], BF16)
    vB_s = dscr("vB_s", [16, 128, S], BF16)
    oT_s = dscr("oT_s", [32, 128, T], BF16)
    s1_s = dscr("s1_s", [8, T, 128], F32)
    B_s1s = Buf("s1_s")
    B_gTs = Buf("gT_s")
    B_ggs = Buf("ggT_s")
    gT_s = dscr("gT_s", [16, 128, 128 * 64], BF16)
    ggT_s = dscr("ggT_s", [128, 128, T], BF16)

    dbg_out = {}

    with ExitStack() as es:
        p = Prog(nc, es)
        arena_t = es.enter_context(nc.sbuf_tensor("arena", [128, ARENA_WORDS], F32))
        psum_t = es.enter_context(nc.psum_tensor("ps", [128, 4096], F32))
        A = Arena(arena_t, ARENA_WORDS)
        PS = [psum_t[:, b * 512:(b + 1) * 512] for b in range(8)]
        PSB = [Buf("ps%d" % b) for b in range(8)]

        def dbg_dump(name, ap, shape, buf, dt=F32):
            if name in dbg:
                o = dout("dbg_" + name, shape, dt)
                dbg_out[name] = o
                p.dma("sp", o, ap, reads=[buf])

        ident_f = A.f32(128)
        ident_b = A.bf16(128)
        ones_f = A.f32(128)
        ones_b = A.bf16(128)
        B_const = Buf("const")
        p.dma("sp", ident_f, inp("ident"), writes=[B_const])
        p.op("dve", lambda e: e.tensor_copy(out=ident_b, in_=ident_f), reads=[B_const], writes=[B_const])
        p.op("dve", lambda e: e.memset(ones_f, 1.0), writes=[B_const])
        p.op("dve", lambda e: e.memset(ones_b, 1.0), writes=[B_const])
        A_BASE = A.off

        def arena_reset():
            p.barrier()
            A.off = A_BASE
            A.top = ARENA_WORDS
            A.marks = []

        def gemm(xT, KC, ntok, w_dram, chunks, evac, xbufs, wtiles, ps_slots, q="pool"):
            nb = ntok // 512
            for i, c in enumerate(chunks):
                wt, wb = wtiles[i % len(wtiles)]
                p.dma(q, wt.rearrange("p (k n) -> p k n", k=KC), w_dram[c], writes=[wb])
                banks = ps_slots[i % len(ps_slots)]
                for kc in range(KC):
                    for t in range(nb):
                        b = banks[t]
                        p.op("pe", lambda e, b=b, kc=kc, t=t, wt=wt: e.matmul(
                            PS[b], wt[:, kc * 128:(kc + 1) * 128], xT[:, kc, t * 512:(t + 1) * 512],
                            start=(kc == 0), stop=(kc == KC - 1)),
                            reads=[wb] + list(xbufs), writes=[PSB[b]],
                            inc=(kc == KC - 1 and t == nb - 1))
                evac(i, c, banks)

        ckvn = A.bf16(2 * S).rearrange("p (c t) -> p c t", c=2)
        B_ckvn = Buf("ckvn")
        B_selT = Buf("selT")
        kpos_b = A.f32(S)
        qpos_b = A.f32(T)
        kpos_col = A.f32(16)
        cend_col = A.f32(8)
        glat = A.f32(2)
        negb = A.f32(1)
        B_pos = Buf("pos")
        A.push()
        qidxT = A.bf16(16 * T).rearrange("p (c t) -> p c t", c=16)
        B_qidx = Buf("qidx")
        kidxT = A.bf16(S)
        B_kidx = Buf("kidx")
        w_tok = A.f32(8 * 32).rearrange("p (j h) -> p j h", j=8)
        B_wtok = Buf("wtok")
        A.push()
        ckvT = A.f32(2 * S).rearrange("p (c t) -> p c t", c=2)
        B_ckv = Buf("ckv")
        fT = A.f32(S)
        B_f = Buf("fT")
        widxT = A.f32(T)
        B_widxT = Buf("widxT")
        A.push()
        xT = A.bf16(32 * T).rearrange("p (k t) -> p k t", k=32)
        B_x = Buf("xT")
        wtiles = [(A.bf16(32 * 128), Buf("w%d" % i)) for i in range(3)]
        stg = [(A.bf16(T), Buf("stg%d" % i)) for i in range(3)]
        xT_v = inp("xT").rearrange("(k p) t -> p k t", p=128)

        def load_x(half):
            for k4 in range(4):
                p.dma("pool", xT[:, k4 * 8:(k4 + 1) * 8, :], xT_v[:, k4 * 8:(k4 + 1) * 8, half * T:(half + 1) * T],
                      writes=[B_x], group=(k4 > 0))

        slots2 = [[0, 1], [2, 3], [4, 5], [6, 7]]
        stg_i = [0]

        def evac_A(half):
            tok0 = half * T

            def ev(i, c, banks):
                def to_scratch(dst, scale):
                    st, sb = stg[stg_i[0] % 3]
                    stg_i[0] += 1
                    for t, b in enumerate(banks):
                        p.op("act", lambda e, b=b, t=t, st=st: e.activation(
                            out=st[:, t * 512:(t + 1) * 512], in_=PS[b], func=AF.Copy, scale=scale),
                            reads=[PSB[b]], writes=[sb])
                    p.dma("sp", dst, st, reads=[sb])

                if c < 16:
                    to_scratch(qA_s[c], SCALE)
                elif c < 32:
                    to_scratch(qB_s[c - 16], SCALE)
                elif c < 48:
                    for t, b in enumerate(banks):
                        p.op("act", lambda e, b=b, t=t: e.activation(
                            out=qidxT[:, c - 32, t * 512:(t + 1) * 512], in_=PS[b], func=AF.Copy),
                            reads=[PSB[b]], writes=[B_qidx])
                elif c == 48:
                    for t, b in enumerate(banks):
                        p.op("dve", lambda e, b=b, t=t: e.tensor_copy(out=widxT[:, t * 512:(t + 1) * 512], in_=PS[b]),
                             reads=[PSB[b]], writes=[B_widxT])
                elif c < 51:
                    for t, b in enumerate(banks):
                        p.op("dve", lambda e, b=b, t=t: e.tensor_copy(
                            out=ckvT[:, c - 49, tok0 + t * 512: tok0 + (t + 1) * 512], in_=PS[b]),
                            reads=[PSB[b]], writes=[B_ckv])
                elif c == 51:
                    for t, b in enumerate(banks):
                        p.op("act", lambda e, b=b, t=t: e.activation(
                            out=kidxT[:, tok0 + t * 512: tok0 + (t + 1) * 512], in_=PS[b], func=AF.Copy),
                            reads=[PSB[b]], writes=[B_kidx])
                elif c < 68:
                    to_scratch(kB_s[c - 52][:, tok0:tok0 + T], 1.0)
                elif c < 84:
                    to_scratch(vB_s[c - 68][:, tok0:tok0 + T], 1.0)
                else:
                    for t, b in enumerate(banks):
                        p.op("dve", lambda e, b=b, t=t: e.tensor_copy(
                            out=fT[:, tok0 + t * 512: tok0 + (t + 1) * 512], in_=PS[b]),
                            reads=[PSB[b]], writes=[B_f])
            return ev

        load_x(1)
        gemm(xT, 32, T, inp("w_in_t"), list(range(NQ_CH, NQ_CH + NK_CH)), evac_A(1), [B_x], wtiles, slots2)
        load_x(0)
        gemm(xT, 32, T, inp("w_in_t"), list(range(NQ_CH + NK_CH)), evac_A(0), [B_x], wtiles, slots2)

        dbg_dump("ckv", ckvT, [128, 2, S], B_ckv)
        dbg_dump("fT", fT, [128, S], B_f)
        dbg_dump("widxT", widxT, [128, T], B_widxT)
        dbg_dump("kidxT", kidxT, [128, S], B_kidx, BF16)
        dbg_dump("qidxT", qidxT, [128, 16, T], B_qidx, BF16)

        if stage <= 1:
            p.wait_all_on("sp")
            p.emit()
            return nc, dbg_out, list(IN)

        p.barrier()
        A.pop()
        selT = A.alloc_top(2 * 16 * T, BF16).rearrange("p (k t) -> p k t", k=16)
        p.dma("sp", kpos_b, inp("kpos_b"), writes=[B_pos])
        p.dma("sp", qpos_b, inp("qpos_b"), writes=[B_pos])
        p.dma("sp", kpos_col, inp("kpos_col"), writes=[B_pos])
        p.dma("sp", cend_col, inp("cend_col"), writes=[B_pos])
        p.dma("sp", glat, inp("glat"), writes=[B_pos])
        p.dma("sp", negb, inp("bfor"), writes=[B_pos])
        p.op("dve", lambda e: e.tensor_scalar(out=negb, in0=negb, scalar1=-1.0, scalar2=None, op0=ALU.mult),
             reads=[B_pos], writes=[B_pos])
        A.push()
        sq = A.f32(2 * S).rearrange("p (c t) -> p c t", c=2)
        rstd = A.f32(S)
        B_sq, B_rstd = Buf("sq"), Buf("rstd")
        for c in range(2):
            p.op("act", lambda e, c=c: e.activation(out=sq[:, c, :], in_=ckvT[:, c, :], func=AF.Square),
                 reads=[B_ckv], writes=[B_sq])
        for t in range(4):
            for c in range(2):
                p.op("pe", lambda e, t=t, c=c: e.matmul(PS[t], ones_f, sq[:, c, t * 512:(t + 1) * 512],
                                                        start=(c == 0), stop=(c == 1)),
                     reads=[B_sq, B_const], writes=[PSB[t]], inc=(c == 1))
            p.op("dve", lambda e, t=t: e.tensor_scalar(out=rstd[:, t * 512:(t + 1) * 512], in0=PS[t],
                                                       scalar1=1.0 / 256, scalar2=EPS, op0=ALU.mult, op1=ALU.add),
                 reads=[PSB[t]], writes=[B_rstd])
        p.op("act", lambda e: e.activation(out=rstd, in_=rstd, func=AF.Sqrt), reads=[B_rstd], writes=[B_rstd])
        p.op("dve", lambda e: e.reciprocal(out=rstd, in_=rstd), reads=[B_rstd], writes=[B_rstd])
        for c in range(2):
            p.op("dve", lambda e, c=c: e.scalar_tensor_tensor(out=ckvn[:, c, :], in0=ckvT[:, c, :], scalar=glat[:, c:c + 1],
                                                              in1=rstd, op0=ALU.mult, op1=ALU.mult),
                 reads=[B_ckv, B_rstd, B_pos], writes=[B_ckvn])
        dbg_dump("ckvn", ckvn, [128, 2, S], B_ckvn, BF16)
        A.pop()

        for j in range(8):
            p.op("pe", lambda e, j=j: e.matmul(PS[4][:, j * 32:(j + 1) * 32], widxT[0:32, j * 128:(j + 1) * 128],
                                               ident_f[0:32, 0:32], start=True, stop=True),
                 reads=[B_widxT, B_const], writes=[PSB[4]], inc=(j == 7))
        p.op("dve", lambda e: e.tensor_copy(out=w_tok.rearrange("p j h -> p (j h)"), in_=PS[4][:, 0:256]),
             reads=[PSB[4]], writes=[B_wtok])

        A.push()
        l2 = A.f32(S)
        B_l2 = Buf("l2")
        p.op("act", lambda e: e.activation(out=l2[0:16, :], in_=fT[0:16, :], func=AF.Exp, scale=-1.0, bias=negb[0:16, :]),
             reads=[B_f, B_pos], writes=[B_l2])
        p.op("act", lambda e: e.activation(out=l2[0:16, :], in_=l2[0:16, :], func=AF.Ln, scale=1.0, bias=1.0),
             reads=[B_l2], writes=[B_l2])
        for i in range(16):
            p.op("pe", lambda e, i=i: e.matmul(PS[5][:, i * 16:(i + 1) * 16], l2[0:16, i * 128:(i + 1) * 128],
                                               ident_f[0:16, 0:16], start=True, stop=True),
                 reads=[B_l2, B_const], writes=[PSB[5]], inc=(i == 15))
        l2t = A.f32(256)
        r1 = A.f32(256)
        tmpf = A.f32(256)
        parts = [A.bf16(256) for _ in range(3)]
        B_sp = Buf("split")
        p.op("dve", lambda e: e.tensor_copy(out=l2t, in_=PS[5][:, 0:256]), reads=[PSB[5]], writes=[B_sp])

        def split3(src, res, tmp, outs, n_part):
            sl = lambda a: a[0:n_part]
            o0, o1, o2 = outs
            p.op("dve", lambda e: e.tensor_copy(out=sl(o0), in_=sl(src)), reads=[B_sp], writes=[B_sp])
            p.op("dve", lambda e: e.tensor_copy(out=sl(tmp), in_=sl(o0)), reads=[B_sp], writes=[B_sp])
            p.op("dve", lambda e: e.tensor_sub(out=sl(res), in0=sl(src), in1=sl(tmp)), reads=[B_sp], writes=[B_sp])
            p.op("dve", lambda e: e.tensor_copy(out=sl(o1), in_=sl(res)), reads=[B_sp], writes=[B_sp])
            p.op("dve", lambda e: e.tensor_copy(out=sl(tmp), in_=sl(o1)), reads=[B_sp], writes=[B_sp])
            p.op("dve", lambda e: e.tensor_sub(out=sl(res), in0=sl(res), in1=sl(tmp)), reads=[B_sp], writes=[B_sp])
            p.op("dve", lambda e: e.tensor_copy(out=sl(o2), in_=sl(res)), reads=[B_sp], writes=[B_sp])

        split3(l2t, r1, tmpf, parts, 128)
        TtR = A.f32(S)
        Tt = [(TtR[:, i * 1024:(i + 1) * 1024].bitcast(BF16), Buf("Tt%d" % i)) for i in range(2)]
        for i in range(16):
            tt, tb = Tt[i % 2]
            p.op("dve", lambda e, i=i, tt=tt: e.tensor_scalar(out=tt, in0=kpos_b, scalar1=kpos_col[:, i:i + 1], scalar2=None,
                                                             op0=ALU.is_ge),
                 reads=[B_pos], writes=[tb])
            for t in range(4):
                for k in range(3):
                    p.op("pe", lambda e, i=i, t=t, k=k, tt=tt: e.matmul(
                        PS[t][0:16, :], parts[k][:, i * 16:(i + 1) * 16], tt[:, t * 512:(t + 1) * 512],
                        start=(i == 0 and k == 0), stop=(i == 15 and k == 2)),
                        reads=[B_sp, tb], writes=[PSB[t]], inc=(k == 2 and t == 3))
        cn = A.f32(S)
        cres = l2
        ctmp = TtR
        cparts = [A.bf16(S) for _ in range(3)]
        nparts = [A.bf16(S) for _ in range(3)]
        for t in range(4):
            p.op("dve", lambda e, t=t: e.tensor_copy(out=cn[0:16, t * 512:(t + 1) * 512], in_=PS[t][0:16, :]),
                 reads=[PSB[t]], writes=[B_sp])
        p.barrier()
        split3(cn, cres, ctmp, cparts, 16)
        cum_s = dscr("cum_s", [6, 16, S], BF16)
        B_cums = Buf("cum_s")
        for k in range(3):
            p.op("dve", lambda e, k=k: e.tensor_scalar(out=nparts[k][0:16], in0=cparts[k][0:16], scalar1=-1.0, scalar2=None,
                                                       op0=ALU.mult), reads=[B_sp], writes=[B_sp])
        for k in range(3):
            p.dma("sp", cum_s[k], cparts[k][0:16], reads=[B_sp], writes=[B_cums])
            p.dma("sp", cum_s[3 + k], nparts[k][0:16], reads=[B_sp], writes=[B_cums])
        dbg_dump("cn", cn, [128, S], B_sp)
        p.barrier()
        A.pop()

        A.pop()
        A.push()
        penA = A.bf16(T)
        penB = A.bf16(S)
        B_pen = Buf("pen")
        p.dma("pool", penA[0:32, :], inp("penA"), writes=[B_pen])
        p.dma("pool", penB[0:32, :], inp("penB"), writes=[B_pen])
        scmS = [(A.f32(S), Buf("scm%d" % i)) for i in range(4)]
        workS = [(A.f32(S), Buf("work%d" % i)) for i in range(4)]
        selcS = [(A.bf16(S), Buf("selc%d" % i)) for i in range(2)]
        m8S = [(A.f32(8), Buf("m8%d" % i)) for i in range(2)]
        thrS = [A.f32(1) for i in range(2)]
        rb = [(A.bf16(512), Buf("r%d" % i)) for i in range(4)]
        dgb = [(A.bf16(128), Buf("dg%d" % i)) for i in range(4)]
        ri = [0]
        pi = [0]

        def score_phase(j):
            scm, B_scm = scmS[j % 4]
            work, B_work = workS[j % 4]
            pieces = [0, 2] if j < 4 else [0, 1, 2, 3]
            for ip, pc in enumerate(pieces):
                p.op("pe", lambda e, ip=ip, pc=pc: e.matmul(
                    PS[4 + ip], penA[0:32, j * 128:(j + 1) * 128], penB[0:32, pc * 512:(pc + 1) * 512],
                    start=True, stop=False), reads=[B_pen], writes=[PSB[4 + ip]], inc=False)
            tiles = [(hi, ip, pc) for hi in range(32) for ip, pc in enumerate(pieces)]
            nt = len(tiles)

            def emit_R(t):
                hi, ip, pc = tiles[t]
                c, hb = hi // 2, (hi % 2) * 64
                if ip == 0:
                    dg, B_dg = dgb[hi % 4]
                    p.op("pool", lambda e, dg=dg, hi=hi: e.tensor_scalar(
                        out=dg, in0=ident_f, scalar1=w_tok[:, j, hi:hi + 1], scalar2=None, op0=ALU.mult),
                        reads=[B_const, B_wtok], writes=[B_dg])
                b = t % 4
                p.op("pe", lambda e, b=b, c=c, hb=hb, pc=pc: e.matmul(
                    PS[b], qidxT[hb:hb + 64, c, j * 128:(j + 1) * 128], kidxT[hb:hb + 64, pc * 512:(pc + 1) * 512],
                    start=True, stop=True), reads=[B_qidx, B_kidx], writes=[PSB[b]])

            LA = 3
            for t in range(min(LA, nt)):
                emit_R(t)
            for t in range(nt):
                if t + LA < nt:
                    emit_R(t + LA)
                hi, ip, pc = tiles[t]
                b = t % 4
                r, rbuf = rb[ri[0] % 4]
                ri[0] += 1
                dg, B_dg = dgb[hi % 4]
                p.op("act", lambda e, b=b, r=r: e.activation(out=r, in_=PS[b], func=AF.Relu),
                     reads=[PSB[b]], writes=[rbuf])
                p.op("pe", lambda e, ip=ip, dg=dg, r=r, hi=hi: e.matmul(
                    PS[4 + ip], dg, r, start=False, stop=(hi == 31)),
                    reads=[B_dg, rbuf], writes=[PSB[4 + ip]])
            nv = (j + 1) * 128
            for ip, pc in enumerate(pieces):
                wv = min(512, nv - (pc % 2) * 512)
                if wv <= 0:
                    continue
                d0 = (0 if pc < 2 else nv) + (pc % 2) * 512
                p.op("act", lambda e, ip=ip, d0=d0, wv=wv: e.activation(out=scm[:, d0:d0 + wv], in_=PS[4 + ip][:, 0:wv], func=AF.Copy),
                     reads=[PSB[4 + ip]], writes=[B_scm])
                if j > 0:
                    p.op("act", lambda e, ip=ip, d0=d0, wv=wv: e.activation(out=work[:, d0:d0 + wv], in_=PS[4 + ip][:, 0:wv], func=AF.Copy),
                         reads=[PSB[4 + ip]], writes=[B_work])

        def topk_pair(js):
            Ws = [2 * (j + 1) * 128 for j in js]
            for rnd in range(32):
                for a, j in enumerate(js):
                    if j == 0:
                        continue
                    work, B_work = workS[j % 4]
                    m8, B_m8 = m8S[a]
                    W = Ws[a]
                    p.op("dve", lambda e, W=W, work=work, m8=m8: e.max(out=m8, in_=work[:, 0:W]), reads=[B_work], writes=[B_m8])
                if rnd < 31:
                    for a, j in enumerate(js):
                        if j == 0:
                            continue
                        work, B_work = workS[j % 4]
                        m8, B_m8 = m8S[a]
                        W = Ws[a]
                        p.op("dve", lambda e, W=W, work=work, m8=m8: e.match_replace(
                            out=work[:, 0:W], in_to_replace=m8, in_values=work[:, 0:W], imm_value=-1e30),
                            reads=[B_m8, B_work], writes=[B_work])
            for a, j in enumerate(js):
                scm, B_scm = scmS[j % 4]
                m8, B_m8 = m8S[a]
                selc, B_selc = selcS[a]
                thr = thrS[a]
                W = Ws[a]
                if j == 0:
                    p.op("dve", lambda e, thr=thr: e.memset(thr, -1e29), reads=[B_m8], writes=[B_m8])
                else:
                    p.op("dve", lambda e, thr=thr, m8=m8: e.tensor_scalar(out=thr, in0=m8[:, 7:8], scalar1=-1e29, scalar2=None,
                                                                         op0=ALU.max), reads=[B_m8], writes=[B_m8])
                p.op("dve", lambda e, W=W, selc=selc, scm=scm, thr=thr: e.tensor_scalar(
                    out=selc[:, 0:W], in0=scm[:, 0:W], scalar1=thr, scalar2=None, op0=ALU.is_ge),
                    reads=[B_m8, B_scm], writes=[B_selc])
                for base_blk, kt_base in ((0, 0), (j + 1, 8)):
                    for g0 in range(0, j + 1, 4):
                        n4 = min(4, j + 1 - g0)
                        b = pi[0] % 4
                        pi[0] += 1
                        psb = PS[b].bitcast(BF16)
                        for i4 in range(n4):
                            i = base_blk + g0 + i4
                            p.op("pe", lambda e, psb=psb, i=i, i4=i4, selc=selc: e.transpose(
                                psb[:, i4 * 128:(i4 + 1) * 128], selc[:, i * 128:(i + 1) * 128], ident_b),
                                 reads=[B_selc, B_const], writes=[PSB[b]], inc=(i4 == n4 - 1))
                        kt0 = kt_base + g0
                        p.op("act", lambda e, psb=psb, kt0=kt0, j=j, n4=n4: e.activation(
                            out=selT[:, kt0:kt0 + n4, j * 128:(j + 1) * 128],
                            in_=psb[:, 0:n4 * 128].rearrange("p (k q) -> p k q", k=n4),
                            func=AF.Copy), reads=[PSB[b]], writes=[B_selT])

        p.op("dve", lambda e: e.memset(selT.rearrange("p k t -> p (k t)"), 0.0), writes=[B_selT])
        score_phase(0)
        score_phase(1)
        for i in range(4):
            if i < 3:
                score_phase(2 * i + 2)
                score_phase(2 * i + 3)
            topk_pair((2 * i, 2 * i + 1))
        dbg_dump("selT", selT, [128, 16, T], B_selT, BF16)
        p.barrier()
        A.pop()

        if stage <= 2:
            p.wait_all_on("sp")
            p.emit()
            return nc, dbg_out, list(IN)

        A.pop()
        KTS = [[0, 1, 2, 3, 8, 9, 10, 11], list(range(16))]
        tmpb = [(A.f32(512), Buf("tmp%d" % i)) for i in range(4)]
        ptb = [(A.bf16(512), Buf("pt%d" % i)) for i in range(4)]
        rec = A.f32(512)
        B_rec = Buf("rec")
        ostb = [(A.bf16(512), Buf("ost%d" % i)) for i in range(2)]
        qTb = [(A.bf16(T), Buf("qT%d" % i)) for i in range(2)]
        kTb = [(A.bf16(S), Buf("kT%d" % i)) for i in range(2)]
        Vb = [(A.bf16(S).rearrange("p (k d) -> p k d", k=16), Buf("V%d" % i)) for i in range(2)]
        cnt = {"s": 0, "tmp": 0, "pt": 0, "ost": 0, "acc": 0}

        def attn_core(h_glob, qT, B_q, kT, B_k, V, B_V, pre_exp, bias_mm):
            for J in range(2):
                kts = KTS[J]
                n = len(kts)
                oacc = 4 + (cnt["acc"] % 2)
                dacc = 6 + (cnt["acc"] % 2)
                cnt["acc"] += 1
                sbank = {}

                def emit_S(idx):
                    kt = kts[idx]
                    b = cnt["s"] % 4
                    cnt["s"] += 1
                    sbank[idx] = b
                    last = bias_mm is None
                    p.op("pe", lambda e, b=b, kt=kt, J=J, last=last: e.matmul(
                        PS[b], kT[:, kt * 128:(kt + 1) * 128], qT[:, J * 512:(J + 1) * 512], start=True, stop=last),
                         reads=[B_k, B_q], writes=[PSB[b]], inc=last)
                    if bias_mm is not None:
                        bias_mm(J, kt, b)

                LA = 3
                for i0 in range(min(LA, n)):
                    emit_S(i0)
                for idx in range(n):
                    kt = kts[idx]
                    if idx + LA < n:
                        emit_S(idx + LA)
                    b = sbank[idx]
                    tmp, B_tmp = tmpb[cnt["tmp"] % 4]
                    cnt["tmp"] += 1
                    pt, B_pt = ptb[cnt["pt"] % 4]
                    cnt["pt"] += 1
                    pre_exp(J, kt, b, tmp, B_tmp)
                    p.op("act", lambda e, tmp=tmp, pt=pt: e.activation(out=pt, in_=tmp, func=AF.Exp),
                         reads=[B_tmp], writes=[B_pt])
                    p.op("pe", lambda e, kt=kt, pt=pt, idx=idx, oacc=oacc, n=n: e.matmul(
                        PS[oacc], V[:, kt, :], pt, start=(idx == 0), stop=(idx == n - 1)),
                         reads=[B_V, B_pt], writes=[PSB[oacc]], inc=False)
                    p.op("pe", lambda e, pt=pt, idx=idx, dacc=dacc, n=n: e.matmul(
                        PS[dacc], ones_b, pt, start=(idx == 0), stop=(idx == n - 1)),
                         reads=[B_const, B_pt], writes=[PSB[dacc]], inc=True)
                ost, B_ost = ostb[cnt["ost"] % 2]
                cnt["ost"] += 1
                p.op("dve", lambda e, dacc=dacc: e.reciprocal(out=rec, in_=PS[dacc]), reads=[PSB[dacc]], writes=[B_rec])
                p.op("dve", lambda e, ost=ost, oacc=oacc: e.tensor_tensor(out=ost, in0=PS[oacc], in1=rec, op=ALU.mult),
                     reads=[PSB[oacc], B_rec], writes=[B_ost])
                p.dma("sp", oT_s[h_glob][:, J * 512:(J + 1) * 512], ost, reads=[B_ost], writes=[B_oTs])

        B_oTs = Buf("oT_s")
        B_scr = Buf("scrA")

        A.push()
        wuk = A.bf16(2 * 2048).rearrange("p (c n) -> p c n", c=2)
        wuv = A.bf16(2 * 2048).rearrange("p (c n) -> p c n", c=2)
        B_wu = Buf("wu")
        p.dma("pool", wuk, inp("wuk_t"), writes=[B_wu])
        p.dma("pool", wuv, inp("wuv_t"), writes=[B_wu])
        Dm = A.f32(24 * 512).rearrange("p (k q) -> p k q", k=24)
        B_Dm = Buf("Dm")
        dmi = {}
        for J in range(2):
            for kt in KTS[J]:
                i = len(dmi)
                dmi[(J, kt)] = i
                dst = Dm[:, i, :]
                p.op("dve", lambda e, dst=dst, J=J, kt=kt: e.tensor_scalar(
                    out=dst, in0=qpos_b[:, J * 512:(J + 1) * 512], scalar1=kpos_col[:, kt:kt + 1], scalar2=None,
                    op0=ALU.subtract), reads=[B_pos], writes=[B_Dm])
                p.op("dve", lambda e, dst=dst: e.scalar_tensor_tensor(out=dst, in0=dst, scalar=-1.0, in1=dst,
                                                                      op0=ALU.mult, op1=ALU.max),
                     reads=[B_Dm], writes=[B_Dm])
                p.op("dve", lambda e, dst=dst: e.tensor_scalar(out=dst, in0=dst, scalar1=1.0e6, scalar2=None, op0=ALU.add),
                     reads=[B_Dm], writes=[B_Dm])
                p.op("dve", lambda e, dst=dst, J=J, kt=kt: e.scalar_tensor_tensor(
                    out=dst, in0=selT[:, kt, J * 512:(J + 1) * 512], scalar=-1.0e6, in1=dst, op0=ALU.mult, op1=ALU.add),
                    reads=[B_Dm, B_selT], writes=[B_Dm])
        if NH_A:
            p.dma("sp", qTb[0][0], qA_s[0], writes=[qTb[0][1]])
        for h in range(NH_A):
            qT, B_q = qTb[h % 2]
            kT, B_k = kTb[h % 2]
            V, B_V = Vb[h % 2]
            if h + 1 < NH_A:
                p.dma("sp", qTb[(h + 1) % 2][0], qA_s[h + 1], writes=[qTb[(h + 1) % 2][1]])
            for t in range(4):
                for c in range(2):
                    p.op("pe", lambda e, t=t, c=c, h=h: e.matmul(PS[t], wuk[:, c, h * 128:(h + 1) * 128],
                                                                 ckvn[:, c, t * 512:(t + 1) * 512], start=(c == 0), stop=(c == 1)),
                         reads=[B_wu, B_ckvn], writes=[PSB[t]], inc=(c == 1))
                p.op("act", lambda e, t=t, kT=kT: e.activation(out=kT[:, t * 512:(t + 1) * 512], in_=PS[t], func=AF.Copy),
                     reads=[PSB[t]], writes=[B_k])
            for t in range(4):
                for k4 in range(4):
                    kt = t * 4 + k4
                    for c in range(2):
                        p.op("pe", lambda e, t=t, k4=k4, kt=kt, c=c, h=h: e.matmul(
                            PS[t][:, k4 * 128:(k4 + 1) * 128], ckvn[:, c, kt * 128:(kt + 1) * 128],
                            wuv[:, c, h * 128:(h + 1) * 128], start=(c == 0), stop=(c == 1)),
                            reads=[B_wu, B_ckvn], writes=[PSB[t]], inc=(c == 1 and k4 == 3))
                p.op("act", lambda e, t=t, V=V: e.activation(out=V[:, t * 4:(t + 1) * 4, :],
                                                             in_=PS[t].rearrange("p (k d) -> p k d", k=4), func=AF.Copy),
                     reads=[PSB[t]], writes=[B_V])

            def pre_exp(J, kt, b, tmp, B_tmp, h=h):
                i = dmi[(J, kt)]
                p.op("dve", lambda e: e.scalar_tensor_tensor(out=tmp, in0=Dm[:, i, :], scalar=-SLOPES[h], in1=PS[b],
                                                             op0=ALU.mult, op1=ALU.add),
                     reads=[B_Dm, PSB[b]], writes=[B_tmp])

            attn_core(h, qT, B_q, kT, B_k, V, B_V, pre_exp, None)
        p.barrier()
        A.pop()

        A.push()
        Mk = A.bf16(24 * 512).rearrange("p (k q) -> p k q", k=24)
        B_Mk = Buf("Mk")
        for J in range(2):
            for kt in KTS[J]:
                i = dmi[(J, kt)]
                p.op("dve", lambda e, i=i, J=J, kt=kt: e.tensor_scalar(
                    out=Mk[:, i, :], in0=qpos_b[:, J * 512:(J + 1) * 512], scalar1=kpos_col[:, kt:kt + 1], scalar2=NEG,
                    op0=ALU.is_lt, op1=ALU.mult), reads=[B_pos], writes=[B_Mk])
        vTb = [(A.bf16(S), Buf("vT%d" % i)) for i in range(2)]
        Aopb = [(A.bf16(S), Buf("Aop%d" % i)) for i in range(2)]
        Bopb = [(A.bf16(T), Buf("Bop%d" % i)) for i in range(2)]
        for i in range(2):
            p.op("dve", lambda e, i=i: e.memset(Aopb[i][0][0:32, :], 1.0), writes=[Aopb[i][1]])
            p.op("dve", lambda e, i=i: e.memset(Bopb[i][0][0:32, :], 1.0), writes=[Bopb[i][1]])
        def fox_loads(h):
            p.dma("sp", qTb[h % 2][0], qB_s[h], writes=[qTb[h % 2][1]])
            p.dma("sp", kTb[h % 2][0], kB_s[h], writes=[kTb[h % 2][1]])
            p.dma("sp", vTb[h % 2][0], vB_s[h], writes=[vTb[h % 2][1]])
            p.dma("sp", Aopb[h % 2][0][3:6, :], cum_s[0:3, h, :], writes=[Aopb[h % 2][1]])
            p.dma("sp", Bopb[h % 2][0][0:3, :], cum_s[3:6, h, 0:T], writes=[Bopb[h % 2][1]])

        if NH_B:
            fox_loads(0)
        for h in range(NH_B):
            qT, B_q = qTb[h % 2]
            kT, B_k = kTb[h % 2]
            V, B_V = Vb[h % 2]
            vT, B_vT = vTb[h % 2]
            Aop, B_A = Aopb[h % 2]
            Bop, B_B = Bopb[h % 2]
            if h + 1 < NH_B:
                fox_loads(h + 1)
            for half in range(2):
                psb = PS[half].bitcast(BF16)
                for k8 in range(8):
                    kt = half * 8 + k8
                    p.op("pe", lambda e, psb=psb, k8=k8, kt=kt, vT=vT: e.transpose(
                        psb[:, k8 * 128:(k8 + 1) * 128], vT[:, kt * 128:(kt + 1) * 128], ident_b),
                        reads=[B_vT, B_const], writes=[PSB[half]], inc=(k8 == 7))
                p.op("act", lambda e, psb=psb, half=half, V=V: e.activation(
                    out=V[:, half * 8:(half + 1) * 8, :], in_=psb.rearrange("p (k d) -> p k d", k=8), func=AF.Copy),
                    reads=[PSB[half]], writes=[B_V])

            def bias_mm(J, kt, b, Aop=Aop, Bop=Bop, B_A=B_A, B_B=B_B):
                p.op("pe", lambda e: e.matmul(PS[b], Aop[0:6, kt * 128:(kt + 1) * 128], Bop[0:6, J * 512:(J + 1) * 512],
                                              start=False, stop=True),
                     reads=[B_A, B_B], writes=[PSB[b]], inc=True)

            def pre_exp(J, kt, b, tmp, B_tmp):
                i = dmi[(J, kt)]
                p.op("dve", lambda e: e.tensor_tensor(out=tmp, in0=PS[b], in1=Mk[:, i, :], op=ALU.add),
                     reads=[B_Mk, PSB[b]], writes=[B_tmp])

            attn_core(16 + h, qT, B_q, kT, B_k, V, B_V, pre_exp, bias_mm)
        p.barrier()
        A.pop()
        if "oT" in dbg:
            o = dout("dbg_oT", [32, 128, T], BF16)
            ot_sb = A.bf16(T)
            B_otsb = Buf("otsb")
            for i in range(32):
                p.dma("sp", ot_sb, oT_s[i], reads=[B_oTs], writes=[B_otsb])
                p.dma("sp", o[i], ot_sb, reads=[B_otsb])

        if stage <= 3:
            p.wait_all_on("sp")
            p.emit()
            return nc, dbg_out, list(IN)

        arena_reset()
        mg_s = dscr("mg_s", [32, 128, T], BF16)
        B_mgs = Buf("mg_s")
        xT = A.bf16(32 * T).rearrange("p (k t) -> p k t", k=32)
        oT = A.bf16(32 * T).rearrange("p (k t) -> p k t", k=32)
        B_x, B_o = Buf("xT"), Buf("oT")
        wtiles = [(A.bf16(32 * 128), Buf("w%d" % i)) for i in range(4)]
        bgate = A.f32(64)
        B_bg = Buf("bgate")
        p.dma("sp", bgate, inp("b_gate_t"), writes=[B_bg])
        for k4 in range(4):
            p.dma("pool", xT[:, k4 * 8:(k4 + 1) * 8, :], xT_v[:, k4 * 8:(k4 + 1) * 8, 0:T], writes=[B_x], group=(k4 > 0))
        for k4 in range(8):
            p.dma("sp", oT[:, k4 * 4:(k4 + 1) * 4, :], oT_s[k4 * 4:(k4 + 1) * 4].rearrange("c p t -> p c t"),
                  reads=[B_oTs], writes=[B_o], group=(k4 > 0))
        gsig = [(A.f32(T), Buf("gsig%d" % i)) for i in range(2)]
        m1 = A.f32(T)
        B_m1 = Buf("m1")
        mgst = [(A.bf16(T), Buf("mgst%d" % i)) for i in range(2)]
        slots2 = [[0, 1], [2, 3], [4, 5], [6, 7]]
        gi = [0]
        wi = [0]
        si = [0]

        def one_chunk(xin, xbuf, KC, w_dram, c, evac):
            wt, wb = wtiles[wi[0] % 4]
            wi[0] += 1
            banks = slots2[si[0] % 4]
            si[0] += 1
            p.dma("pool", wt[:, 0:KC * 128].rearrange("p (k n) -> p k n", k=KC), w_dram[c], writes=[wb])
            for kc in range(KC):
                for t in range(2):
                    b = banks[t]
                    p.op("pe", lambda e, b=b, kc=kc, t=t, wt=wt: e.matmul(
                        PS[b], wt[:, kc * 128:(kc + 1) * 128], xin[:, kc, t * 512:(t + 1) * 512],
                        start=(kc == 0), stop=(kc == KC - 1)),
                        reads=[wb, xbuf], writes=[PSB[b]], inc=(kc == KC - 1 and t == 1))
            evac(banks)

        w_gate_d, w_ba_d, w_bb_d = inp("w_gate_t"), inp("w_ba_t"), inp("w_bb_t")
        for n in range(32):
            ga, B_ga = gsig[0]
            gb, B_gb = gsig[1]
            mst, B_mst = mgst[n % 2]

            def ev_gate(dst, B_dst, col):
                def ev(banks):
                    for t, b in enumerate(banks):
                        p.op("act", lambda e, b=b, t=t: e.activation(
                            out=dst[:, t * 512:(t + 1) * 512], in_=PS[b], func=AF.Sigmoid, bias=bgate[:, col:col + 1], scale=1.0),
                            reads=[PSB[b], B_bg], writes=[B_dst])
                return ev

            def ev_ba(banks):
                for t, b in enumerate(banks):
                    p.op("dve", lambda e, b=b, t=t: e.tensor_tensor(
                        out=m1[:, t * 512:(t + 1) * 512], in0=PS[b], in1=ga[:, t * 512:(t + 1) * 512], op=ALU.mult),
                        reads=[PSB[b], B_ga], writes=[B_m1])

            def ev_bb(banks, mst=mst, B_mst=B_mst, n=n):
                for t, b in enumerate(banks):
                    p.op("dve", lambda e, b=b, t=t: e.tensor_tensor(
                        out=gb[:, t * 512:(t + 1) * 512], in0=PS[b], in1=gb[:, t * 512:(t + 1) * 512], op=ALU.mult),
                        reads=[PSB[b], B_gb], writes=[B_gb])
                p.op("dve", lambda e: e.tensor_tensor(out=mst, in0=gb, in1=m1, op=ALU.add),
                     reads=[B_gb, B_m1], writes=[B_mst])
                p.dma("sp", mg_s[n], mst, reads=[B_mst], writes=[B_mgs])

            one_chunk(xT, B_x, 32, w_gate_d, n, ev_gate(ga, B_ga, n))
            one_chunk(oT[:, 0:16, :], B_o, 16, w_ba_d, n, ev_ba)
            one_chunk(xT, B_x, 32, w_gate_d, 32 + n, ev_gate(gb, B_gb, 32 + n))
            one_chunk(oT[:, 16:32, :], B_o, 16, w_bb_d, n, ev_bb)
        if "mg" in dbg:
            o = dout("dbg_mg", [32, 128, T], BF16)
            for i in range(32):
                p.dma("sp", mgst[0][0], mg_s[i], reads=[B_mgs], writes=[mgst[0][1]])
                p.dma("sp", o[i], mgst[0][0], reads=[mgst[0][1]])

        if stage <= 3.3:
            p.wait_all_on("sp")
            p.emit()
            return nc, dbg_out, list(IN)

        arena_reset()
        mgT = A.bf16(32 * T).rearrange("p (k t) -> p k t", k=32)
        B_mg = Buf("mgT")
        for k4 in range(8):
            p.dma("sp", mgT[:, k4 * 4:(k4 + 1) * 4, :], mg_s[k4 * 4:(k4 + 1) * 4].rearrange("c p t -> p c t"),
                  reads=[B_mgs], writes=[B_mg], group=(k4 > 0))
        wtiles = [(A.bf16(32 * 128), Buf("w%d" % i)) for i in range(3)]
        xf = [(A.f32(T), Buf("xf%d" % i)) for i in range(2)]
        zf = [(A.f32(T), Buf("zf%d" % i)) for i in range(2)]
        zq = [(A.bf16(T), Buf("zq%d" % i)) for i in range(2)]
        zbb = [(A.bf16(T), Buf("zbb%d" % i)) for i in range(2)]
        z1_s = dscr("z1_s", [32, 128, T], F32)
        B_z1s = Buf("z1_s")
        slots_c2 = [[0, 1], [2, 3]]
        w_out_d = inp("w_out_t")
        xT_rows = inp("xT")

        def ln_stats_mm(n, z, B_z, q, B_q, nchunks):
            for t in range(2):
                p.op("pe", lambda e, t=t: e.matmul(PS[4 + t], ones_b, z[:, t * 512:(t + 1) * 512],
                                                   start=(n == 0), stop=(n == nchunks - 1)),
                     reads=[B_z, B_const], writes=[PSB[4 + t]], inc=False)
                p.op("pe", lambda e, t=t: e.matmul(PS[6 + t], ones_b, q[:, t * 512:(t + 1) * 512],
                                                   start=(n == 0), stop=(n == nchunks - 1)),
                     reads=[B_q, B_const], writes=[PSB[6 + t]], inc=True)

        pending_stats = []
        for n in range(32):
            wt, wb = wtiles[n % 3]
            banks = slots_c2[n % 2]
            x_f, B_xf = xf[n % 2]
            z_f, B_zf = zf[n % 2]
            z_q, B_zq = zq[n % 2]
            p.dma("pool", wt.rearrange("p (k n) -> p k n", k=32), w_out_d[n], writes=[wb])
            p.dma("sp", x_f, xT_rows[n * 128:(n + 1) * 128, 0:T], writes=[B_xf])
            for kc in range(32):
                for t in range(2):
                    b = banks[t]
                    p.op("pe", lambda e, b=b, kc=kc, t=t, wt=wt: e.matmul(
                        PS[b], wt[:, kc * 128:(kc + 1) * 128], mgT[:, kc, t * 512:(t + 1) * 512],
                        start=(kc == 0), stop=(kc == 31)),
                        reads=[wb, B_mg], writes=[PSB[b]], inc=(kc == 31 and t == 1))
            while pending_stats:
                a_ = pending_stats.pop(0)
                ln_stats_mm(a_[0], a_[1], a_[2], a_[3], a_[4], 32)
            for t, b in enumerate(banks):
                p.op("dve", lambda e, b=b, t=t, x_f=x_f, z_f=z_f: e.scalar_tensor_tensor(
                    out=z_f[:, t * 512:(t + 1) * 512], in0=x_f[:, t * 512:(t + 1) * 512], scalar=ALPHA, in1=PS[b],
                    op0=ALU.mult, op1=ALU.add), reads=[PSB[b], B_xf], writes=[B_zf])
            z_b, B_zb = zbb[n % 2]
            p.op("act", lambda e, z_f=z_f, z_q=z_q: e.activation(out=z_q, in_=z_f, func=AF.Square),
                 reads=[B_zf], writes=[B_zq])
            p.op("act", lambda e, z_f=z_f, z_b=z_b: e.activation(out=z_b, in_=z_f, func=AF.Copy),
                 reads=[B_zf], writes=[B_zb])
            p.dma("sp", z1_s[n], z_f, reads=[B_zf], writes=[B_z1s])
            pending_stats.append((n, z_b, B_zb, z_q, B_zq))

        if stage <= 3.6:
            p.wait_all_on("sp")
            p.emit()
            return nc, dbg_out, list(IN)

        while pending_stats:
            a_ = pending_stats.pop(0)
            ln_stats_mm(a_[0], a_[1], a_[2], a_[3], a_[4], 32)

        def ln_finish():
            Mt = A.f32(T)
            Rt = A.f32(T)
            B_MR = Buf("MR")
            for t in range(2):
                sl = slice(t * 512, (t + 1) * 512)
                p.op("dve", lambda e, t=t, sl=sl: e.tensor_scalar(out=Mt[:, sl], in0=PS[4 + t], scalar1=1.0 / D, scalar2=None,
                                                                 op0=ALU.mult), reads=[PSB[4 + t]], writes=[B_MR])
                p.op("dve", lambda e, t=t, sl=sl: e.tensor_scalar(out=Rt[:, sl], in0=PS[6 + t], scalar1=1.0 / D, scalar2=EPS,
                                                                 op0=ALU.mult, op1=ALU.add), reads=[PSB[6 + t]], writes=[B_MR])
            msq = A.f32(T)
            p.op("dve", lambda e: e.tensor_tensor(out=msq, in0=Mt, in1=Mt, op=ALU.mult), reads=[B_MR], writes=[B_MR])
            p.op("dve", lambda e: e.tensor_sub(out=Rt, in0=Rt, in1=msq), reads=[B_MR], writes=[B_MR])
            p.op("act", lambda e: e.activation(out=Rt, in_=Rt, func=AF.Sqrt), reads=[B_MR], writes=[B_MR])
            p.op("dve", lambda e: e.reciprocal(out=Rt, in_=Rt), reads=[B_MR], writes=[B_MR])
            return Mt, Rt, B_MR

        def ln_apply(src_s, B_src, g_name, b_name, sink):
            Mt, Rt, B_MR = ln_finish()
            gcol = A.f32(32)
            bcol = A.f32(32)
            B_gb = Buf("gb")
            p.dma("sp", gcol, inp(g_name), writes=[B_gb])
            p.dma("sp", bcol, inp(b_name), writes=[B_gb])
            zb = [(A.f32(T), Buf("lz%d" % i)) for i in range(4)]
            for n in range(32):
                z, B_z = zb[n % 4]
                p.dma("sp", z, src_s[n], reads=[B_src], writes=[B_z])
                p.op("dve", lambda e, z=z: e.tensor_sub(out=z, in0=z, in1=Mt), reads=[B_MR, B_z], writes=[B_z])
                p.op("dve", lambda e, z=z: e.tensor_tensor(out=z, in0=z, in1=Rt, op=ALU.mult), reads=[B_MR, B_z], writes=[B_z])
                p.op("dve", lambda e, z=z, n=n: e.tensor_scalar(out=z, in0=z, scalar1=gcol[:, n:n + 1], scalar2=bcol[:, n:n + 1],
                                                               op0=ALU.mult, op1=ALU.add), reads=[B_gb, B_z], writes=[B_z])
                sink(n, z, B_z)

        arena_reset()
        h1_s = dscr("h1_s", [32, 128, T], F32)
        B_h1s = Buf("h1_s")
        h1T = A.bf16(32 * T).rearrange("p (k t) -> p k t", k=32)
        B_h1 = Buf("h1T")

        def sink1(n, z, B_z):
            p.dma("sp", h1_s[n], z, reads=[B_z], writes=[B_h1s])
            p.op("act", lambda e: e.activation(out=h1T[:, n, :], in_=z, func=AF.Copy), reads=[B_z], writes=[B_h1])

        ln_apply(z1_s, B_z1s, "ln1g", "ln1b", sink1)
        if "h1" in dbg:
            o = dout("dbg_h1", [32, 128, T], F32)
            tmp_h = A.f32(T)
            B_th = Buf("tmp_h")
            for i in range(32):
                p.dma("sp", tmp_h, h1_s[i], reads=[B_h1s], writes=[B_th])
                p.dma("sp", o[i], tmp_h, reads=[B_th])

        if stage <= 4:
            p.wait_all_on("sp")
            p.emit()
            return nc, dbg_out, list(IN)

        z2_s = dscr("z2_s", [32, 128, T], F32)
        B_z2s = Buf("z2_s")
        A.off = A_BASE + (2 * 32 * T + 3) // 4
        pTb = A.bf16(2 * T).rearrange("p (k t) -> p k t", k=2)
        B_pT = Buf("pT")
        p.barrier()
        p.dma("pool", pTb, inp("pT").rearrange("(k p) t -> p k t", p=128), writes=[B_pT])
        bpg = A.f32(32)
        B_bpg = Buf("bpg")
        p.dma("sp", bpg, inp("b_pg_t"), writes=[B_bpg])
        wtiles = [(A.bf16(32 * 128), Buf("w%d" % i)) for i in range(3)]
        wple = [(A.bf16(2 * 128), Buf("wple%d" % i)) for i in range(2)]
        sg = [(A.f32(T), Buf("sg%d" % i)) for i in range(2)]
        hf = [(A.f32(T), Buf("hf%d" % i)) for i in range(2)]
        w_pg_d, w_ple_d = inp("w_pg_t"), inp("w_ple_t")
        slots_d = [[0, 1], [2, 3], [4, 5], [6, 7]]
        for n in range(32):
            wt, wb = wtiles[n % 3]
            wp, wpb = wple[n % 2]
            s_g, B_sg = sg[n % 2]
            h_f, B_hf = hf[n % 2]
            bg_ = slots_d[(2 * n) % 4]
            bp_ = slots_d[(2 * n + 1) % 4]
            p.dma("pool", wt.rearrange("p (k n) -> p k n", k=32), w_pg_d[n], writes=[wb])
            p.dma("pool", wp.rearrange("p (k n) -> p k n", k=2), w_ple_d[n], writes=[wpb])
            p.dma("sp", h_f, h1_s[n], reads=[B_h1s], writes=[B_hf])
            for kc in range(32):
                for t in range(2):
                    b = bg_[t]
                    p.op("pe", lambda e, b=b, kc=kc, t=t, wt=wt: e.matmul(
                        PS[b], wt[:, kc * 128:(kc + 1) * 128], h1T[:, kc, t * 512:(t + 1) * 512],
                        start=(kc == 0), stop=(kc == 31)),
                        reads=[wb, B_h1], writes=[PSB[b]], inc=(kc == 31 and t == 1))
            for kc in range(2):
                for t in range(2):
                    b = bp_[t]
                    p.op("pe", lambda e, b=b, kc=kc, t=t, wp=wp: e.matmul(
                        PS[b], wp[:, kc * 128:(kc + 1) * 128], pTb[:, kc, t * 512:(t + 1) * 512],
                        start=(kc == 0), stop=(kc == 1)),
                        reads=[wpb, B_pT], writes=[PSB[b]], inc=(kc == 1 and t == 1))
            for t in range(2):
                sl = slice(t * 512, (t + 1) * 512)
                p.op("act", lambda e, t=t, sl=sl, s_g=s_g, n=n, b=bg_[t]: e.activation(
                    out=s_g[:, sl], in_=PS[b], func=AF.Sigmoid, bias=bpg[:, n:n + 1], scale=1.0),
                    reads=[PSB[bg_[t]], B_bpg], writes=[B_sg])
                p.op("dve", lambda e, sl=sl, s_g=s_g, b=bp_[t]: e.tensor_tensor(
                    out=s_g[:, sl], in0=PS[b], in1=s_g[:, sl], op=ALU.mult),
                    reads=[PSB[bp_[t]], B_sg], writes=[B_sg])
            p.op("dve", lambda e, s_g=s_g, h_f=h_f: e.scalar_tensor_tensor(
                out=h_f, in0=h_f, scalar=ALPHA, in1=s_g, op0=ALU.mult, op1=ALU.add),
                reads=[B_sg, B_hf], writes=[B_hf])
            p.dma("sp", z2_s[n], h_f, reads=[B_hf], writes=[B_z2s])

        if stage >= 6:
            p.barrier()
            A.off = A_BASE + (2 * 32 * T + 3) // 4
            qpT = A.alloc_top(2 * 16 * T, BF16).rearrange("p (g t) -> p g t", g=16)
            B_qp = Buf("qpT")
            wtiles = [(A.bf16(32 * 128), Buf("w%d" % i)) for i in range(3)]

            def ev_q(i, c, banks):
                for t, b in enumerate(banks):
                    p.op("act", lambda e, b=b, t=t: e.activation(out=qpT[:, c, t * 512:(t + 1) * 512], in_=PS[b], func=AF.Copy),
                         reads=[PSB[b]], writes=[B_qp])

            gemm(h1T, 32, T, inp("peer_wq_t"), list(range(16)), ev_q, [B_h1], wtiles, [[0, 1], [2, 3], [4, 5], [6, 7]])
            p.barrier()
            A.off = A_BASE
            skb = A.bf16(16 * 128).rearrange("p (g n) -> p g n", g=16)
            iota3 = A.bf16(32 * 128).rearrange("p (t n) -> p t n", t=32)
            B_ec = Buf("e1const")
            p.dma("pool", skb, inp("sk_t"), writes=[B_ec])
            p.dma("pool", iota3, inp("iota_t"), writes=[B_ec])
            S_sb = A.f32(16 * 128).rearrange("p (g n) -> p g n", g=16)
            B_Sb = [Buf("S_sb%d" % i) for i in range(4)]
            V16 = A.f32(16 * 16).rearrange("p (g k) -> p g k", g=16)
            B_Vg = [Buf("V16_%d" % i) for i in range(16)]
            w128g = A.f32(16 * 128).rearrange("p (g n) -> p g n", g=16)
            B_wg = [Buf("w128_%d" % i) for i in range(16)]
            idxu = A.alloc(4 * 128, U32)[:, 0:128].rearrange("p (h k) -> p h k", h=8)
            B_ix = [Buf("ix%d" % i) for i in range(8)]
            cand = A.f32(8 * 256).rearrange("p (h c) -> p h c", h=8)
            B_cd = [Buf("cand%d" % i) for i in range(8)]
            workc = A.f32(8 * 256).rearrange("p (h c) -> p h c", h=8)
            B_wc = [Buf("workc%d" % i) for i in range(8)]
            vals = A.f32(128).rearrange("p (h k) -> p h k", h=8)
            B_vl = [Buf("vals%d" % i) for i in range(8)]
            ev_ = A.f32(128).rearrange("p (h k) -> p h k", h=8)
            Zs = A.f32(8)
            rZ = A.f32(8)
            X = [A.f32(128).rearrange("p (h k) -> p h k", h=8) for _ in range(4)]
            B_X = [Buf("X%d" % i) for i in range(4)]
            XTb = [A.f32(4 * 128).rearrange("p (i t) -> p i t", i=4) for _ in range(2)]
            B_XTb = [Buf("XT0"), Buf("XT1")]
            B_sm = Buf("small")
            S1rep = [(A.f32(32 * 128).rearrange("p (t n) -> p t n", t=32), Buf("S1rep%d" % i)) for i in range(2)]
            L3b = [(A.bf16(32 * 128).rearrange("p (t n) -> p t n", t=32), Buf("L3%d" % i)) for i in range(2)]
            R3b = [(A.bf16(32 * 128).rearrange("p (t n) -> p t n", t=32), Buf("R3%d" % i)) for i in range(2)]
            GTb = [(A.bf16(128 * 64).rearrange("p (y t) -> p y t", y=128), Buf("GT%d" % i)) for i in range(2)]
            gT_h = gT_s
            V1v = V16.rearrange("p (h two) k -> p h two k", two=2)[:, :, 0, :]
            V2v = V16.rearrange("p (h two) k -> p h two k", two=2)[:, :, 1, :]
            gbank = [0]

            def chain(tt):
                for g in range(16):
                    p.op("pe", lambda e, g=g: e.matmul(PS[g // 4][:, (g % 4) * 128:(g % 4 + 1) * 128],
                                                       qpT[:, g, tt * 128:(tt + 1) * 128], skb[:, g, :],
                                                       start=True, stop=True),
                         reads=[B_qp, B_ec], writes=[PSB[g // 4]], inc=(g % 4 == 3))
                for b in range(4):
                    p.op("act", lambda e, b=b: e.activation(out=S_sb[:, b * 4:(b + 1) * 4, :],
                                                            in_=PS[b].rearrange("p (g n) -> p g n", g=4), func=AF.Copy),
                         reads=[PSB[b]], writes=[B_Sb[b]])
                p.dma("sp", s1_s[:, tt * 128:(tt + 1) * 128, :].rearrange("h t n -> t h n"),
                      S_sb.rearrange("p (h two) n -> p h two n", two=2)[:, :, 0, :], reads=B_Sb, writes=[B_s1s])
                for g in range(16):
                    p.op("dve", lambda e, g=g: e.max(out=V16[:, g, 0:8], in_=S_sb[:, g, :]),
                         reads=[B_Sb[g // 4]], writes=[B_Vg[g]])
                for g in range(16):
                    p.op("dve", lambda e, g=g: e.match_replace(out=w128g[:, g, :], in_to_replace=V16[:, g, 0:8],
                                                               in_values=S_sb[:, g, :], imm_value=-1e30),
                         reads=[B_Sb[g // 4], B_Vg[g]], writes=[B_wg[g]])
                for g in range(16):
                    p.op("dve", lambda e, g=g: e.max(out=V16[:, g, 8:16], in_=w128g[:, g, :]),
                         reads=[B_wg[g]], writes=[B_Vg[g]])
                for hd in range(8):
                    g = 2 * hd + 1
                    p.op("dve", lambda e, g=g, hd=hd: e.max_index(out=idxu[:, hd, 0:8], in_max=V16[:, g, 0:8], in_values=S_sb[:, g, :]),
                         reads=[B_Sb[g // 4], B_Vg[g]], writes=[B_ix[hd]])
                for hd in range(8):
                    g = 2 * hd + 1
                    p.op("dve", lambda e, g=g, hd=hd: e.max_index(out=idxu[:, hd, 8:16], in_max=V16[:, g, 8:16], in_values=S_sb[:, g, :]),
                         reads=[B_Sb[g // 4], B_Vg[g]], writes=[B_ix[hd]])
                for hd in range(8):
                    p.op("dve", lambda e, hd=hd: e.tensor_tensor(
                        out=cand[:, hd, :].rearrange("p (a b) -> p a b", a=16),
                        in0=V16[:, 2 * hd, :].unsqueeze(2).to_broadcast([128, 16, 16]),
                        in1=V16[:, 2 * hd + 1, :].unsqueeze(1).to_broadcast([128, 16, 16]), op=ALU.add),
                        reads=[B_Vg[2 * hd], B_Vg[2 * hd + 1]], writes=[B_cd[hd]])
                for hd in range(8):
                    p.op("dve", lambda e, hd=hd: e.max(out=vals[:, hd, 0:8], in_=cand[:, hd, :]),
                         reads=[B_cd[hd]], writes=[B_vl[hd]])
                for hd in range(8):
                    p.op("dve", lambda e, hd=hd: e.match_replace(out=workc[:, hd, :], in_to_replace=vals[:, hd, 0:8],
                                                                 in_values=cand[:, hd, :], imm_value=-1e30),
                         reads=[B_cd[hd], B_vl[hd]], writes=[B_wc[hd]])
                for hd in range(8):
                    p.op("dve", lambda e, hd=hd: e.max(out=vals[:, hd, 8:16], in_=workc[:, hd, :]),
                         reads=[B_wc[hd]], writes=[B_vl[hd]])
                p.op("dve", lambda e: e.tensor_copy(out=X[2], in_=idxu), reads=B_ix, writes=[B_X[2]])
                p.op("dve", lambda e: e.tensor_tensor(out=ev_, in0=vals, in1=vals[:, :, 0:1].to_broadcast([128, 8, 16]),
                                                      op=ALU.subtract), reads=B_vl + [B_sm], writes=[B_sm])
                p.op("act", lambda e: e.activation(out=ev_, in_=ev_, func=AF.Exp), reads=[B_sm], writes=[B_sm])
                p.op("dve", lambda e: e.tensor_tensor(out=X[0], in0=vals[:, :, 15:16].to_broadcast([128, 8, 16]), in1=V2v,
                                                      op=ALU.subtract), reads=B_vl + B_Vg, writes=[B_X[0]])
                p.op("dve", lambda e: e.tensor_tensor(out=X[1], in0=V2v, in1=V2v[:, :, 0:1].to_broadcast([128, 8, 16]),
                                                      op=ALU.subtract), reads=B_Vg, writes=[B_X[1]])
                p.op("dve", lambda e: e.tensor_scalar(out=X[3], in0=V1v[:, :, 0:1].to_broadcast([128, 8, 16]), scalar1=-1.0,
                                                      scalar2=None, op0=ALU.mult), reads=B_Vg, writes=[B_X[3]])
                p.op("dve", lambda e: e.scalar_tensor_tensor(out=X[0], in0=X[0], scalar=-3e-5, in1=X[3], op0=ALU.add, op1=ALU.add),
                     reads=[B_X[0], B_X[3]], writes=[B_X[0]])
                p.op("act", lambda e: e.activation(out=X[0], in_=X[0], func=AF.Exp), reads=[B_X[0]], writes=[B_X[0]])
                p.op("act", lambda e: e.activation(out=X[1], in_=X[1], func=AF.Exp), reads=[B_X[1]], writes=[B_X[1]])
                p.op("dve", lambda e: e.tensor_reduce(out=Zs, in_=ev_, axis=AX.X, op=ALU.add), reads=[B_sm], writes=[B_sm])
                p.op("dve", lambda e: e.reciprocal(out=rZ, in_=Zs), reads=[B_sm], writes=[B_sm])
                p.op("dve", lambda e: e.tensor_tensor(out=X[1], in0=X[1], in1=rZ.unsqueeze(2).to_broadcast([128, 8, 16]),
                                                      op=ALU.mult), reads=[B_sm, B_X[1]], writes=[B_X[1]])
                for i in range(4):
                    p.op("pe", lambda e, i=i: e.matmul(PS[4][:, i * 128:(i + 1) * 128], X[i].rearrange("p h k -> p (h k)"),
                                                       ident_f, start=True, stop=True),
                         reads=[B_X[i], B_const], writes=[PSB[4]], inc=(i == 3))
                p.op("act", lambda e: e.activation(out=XTb[tt % 2].rearrange("p i t -> p (i t)"), in_=PS[4], func=AF.Copy),
                     reads=[PSB[4]], writes=[B_XTb[tt % 2]])

            B_srD = [Buf("srD%d" % i) for i in range(2)]

            def stage_A(tt, sub, k):
                XT, B_XT = XTb[tt % 2], B_XTb[tt % 2]
                t0 = tt * 128 + sub * 32
                sr, B_E = S1rep[k % 2]
                B_D = B_srD[k % 2]
                R3, B_R3 = R3b[k % 2]
                for hd in range(8):
                    p.dma("sp", sr[hd * 16:(hd + 1) * 16, :, :],
                          s1_s[hd, t0:t0 + 32, :].partition_broadcast(16), reads=[B_s1s], writes=[B_D, B_E], group=(hd > 0))
                for t in range(32):
                    tc = sub * 32 + t
                    p.op("act", lambda e, t=t, tc=tc: e.activation(out=sr[:, t, :], in_=sr[:, t, :], func=AF.Exp,
                                                                  bias=XT[:, 3, tc:tc + 1], scale=1.0),
                         reads=[B_D, B_XT], writes=[B_E], skip_own=(t > 0))
                for t in range(32):
                    tc = sub * 32 + t
                    p.op("dve", lambda e, t=t, tc=tc: e.tensor_scalar(
                        out=R3[:, t, :], in0=iota3[:, 0, :], scalar1=XT[:, 2, tc:tc + 1], scalar2=XT[:, 1, tc:tc + 1],
                        op0=ALU.is_equal, op1=ALU.mult), reads=[B_ec, B_XT], writes=[B_R3], skip_own=(t > 0))

            def stage_B(tt, sub, k):
                XT, B_XT = XTb[tt % 2], B_XTb[tt % 2]
                ht = tt * 2 + sub // 2
                GT, B_GT = GTb[ht % 2]
                sr, B_E = S1rep[k % 2]
                L3, B_L3 = L3b[k % 2]
                R3, B_R3 = R3b[k % 2]
                for t in range(32):
                    tc = sub * 32 + t
                    p.op("dve", lambda e, t=t, tc=tc: e.scalar_tensor_tensor(
                        out=L3[:, t, :], in0=sr[:, t, :], scalar=XT[:, 0, tc:tc + 1], in1=sr[:, t, :],
                        op0=ALU.is_ge, op1=ALU.mult), reads=[B_E, B_XT], writes=[B_L3], skip_own=(t > 0))
                for q4 in range(8):
                    b = 5 + gbank[0] % 3
                    gbank[0] += 1
                    for kk in range(4):
                        tk = q4 * 4 + kk
                        p.op("pe", lambda e, b=b, kk=kk, tk=tk: e.matmul(
                            PS[b][:, kk * 128:(kk + 1) * 128], L3[:, tk, :], R3[:, tk, :], start=True, stop=True),
                             reads=[B_L3, B_R3], writes=[PSB[b]], inc=(kk == 3))
                    tl0 = (sub % 2) * 32 + q4 * 4
                    p.op("act", lambda e, b=b, tl0=tl0: e.activation(
                        out=GT[:, :, tl0:tl0 + 4], in_=PS[b].rearrange("p (t y) -> p y t", t=4), func=AF.Copy),
                        reads=[PSB[b]], writes=[B_GT])
                if sub % 2 == 1:
                    p.dma("sp", gT_h[ht], GT.rearrange("p y t -> p (y t)"), reads=[B_GT], writes=[B_gTs])

            jobs = [(tt, sub) for tt in range(8) for sub in range(4)]
            chain(0)
            stage_A(0, 0, 0)
            for k, (tt, sub) in enumerate(jobs):
                if k + 1 < len(jobs):
                    tt2, sub2 = jobs[k + 1]
                    if sub2 == 0:
                        chain(tt2)
                    stage_A(tt2, sub2, k + 1)
                stage_B(tt, sub, k)
            if stage <= 6:
                p.wait_all_on("sp")
                p.emit()
                return nc, dbg_out, list(IN)

            arena_reset()
            h1T = A.bf16(32 * T).rearrange("p (k t) -> p k t", k=32)
            B_h1 = Buf("h1T")
            for k4 in range(8):
                p.dma("pool", h1T[:, k4 * 4:(k4 + 1) * 4, :], h1_s[k4 * 4:(k4 + 1) * 4].rearrange("c p t -> p c t"),
                      reads=[B_h1s], writes=[B_h1], group=(k4 > 0))
            wtiles = [(A.bf16(32 * 128), Buf("w%d" % i)) for i in range(3)]
            gty = [(A.bf16(T), Buf("gty%d" % i)) for i in range(3)]
            actb = [(A.bf16(T), Buf("actb%d" % i)) for i in range(2)]
            ggb = [(A.bf16(T), Buf("ggb%d" % i)) for i in range(3)]
            gT_v = gT_s.rearrange("ht x (y t) -> x ht y t", y=128)

            def ev_e2(i, y, banks):
                g_y, B_gy = gty[i % 3]
                a_b, B_ab = actb[i % 2]
                g_g, B_gg = ggb[i % 3]
                p.dma("sp", g_y.rearrange("p (ht t) -> p ht t", ht=16), gT_v[:, :, y, :], reads=[B_gTs], writes=[B_gy])
                for t, b in enumerate(banks):
                    p.op("act", lambda e, b=b, t=t: e.activation(out=a_b[:, t * 512:(t + 1) * 512], in_=PS[b], func=AF.Gelu),
                         reads=[PSB[b]], writes=[B_ab])
                p.op("dve", lambda e: e.tensor_tensor(out=g_g, in0=a_b, in1=g_y, op=ALU.mult),
                     reads=[B_ab, B_gy], writes=[B_gg])
                p.dma("sp", ggT_s[y], g_g, reads=[B_gg], writes=[B_ggs])

            gemm(h1T, 32, T, inp("uT_t"), list(range(128)), ev_e2, [B_h1], wtiles, [[0, 1], [2, 3], [4, 5], [6, 7]])

            arena_reset()
            vtb = [(A.bf16(2 * 512).rearrange("p (y d) -> p y d", y=2), Buf("vt%d" % i)) for i in range(4)]
            gyb = [(A.bf16(2 * T).rearrange("p (y t) -> p y t", y=2), Buf("gy%d" % i)) for i in range(4)]
            ztb = [(A.f32(T), Buf("zt%d" % i)) for i in range(4)]
            NRES = 32
            GGR = A.bf16(NRES * 2 * T).rearrange("p (r y t) -> p r y t", r=NRES, y=2)
            B_ggr = [Buf("ggr%d" % i) for i in range(NRES)]
            v_d = inp("v_t")
            zi = 0
            for dg in range(8):
                zts = []
                for dc in range(4):
                    n = dg * 4 + dc
                    zt, B_zt = ztb[zi % 4]
                    zi += 1
                    p.dma("sp", zt, z2_s[n], reads=[B_z2s], writes=[B_zt])
                    zts.append((zt, B_zt))
                for y2 in range(64):
                    vt, B_vt = vtb[y2 % 4]
                    p.dma("pool", vt, v_d[2 * y2:2 * y2 + 2][:, :, dg * 512:(dg + 1) * 512].rearrange("y x d -> x y d"),
                          writes=[B_vt])
                    if y2 < NRES:
                        gy, B_gy = GGR[:, y2, :, :], B_ggr[y2]
                        if dg == 0:
                            p.dma("sp" if y2 % 2 else "act", gy, ggT_s[2 * y2:2 * y2 + 2].rearrange("y x t -> x y t"),
                                  reads=[B_ggs], writes=[B_gy])
                    else:
                        gy, B_gy = gyb[y2 % 4]
                        p.dma("sp" if y2 % 2 else "act", gy, ggT_s[2 * y2:2 * y2 + 2].rearrange("y x t -> x y t"),
                              reads=[B_ggs], writes=[B_gy])
                    for yy in range(2):
                        y = 2 * y2 + yy
                        for dc in range(4):
                            for th in range(2):
                                b = dc * 2 + th
                                p.op("pe", lambda e, b=b, dc=dc, th=th, vt=vt, gy=gy, y=y, yy=yy: e.matmul(
                                    PS[b], vt[:, yy, dc * 128:(dc + 1) * 128], gy[:, yy, th * 512:(th + 1) * 512],
                                    start=(y == 0), stop=(y == 127)),
                                    reads=[B_vt, B_gy], writes=[PSB[b]], inc=(dc == 3 and th == 1))
                for dc in range(4):
                    n = dg * 4 + dc
                    zt, B_zt = zts[dc]
                    for th in range(2):
                        b = dc * 2 + th
                        p.op("dve", lambda e, b=b, th=th, zt=zt: e.tensor_tensor(
                            out=zt[:, th * 512:(th + 1) * 512], in0=PS[b], in1=zt[:, th * 512:(th + 1) * 512], op=ALU.add),
                            reads=[PSB[b], B_zt], writes=[B_zt])
                    p.dma("sp", z2_s[n], zt, reads=[B_zt], writes=[B_z2s])

        arena_reset()
        zb2 = [(A.f32(T), Buf("fz%d" % i)) for i in range(4)]
        zq2 = [(A.bf16(T), Buf("fq%d" % i)) for i in range(4)]
        zc2 = [(A.bf16(T), Buf("fc%d" % i)) for i in range(4)]
        for n in range(32):
            z, B_z = zb2[n % 4]
            q, B_q = zq2[n % 4]
            zc, B_zc = zc2[n % 4]
            p.dma("sp", z, z2_s[n], reads=[B_z2s], writes=[B_z])
            p.op("act", lambda e, z=z, q=q: e.activation(out=q, in_=z, func=AF.Square), reads=[B_z], writes=[B_q])
            p.op("act", lambda e, z=z, zc=zc: e.activation(out=zc, in_=z, func=AF.Copy), reads=[B_z], writes=[B_zc])
            ln_stats_mm(n, zc, B_zc, q, B_q, 32)
        B_out = Buf("out")

        def sink2(n, z, B_z):
            p.dma("sp", outT_d[n * 128:(n + 1) * 128, :], z, reads=[B_z], writes=[B_out])

        ln_apply(z2_s, B_z2s, "ln2g", "ln2b", sink2)

        p.wait_all_on("sp")
        p.emit()
    return nc, dbg_out, list(IN)


def _chunked(w, kc):
    K, N = w.shape
    assert K == kc * 128 and N % 128 == 0
    return np.ascontiguousarray(w.reshape(kc, 128, N // 128, 128).transpose(2, 1, 0, 3))


def _col(v, n):
    return np.ascontiguousarray(v.reshape(n, 128).T)


def prep_shared(inp, used=None):
    f = lambda a: np.asarray(a, dtype=np.float32)

    def w_in_t():
        w_in = f(inp["w_in"])[0]
        W = [2048, 256, 2048, 64, 32, 2048, 2048, 2048, 16]
        off = np.concatenate([[0], np.cumsum(W)])
        seg = lambda i: w_in[:, off[i]:off[i + 1]]
        z = lambda n: np.zeros((D, n), np.float32)
        cols = np.concatenate([
            seg(0), seg(5), seg(2), seg(4), z(96),
            seg(1), seg(3), seg(3), seg(6), seg(7), seg(8), z(112)], axis=1)
        assert cols.shape[1] == (NQ_CH + NK_CH) * 128
        return _chunked(cols, 32)

    def bfor():
        bf = np.zeros((128, 1), np.float32)
        bf[:16, 0] = f(inp["b_forget"])[0]
        return bf

    th = {
        "w_in_t": w_in_t,
        "ident": lambda: np.eye(128, dtype=np.float32),
        "glat": lambda: _col(f(inp["g_latent"])[0], 2),
        "bfor": bfor,
        "wuk_t": lambda: np.ascontiguousarray(f(inp["w_uk"])[0].reshape(2, 128, 2048).transpose(1, 0, 2)),
        "wuv_t": lambda: np.ascontiguousarray(f(inp["w_uv"])[0].reshape(2, 128, 2048).transpose(1, 0, 2)),
        "w_gate_t": lambda: _chunked(f(inp["w_gate"])[0], 32),
        "b_gate_t": lambda: _col(f(inp["b_gate"])[0], 64),
        "w_ba_t": lambda: _chunked(f(inp["w_branch_a"])[0], 16),
        "w_bb_t": lambda: _chunked(f(inp["w_branch_b"])[0], 16),
        "w_out_t": lambda: _chunked(f(inp["w_out"])[0], 32),
        "ln1g": lambda: _col(f(inp["ln1_g"])[0], 32),
        "ln1b": lambda: _col(f(inp["ln1_b"])[0], 32),
        "peer_wq_t": lambda: _chunked(f(inp["peer_wq"])[0].reshape(D, 2048), 32),
        "sk_t": lambda: np.ascontiguousarray(f(inp["peer_subkeys"])[0].reshape(16, 128, 128).transpose(2, 0, 1)),
        "uT_t": lambda: np.ascontiguousarray(f(inp["peer_u"])[0].reshape(128, 128, 32, 128).transpose(1, 3, 2, 0)),
        "v_t": lambda: np.ascontiguousarray(f(inp["peer_v"])[0].reshape(128, 128, D).transpose(1, 0, 2)),
        "w_pg_t": lambda: _chunked(f(inp["w_ple_gate"])[0], 32),
        "b_pg_t": lambda: _col(f(inp["b_ple_gate"])[0], 32),
        "w_ple_t": lambda: _chunked(f(inp["w_ple"])[0], 2),
        "ln2g": lambda: _col(f(inp["ln2_g"])[0], 32),
        "ln2b": lambda: _col(f(inp["ln2_b"])[0], 32),
        "iota_t": lambda: np.ascontiguousarray(np.broadcast_to(np.arange(128, dtype=np.float32), (128, 32, 128))),
    }
    return {k: fn() for k, fn in th.items() if used is None or k in used}


def core_positions(par):
    j = np.arange(8)
    own = ((2 * j + par)[:, None] * 128 + np.arange(128)[None, :]).reshape(-1)
    oth = ((2 * j + 1 - par)[:, None] * 128 + np.arange(128)[None, :]).reshape(-1)
    return own, oth


def prep_core(inp, b, par):
    x = np.asarray(inp["x"], dtype=np.float32)[b]
    pp = np.asarray(inp["p"], dtype=np.float32)[0, b]
    own, oth = core_positions(par)
    kpos = np.concatenate([own, oth])
    d = {}
    d["xT"] = np.ascontiguousarray(x[kpos].T)
    d["pT"] = np.ascontiguousarray(pp[own].T)
    d["qpos_b"] = np.ascontiguousarray(np.broadcast_to(own.astype(np.float32), (128, T)))
    d["kpos_b"] = np.ascontiguousarray(np.broadcast_to(kpos.astype(np.float32), (128, S)))
    d["kpos_col"] = _col(kpos.astype(np.float32), 16)
    d["cend_col"] = _col(((own // 64 + 1) * 64).astype(np.float32), 8)
    ce = own // 64 + 1
    cidx = np.arange(32)
    d["penA"] = np.where(cidx[:, None] >= ce[None, :], np.float32(-1e30), np.float32(0)).astype(np.float32)
    d["penB"] = (cidx[:, None] == (kpos // 64)[None, :]).astype(np.float32)
    return d


_CACHE = {}


def kernel(**inputs):
    if "nc" not in _CACHE:
        _CACHE["nc"] = build()
    nc, _, used = _CACHE["nc"]
    sh = prep_shared(inputs, used)
    in_maps = []
    for c in range(8):
        d = dict(sh)
        d.update(prep_core(inputs, c // 2, c % 2))
        in_maps.append({k: d[k] for k in used})
    res = run_bass_kernel_spmd(nc, in_maps, core_ids=list(range(8)))
    out = np.zeros((4, S, D), np.float32)
    for c in range(8):
        own, _ = core_positions(c % 2)
        out[c // 2, own, :] = res.results[c]["outT"].T
    return out
```

```python
from contextlib import ExitStack
import numpy as np
import concourse.bass as bass
import concourse.mybir as mybir
from concourse.bass_utils import run_bass_kernel_spmd

F32 = mybir.dt.float32
BF16 = mybir.dt.bfloat16
U32 = mybir.dt.uint32
ALU = mybir.AluOpType
AF = mybir.ActivationFunctionType
AX = mybir.AxisListType

ENG = ("sp", "act", "dve", "pool", "pe")
NDMASEM = 8


class Buf:
    __slots__ = ("name", "w", "ws", "r")

    def __init__(self, name=""):
        self.name = name
        self.w = None
        self.ws = []
        self.r = []


class Prog:
    def __init__(self, nc, es):
        self.nc = nc
        self.streams = {e: [] for e in ENG}
        self.cnt = {e: 0 for e in ENG}
        self.sems = {}
        for e in ENG:
            self.sems[("e", e)] = es.enter_context(nc.semaphore("s_" + e))
        self.dcnt = {}
        self.dnext = {}
        for q in ("sp", "act", "pool"):
            self.dnext[q] = 0
            for i in range(NDMASEM):
                k = ("d", q, i)
                self.sems[k] = es.enter_context(nc.semaphore("d_%s%d" % (q, i)))
                self.dcnt[k] = 0
        self.waited = {e: {} for e in ENG}
        self.ninstr = 0

    def _wait(self, e, deps, skip_own=False):
        best = {}
        for d in deps:
            if d is None:
                continue
            k, v = d
            if skip_own and k == ("e", e):
                continue
            if best.get(k, 0) < v:
                best[k] = v
        for k, v in best.items():
            if self.waited[e].get(k, 0) >= v:
                continue
            if k == ("e", e) and v > self.cnt[e]:
                continue
            self.waited[e][k] = v
            sem = self.sems[k]
            self.streams[e].append(lambda eng, sem=sem, v=v: eng.wait_ge(sem, v))

    @staticmethod
    def _deps(reads, writes, group=False):
        deps = []
        for b in reads:
            deps.append(b.w)
            deps.extend(b.ws)
        for b in writes:
            if not group:
                deps.append(b.w)
                deps.extend(b.ws)
            deps.extend(b.r)
        return deps

    def _mark(self, tok, reads, writes, group=False):
        for b in reads:
            b.r.append(tok)
            if len(b.r) > 64:
                best = {}
                for k, v in b.r:
                    if best.get(k, 0) < v:
                        best[k] = v
                b.r = list(best.items())
        for b in writes:
            if group:
                b.ws.append(tok)
            else:
                b.w = tok
                b.ws = []
                b.r = []

    def op(self, e, fn, reads=(), writes=(), inc=True, skip_own=False):
        self._wait(e, self._deps(reads, writes), skip_own)
        tok_val = self.cnt[e] + 1
        key = ("e", e)
        if inc:
            self.cnt[e] += 1
            sem = self.sems[key]
            self.streams[e].append(lambda eng, fn=fn, sem=sem: fn(eng).then_inc(sem, 1))
        else:
            self.streams[e].append(lambda eng, fn=fn: fn(eng))
        tok = (key, tok_val)
        self._mark(tok, reads, writes)
        self.ninstr += 1
        return tok

    def dma(self, q, out, in_, reads=(), writes=(), group=False, **kw):
        i = self.dnext[q]
        self.dnext[q] = (i + 1) % NDMASEM
        k = ("d", q, i)
        deps = self._deps(reads, writes, group)
        if self.dcnt[k] > 0:
            deps.append((k, self.dcnt[k]))
        self._wait(q, deps)
        self.dcnt[k] += 16
        sem = self.sems[k]
        self.streams[q].append(
            lambda eng, out=out, in_=in_, sem=sem, kw=kw: eng.dma_start(out=out, in_=in_, **kw).then_inc(sem, 16))
        tok = (k, self.dcnt[k])
        self._mark(tok, reads, writes, group)
        self.ninstr += 1
        return tok

    def barrier(self):
        deps = [(("e", e), self.cnt[e]) for e in ENG if self.cnt[e] > 0]
        deps += [(k, v) for k, v in self.dcnt.items() if v > 0]
        for e in ENG:
            self._wait(e, deps)

    def wait_all_on(self, e):
        deps = [(("e", x), self.cnt[x]) for x in ENG if self.cnt[x] > 0]
        deps += [(k, v) for k, v in self.dcnt.items() if v > 0]
        self._wait(e, deps)

    def emit(self):
        nc = self.nc
        with nc.Block() as block:
            @block.sync
            def _(eng):
                for f in self.streams["sp"]:
                    f(eng)

            @block.scalar
            def _(eng):
                for f in self.streams["act"]:
                    f(eng)

            @block.vector
            def _(eng):
                for f in self.streams["dve"]:
                    f(eng)

            @block.gpsimd
            def _(eng):
                for f in self.streams["pool"]:
                    f(eng)

            @block.tensor
            def _(eng):
                for f in self.streams["pe"]:
                    f(eng)


class Arena:
    def __init__(self, ap_full, nwords):
        self.a = ap_full
        self.n = nwords
        self.off = 0
        self.top = nwords
        self.marks = []

    def alloc(self, nbytes, dt=F32):
        nw = (nbytes + 3) // 4
        nw = (nw + 15) // 16 * 16
        assert self.off + nw <= self.top, "SBUF arena overflow %d + %d > %d" % (self.off, nw, self.top)
        v = self.a[:, self.off:self.off + nw]
        self.off += nw
        if dt != F32:
            v = v.bitcast(dt)
        return v

    def alloc_top(self, nbytes, dt=F32):
        nw = ((nbytes + 3) // 4 + 15) // 16 * 16
        assert self.top - nw >= self.off
        self.top -= nw
        v = self.a[:, self.top:self.top + nw]
        return v.bitcast(dt) if dt != F32 else v

    def f32(self, n):
        return self.alloc(4 * n)[:, 0:n]

    def bf16(self, n):
        return self.alloc(2 * n, BF16)[:, 0:n]

    def push(self):
        self.marks.append(self.off)

    def pop(self):
        self.off = self.marks.pop()


D = 4096
S = 2048
T = 1024
NQ_CH = 49
NK_CH = 36
ALPHA = 2.0 ** 0.25
SCALE = 128.0 ** -0.5
EPS = 1e-5
NEG = -30000.0
SLOPES = [2.0 ** (-8.0 * (h + 1) / 16) for h in range(16)]

ARENA_WORDS = 184 * 256
import os
NH_A = int(os.environ.get('NH_A', 16))
NH_B = int(os.environ.get('NH_B', 16))


def build(stage=99, dbg=()):
    nc = bass.Bass("TRN2", target_bir_lowering=False)

    def din(name, shape, dt=F32):
        return nc.dram_tensor(name, list(shape), dt, kind="ExternalInput").ap()

    def dscr(name, shape, dt=F32):
        return nc.dram_tensor(name, list(shape), dt, kind="Internal").ap()

    def dout(name, shape, dt=F32):
        return nc.dram_tensor(name, list(shape), dt, kind="ExternalOutput").ap()

    IN_SHAPES = {
        "xT": [D, S], "pT": [256, T], "w_in_t": [NQ_CH + NK_CH, 128, 32, 128], "ident": [128, 128],
        "qpos_b": [128, T], "kpos_b": [128, S], "kpos_col": [128, 16], "cend_col": [128, 8], "penA": [32, T], "penB": [32, S],
        "glat": [128, 2], "bfor": [128, 1], "wuk_t": [128, 2, 2048], "wuv_t": [128, 2, 2048],
        "w_gate_t": [64, 128, 32, 128], "b_gate_t": [128, 64], "w_ba_t": [32, 128, 16, 128],
        "w_bb_t": [32, 128, 16, 128], "w_out_t": [32, 128, 32, 128], "ln1g": [128, 32], "ln1b": [128, 32],
        "peer_wq_t": [16, 128, 32, 128], "sk_t": [128, 16, 128], "uT_t": [128, 128, 32, 128],
        "v_t": [128, 128, D], "w_pg_t": [32, 128, 32, 128], "b_pg_t": [128, 32],
        "w_ple_t": [32, 128, 2, 128], "ln2g": [128, 32], "ln2b": [128, 32], "iota_t": [128, 32, 128],
    }
    IN = {}

    def inp(name):
        if name not in IN:
            IN[name] = din(name, IN_SHAPES[name])
        return IN[name]

    outT_d = dout("outT", [D, T])

    qA_s = dscr("qA_s", [16, 128, T], BF16)
    qB_s = dscr("qB_s", [16, 128, T], BF16)
    kB_s = dscr("kB_s", [16, 128, S], BF16)
    vB_s = dscr("vB_s", [16, 128, S], BF16)
    oT_s = dscr("oT_s", [32, 128, T], BF16)
    s1_s = dscr("s1_s", [8, T, 128], F32)
    B_s1s = Buf("s1_s")
    B_gTs = Buf("gT_s")
    B_ggs = Buf("ggT_s")
    gT_s = dscr("gT_s", [16, 128, 128 * 64], BF16)
    ggT_s = dscr("ggT_s", [128, 128, T], BF16)

    dbg_out = {}

    with ExitStack() as es:
        p = Prog(nc, es)
        arena_t = es.enter_context(nc.sbuf_tensor("arena", [128, ARENA_WORDS], F32))
        psum_t = es.enter_context(nc.psum_tensor("ps", [128, 4096], F32))
        A = Arena(arena_t, ARENA_WORDS)
        PS = [psum_t[:, b * 512:(b + 1) * 512] for b in range(8)]
        PSB = [Buf("ps%d" % b) for b in range(8)]

        def dbg_dump(name, ap, shape, buf, dt=F32):
            if name in dbg:
                o = dout("dbg_" + name, shape, dt)
                dbg_out[name] = o
                p.dma("sp", o, ap, reads=[buf])

        ident_f = A.f32(128)
        ident_b = A.bf16(128)
        ones_f = A.f32(128)
        ones_b = A.bf16(128)
        B_const = Buf("const")
        p.dma("sp", ident_f, inp("ident"), writes=[B_const])
        p.op("dve", lambda e: e.tensor_copy(out=ident_b, in_=ident_f), reads=[B_const], writes=[B_const])
        p.op("dve", lambda e: e.memset(ones_f, 1.0), writes=[B_const])
        p.op("dve", lambda e: e.memset(ones_b, 1.0), writes=[B_const])
        A_BASE = A.off

        def arena_reset():
            p.barrier()
            A.off = A_BASE
            A.top = ARENA_WORDS
            A.marks = []

        def gemm(xT, KC, ntok, w_dram, chunks, evac, xbufs, wtiles, ps_slots, q="pool"):
            nb = ntok // 512
            for i, c in enumerate(chunks):
                wt, wb = wtiles[i % len(wtiles)]
                p.dma(q, wt.rearrange("p (k n) -> p k n", k=KC), w_dram[c], writes=[wb])
                banks = ps_slots[i % len(ps_slots)]
                for kc in range(KC):
                    for t in range(nb):
                        b = banks[t]
                        p.op("pe", lambda e, b=b, kc=kc, t=t, wt=wt: e.matmul(
                            PS[b], wt[:, kc * 128:(kc + 1) * 128], xT[:, kc, t * 512:(t + 1) * 512],
                            start=(kc == 0), stop=(kc == KC - 1)),
                            reads=[wb] + list(xbufs), writes=[PSB[b]],
                            inc=(kc == KC - 1 and t == nb - 1))
                evac(i, c, banks)

        ckvn = A.bf16(2 * S).rearrange("p (c t) -> p c t", c=2)
        B_ckvn = Buf("ckvn")
        B_selT = Buf("selT")
        kpos_b = A.f32(S)
        qpos_b = A.f32(T)
        kpos_col = A.f32(16)
        cend_col = A.f32(8)
        glat = A.f32(2)
        negb = A.f32(1)
        B_pos = Buf("pos")
        A.push()
        qidxT = A.bf16(16 * T).rearrange("p (c t) -> p c t", c=16)
        B_qidx = Buf("qidx")
        kidxT = A.bf16(S)
        B_kidx = Buf("kidx")
        w_tok = A.f32(8 * 32).rearrange("p (j h) -> p j h", j=8)
        B_wtok = Buf("wtok")
        A.push()
        ckvT = A.f32(2 * S).rearrange("p (c t) -> p c t", c=2)
        B_ckv = Buf("ckv")
        fT = A.f32(S)
        B_f = Buf("fT")
        widxT = A.f32(T)
        B_widxT = Buf("widxT")
        A.push()
        xT = A.bf16(32 * T).rearrange("p (k t) -> p k t", k=32)
        B_x = Buf("xT")
        wtiles = [(A.bf16(32 * 128), Buf("w%d" % i)) for i in range(3)]
        stg = [(A.bf16(T), Buf("stg%d" % i)) for i in range(3)]
        xT_v = inp("xT").rearrange("(k p) t -> p k t", p=128)

        def load_x(half):
            for k4 in range(4):
                p.dma("pool", xT[:, k4 * 8:(k4 + 1) * 8, :], xT_v[:, k4 * 8:(k4 + 1) * 8, half * T:(half + 1) * T],
                      writes=[B_x], group=(k4 > 0))

        slots2 = [[0, 1], [2, 3], [4, 5], [6, 7]]
        stg_i = [0]

        def evac_A(half):
            tok0 = half * T

            def ev(i, c, banks):
                def to_scratch(dst, scale):
                    st, sb = stg[stg_i[0] % 3]
                    stg_i[0] += 1
                    for t, b in enumerate(banks):
                        p.op("act", lambda e, b=b, t=t, st=st: e.activation(
                            out=st[:, t * 512:(t + 1) * 512], in_=PS[b], func=AF.Copy, scale=scale),
                            reads=[PSB[b]], writes=[sb])
                    p.dma("sp", dst, st, reads=[sb])

                if c < 16:
                    to_scratch(qA_s[c], SCALE)
                elif c < 32:
                    to_scratch(qB_s[c - 16], SCALE)
                elif c < 48:
                    for t, b in enumerate(banks):
                        p.op("act", lambda e, b=b, t=t: e.activation(
                            out=qidxT[:, c - 32, t * 512:(t + 1) * 512], in_=PS[b], func=AF.Copy),
                            reads=[PSB[b]], writes=[B_qidx])
                elif c == 48:
                    for t, b in enumerate(banks):
                        p.op("dve", lambda e, b=b, t=t: e.tensor_copy(out=widxT[:, t * 512:(t + 1) * 512], in_=PS[b]),
                             reads=[PSB[b]], writes=[B_widxT])
                elif c < 51:
                    for t, b in enumerate(banks):
                        p.op("dve", lambda e, b=b, t=t: e.tensor_copy(
                            out=ckvT[:, c - 49, tok0 + t * 512: tok0 + (t + 1) * 512], in_=PS[b]),
                            reads=[PSB[b]], writes=[B_ckv])
                elif c == 51:
                    for t, b in enumerate(banks):
                        p.op("act", lambda e, b=b, t=t: e.activation(
                            out=kidxT[:, tok0 + t * 512: tok0 + (t + 1) * 512], in_=PS[b], func=AF.Copy),
                            reads=[PSB[b]], writes=[B_kidx])
                elif c < 68:
                    to_scratch(kB_s[c - 52][:, tok0:tok0 + T], 1.0)
                elif c < 84:
                    to_scratch(vB_s[c - 68][:, tok0:tok0 + T], 1.0)
                else:
                    for t, b in enumerate(banks):
                        p.op("dve", lambda e, b=b, t=t: e.tensor_copy(
                            out=fT[:, tok0 + t * 512: tok0 + (t + 1) * 512], in_=PS[b]),
                            reads=[PSB[b]], writes=[B_f])
            return ev

        load_x(1)
        gemm(xT, 32, T, inp("w_in_t"), list(range(NQ_CH, NQ_CH + NK_CH)), evac_A(1), [B_x], wtiles, slots2)
        load_x(0)
        gemm(xT, 32, T, inp("w_in_t"), list(range(NQ_CH + NK_CH)), evac_A(0), [B_x], wtiles, slots2)

        dbg_dump("ckv", ckvT, [128, 2, S], B_ckv)
        dbg_dump("fT", fT, [128, S], B_f)
        dbg_dump("widxT", widxT, [128, T], B_widxT)
        dbg_dump("kidxT", kidxT, [128, S], B_kidx, BF16)
        dbg_dump("qidxT", qidxT, [128, 16, T], B_qidx, BF16)

        if stage <= 1:
            p.wait_all_on("sp")
            p.emit()
            return nc, dbg_out, list(IN)

        p.barrier()
        A.pop()
        selT = A.alloc_top(2 * 16 * T, BF16).rearrange("p (k t) -> p k t", k=16)
        p.dma("sp", kpos_b, inp("kpos_b"), writes=[B_pos])
        p.dma("sp", qpos_b, inp("qpos_b"), writes=[B_pos])
        p.dma("sp", kpos_col, inp("kpos_col"), writes=[B_pos])
        p.dma("sp", cend_col, inp("cend_col"), writes=[B_pos])
        p.dma("sp", glat, inp("glat"), writes=[B_pos])
        p.dma("sp", negb, inp("bfor"), writes=[B_pos])
        p.op("dve", lambda e: e.tensor_scalar(out=negb, in0=negb, scalar1=-1.0, scalar2=None, op0=ALU.mult),
             reads=[B_pos], writes=[B_pos])
        A.push()
        sq = A.f32(2 * S).rearrange("p (c t) -> p c t", c=2)
        rstd = A.f32(S)
        B_sq, B_rstd = Buf("sq"), Buf("rstd")
        for c in range(2):
            p.op("act", lambda e, c=c: e.activation(out=sq[:, c, :], in_=ckvT[:, c, :], func=AF.Square),
                 reads=[B_ckv], writes=[B_sq])
        for t in range(4):
            for c in range(2):
                p.op("pe", lambda e, t=t, c=c: e.matmul(PS[t], ones_f, sq[:, c, t * 512:(t + 1) * 512],
                                                        start=(c == 0), stop=(c == 1)),
                     reads=[B_sq, B_const], writes=[PSB[t]], inc=(c == 1))
            p.op("dve", lambda e, t=t: e.tensor_scalar(out=rstd[:, t * 512:(t + 1) * 512], in0=PS[t],
                                                       scalar1=1.0 / 256, scalar2=EPS, op0=ALU.mult, op1=ALU.add),
                 reads=[PSB[t]], writes=[B_rstd])
        p.op("act", lambda e: e.activation(out=rstd, in_=rstd, func=AF.Sqrt), reads=[B_rstd], writes=[B_rstd])
        p.op("dve", lambda e: e.reciprocal(out=rstd, in_=rstd), reads=[B_rstd], writes=[B_rstd])
        for c in range(2):
            p.op("dve", lambda e, c=c: e.scalar_tensor_tensor(out=ckvn[:, c, :], in0=ckvT[:, c, :], scalar=glat[:, c:c + 1],
                                                              in1=rstd, op0=ALU.mult, op1=ALU.mult),
                 reads=[B_ckv, B_rstd, B_pos], writes=[B_ckvn])
        dbg_dump("ckvn", ckvn, [128, 2, S], B_ckvn, BF16)
        A.pop()

        for j in range(8):
            p.op("pe", lambda e, j=j: e.matmul(PS[4][:, j * 32:(j + 1) * 32], widxT[0:32, j * 128:(j + 1) * 128],
                                               ident_f[0:32, 0:32], start=True, stop=True),
                 reads=[B_widxT, B_const], writes=[PSB[4]], inc=(j == 7))
        p.op("dve", lambda e: e.tensor_copy(out=w_tok.rearrange("p j h -> p (j h)"), in_=PS[4][:, 0:256]),
             reads=[PSB[4]], writes=[B_wtok])

        A.push()
        l2 = A.f32(S)
        B_l2 = Buf("l2")
        p.op("act", lambda e: e.activation(out=l2[0:16, :], in_=fT[0:16, :], func=AF.Exp, scale=-1.0, bias=negb[0:16, :]),
             reads=[B_f, B_pos], writes=[B_l2])
        p.op("act", lambda e: e.activation(out=l2[0:16, :], in_=l2[0:16, :], func=AF.Ln, scale=1.0, bias=1.0),
             reads=[B_l2], writes=[B_l2])
        for i in range(16):
            p.op("pe", lambda e, i=i: e.matmul(PS[5][:, i * 16:(i + 1) * 16], l2[0:16, i * 128:(i + 1) * 128],
                                               ident_f[0:16, 0:16], start=True, stop=True),
                 reads=[B_l2, B_const], writes=[PSB[5]], inc=(i == 15))
        l2t = A.f32(256)
        r1 = A.f32(256)
        tmpf = A.f32(256)
        parts = [A.bf16(256) for _ in range(3)]
        B_sp = Buf("split")
        p.op("dve", lambda e: e.tensor_copy(out=l2t, in_=PS[5][:, 0:256]), reads=[PSB[5]], writes=[B_sp])

        def split3(src, res, tmp, outs, n_part):
            sl = lambda a: a[0:n_part]
            o0, o1, o2 = outs
            p.op("dve", lambda e: e.tensor_copy(out=sl(o0), in_=sl(src)), reads=[B_sp], writes=[B_sp])
            p.op("dve", lambda e: e.tensor_copy(out=sl(tmp), in_=sl(o0)), reads=[B_sp], writes=[B_sp])
            p.op("dve", lambda e: e.tensor_sub(out=sl(res), in0=sl(src), in1=sl(tmp)), reads=[B_sp], writes=[B_sp])
            p.op("dve", lambda e: e.tensor_copy(out=sl(o1), in_=sl(res)), reads=[B_sp], writes=[B_sp])
            p.op("dve", lambda e: e.tensor_copy(out=sl(tmp), in_=sl(o1)), reads=[B_sp], writes=[B_sp])
            p.op("dve", lambda e: e.tensor_sub(out=sl(res), in0=sl(res), in1=sl(tmp)), reads=[B_sp], writes=[B_sp])
            p.op("dve", lambda e: e.tensor_copy(out=sl(o2), in_=sl(res)), reads=[B_sp], writes=[B_sp])

        split3(l2t, r1, tmpf, parts, 128)
        TtR = A.f32(S)
        Tt = [(TtR[:, i * 1024:(i + 1) * 1024].bitcast(BF16), Buf("Tt%d" % i)) for i in range(2)]
        for i in range(16):
            tt, tb = Tt[i % 2]
            p.op("dve", lambda e, i=i, tt=tt: e.tensor_scalar(out=tt, in0=kpos_b, scalar1=kpos_col[:, i:i + 1], scalar2=None,
                                                             op0=ALU.is_ge),
                 reads=[B_pos], writes=[tb])
            for t in range(4):
                for k in range(3):
                    p.op("pe", lambda e, i=i, t=t, k=k, tt=tt: e.matmul(
                        PS[t][0:16, :], parts[k][:, i * 16:(i + 1) * 16], tt[:, t * 512:(t + 1) * 512],
                        start=(i == 0 and k == 0), stop=(i == 15 and k == 2)),
                        reads=[B_sp, tb], writes=[PSB[t]], inc=(k == 2 and t == 3))
        cn = A.f32(S)
        cres = l2
        ctmp = TtR
        cparts = [A.bf16(S) for _ in range(3)]
        nparts = [A.bf16(S) for _ in range(3)]
        for t in range(4):
            p.op("dve", lambda e, t=t: e.tensor_copy(out=cn[0:16, t * 512:(t + 1) * 512], in_=PS[t][0:16, :]),
                 reads=[PSB[t]], writes=[B_sp])
        p.barrier()
        split3(cn, cres, ctmp, cparts, 16)
        cum_s = dscr("cum_s", [6, 16, S], BF16)
        B_cums = Buf("cum_s")
        for k in range(3):
            p.op("dve", lambda e, k=k: e.tensor_scalar(out=nparts[k][0:16], in0=cparts[k][0:16], scalar1=-1.0, scalar2=None,
                                                       op0=ALU.mult), reads=[B_sp], writes=[B_sp])
        for k in range(3):
            p.dma("sp", cum_s[k], cparts[k][0:16], reads=[B_sp], writes=[B_cums])
            p.dma("sp", cum_s[3 + k], nparts[k][0:16], reads=[B_sp], writes=[B_cums])
        dbg_dump("cn", cn, [128, S], B_sp)
        p.barrier()
        A.pop()

        A.pop()
        A.push()
        penA = A.bf16(T)
        penB = A.bf16(S)
        B_pen = Buf("pen")
        p.dma("pool", penA[0:32, :], inp("penA"), writes=[B_pen])
        p.dma("pool", penB[0:32, :], inp("penB"), writes=[B_pen])
        scmS = [(A.f32(S), Buf("scm%d" % i)) for i in range(4)]
        workS = [(A.f32(S), Buf("work%d" % i)) for i in range(4)]
        selcS = [(A.bf16(S), Buf("selc%d" % i)) for i in range(2)]
        m8S = [(A.f32(8), Buf("m8%d" % i)) for i in range(2)]
        thrS = [A.f32(1) for i in range(2)]
        rb = [(A.bf16(512), Buf("r%d" % i)) for i in range(4)]
        dgb = [(A.bf16(128), Buf("dg%d" % i)) for i in range(4)]
        ri = [0]
        pi = [0]

        def score_phase(j):
            scm, B_scm = scmS[j % 4]
            work, B_work = workS[j % 4]
            pieces = [0, 2] if j < 4 else [0, 1, 2, 3]
            for ip, pc in enumerate(pieces):
                p.op("pe", lambda e, ip=ip, pc=pc: e.matmul(
                    PS[4 + ip], penA[0:32, j * 128:(j + 1) * 128], penB[0:32, pc * 512:(pc + 1) * 512],
                    start=True, stop=False), reads=[B_pen], writes=[PSB[4 + ip]], inc=False)
            tiles = [(hi, ip, pc) for hi in range(32) for ip, pc in enumerate(pieces)]
            nt = len(tiles)

            def emit_R(t):
                hi, ip, pc = tiles[t]
                c, hb = hi // 2, (hi % 2) * 64
                if ip == 0:
                    dg, B_dg = dgb[hi % 4]
                    p.op("pool", lambda e, dg=dg, hi=hi: e.tensor_scalar(
                        out=dg, in0=ident_f, scalar1=w_tok[:, j, hi:hi + 1], scalar2=None, op0=ALU.mult),
                        reads=[B_const, B_wtok], writes=[B_dg])
                b = t % 4
                p.op("pe", lambda e, b=b, c=c, hb=hb, pc=pc: e.matmul(
                    PS[b], qidxT[hb:hb + 64, c, j * 128:(j + 1) * 128], kidxT[hb:hb + 64, pc * 512:(pc + 1) * 512],
                    start=True, stop=True), reads=[B_qidx, B_kidx], writes=[PSB[b]])

            LA = 3
            for t in range(min(LA, nt)):
                emit_R(t)
            for t in range(nt):
                if t + LA < nt:
                    emit_R(t + LA)
                hi, ip, pc = tiles[t]
                b = t % 4
                r, rbuf = rb[ri[0] % 4]
                ri[0] += 1
                dg, B_dg = dgb[hi % 4]
                p.op("act", lambda e, b=b, r=r: e.activation(out=r, in_=PS[b], func=AF.Relu),
                     reads=[PSB[b]], writes=[rbuf])
                p.op("pe", lambda e, ip=ip, dg=dg, r=r, hi=hi: e.matmul(
                    PS[4 + ip], dg, r, start=False, stop=(hi == 31)),
                    reads=[B_dg, rbuf], writes=[PSB[4 + ip]])
            nv = (j + 1) * 128
            for ip, pc in enumerate(pieces):
                wv = min(512, nv - (pc % 2) * 512)
                if wv <= 0:
                    continue
                d0 = (0 if pc < 2 else nv) + (pc % 2) * 512
                p.op("act", lambda e, ip=ip, d0=d0, wv=wv: e.activation(out=scm[:, d0:d0 + wv], in_=PS[4 + ip][:, 0:wv], func=AF.Copy),
                     reads=[PSB[4 + ip]], writes=[B_scm])
                if j > 0:
                    p.op("act", lambda e, ip=ip, d0=d0, wv=wv: e.activation(out=work[:, d0:d0 + wv], in_=PS[4 + ip][:, 0:wv], func=AF.Copy),
                         reads=[PSB[4 + ip]], writes=[B_work])

        def topk_pair(js):
            Ws = [2 * (j + 1) * 128 for j in js]
            for rnd in range(32):
                for a, j in enumerate(js):
                    if j == 0:
                        continue
                    work, B_work = workS[j % 4]
                    m8, B_m8 = m8S[a]
                    W = Ws[a]
                    p.op("dve", lambda e, W=W, work=work, m8=m8: e.max(out=m8, in_=work[:, 0:W]), reads=[B_work], writes=[B_m8])
                if rnd < 31:
                    for a, j in enumerate(js):
                        if j == 0:
                            continue
                        work, B_work = workS[j % 4]
                        m8, B_m8 = m8S[a]
                        W = Ws[a]
                        p.op("dve", lambda e, W=W, work=work, m8=m8: e.match_replace(
                            out=work[:, 0:W], in_to_replace=m8, in_values=work[:, 0:W], imm_value=-1e30),
                            reads=[B_m8, B_work], writes=[B_work])
            for a, j in enumerate(js):
                scm, B_scm = scmS[j % 4]
                m8, B_m8 = m8S[a]
                selc, B_selc = selcS[a]
                thr = thrS[a]
                W = Ws[a]
                if j == 0:
                    p.op("dve", lambda e, thr=thr: e.memset(thr, -1e29), reads=[B_m8], writes=[B_m8])
                else:
                    p.op("dve", lambda e, thr=thr, m8=m8: e.tensor_scalar(out=thr, in0=m8[:, 7:8], scalar1=-1e29, scalar2=None,
                                                                         op0=ALU.max), reads=[B_m8], writes=[B_m8])
                p.op("dve", lambda e, W=W, selc=selc, scm=scm, thr=thr: e.tensor_scalar(
                    out=selc[:, 0:W], in0=scm[:, 0:W], scalar1=thr, scalar2=None, op0=ALU.is_ge),
                    reads=[B_m8, B_scm], writes=[B_selc])
                for base_blk, kt_base in ((0, 0), (j + 1, 8)):
                    for g0 in range(0, j + 1, 4):
                        n4 = min(4, j + 1 - g0)
                        b = pi[0] % 4
                        pi[0] += 1
                        psb = PS[b].bitcast(BF16)
                        for i4 in range(n4):
                            i = base_blk + g0 + i4
                            p.op("pe", lambda e, psb=psb, i=i, i4=i4, selc=selc: e.transpose(
                                psb[:, i4 * 128:(i4 + 1) * 128], selc[:, i * 128:(i + 1) * 128], ident_b),
                                 reads=[B_selc, B_const], writes=[PSB[b]], inc=(i4 == n4 - 1))
                        kt0 = kt_base + g0
                        p.op("act", lambda e, psb=psb, kt0=kt0, j=j, n4=n4: e.activation(
                            out=selT[:, kt0:kt0 + n4, j * 128:(j + 1) * 128],
                            in_=psb[:, 0:n4 * 128].rearrange("p (k q) -> p k q", k=n4),
                            func=AF.Copy), reads=[PSB[b]], writes=[B_selT])

        p.op("dve", lambda e: e.memset(selT.rearrange("p k t -> p (k t)"), 0.0), writes=[B_selT])
        score_phase(0)
        score_phase(1)
        for i in range(4):
            if i < 3:
                score_phase(2 * i + 2)
                score_phase(2 * i + 3)
            topk_pair((2 * i, 2 * i + 1))
        dbg_dump("selT", selT, [128, 16, T], B_selT, BF16)
        p.barrier()
        A.pop()

        if stage <= 2:
            p.wait_all_on("sp")
            p.emit()
            return nc, dbg_out, list(IN)

        A.pop()
        KTS = [[0, 1, 2, 3, 8, 9, 10, 11], list(range(16))]
        tmpb = [(A.f32(512), Buf("tmp%d" % i)) for i in range(4)]
        ptb = [(A.bf16(512), Buf("pt%d" % i)) for i in range(4)]
        rec = A.f32(512)
        B_rec = Buf("rec")
        ostb = [(A.bf16(512), Buf("ost%d" % i)) for i in range(2)]
        qTb = [(A.bf16(T), Buf("qT%d" % i)) for i in range(2)]
        kTb = [(A.bf16(S), Buf("kT%d" % i)) for i in range(2)]
        Vb = [(A.bf16(S).rearrange("p (k d) -> p k d", k=16), Buf("V%d" % i)) for i in range(2)]
        cnt = {"s": 0, "tmp": 0, "pt": 0, "ost": 0, "acc": 0}

        def attn_core(h_glob, qT, B_q, kT, B_k, V, B_V, pre_exp, bias_mm):
            for J in range(2):
                kts = KTS[J]
                n = len(kts)
                oacc = 4 + (cnt["acc"] % 2)
                dacc = 6 + (cnt["acc"] % 2)
                cnt["acc"] += 1
                sbank = {}

                def emit_S(idx):
                    kt = kts[idx]
                    b = cnt["s"] % 4
                    cnt["s"] += 1
                    sbank[idx] = b
                    last = bias_mm is None
                    p.op("pe", lambda e, b=b, kt=kt, J=J, last=last: e.matmul(
                        PS[b], kT[:, kt * 128:(kt + 1) * 128], qT[:, J * 512:(J + 1) * 512], start=True, stop=last),
                         reads=[B_k, B_q], writes=[PSB[b]], inc=last)
                    if bias_mm is not None:
                        bias_mm(J, kt, b)

                LA = 3
                for i0 in range(min(LA, n)):
                    emit_S(i0)
                for idx in range(n):
                    kt = kts[idx]
                    if idx + LA < n:
                        emit_S(idx + LA)
                    b = sbank[idx]
                    tmp, B_tmp = tmpb[cnt["tmp"] % 4]
                    cnt["tmp"] += 1
                    pt, B_pt = ptb[cnt["pt"] % 4]
                    cnt["pt"] += 1
                    pre_exp(J, kt, b, tmp, B_tmp)
                    p.op("act", lambda e, tmp=tmp, pt=pt: e.activation(out=pt, in_=tmp, func=AF.Exp),
                         reads=[B_tmp], writes=[B_pt])
                    p.op("pe", lambda e, kt=kt, pt=pt, idx=idx, oacc=oacc, n=n: e.matmul(
                        PS[oacc], V[:, kt, :], pt, start=(idx == 0), stop=(idx == n - 1)),
                         reads=[B_V, B_pt], writes=[PSB[oacc]], inc=False)
                    p.op("pe", lambda e, pt=pt, idx=idx, dacc=dacc, n=n: e.matmul(
                        PS[dacc], ones_b, pt, start=(idx == 0), stop=(idx == n - 1)),
                         reads=[B_const, B_pt], writes=[PSB[dacc]], inc=True)
                ost, B_ost = ostb[cnt["ost"] % 2]
                cnt["ost"] += 1
                p.op("dve", lambda e, dacc=dacc: e.reciprocal(out=rec, in_=PS[dacc]), reads=[PSB[dacc]], writes=[B_rec])
                p.op("dve", lambda e, ost=ost, oacc=oacc: e.tensor_tensor(out=ost, in0=PS[oacc], in1=rec, op=ALU.mult),
                     reads=[PSB[oacc], B_rec], writes=[B_ost])
                p.dma("sp", oT_s[h_glob][:, J * 512:(J + 1) * 512], ost, reads=[B_ost], writes=[B_oTs])

        B_oTs = Buf("oT_s")
        B_scr = Buf("scrA")

        A.push()
        wuk = A.bf16(2 * 2048).rearrange("p (c n) -> p c n", c=2)
        wuv = A.bf16(2 * 2048).rearrange("p (c n) -> p c n", c=2)
        B_wu = Buf("wu")
        p.dma("pool", wuk, inp("wuk_t"), writes=[B_wu])
        p.dma("pool", wuv, inp("wuv_t"), writes=[B_wu])
        Dm = A.f32(24 * 512).rearrange("p (k q) -> p k q", k=24)
        B_Dm = Buf("Dm")
        dmi = {}
        for J in range(2):
            for kt in KTS[J]:
                i = len(dmi)
                dmi[(J, kt)] = i
                dst = Dm[:, i, :]
                p.op("dve", lambda e, dst=dst, J=J, kt=kt: e.tensor_scalar(
                    out=dst, in0=qpos_b[:, J * 512:(J + 1) * 512], scalar1=kpos_col[:, kt:kt + 1], scalar2=None,
                    op0=ALU.subtract), reads=[B_pos], writes=[B_Dm])
                p.op("dve", lambda e, dst=dst: e.scalar_tensor_tensor(out=dst, in0=dst, scalar=-1.0, in1=dst,
                                                                      op0=ALU.mult, op1=ALU.max),
                     reads=[B_Dm], writes=[B_Dm])
                p.op("dve", lambda e, dst=dst: e.tensor_scalar(out=dst, in0=dst, scalar1=1.0e6, scalar2=None, op0=ALU.add),
                     reads=[B_Dm], writes=[B_Dm])
                p.op("dve", lambda e, dst=dst, J=J, kt=kt: e.scalar_tensor_tensor(
                    out=dst, in0=selT[:, kt, J * 512:(J + 1) * 512], scalar=-1.0e6, in1=dst, op0=ALU.mult, op1=ALU.add),
                    reads=[B_Dm, B_selT], writes=[B_Dm])
        if NH_A:
            p.dma("sp", qTb[0][0], qA_s[0], writes=[qTb[0][1]])
        for h in range(NH_A):
            qT, B_q = qTb[h % 2]
            kT, B_k = kTb[h % 2]
            V, B_V = Vb[h % 2]
            if h + 1 < NH_A:
                p.dma("sp", qTb[(h + 1) % 2][0], qA_s[h + 1], writes=[qTb[(h + 1) % 2][1]])
            for t in range(4):
                for c in range(2):
                    p.op("pe", lambda e, t=t, c=c, h=h: e.matmul(PS[t], wuk[:, c, h * 128:(h + 1) * 128],
                                                                 ckvn[:, c, t * 512:(t + 1) * 512], start=(c == 0), stop=(c == 1)),
                         reads=[B_wu, B_ckvn], writes=[PSB[t]], inc=(c == 1))
                p.op("act", lambda e, t=t, kT=kT: e.activation(out=kT[:, t * 512:(t + 1) * 512], in_=PS[t], func=AF.Copy),
                     reads=[PSB[t]], writes=[B_k])
            for t in range(4):
                for k4 in range(4):
                    kt = t * 4 + k4
                    for c in range(2):
                        p.op("pe", lambda e, t=t, k4=k4, kt=kt, c=c, h=h: e.matmul(
                            PS[t][:, k4 * 128:(k4 + 1) * 128], ckvn[:, c, kt * 128:(kt + 1) * 128],
                            wuv[:, c, h * 128:(h + 1) * 128], start=(c == 0), stop=(c == 1)),
                            reads=[B_wu, B_ckvn], writes=[PSB[t]], inc=(c == 1 and k4 == 3))
                p.op("act", lambda e, t=t, V=V: e.activation(out=V[:, t * 4:(t + 1) * 4, :],
                                                             in_=PS[t].rearrange("p (k d) -> p k d", k=4), func=AF.Copy),
                     reads=[PSB[t]], writes=[B_V])

            def pre_exp(J, kt, b, tmp, B_tmp, h=h):
                i = dmi[(J, kt)]
                p.op("dve", lambda e: e.scalar_tensor_tensor(out=tmp, in0=Dm[:, i, :], scalar=-SLOPES[h], in1=PS[b],
                                                             op0=ALU.mult, op1=ALU.add),
                     reads=[B_Dm, PSB[b]], writes=[B_tmp])

            attn_core(h, qT, B_q, kT, B_k, V, B_V, pre_exp, None)
        p.barrier()
        A.pop()

        A.push()
        Mk = A.bf16(24 * 512).rearrange("p (k q) -> p k q", k=24)
        B_Mk = Buf("Mk")
        for J in range(2):
            for kt in KTS[J]:
                i = dmi[(J, kt)]
                p.op("dve", lambda e, i=i, J=J, kt=kt: e.tensor_scalar(
                    out=Mk[:, i, :], in0=qpos_b[:, J * 512:(J + 1) * 512], scalar1=kpos_col[:, kt:kt + 1], scalar2=NEG,
                    op0=ALU.is_lt, op1=ALU.mult), reads=[B_pos], writes=[B_Mk])
        vTb = [(A.bf16(S), Buf("vT%d" % i)) for i in range(2)]
        Aopb = [(A.bf16(S), Buf("Aop%d" % i)) for i in range(2)]
        Bopb = [(A.bf16(T), Buf("Bop%d" % i)) for i in range(2)]
        for i in range(2):
            p.op("dve", lambda e, i=i: e.memset(Aopb[i][0][0:32, :], 1.0), writes=[Aopb[i][1]])
            p.op("dve", lambda e, i=i: e.memset(Bopb[i][0][0:32, :], 1.0), writes=[Bopb[i][1]])
        def fox_loads(h):
            p.dma("sp", qTb[h % 2][0], qB_s[h], writes=[qTb[h % 2][1]])
            p.dma("sp", kTb[h % 2][0], kB_s[h], writes=[kTb[h % 2][1]])
            p.dma("sp", vTb[h % 2][0], vB_s[h], writes=[vTb[h % 2][1]])
            p.dma("sp", Aopb[h % 2][0][3:6, :], cum_s[0:3, h, :], writes=[Aopb[h % 2][1]])
            p.dma("sp", Bopb[h % 2][0][0:3, :], cum_s[3:6, h, 0:T], writes=[Bopb[h % 2][1]])

        if NH_B:
            fox_loads(0)
        for h in range(NH_B):
            qT, B_q = qTb[h % 2]
            kT, B_k = kTb[h % 2]
            V, B_V = Vb[h % 2]
            vT, B_vT = vTb[h % 2]
            Aop, B_A = Aopb[h % 2]
            Bop, B_B = Bopb[h % 2]
            if h + 1 < NH_B:
                fox_loads(h + 1)
            for half in range(2):
                psb = PS[half].bitcast(BF16)
                for k8 in range(8):
                    kt = half * 8 + k8
                    p.op("pe", lambda e, psb=psb, k8=k8, kt=kt, vT=vT: e.transpose(
                        psb[:, k8 * 128:(k8 + 1) * 128], vT[:, kt * 128:(kt + 1) * 128], ident_b),
                        reads=[B_vT, B_const], writes=[PSB[half]], inc=(k8 == 7))
                p.op("act", lambda e, psb=psb, half=half, V=V: e.activation(
                    out=V[:, half * 8:(half + 1) * 8, :], in_=psb.rearrange("p (k d) -> p k d", k=8), func=AF.Copy),
                    reads=[PSB[half]], writes=[B_V])

            def bias_mm(J, kt, b, Aop=Aop, Bop=Bop, B_A=B_A, B_B=B_B):
                p.op("pe", lambda e: e.matmul(PS[b], Aop[0:6, kt * 128:(kt + 1) * 128], Bop[0:6, J * 512:(J + 1) * 512],
                                              start=False, stop=True),
                     reads=[B_A, B_B], writes=[PSB[b]], inc=True)

            def pre_exp(J, kt, b, tmp, B_tmp):
                i = dmi[(J, kt)]
                p.op("dve", lambda e: e.tensor_tensor(out=tmp, in0=PS[b], in1=Mk[:, i, :], op=ALU.add),
                     reads=[B_Mk, PSB[b]], writes=[B_tmp])

            attn_core(16 + h, qT, B_q, kT, B_k, V, B_V, pre_exp, bias_mm)
        p.barrier()
        A.pop()
        if "oT" in dbg:
            o = dout("dbg_oT", [32, 128, T], BF16)
            ot_sb = A.bf16(T)
            B_otsb = Buf("otsb")
            for i in range(32):
                p.dma("sp", ot_sb, oT_s[i], reads=[B_oTs], writes=[B_otsb])
                p.dma("sp", o[i], ot_sb, reads=[B_otsb])

        if stage <= 3:
            p.wait_all_on("sp")
            p.emit()
            return nc, dbg_out, list(IN)

        arena_reset()
        mg_s = dscr("mg_s", [32, 128, T], BF16)
        B_mgs = Buf("mg_s")
        xT = A.bf16(32 * T).rearrange("p (k t) -> p k t", k=32)
        oT = A.bf16(32 * T).rearrange("p (k t) -> p k t", k=32)
        B_x, B_o = Buf("xT"), Buf("oT")
        wtiles = [(A.bf16(32 * 128), Buf("w%d" % i)) for i in range(4)]
        bgate = A.f32(64)
        B_bg = Buf("bgate")
        p.dma("sp", bgate, inp("b_gate_t"), writes=[B_bg])
        for k4 in range(4):
            p.dma("pool", xT[:, k4 * 8:(k4 + 1) * 8, :], xT_v[:, k4 * 8:(k4 + 1) * 8, 0:T], writes=[B_x], group=(k4 > 0))
        for k4 in range(8):
            p.dma("sp", oT[:, k4 * 4:(k4 + 1) * 4, :], oT_s[k4 * 4:(k4 + 1) * 4].rearrange("c p t -> p c t"),
                  reads=[B_oTs], writes=[B_o], group=(k4 > 0))
        gsig = [(A.f32(T), Buf("gsig%d" % i)) for i in range(2)]
        m1 = A.f32(T)
        B_m1 = Buf("m1")
        mgst = [(A.bf16(T), Buf("mgst%d" % i)) for i in range(2)]
        slots2 = [[0, 1], [2, 3], [4, 5], [6, 7]]
        gi = [0]
        wi = [0]
        si = [0]

        def one_chunk(xin, xbuf, KC, w_dram, c, evac):
            wt, wb = wtiles[wi[0] % 4]
            wi[0] += 1
            banks = slots2[si[0] % 4]
            si[0] += 1
            p.dma("pool", wt[:, 0:KC * 128].rearrange("p (k n) -> p k n", k=KC), w_dram[c], writes=[wb])
            for kc in range(KC):
                for t in range(2):
                    b = banks[t]
                    p.op("pe", lambda e, b=b, kc=kc, t=t, wt=wt: e.matmul(
                        PS[b], wt[:, kc * 128:(kc + 1) * 128], xin[:, kc, t * 512:(t + 1) * 512],
                        start=(kc == 0), stop=(kc == KC - 1)),
                        reads=[wb, xbuf], writes=[PSB[b]], inc=(kc == KC - 1 and t == 1))
            evac(banks)

        w_gate_d, w_ba_d, w_bb_d = inp("w_gate_t"), inp("w_ba_t"), inp("w_bb_t")
        for n in range(32):
            ga, B_ga = gsig[0]
            gb, B_gb = gsig[1]
            mst, B_mst = mgst[n % 2]

            def ev_gate(dst, B_dst, col):
                def ev(banks):
                    for t, b in enumerate(banks):
                        p.op("act", lambda e, b=b, t=t: e.activation(
                            out=dst[:, t * 512:(t + 1) * 512], in_=PS[b], func=AF.Sigmoid, bias=bgate[:, col:col + 1], scale=1.0),
                            reads=[PSB[b], B_bg], writes=[B_dst])
                return ev

            def ev_ba(banks):
                for t, b in enumerate(banks):
                    p.op("dve", lambda e, b=b, t=t: e.tensor_tensor(
                        out=m1[:, t * 512:(t + 1) * 512], in0=PS[b], in1=ga[:, t * 512:(t + 1) * 512], op=ALU.mult),
                        reads=[PSB[b], B_ga], writes=[B_m1])

            def ev_bb(banks, mst=mst, B_mst=B_mst, n=n):
                for t, b in enumerate(banks):
                    p.op("dve", lambda e, b=b, t=t: e.tensor_tensor(
                        out=gb[:, t * 512:(t + 1) * 512], in0=PS[b], in1=gb[:, t * 512:(t + 1) * 512], op=ALU.mult),
                        reads=[PSB[b], B_gb], writes=[B_gb])
                p.op("dve", lambda e: e.tensor_tensor(out=mst, in0=gb, in1=m1, op=ALU.add),
                     reads=[B_gb, B_m1], writes=[B_mst])
                p.dma("sp", mg_s[n], mst, reads=[B_mst], writes=[B_mgs])

            one_chunk(xT, B_x, 32, w_gate_d, n, ev_gate(ga, B_ga, n))
            one_chunk(oT[:, 0:16, :], B_o, 16, w_ba_d, n, ev_ba)
            one_chunk(xT, B_x, 32, w_gate_d, 32 + n, ev_gate(gb, B_gb, 32 + n))
            one_chunk(oT[:, 16:32, :], B_o, 16, w_bb_d, n, ev_bb)
        if "mg" in dbg:
            o = dout("dbg_mg", [32, 128, T], BF16)
            for i in range(32):
                p.dma("sp", mgst[0][0], mg_s[i], reads=[B_mgs], writes=[mgst[0][1]])
                p.dma("sp", o[i], mgst[0][0], reads=[mgst[0][1]])

        if stage <= 3.3:
            p.wait_all_on("sp")
            p.emit()
            return nc, dbg_out, list(IN)

        arena_reset()
        mgT = A.bf16(32 * T).rearrange("p (k t) -> p k t", k=32)
        B_mg = Buf("mgT")
        for k4 in range(8):
            p.dma("sp", mgT[:, k4 * 4:(k4 + 1) * 4, :], mg_s[k4 * 4:(k4 + 1) * 4].rearrange("c p t -> p c t"),
                  reads=[B_mgs], writes=[B_mg], group=(k4 > 0))
        wtiles = [(A.bf16(32 * 128), Buf("w%d" % i)) for i in range(3)]
        xf = [(A.f32(T), Buf("xf%d" % i)) for i in range(2)]
        zf = [(A.f32(T), Buf("zf%d" % i)) for i in range(2)]
        zq = [(A.bf16(T), Buf("zq%d" % i)) for i in range(2)]
        zbb = [(A.bf16(T), Buf("zbb%d" % i)) for i in range(2)]
        z1_s = dscr("z1_s", [32, 128, T], F32)
        B_z1s = Buf("z1_s")
        slots_c2 = [[0, 1], [2, 3]]
        w_out_d = inp("w_out_t")
        xT_rows = inp("xT")

        def ln_stats_mm(n, z, B_z, q, B_q, nchunks):
            for t in range(2):
                p.op("pe", lambda e, t=t: e.matmul(PS[4 + t], ones_b, z[:, t * 512:(t + 1) * 512],
                                                   start=(n == 0), stop=(n == nchunks - 1)),
                     reads=[B_z, B_const], writes=[PSB[4 + t]], inc=False)
                p.op("pe", lambda e, t=t: e.matmul(PS[6 + t], ones_b, q[:, t * 512:(t + 1) * 512],
                                                   start=(n == 0), stop=(n == nchunks - 1)),
                     reads=[B_q, B_const], writes=[PSB[6 + t]], inc=True)

        pending_stats = []
        for n in range(32):
            wt, wb = wtiles[n % 3]
            banks = slots_c2[n % 2]
            x_f, B_xf = xf[n % 2]
            z_f, B_zf = zf[n % 2]
            z_q, B_zq = zq[n % 2]
            p.dma("pool", wt.rearrange("p (k n) -> p k n", k=32), w_out_d[n], writes=[wb])
            p.dma("sp", x_f, xT_rows[n * 128:(n + 1) * 128, 0:T], writes=[B_xf])
            for kc in range(32):
                for t in range(2):
                    b = banks[t]
                    p.op("pe", lambda e, b=b, kc=kc, t=t, wt=wt: e.matmul(
                        PS[b], wt[:, kc * 128:(kc + 1) * 128], mgT[:, kc, t * 512:(t + 1) * 512],
                        start=(kc == 0), stop=(kc == 31)),
                        reads=[wb, B_mg], writes=[PSB[b]], inc=(kc == 31 and t == 1))
            while pending_stats:
                a_ = pending_stats.pop(0)
                ln_stats_mm(a_[0], a_[1], a_[2], a_[3], a_[4], 32)
            for t, b in enumerate(banks):
                p.op("dve", lambda e, b=b, t=t, x_f=x_f, z_f=z_f: e.scalar_tensor_tensor(
                    out=z_f[:, t * 512:(t + 1) * 512], in0=x_f[:, t * 512:(t + 1) * 512], scalar=ALPHA, in1=PS[b],
                    op0=ALU.mult, op1=ALU.add), reads=[PSB[b], B_xf], writes=[B_zf])
            z_b, B_zb = zbb[n % 2]
            p.op("act", lambda e, z_f=z_f, z_q=z_q: e.activation(out=z_q, in_=z_f, func=AF.Square),
                 reads=[B_zf], writes=[B_zq])
            p.op("act", lambda e, z_f=z_f, z_b=z_b: e.activation(out=z_b, in_=z_f, func=AF.Copy),
                 reads=[B_zf], writes=[B_zb])
            p.dma("sp", z1_s[n], z_f, reads=[B_zf], writes=[B_z1s])
            pending_stats.append((n, z_b, B_zb, z_q, B_zq))

        if stage <= 3.6:
            p.wait_all_on("sp")
            p.emit()
            return nc, dbg_out, list(IN)

        while pending_stats:
            a_ = pending_stats.pop(0)
            ln_stats_mm(a_[0], a_[1], a_[2], a_[3], a_[4], 32)

        def ln_finish():
            Mt = A.f32(T)
            Rt = A.f32(T)
            B_MR = Buf("MR")
            for t in range(2):
                sl = slice(t * 512, (t + 1) * 512)
                p.op("dve", lambda e, t=t, sl=sl: e.tensor_scalar(out=Mt[:, sl], in0=PS[4 + t], scalar1=1.0 / D, scalar2=None,
                                                                 op0=ALU.mult), reads=[PSB[4 + t]], writes=[B_MR])
                p.op("dve", lambda e, t=t, sl=sl: e.tensor_scalar(out=Rt[:, sl], in0=PS[6 + t], scalar1=1.0 / D, scalar2=EPS,
                                                                 op0=ALU.mult, op1=ALU.add), reads=[PSB[6 + t]], writes=[B_MR])
            msq = A.f32(T)
            p.op("dve", lambda e: e.tensor_tensor(out=msq, in0=Mt, in1=Mt, op=ALU.mult), reads=[B_MR], writes=[B_MR])
            p.op("dve", lambda e: e.tensor_sub(out=Rt, in0=Rt, in1=msq), reads=[B_MR], writes=[B_MR])
            p.op("act", lambda e: e.activation(out=Rt, in_=Rt, func=AF.Sqrt), reads=[B_MR], writes=[B_MR])
            p.op("dve", lambda e: e.reciprocal(out=Rt, in_=Rt), reads=[B_MR], writes=[B_MR])
            return Mt, Rt, B_MR

        def ln_apply(src_s, B_src, g_name, b_name, sink):
            Mt, Rt, B_MR = ln_finish()
            gcol = A.f32(32)
            bcol = A.f32(32)
            B_gb = Buf("gb")
            p.dma("sp", gcol, inp(g_name), writes=[B_gb])
            p.dma("sp", bcol, inp(b_name), writes=[B_gb])
            zb = [(A.f32(T), Buf("lz%d" % i)) for i in range(4)]
            for n in range(32):
                z, B_z = zb[n % 4]
                p.dma("sp", z, src_s[n], reads=[B_src], writes=[B_z])
                p.op("dve", lambda e, z=z: e.tensor_sub(out=z, in0=z, in1=Mt), reads=[B_MR, B_z], writes=[B_z])
                p.op("dve", lambda e, z=z: e.tensor_tensor(out=z, in0=z, in1=Rt, op=ALU.mult), reads=[B_MR, B_z], writes=[B_z])
                p.op("act", lambda e, z=z, n=n: e.activation(out=z, in_=z, func=AF.Identity, scale=gcol[:, n:n + 1],
                                                            bias=bcol[:, n:n + 1]), reads=[B_gb, B_z], writes=[B_z])
                sink(n, z, B_z)

        arena_reset()
        h1_s = dscr("h1_s", [32, 128, T], F32)
        B_h1s = Buf("h1_s")
        h1T = A.bf16(32 * T).rearrange("p (k t) -> p k t", k=32)
        B_h1 = Buf("h1T")

        def sink1(n, z, B_z):
            p.dma("sp", h1_s[n], z, reads=[B_z], writes=[B_h1s])
            p.op("act", lambda e: e.activation(out=h1T[:, n, :], in_=z, func=AF.Copy), reads=[B_z], writes=[B_h1])

        ln_apply(z1_s, B_z1s, "ln1g", "ln1b", sink1)
        if "h1" in dbg:
            o = dout("dbg_h1", [32, 128, T], F32)
            tmp_h = A.f32(T)
            B_th = Buf("tmp_h")
            for i in range(32):
                p.dma("sp", tmp_h, h1_s[i], reads=[B_h1s], writes=[B_th])
                p.dma("sp", o[i], tmp_h, reads=[B_th])

        if stage <= 4:
            p.wait_all_on("sp")
            p.emit()
            return nc, dbg_out, list(IN)

        z2_s = dscr("z2_s", [32, 128, T], F32)
        B_z2s = Buf("z2_s")
        A.off = A_BASE + (2 * 32 * T + 3) // 4
        pTb = A.bf16(2 * T).rearrange("p (k t) -> p k t", k=2)
        B_pT = Buf("pT")
        p.barrier()
        p.dma("pool", pTb, inp("pT").rearrange("(k p) t -> p k t", p=128), writes=[B_pT])
        bpg = A.f32(32)
        B_bpg = Buf("bpg")
        p.dma("sp", bpg, inp("b_pg_t"), writes=[B_bpg])
        wtiles = [(A.bf16(32 * 128), Buf("w%d" % i)) for i in range(3)]
        wple = [(A.bf16(2 * 128), Buf("wple%d" % i)) for i in range(2)]
        sg = [(A.f32(T), Buf("sg%d" % i)) for i in range(2)]
        hf = [(A.f32(T), Buf("hf%d" % i)) for i in range(2)]
        w_pg_d, w_ple_d = inp("w_pg_t"), inp("w_ple_t")
        slots_d = [[0, 1], [2, 3], [4, 5], [6, 7]]
        for n in range(32):
            wt, wb = wtiles[n % 3]
            wp, wpb = wple[n % 2]
            s_g, B_sg = sg[n % 2]
            h_f, B_hf = hf[n % 2]
            bg_ = slots_d[(2 * n) % 4]
            bp_ = slots_d[(2 * n + 1) % 4]
            p.dma("pool", wt.rearrange("p (k n) -> p k n", k=32), w_pg_d[n], writes=[wb])
            p.dma("pool", wp.rearrange("p (k n) -> p k n", k=2), w_ple_d[n], writes=[wpb])
            p.dma("sp", h_f, h1_s[n], reads=[B_h1s], writes=[B_hf])
            for kc in range(32):
                for t in range(2):
                    b = bg_[t]
                    p.op("pe", lambda e, b=b, kc=kc, t=t, wt=wt: e.matmul(
                        PS[b], wt[:, kc * 128:(kc + 1) * 128], h1T[:, kc, t * 512:(t + 1) * 512],
                        start=(kc == 0), stop=(kc == 31)),
                        reads=[wb, B_h1], writes=[PSB[b]], inc=(kc == 31 and t == 1))
            for kc in range(2):
                for t in range(2):
                    b = bp_[t]
                    p.op("pe", lambda e, b=b, kc=kc, t=t, wp=wp: e.matmul(
                        PS[b], wp[:, kc * 128:(kc + 1) * 128], pTb[:, kc, t * 512:(t + 1) * 512],
                        start=(kc == 0), stop=(kc == 1)),
                        reads=[wpb, B_pT], writes=[PSB[b]], inc=(kc == 1 and t == 1))
            for t in range(2):
                sl = slice(t * 512, (t + 1) * 512)
                p.op("act", lambda e, t=t, sl=sl, s_g=s_g, n=n, b=bg_[t]: e.activation(
                    out=s_g[:, sl], in_=PS[b], func=AF.Sigmoid, bias=bpg[:, n:n + 1], scale=1.0),
                    reads=[PSB[bg_[t]], B_bpg], writes=[B_sg])
                p.op("dve", lambda e, sl=sl, s_g=s_g, b=bp_[t]: e.tensor_tensor(
                    out=s_g[:, sl], in0=PS[b], in1=s_g[:, sl], op=ALU.mult),
                    reads=[PSB[bp_[t]], B_sg], writes=[B_sg])
            p.op("dve", lambda e, s_g=s_g, h_f=h_f: e.scalar_tensor_tensor(
                out=h_f, in0=h_f, scalar=ALPHA, in1=s_g, op0=ALU.mult, op1=ALU.add),
                reads=[B_sg, B_hf], writes=[B_hf])
            p.dma("sp", z2_s[n], h_f, reads=[B_hf], writes=[B_z2s])

        if stage >= 6:
            p.barrier()
            A.off = A_BASE + (2 * 32 * T + 3) // 4
            qpT = A.alloc_top(2 * 16 * T, BF16).rearrange("p (g t) -> p g t", g=16)
            B_qp = Buf("qpT")
            wtiles = [(A.bf16(32 * 128), Buf("w%d" % i)) for i in range(3)]

            def ev_q(i, c, banks):
                for t, b in enumerate(banks):
                    p.op("act", lambda e, b=b, t=t: e.activation(out=qpT[:, c, t * 512:(t + 1) * 512], in_=PS[b], func=AF.Copy),
                         reads=[PSB[b]], writes=[B_qp])

            gemm(h1T, 32, T, inp("peer_wq_t"), list(range(16)), ev_q, [B_h1], wtiles, [[0, 1], [2, 3], [4, 5], [6, 7]])
            p.barrier()
            A.off = A_BASE
            skb = A.bf16(16 * 128).rearrange("p (g n) -> p g n", g=16)
            iota3 = A.bf16(32 * 128).rearrange("p (t n) -> p t n", t=32)
            B_ec = Buf("e1const")
            p.dma("pool", skb, inp("sk_t"), writes=[B_ec])
            p.dma("pool", iota3, inp("iota_t"), writes=[B_ec])
            S_sb = A.f32(16 * 128).rearrange("p (g n) -> p g n", g=16)
            B_Sb = [Buf("S_sb%d" % i) for i in range(4)]
            V16 = A.f32(16 * 16).rearrange("p (g k) -> p g k", g=16)
            B_Vg = [Buf("V16_%d" % i) for i in range(16)]
            w128g = A.f32(16 * 128).rearrange("p (g n) -> p g n", g=16)
            B_wg = [Buf("w128_%d" % i) for i in range(16)]
            idxu = A.alloc(4 * 128, U32)[:, 0:128].rearrange("p (h k) -> p h k", h=8)
            B_ix = [Buf("ix%d" % i) for i in range(8)]
            cand = A.f32(8 * 256).rearrange("p (h c) -> p h c", h=8)
            B_cd = [Buf("cand%d" % i) for i in range(8)]
            workc = A.f32(8 * 256).rearrange("p (h c) -> p h c", h=8)
            B_wc = [Buf("workc%d" % i) for i in range(8)]
            vals = A.f32(128).rearrange("p (h k) -> p h k", h=8)
            B_vl = [Buf("vals%d" % i) for i in range(8)]
            ev_ = A.f32(128).rearrange("p (h k) -> p h k", h=8)
            Zs = A.f32(8)
            rZ = A.f32(8)
            X = [A.f32(128).rearrange("p (h k) -> p h k", h=8) for _ in range(4)]
            B_X = [Buf("X%d" % i) for i in range(4)]
            XTb = [A.f32(4 * 128).rearrange("p (i t) -> p i t", i=4) for _ in range(2)]
            B_XTb = [Buf("XT0"), Buf("XT1")]
            B_sm = Buf("small")
            S1rep = [(A.f32(32 * 128).rearrange("p (t n) -> p t n", t=32), Buf("S1rep%d" % i)) for i in range(2)]
            L3b = [(A.bf16(32 * 128).rearrange("p (t n) -> p t n", t=32), Buf("L3%d" % i)) for i in range(2)]
            R3b = [(A.bf16(32 * 128).rearrange("p (t n) -> p t n", t=32), Buf("R3%d" % i)) for i in range(2)]
            GTb = [(A.bf16(128 * 64).rearrange("p (y t) -> p y t", y=128), Buf("GT%d" % i)) for i in range(2)]
            gT_h = gT_s
            V1v = V16.rearrange("p (h two) k -> p h two k", two=2)[:, :, 0, :]
            V2v = V16.rearrange("p (h two) k -> p h two k", two=2)[:, :, 1, :]
            gbank = [0]

            def chain(tt):
                for g in range(16):
                    p.op("pe", lambda e, g=g: e.matmul(PS[g // 4][:, (g % 4) * 128:(g % 4 + 1) * 128],
                                                       qpT[:, g, tt * 128:(tt + 1) * 128], skb[:, g, :],
                                                       start=True, stop=True),
                         reads=[B_qp, B_ec], writes=[PSB[g // 4]], inc=(g % 4 == 3))
                for b in range(4):
                    p.op("act", lambda e, b=b: e.activation(out=S_sb[:, b * 4:(b + 1) * 4, :],
                                                            in_=PS[b].rearrange("p (g n) -> p g n", g=4), func=AF.Copy),
                         reads=[PSB[b]], writes=[B_Sb[b]])
                p.dma("sp", s1_s[:, tt * 128:(tt + 1) * 128, :].rearrange("h t n -> t h n"),
                      S_sb.rearrange("p (h two) n -> p h two n", two=2)[:, :, 0, :], reads=B_Sb, writes=[B_s1s])
                for g in range(16):
                    p.op("dve", lambda e, g=g: e.max(out=V16[:, g, 0:8], in_=S_sb[:, g, :]),
                         reads=[B_Sb[g // 4]], writes=[B_Vg[g]])
                for g in range(16):
                    p.op("dve", lambda e, g=g: e.match_replace(out=w128g[:, g, :], in_to_replace=V16[:, g, 0:8],
                                                               in_values=S_sb[:, g, :], imm_value=-1e30),
                         reads=[B_Sb[g // 4], B_Vg[g]], writes=[B_wg[g]])
                for g in range(16):
                    p.op("dve", lambda e, g=g: e.max(out=V16[:, g, 8:16], in_=w128g[:, g, :]),
                         reads=[B_wg[g]], writes=[B_Vg[g]])
                for hd in range(8):
                    g = 2 * hd + 1
                    p.op("dve", lambda e, g=g, hd=hd: e.max_index(out=idxu[:, hd, 0:8], in_max=V16[:, g, 0:8], in_values=S_sb[:, g, :]),
                         reads=[B_Sb[g // 4], B_Vg[g]], writes=[B_ix[hd]])
                for hd in range(8):
                    g = 2 * hd + 1
                    p.op("dve", lambda e, g=g, hd=hd: e.max_index(out=idxu[:, hd, 8:16], in_max=V16[:, g, 8:16], in_values=S_sb[:, g, :]),
                         reads=[B_Sb[g // 4], B_Vg[g]], writes=[B_ix[hd]])
                for hd in range(8):
                    p.op("dve", lambda e, hd=hd: e.tensor_tensor(
                        out=cand[:, hd, :].rearrange("p (a b) -> p a b", a=16),
                        in0=V16[:, 2 * hd, :].unsqueeze(2).to_broadcast([128, 16, 16]),
                        in1=V16[:, 2 * hd + 1, :].unsqueeze(1).to_broadcast([128, 16, 16]), op=ALU.add),
                        reads=[B_Vg[2 * hd], B_Vg[2 * hd + 1]], writes=[B_cd[hd]])
                for hd in range(8):
                    p.op("dve", lambda e, hd=hd: e.max(out=vals[:, hd, 0:8], in_=cand[:, hd, :]),
                         reads=[B_cd[hd]], writes=[B_vl[hd]])
                for hd in range(8):
                    p.op("dve", lambda e, hd=hd: e.match_replace(out=workc[:, hd, :], in_to_replace=vals[:, hd, 0:8],
                                                                 in_values=cand[:, hd, :], imm_value=-1e30),
                         reads=[B_cd[hd], B_vl[hd]], writes=[B_wc[hd]])
                for hd in range(8):
                    p.op("dve", lambda e, hd=hd: e.max(out=vals[:, hd, 8:16], in_=workc[:, hd, :]),
                         reads=[B_wc[hd]], writes=[B_vl[hd]])
                p.op("dve", lambda e: e.tensor_copy(out=X[2], in_=idxu), reads=B_ix, writes=[B_X[2]])
                p.op("dve", lambda e: e.tensor_tensor(out=ev_, in0=vals, in1=vals[:, :, 0:1].to_broadcast([128, 8, 16]),
                                                      op=ALU.subtract), reads=B_vl + [B_sm], writes=[B_sm])
                p.op("act", lambda e: e.activation(out=ev_, in_=ev_, func=AF.Exp), reads=[B_sm], writes=[B_sm])
                p.op("dve", lambda e: e.tensor_tensor(out=X[0], in0=vals[:, :, 15:16].to_broadcast([128, 8, 16]), in1=V2v,
                                                      op=ALU.subtract), reads=B_vl + B_Vg, writes=[B_X[0]])
                p.op("dve", lambda e: e.tensor_tensor(out=X[1], in0=V2v, in1=V2v[:, :, 0:1].to_broadcast([128, 8, 16]),
                                                      op=ALU.subtract), reads=B_Vg, writes=[B_X[1]])
                p.op("dve", lambda e: e.tensor_scalar(out=X[3], in0=V1v[:, :, 0:1].to_broadcast([128, 8, 16]), scalar1=-1.0,
                                                      scalar2=None, op0=ALU.mult), reads=B_Vg, writes=[B_X[3]])
                p.op("dve", lambda e: e.scalar_tensor_tensor(out=X[0], in0=X[0], scalar=-3e-5, in1=X[3], op0=ALU.add, op1=ALU.add),
                     reads=[B_X[0], B_X[3]], writes=[B_X[0]])
                p.op("act", lambda e: e.activation(out=X[0], in_=X[0], func=AF.Exp), reads=[B_X[0]], writes=[B_X[0]])
                p.op("act", lambda e: e.activation(out=X[1], in_=X[1], func=AF.Exp), reads=[B_X[1]], writes=[B_X[1]])
                p.op("dve", lambda e: e.tensor_reduce(out=Zs, in_=ev_, axis=AX.X, op=ALU.add), reads=[B_sm], writes=[B_sm])
                p.op("dve", lambda e: e.reciprocal(out=rZ, in_=Zs), reads=[B_sm], writes=[B_sm])
                p.op("dve", lambda e: e.tensor_tensor(out=X[1], in0=X[1], in1=rZ.unsqueeze(2).to_broadcast([128, 8, 16]),
                                                      op=ALU.mult), reads=[B_sm, B_X[1]], writes=[B_X[1]])
                for i in range(4):
                    p.op("pe", lambda e, i=i: e.matmul(PS[4][:, i * 128:(i + 1) * 128], X[i].rearrange("p h k -> p (h k)"),
                                                       ident_f, start=True, stop=True),
                         reads=[B_X[i], B_const], writes=[PSB[4]], inc=(i == 3))
                p.op("act", lambda e: e.activation(out=XTb[tt % 2].rearrange("p i t -> p (i t)"), in_=PS[4], func=AF.Copy),
                     reads=[PSB[4]], writes=[B_XTb[tt % 2]])

            B_srD = [Buf("srD%d" % i) for i in range(2)]

            def stage_A(tt, sub, k):
                XT, B_XT = XTb[tt % 2], B_XTb[tt % 2]
                t0 = tt * 128 + sub * 32
                sr, B_E = S1rep[k % 2]
                B_D = B_srD[k % 2]
                R3, B_R3 = R3b[k % 2]
                for hd in range(8):
                    p.dma("sp", sr[hd * 16:(hd + 1) * 16, :, :],
                          s1_s[hd, t0:t0 + 32, :].partition_broadcast(16), reads=[B_s1s], writes=[B_D, B_E], group=(hd > 0))
                for t in range(32):
                    tc = sub * 32 + t
                    p.op("act", lambda e, t=t, tc=tc: e.activation(out=sr[:, t, :], in_=sr[:, t, :], func=AF.Exp,
                                                                  bias=XT[:, 3, tc:tc + 1], scale=1.0),
                         reads=[B_D, B_XT], writes=[B_E], skip_own=(t > 0))
                for t in range(32):
                    tc = sub * 32 + t
                    p.op("dve", lambda e, t=t, tc=tc: e.tensor_scalar(
                        out=R3[:, t, :], in0=iota3[:, 0, :], scalar1=XT[:, 2, tc:tc + 1], scalar2=XT[:, 1, tc:tc + 1],
                        op0=ALU.is_equal, op1=ALU.mult), reads=[B_ec, B_XT], writes=[B_R3], skip_own=(t > 0))

            def stage_B(tt, sub, k):
                XT, B_XT = XTb[tt % 2], B_XTb[tt % 2]
                ht = tt * 2 + sub // 2
                GT, B_GT = GTb[ht % 2]
                sr, B_E = S1rep[k % 2]
                L3, B_L3 = L3b[k % 2]
                R3, B_R3 = R3b[k % 2]
                for t in range(32):
                    tc = sub * 32 + t
                    p.op("dve", lambda e, t=t, tc=tc: e.scalar_tensor_tensor(
                        out=L3[:, t, :], in0=sr[:, t, :], scalar=XT[:, 0, tc:tc + 1], in1=sr[:, t, :],
                        op0=ALU.is_ge, op1=ALU.mult), reads=[B_E, B_XT], writes=[B_L3], skip_own=(t > 0))
                for q4 in range(8):
                    b = 5 + gbank[0] % 3
                    gbank[0] += 1
                    for kk in range(4):
                        tk = q4 * 4 + kk
                        p.op("pe", lambda e, b=b, kk=kk, tk=tk: e.matmul(
                            PS[b][:, kk * 128:(kk + 1) * 128], L3[:, tk, :], R3[:, tk, :], start=True, stop=True),
                             reads=[B_L3, B_R3], writes=[PSB[b]], inc=(kk == 3))
                    tl0 = (sub % 2) * 32 + q4 * 4
                    p.op("act", lambda e, b=b, tl0=tl0: e.activation(
                        out=GT[:, :, tl0:tl0 + 4], in_=PS[b].rearrange("p (t y) -> p y t", t=4), func=AF.Copy),
                        reads=[PSB[b]], writes=[B_GT])
                if sub % 2 == 1:
                    p.dma("sp", gT_h[ht], GT.rearrange("p y t -> p (y t)"), reads=[B_GT], writes=[B_gTs])

            jobs = [(tt, sub) for tt in range(8) for sub in range(4)]
            chain(0)
            stage_A(0, 0, 0)
            for k, (tt, sub) in enumerate(jobs):
                if k + 1 < len(jobs):
                    tt2, sub2 = jobs[k + 1]
                    if sub2 == 0:
                        chain(tt2)
                    stage_A(tt2, sub2, k + 1)
                stage_B(tt, sub, k)
            if stage <= 6:
                p.wait_all_on("sp")
                p.emit()
                return nc, dbg_out, list(IN)

            arena_reset()
            h1T = A.bf16(32 * T).rearrange("p (k t) -> p k t", k=32)
            B_h1 = Buf("h1T")
            for k4 in range(8):
                p.dma("pool", h1T[:, k4 * 4:(k4 + 1) * 4, :], h1_s[k4 * 4:(k4 + 1) * 4].rearrange("c p t -> p c t"),
                      reads=[B_h1s], writes=[B_h1], group=(k4 > 0))
            wtiles = [(A.bf16(32 * 128), Buf("w%d" % i)) for i in range(3)]
            gty = [(A.bf16(T), Buf("gty%d" % i)) for i in range(3)]
            actb = [(A.bf16(T), Buf("actb%d" % i)) for i in range(2)]
            ggb = [(A.bf16(T), Buf("ggb%d" % i)) for i in range(3)]
            gT_v = gT_s.rearrange("ht x (y t) -> x ht y t", y=128)

            def ev_e2(i, y, banks):
                g_y, B_gy = gty[i % 3]
                a_b, B_ab = actb[i % 2]
                g_g, B_gg = ggb[i % 3]
                p.dma("sp", g_y.rearrange("p (ht t) -> p ht t", ht=16), gT_v[:, :, y, :], reads=[B_gTs], writes=[B_gy])
                for t, b in enumerate(banks):
                    p.op("act", lambda e, b=b, t=t: e.activation(out=a_b[:, t * 512:(t + 1) * 512], in_=PS[b], func=AF.Gelu),
                         reads=[PSB[b]], writes=[B_ab])
                p.op("dve", lambda e: e.tensor_tensor(out=g_g, in0=a_b, in1=g_y, op=ALU.mult),
                     reads=[B_ab, B_gy], writes=[B_gg])
                p.dma("sp", ggT_s[y], g_g, reads=[B_gg], writes=[B_ggs])

            gemm(h1T, 32, T, inp("uT_t"), list(range(128)), ev_e2, [B_h1], wtiles, [[0, 1], [2, 3], [4, 5], [6, 7]])

            arena_reset()
            vtb = [(A.bf16(2 * 512).rearrange("p (y d) -> p y d", y=2), Buf("vt%d" % i)) for i in range(4)]
            gyb = [(A.bf16(2 * T).rearrange("p (y t) -> p y t", y=2), Buf("gy%d" % i)) for i in range(4)]
            ztb = [(A.f32(T), Buf("zt%d" % i)) for i in range(4)]
            NRES = 32
            GGR = A.bf16(NRES * 2 * T).rearrange("p (r y t) -> p r y t", r=NRES, y=2)
            B_ggr = [Buf("ggr%d" % i) for i in range(NRES)]
            v_d = inp("v_t")
            zi = 0
            for dg in range(8):
                zts = []
                for dc in range(4):
                    n = dg * 4 + dc
                    zt, B_zt = ztb[zi % 4]
                    zi += 1
                    p.dma("sp", zt, z2_s[n], reads=[B_z2s], writes=[B_zt])
                    zts.append((zt, B_zt))
                for y2 in range(64):
                    vt, B_vt = vtb[y2 % 4]
                    p.dma("pool", vt, v_d[2 * y2:2 * y2 + 2][:, :, dg * 512:(dg + 1) * 512].rearrange("y x d -> x y d"),
                          writes=[B_vt])
                    if y2 < NRES:
                        gy, B_gy = GGR[:, y2, :, :], B_ggr[y2]
                        if dg == 0:
                            p.dma("sp" if y2 % 2 else "act", gy, ggT_s[2 * y2:2 * y2 + 2].rearrange("y x t -> x y t"),
                                  reads=[B_ggs], writes=[B_gy])
                    else:
                        gy, B_gy = gyb[y2 % 4]
                        p.dma("sp" if y2 % 2 else "act", gy, ggT_s[2 * y2:2 * y2 + 2].rearrange("y x t -> x y t"),
                              reads=[B_ggs], writes=[B_gy])
                    for yy in range(2):
                        y = 2 * y2 + yy
                        for dc in range(4):
                            for th in range(2):
                                b = dc * 2 + th
                                p.op("pe", lambda e, b=b, dc=dc, th=th, vt=vt, gy=gy, y=y, yy=yy: e.matmul(
                                    PS[b], vt[:, yy, dc * 128:(dc + 1) * 128], gy[:, yy, th * 512:(th + 1) * 512],
                                    start=(y == 0), stop=(y == 127)),
                                    reads=[B_vt, B_gy], writes=[PSB[b]], inc=(dc == 3 and th == 1))
                for dc in range(4):
                    n = dg * 4 + dc
                    zt, B_zt = zts[dc]
                    for th in range(2):
                        b = dc * 2 + th
                        p.op("dve", lambda e, b=b, th=th, zt=zt: e.tensor_tensor(
                            out=zt[:, th * 512:(th + 1) * 512], in0=PS[b], in1=zt[:, th * 512:(th + 1) * 512], op=ALU.add),
                            reads=[PSB[b], B_zt], writes=[B_zt])
                    p.dma("sp", z2_s[n], zt, reads=[B_zt], writes=[B_z2s])

        arena_reset()
        zb2 = [(A.f32(T), Buf("fz%d" % i)) for i in range(4)]
        zq2 = [(A.bf16(T), Buf("fq%d" % i)) for i in range(4)]
        zc2 = [(A.bf16(T), Buf("fc%d" % i)) for i in range(4)]
        for n in range(32):
            z, B_z = zb2[n % 4]
            q, B_q = zq2[n % 4]
            zc, B_zc = zc2[n % 4]
            p.dma("sp", z, z2_s[n], reads=[B_z2s], writes=[B_z])
            p.op("act", lambda e, z=z, q=q: e.activation(out=q, in_=z, func=AF.Square), reads=[B_z], writes=[B_q])
            p.op("act", lambda e, z=z, zc=zc: e.activation(out=zc, in_=z, func=AF.Copy), reads=[B_z], writes=[B_zc])
            ln_stats_mm(n, zc, B_zc, q, B_q, 32)
        B_out = Buf("out")

        def sink2(n, z, B_z):
            p.dma("sp", outT_d[n * 128:(n + 1) * 128, :], z, reads=[B_z], writes=[B_out])

        ln_apply(z2_s, B_z2s, "ln2g", "ln2b", sink2)

        p.wait_all_on("sp")
        p.emit()
    return nc, dbg_out, list(IN)


def _chunked(w, kc):
    K, N = w.shape
    assert K == kc * 128 and N % 128 == 0
    return np.ascontiguousarray(w.reshape(kc, 128, N // 128, 128).transpose(2, 1, 0, 3))


def _col(v, n):
    return np.ascontiguousarray(v.reshape(n, 128).T)


def prep_shared(inp, used=None):
    f = lambda a: np.asarray(a, dtype=np.float32)

    def w_in_t():
        w_in = f(inp["w_in"])[0]
        W = [2048, 256, 2048, 64, 32, 2048, 2048, 2048, 16]
        off = np.concatenate([[0], np.cumsum(W)])
        seg = lambda i: w_in[:, off[i]:off[i + 1]]
        z = lambda n: np.zeros((D, n), np.float32)
        cols = np.concatenate([
            seg(0), seg(5), seg(2), seg(4), z(96),
            seg(1), seg(3), seg(3), seg(6), seg(7), seg(8), z(112)], axis=1)
        assert cols.shape[1] == (NQ_CH + NK_CH) * 128
        return _chunked(cols, 32)

    def bfor():
        bf = np.zeros((128, 1), np.float32)
        bf[:16, 0] = f(inp["b_forget"])[0]
        return bf

    th = {
        "w_in_t": w_in_t,
        "ident": lambda: np.eye(128, dtype=np.float32),
        "glat": lambda: _col(f(inp["g_latent"])[0], 2),
        "bfor": bfor,
        "wuk_t": lambda: np.ascontiguousarray(f(inp["w_uk"])[0].reshape(2, 128, 2048).transpose(1, 0, 2)),
        "wuv_t": lambda: np.ascontiguousarray(f(inp["w_uv"])[0].reshape(2, 128, 2048).transpose(1, 0, 2)),
        "w_gate_t": lambda: _chunked(f(inp["w_gate"])[0], 32),
        "b_gate_t": lambda: _col(f(inp["b_gate"])[0], 64),
        "w_ba_t": lambda: _chunked(f(inp["w_branch_a"])[0], 16),
        "w_bb_t": lambda: _chunked(f(inp["w_branch_b"])[0], 16),
        "w_out_t": lambda: _chunked(f(inp["w_out"])[0], 32),
        "ln1g": lambda: _col(f(inp["ln1_g"])[0], 32),
        "ln1b": lambda: _col(f(inp["ln1_b"])[0], 32),
        "peer_wq_t": lambda: _chunked(f(inp["peer_wq"])[0].reshape(D, 2048), 32),
        "sk_t": lambda: np.ascontiguousarray(f(inp["peer_subkeys"])[0].reshape(16, 128, 128).transpose(2, 0, 1)),
        "uT_t": lambda: np.ascontiguousarray(f(inp["peer_u"])[0].reshape(128, 128, 32, 128).transpose(1, 3, 2, 0)),
        "v_t": lambda: np.ascontiguousarray(f(inp["peer_v"])[0].reshape(128, 128, D).transpose(1, 0, 2)),
        "w_pg_t": lambda: _chunked(f(inp["w_ple_gate"])[0], 32),
        "b_pg_t": lambda: _col(f(inp["b_ple_gate"])[0], 32),
        "w_ple_t": lambda: _chunked(f(inp["w_ple"])[0], 2),
        "ln2g": lambda: _col(f(inp["ln2_g"])[0], 32),
        "ln2b": lambda: _col(f(inp["ln2_b"])[0], 32),
        "iota_t": lambda: np.ascontiguousarray(np.broadcast_to(np.arange(128, dtype=np.float32), (128, 32, 128))),
    }
    return {k: fn() for k, fn in th.items() if used is None or k in used}


def core_positions(par):
    j = np.arange(8)
    own = ((2 * j + par)[:, None] * 128 + np.arange(128)[None, :]).reshape(-1)
    oth = ((2 * j + 1 - par)[:, None] * 128 + np.arange(128)[None, :]).reshape(-1)
    return own, oth


def prep_core(inp, b, par):
    x = np.asarray(inp["x"], dtype=np.float32)[b]
    pp = np.asarray(inp["p"], dtype=np.float32)[0, b]
    own, oth = core_positions(par)
    kpos = np.concatenate([own, oth])
    d = {}
    d["xT"] = np.ascontiguousarray(x[kpos].T)
    d["pT"] = np.ascontiguousarray(pp[own].T)
    d["qpos_b"] = np.ascontiguousarray(np.broadcast_to(own.astype(np.float32), (128, T)))
    d["kpos_b"] = np.ascontiguousarray(np.broadcast_to(kpos.astype(np.float32), (128, S)))
    d["kpos_col"] = _col(kpos.astype(np.float32), 16)
    d["cend_col"] = _col(((own // 64 + 1) * 64).astype(np.float32), 8)
    ce = own // 64 + 1
    cidx = np.arange(32)
    d["penA"] = np.where(cidx[:, None] >= ce[None, :], np.float32(-1e30), np.float32(0)).astype(np.float32)
    d["penB"] = (cidx[:, None] == (kpos // 64)[None, :]).astype(np.float32)
    return d


_CACHE = {}


def kernel(**inputs):
    if "nc" not in _CACHE:
        _CACHE["nc"] = build()
    nc, _, used = _CACHE["nc"]
    sh = prep_shared(inputs, used)
    in_maps = []
    for c in range(8):
        d = dict(sh)
        d.update(prep_core(inputs, c // 2, c % 2))
        in_maps.append({k: d[k] for k in used})
    res = run_bass_kernel_spmd(nc, in_maps, core_ids=list(range(8)))
    out = np.zeros((4, S, D), np.float32)
    for c in range(8):
        own, _ = core_positions(c % 2)
        out[c // 2, own, :] = res.results[c]["outT"].T
    return out
```
